# Optimizing a Trainium2 kernel written in Bass

```python
import math
import jax, jax.numpy as jnp
from jax import lax
import numpy as np

D_MODEL = 2048
BATCH = 2
SEQ = 4096
DEPTH = 1
DEC_BATCH = 32
DEC_SEQ = 8
PAST_LEN = 8192
PAGE_SIZE = 128

D_ATT = D_MODEL // 2
ATT_HEADS = 8
ATT_HD = D_ATT // ATT_HEADS
DILATIONS = ((128, 1), (512, 4), (2048, 16))
WINDOW = 2048
Q_BLOCK = 128
D_SSM = D_MODEL - D_ATT
SSM_GROUP_CH = 16
N_SSM_GROUPS = D_SSM // SSM_GROUP_CH
SSM_STATE = 64
D_MIX = D_ATT + D_SSM
N_MEM = 256
MEM_HEADS = 4
MEM_HD = D_MODEL // MEM_HEADS
N_EXPERT_GROUPS = 4
EXPERTS_PER_GROUP = 8
N_EXPERTS = N_EXPERT_GROUPS * EXPERTS_PER_GROUP
TOP_K_INNER = 2
D_EXPERT = D_MODEL // 4
DEEPNORM_ALPHA = (2.0 * DEPTH) ** 0.25
DEEPNORM_BETA = (8.0 * DEPTH) ** -0.25
LN_EPS = 1e-5
RMS_EPS = 1e-6
DT_MIN = 0.001
DT_MAX = 0.1

kernel_name = 'hymba_s5_dilated_attn_hmoe_decode_step'


def _layernorm(x, g, b):
    xf = x.astype(jnp.float32)
    mu = jnp.mean(xf, -1, keepdims=True)
    var = jnp.mean(jnp.square(xf - mu), -1, keepdims=True)
    y = (xf - mu) * lax.rsqrt(var + LN_EPS) * g.astype(jnp.float32) + b.astype(jnp.float32)
    return y.astype(x.dtype)


def _rmsnorm(x, g):
    xf = x.astype(jnp.float32)
    return xf * lax.rsqrt(jnp.mean(jnp.square(xf), -1, keepdims=True) + RMS_EPS) * g.astype(jnp.float32)


def _dilated_block(q_blk, q_pos, k_all, v_all, kv_start):
    tk = k_all.shape[1]
    qf = q_blk.astype(jnp.float32) * (ATT_HD ** -0.5)
    outs, lses = [], []
    for w, d in DILATIONS:
        steps = jnp.arange(w // d + 1, dtype=jnp.int32) * d
        kpos = q_pos[:, None] - steps[None, :]
        valid = kpos >= kv_start
        idx = jnp.clip(kpos - kv_start, 0, tk - 1)
        kg = jnp.take(k_all, idx, axis=1).astype(jnp.float32)
        vg = jnp.take(v_all, idx, axis=1).astype(jnp.float32)
        s = jnp.einsum('bqhd,bqkhd->bhqk', qf, kg)
        s = jnp.where(valid[None, None], s, -jnp.inf)
        m = jnp.max(s, axis=-1, keepdims=True)
        p = jnp.exp(s - m)
        den = jnp.sum(p, axis=-1)
        o = jnp.einsum('bhqk,bqkhd->bqhd', p, vg) / jnp.transpose(den, (0, 2, 1))[..., None]
        outs.append(o)
        lses.append(jnp.transpose(m[..., 0] + jnp.log(den), (0, 2, 1)))
    wts = jax.nn.softmax(jnp.stack(lses, 0), axis=0)
    return jnp.sum(wts[..., None] * jnp.stack(outs, 0), axis=0)


def _dilated_attention_prompt(q, k, v):
    bsz, t = q.shape[:2]

    def block(i):
        q0 = i * Q_BLOCK
        qb = lax.dynamic_slice_in_dim(q, q0, Q_BLOCK, axis=1)
        pos = q0 + jnp.arange(Q_BLOCK, dtype=jnp.int32)
        return _dilated_block(qb, pos, k, v, 0)

    out = lax.map(block, jnp.arange(t // Q_BLOCK, dtype=jnp.int32))
    return jnp.moveaxis(out, 0, 1).reshape(bsz, t, ATT_HEADS, ATT_HD)


def _s5(u, h0_re, h0_im, lam_re, lam_im, log_dt, b_re, b_im, c_re, c_im, d_skip):
    f32 = jnp.float32
    bsz, t, _ = u.shape
    uf = u.astype(f32).reshape(bsz, t, N_SSM_GROUPS, SSM_GROUP_CH)
    dt = jnp.exp(log_dt.astype(f32))[:, None]
    lr, li = lam_re.astype(f32), lam_im.astype(f32)
    mag = jnp.exp(lr * dt)
    a_re, a_im = mag * jnp.cos(li * dt), mag * jnp.sin(li * dt)
    den = lr * lr + li * li
    nr, ni = a_re - 1.0, a_im
    f_re, f_im = (nr * lr + ni * li) / den, (ni * lr - nr * li) / den
    br, bi = b_re.astype(f32), b_im.astype(f32)
    bb_re = f_re[..., None] * br - f_im[..., None] * bi
    bb_im = f_re[..., None] * bi + f_im[..., None] * br
    x_re = jnp.einsum('btgc,gpc->btgp', uf, bb_re)
    x_im = jnp.einsum('btgc,gpc->btgp', uf, bb_im)
    h0r, h0i = h0_re.astype(f32), h0_im.astype(f32)
    x_re = x_re.at[:, 0].add(a_re * h0r - a_im * h0i)
    x_im = x_im.at[:, 0].add(a_re * h0i + a_im * h0r)
    ar = jnp.broadcast_to(a_re, x_re.shape)
    ai = jnp.broadcast_to(a_im, x_im.shape)

    def comb(e1, e2):
        a1r, a1i, b1r, b1i = e1
        a2r, a2i, b2r, b2i = e2
        return (a2r * a1r - a2i * a1i, a2r * a1i + a2i * a1r,
                a2r * b1r - a2i * b1i + b2r, a2r * b1i + a2i * b1r + b2i)

    _, _, h_re, h_im = lax.associative_scan(comb, (ar, ai, x_re, x_im), axis=1)
    y = (jnp.einsum('btgp,gcp->btgc', h_re, c_re.astype(f32))
         - jnp.einsum('btgp,gcp->btgc', h_im, c_im.astype(f32))
         + d_skip.astype(f32) * uf)
    return y.reshape(bsz, t, D_SSM), h_re[:, -1], h_im[:, -1]


def _mem_attention(x, mem_k, mem_v, w_mq, w_mo):
    bsz, t, _ = x.shape
    q = (x @ w_mq).reshape(bsz, t, MEM_HEADS, MEM_HD).astype(jnp.float32)
    s = jnp.einsum('bthd,bmhd->bhtm', q, mem_k.astype(jnp.float32)) * (MEM_HD ** -0.5)
    p = jax.nn.softmax(s, axis=-1)
    o = jnp.einsum('bhtm,bmhd->bthd', p, mem_v.astype(jnp.float32)).reshape(bsz, t, D_MODEL)
    return o.astype(x.dtype) @ w_mo


def _hier_moe(x, w_r1, b_r1, w_r2, b_r2, w_gate, w_up, w_down):
    f32 = jnp.float32
    g_logits = (x @ w_r1).astype(f32) + b_r1.astype(f32)
    g_prob = jax.nn.softmax(g_logits, axis=-1)
    g_sel = jnp.argmax(g_logits, axis=-1)
    g_w = jnp.take_along_axis(g_prob, g_sel[:, None], axis=-1)[:, 0]
    e_logits = jnp.einsum('nd,dge->nge', x, w_r2).astype(f32) + b_r2.astype(f32)
    e_sel = jnp.take_along_axis(e_logits, g_sel[:, None, None], axis=1)[:, 0]
    top_v, top_i = lax.top_k(e_sel, TOP_K_INNER)
    top_w = jax.nn.softmax(top_v, axis=-1) * g_w[:, None]
    expert_id = g_sel[:, None] * EXPERTS_PER_GROUP + top_i
    gates = jnp.sum(jax.nn.one_hot(expert_id, N_EXPERTS, dtype=f32) * top_w[..., None], axis=1)
    hg = jnp.einsum('nd,edf->nef', x, w_gate)
    hu = jnp.einsum('nd,edf->nef', x, w_up)
    h = jax.nn.silu(hg) * hu * gates[..., None].astype(hg.dtype)
    return jnp.einsum('nef,efd->nd', h, w_down)


def _layer(x, p, mem_k, mem_v, h0_re, h0_im, win_k=None, win_v=None):
    bsz, t, _ = x.shape
    proj = x @ p['w_in']
    q, k, v, u = jnp.split(proj, [D_ATT, 2 * D_ATT, 3 * D_ATT], axis=-1)
    q = q.reshape(bsz, t, ATT_HEADS, ATT_HD)
    k = k.reshape(bsz, t, ATT_HEADS, ATT_HD)
    v = v.reshape(bsz, t, ATT_HEADS, ATT_HD)
    if win_k is None:
        attn = _dilated_attention_prompt(q, k, v)
        wp = min(WINDOW, t)
        new_k, new_v = k[:, t - wp:], v[:, t - wp:]
    else:
        wb = win_k.shape[1]
        k_all = jnp.concatenate([win_k, k.astype(win_k.dtype)], axis=1)
        v_all = jnp.concatenate([win_v, v.astype(win_v.dtype)], axis=1)
        q_pos = PAST_LEN + jnp.arange(t, dtype=jnp.int32)
        attn = _dilated_block(q, q_pos, k_all, v_all, PAST_LEN - wb)
        new_k, new_v = k, v
    attn = attn.reshape(bsz, t, D_ATT)
    y_ssm, h_re, h_im = _s5(u, h0_re, h0_im, p['ssm_lam_re'], p['ssm_lam_im'], p['ssm_log_dt'],
                            p['ssm_b_re'], p['ssm_b_im'], p['ssm_c_re'], p['ssm_c_im'], p['ssm_d'])
    yg = jax.nn.gelu(y_ssm)
    ssm_out = yg * jax.nn.sigmoid(yg @ p['w_glu'].astype(jnp.float32))
    mixed = jnp.concatenate([_rmsnorm(attn, p['g_attn']), _rmsnorm(ssm_out, p['g_ssm'])], axis=-1)
    mix_out = mixed.astype(x.dtype) @ p['w_out']
    x = _layernorm(DEEPNORM_ALPHA * x + mix_out, p['ln1_g'], p['ln1_b'])
    x = _layernorm(DEEPNORM_ALPHA * x + _mem_attention(x, mem_k, mem_v, p['w_mq'], p['w_mo']),
                   p['ln2_g'], p['ln2_b'])
    moe = _hier_moe(x.reshape(bsz * t, D_MODEL), p['w_r1'], p['b_r1'], p['w_r2'], p['b_r2'],
                    p['w_gate'], p['w_up'], p['w_down']).reshape(bsz, t, D_MODEL)
    x = _layernorm(DEEPNORM_ALPHA * x + moe.astype(x.dtype), p['ln3_g'], p['ln3_b'])
    return x, new_k, new_v, h_re, h_im


def setup_inputs(seed: int = 0) -> dict:
    key = jax.random.key(seed)
    ks = iter(jax.random.split(key, 64))
    f32 = jnp.float32

    def nrm(shape, scale):
        return jax.random.normal(next(ks), shape, f32) * scale

    wb = min(WINDOW, PAST_LEN)
    L, G, P, C = DEPTH, N_SSM_GROUPS, SSM_STATE, SSM_GROUP_CH
    w_in = nrm((L, D_MODEL, 3 * D_ATT + D_SSM), D_MODEL ** -0.5)
    w_in = w_in.at[..., 2 * D_ATT:3 * D_ATT].multiply(DEEPNORM_BETA)
    n_idx = jnp.arange(P, dtype=f32)
    lam_re = -0.5 * jnp.exp(nrm((L, G, P), 0.02))
    lam_im = math.pi * n_idx[None, None, :] + nrm((L, G, P), 0.01)
    log_dt = jax.random.uniform(next(ks), (L, G), f32, math.log(DT_MIN), math.log(DT_MAX))
    return {
        'x_prompt': nrm((BATCH, SEQ, D_MODEL), 1.0),
        'x_sample': nrm((DEC_BATCH, DEC_SEQ, D_MODEL), 1.0),
        'cache_win_k': nrm((L, DEC_BATCH, wb, ATT_HEADS, ATT_HD), 1.0),
        'cache_win_v': nrm((L, DEC_BATCH, wb, ATT_HEADS, ATT_HD), 1.0),
        'state_ssm_re': nrm((L, DEC_BATCH, G, P), 0.5),
        'state_ssm_im': nrm((L, DEC_BATCH, G, P), 0.5),
        'cache_mem_k': nrm((L, DEC_BATCH, N_MEM, MEM_HEADS, MEM_HD), 1.0),
        'cache_mem_v': nrm((L, DEC_BATCH, N_MEM, MEM_HEADS, MEM_HD), 1.0),
        'mem_prompt': nrm((BATCH, N_MEM, D_MODEL), 1.0),
        'w_in': w_in,
        'ssm_lam_re': lam_re,
        'ssm_lam_im': lam_im,
        'ssm_log_dt': log_dt,
        'ssm_b_re': nrm((L, G, P, C), (2.0 * C) ** -0.5),
        'ssm_b_im': nrm((L, G, P, C), (2.0 * C) ** -0.5),
        'ssm_c_re': nrm((L, G, C, P), (2.0 * P) ** -0.5),
        'ssm_c_im': nrm((L, G, C, P), (2.0 * P) ** -0.5),
        'ssm_d': nrm((L, G, C), 1.0),
        'w_glu': nrm((L, D_SSM, D_SSM), D_SSM ** -0.5),
        'g_attn': 1.0 + nrm((L, D_ATT), 0.02),
        'g_ssm': 1.0 + nrm((L, D_SSM), 0.02),
        'w_out': nrm((L, D_MIX, D_MODEL), D_MIX ** -0.5 * DEEPNORM_BETA),
        'ln1_g': 1.0 + nrm((L, D_MODEL), 0.02),
        'ln1_b': nrm((L, D_MODEL), 0.02),
        'w_mq': nrm((L, D_MODEL, D_MODEL), D_MODEL ** -0.5),
        'w_mk': nrm((L, D_MODEL, D_MODEL), D_MODEL ** -0.5),
        'w_mv': nrm((L, D_MODEL, D_MODEL), D_MODEL ** -0.5 * DEEPNORM_BETA),
        'w_mo': nrm((L, D_MODEL, D_MODEL), D_MODEL ** -0.5 * DEEPNORM_BETA),
        'ln2_g': 1.0 + nrm((L, D_MODEL), 0.02),
        'ln2_b': nrm((L, D_MODEL), 0.02),
        'w_r1': nrm((L, D_MODEL, N_EXPERT_GROUPS), D_MODEL ** -0.5),
        'b_r1': nrm((L, N_EXPERT_GROUPS), 0.01),
        'w_r2': nrm((L, D_MODEL, N_EXPERT_GROUPS, EXPERTS_PER_GROUP), D_MODEL ** -0.5),
        'b_r2': nrm((L, N_EXPERT_GROUPS, EXPERTS_PER_GROUP), 0.01),
        'w_gate': nrm((L, N_EXPERTS, D_MODEL, D_EXPERT), D_MODEL ** -0.5),
        'w_up': nrm((L, N_EXPERTS, D_MODEL, D_EXPERT), D_MODEL ** -0.5),
        'w_down': nrm((L, N_EXPERTS, D_EXPERT, D_MODEL), D_EXPERT ** -0.5 * DEEPNORM_BETA),
        'ln3_g': 1.0 + nrm((L, D_MODEL), 0.02),
        'ln3_b': nrm((L, D_MODEL), 0.02),
    }


def reference(x_prompt, x_sample, cache_win_k, cache_win_v, state_ssm_re, state_ssm_im,
              cache_mem_k, cache_mem_v, mem_prompt, w_in, ssm_lam_re, ssm_lam_im, ssm_log_dt,
              ssm_b_re, ssm_b_im, ssm_c_re, ssm_c_im, ssm_d, w_glu, g_attn, g_ssm, w_out,
              ln1_g, ln1_b, w_mq, w_mk, w_mv, w_mo, ln2_g, ln2_b, w_r1, b_r1, w_r2, b_r2,
              w_gate, w_up, w_down, ln3_g, ln3_b):
    yp, ys = x_prompt, x_sample
    bsz = x_prompt.shape[0]
    wkp, wvp, wks, wvs = [], [], [], []
    srp, sip, srs, sis = [], [], [], []
    mkp, mvp = [], []
    for l in range(DEPTH):
        p = {
            'w_in': w_in[l], 'ssm_lam_re': ssm_lam_re[l], 'ssm_lam_im': ssm_lam_im[l],
            'ssm_log_dt': ssm_log_dt[l], 'ssm_b_re': ssm_b_re[l], 'ssm_b_im': ssm_b_im[l],
            'ssm_c_re': ssm_c_re[l], 'ssm_c_im': ssm_c_im[l], 'ssm_d': ssm_d[l], 'w_glu': w_glu[l],
            'g_attn': g_attn[l], 'g_ssm': g_ssm[l], 'w_out': w_out[l],
            'ln1_g': ln1_g[l], 'ln1_b': ln1_b[l], 'w_mq': w_mq[l], 'w_mo': w_mo[l],
            'ln2_g': ln2_g[l], 'ln2_b': ln2_b[l], 'w_r1': w_r1[l], 'b_r1': b_r1[l],
            'w_r2': w_r2[l], 'b_r2': b_r2[l], 'w_gate': w_gate[l], 'w_up': w_up[l],
            'w_down': w_down[l], 'ln3_g': ln3_g[l], 'ln3_b': ln3_b[l],
        }
        mem_k = (mem_prompt @ w_mk[l]).reshape(bsz, N_MEM, MEM_HEADS, MEM_HD)
        mem_v = (mem_prompt @ w_mv[l]).reshape(bsz, N_MEM, MEM_HEADS, MEM_HD)
        h0 = jnp.zeros((bsz, N_SSM_GROUPS, SSM_STATE), jnp.float32)
        yp, kp, vp, hrp, hip = _layer(yp, p, mem_k, mem_v, h0, h0)
        ys, kn, vn, hrs, his = _layer(ys, p, cache_mem_k[l], cache_mem_v[l], state_ssm_re[l],
                                      state_ssm_im[l], cache_win_k[l], cache_win_v[l])
        wkp.append(kp); wvp.append(vp); wks.append(kn); wvs.append(vn)
        srp.append(hrp); sip.append(hip); srs.append(hrs); sis.append(his)
        mkp.append(mem_k); mvp.append(mem_v)
    return (yp, ys, jnp.stack(wkp), jnp.stack(wvp), jnp.stack(wks), jnp.stack(wvs),
            jnp.stack(srp), jnp.stack(sip), jnp.stack(srs), jnp.stack(sis),
            jnp.stack(mkp), jnp.stack(mvp))
```

```python
import numpy as np
from contextlib import ExitStack
import concourse.bass as bass
import concourse.mybir as mybir
from concourse.bass_utils import run_bass_kernel_spmd

F32 = mybir.dt.float32
BF16 = mybir.dt.bfloat16
AF = mybir.ActivationFunctionType
ALU = mybir.AluOpType

ENGS = ("pe", "act", "dve", "pool", "sp")
NT = 9
TOK = 1152
KPOS = 3200
UPOS = 4224
ALPHA = 2.0 ** 0.25
SC_ATT = 128 ** -0.5
SC_MEM = 512 ** -0.5
NEG = -30000.0
DBG = {}
PSUM_KEYS = {"ps_big", "ps_n", "ptr", "pmm", "ps_s", "ps_nd", "ptk", "ps_sm", "ps_snd", "ps_ss", "ps_new", "pw", "py", "pz", "pa", "pb",
             "pm", "pk", "pq", "ps_o", "ps_d", "pl", "pg", "pu", "po"}


class Sched:
    def __init__(self, nc, n_dma_sems=22):
        self.nc = nc
        self.stack = []
        self.esem = {}
        for e in ENGS:
            self.esem[e] = self._sem("e_" + e)
        self.ecount = {e: 0 for e in ENGS}
        self.sval = {e: 0 for e in ENGS}
        self.dsems = [self._sem("d%d" % i) for i in range(n_dma_sems)]
        self.dcount = [0] * n_dma_sems
        self.rr = {}
        self.reset_stage()

    def _sem(self, name):
        cm = self.nc.semaphore(name)
        s = cm.__enter__()
        self.stack.append(cm)
        return s

    def reset_stage(self):
        self.ops = []
        self.dmap = {}
        self.lastw = {}
        self.readers = {}
        self.known = {e: {} for e in ENGS}

    def alt(self, name, engines):
        i = self.rr.get(name, 0)
        self.rr[name] = i + 1
        return engines[i % len(engines)]

    def _dsem(self, group):
        if group not in self.dmap:
            idx = len(self.dmap)
            assert idx < len(self.dsems), "too many dma groups in stage"
            self.dmap[group] = idx
        return self.dmap[group]

    def _need(self, eng, dep, waits):
        kind, a, b = dep
        kn = self.known[eng]
        key = (kind, a)
        if kn.get(key, -1) >= b:
            return
        kn[key] = b
        waits.append(dep)

    def op(self, eng, fn, reads=(), writes=(), dma=None):
        waits = []
        deps = []
        if eng != "pe":
            ex = [k for k in reads if (k[0] if isinstance(k, tuple) else k) in PSUM_KEYS]
            if ex:
                writes = list(writes) + [k for k in ex if k not in writes]
        for k in reads:
            if k in self.lastw:
                deps.append(self.lastw[k])
        for k in writes:
            if k in self.lastw:
                deps.append(self.lastw[k])
            deps.extend(self.readers.get(k, ()))
        if dma is not None:
            di = self._dsem(dma)
            if self.dcount[di] > 0:
                deps.append(("d", di, self.dcount[di]))
        for d in deps:
            if d[0] == "e" and d[1] == eng and eng == "pe":
                continue
            self._need(eng, d, waits)
        if dma is not None:
            self.dcount[di] += 16
            tok = ("d", di, self.dcount[di])
        else:
            self.ecount[eng] += 1
            tok = ("e", eng, self.ecount[eng])
        for k in writes:
            self.lastw[k] = tok
            self.readers[k] = []
        for k in reads:
            self.readers.setdefault(k, []).append(tok)
        self.ops.append((eng, fn, waits, tok))

    def touch(self, keys, from_keys):
        best = None
        for k in from_keys:
            t = self.lastw.get(k)
            if t is not None and (best is None or t[2] > best[2]):
                best = t
        if best is not None:
            for k in keys:
                self.lastw[k] = best
                self.readers[k] = []

    def emit(self):
        nc = self.nc
        fin = [("d", di, self.dcount[di]) for g, di in self.dmap.items()]
        per = {e: [] for e in ENGS}
        need = set()
        for o in self.ops:
            per[o[0]].append(o)
            for (kind, a, b) in o[2]:
                if kind == "e":
                    need.add((a, b))
        sval = self.sval
        smap = {}
        for (eng, fn, waits, tok) in self.ops:
            if tok[0] == "e" and (tok[1], tok[2]) in need:
                sval[eng] += 1
                smap[(tok[1], tok[2])] = sval[eng]
        esem, dsems = self.esem, self.dsems

        def run(engname, h):
            for (_, fn, waits, tok) in per[engname]:
                for (kind, a, b) in waits:
                    if kind == "e":
                        h.wait_ge(esem[a], smap[(a, b)])
                    else:
                        h.wait_ge(dsems[a], b)
                ins = fn(h)
                if tok[0] == "e":
                    if (tok[1], tok[2]) in smap:
                        ins.then_inc(esem[engname], 1)
                else:
                    ins.then_inc(dsems[tok[1]], 16)
            if engname == "sp":
                for (kind, a, b) in fin:
                    h.wait_ge(dsems[a], b)

        with nc.Block() as block:
            @block.tensor
            def _(h):
                run("pe", h)

            @block.scalar
            def _(h):
                run("act", h)

            @block.vector
            def _(h):
                run("dve", h)

            @block.gpsimd
            def _(h):
                run("pool", h)

            @block.sync
            def _(h):
                run("sp", h)
        self.reset_stage()

    def close(self):
        for cm in reversed(self.stack):
            cm.__exit__(None, None, None)


def o_mm(S, out, lhsT, rhs, start, stop, reads, writes):
    S.op("pe", lambda h: h.matmul(out, lhsT=lhsT, rhs=rhs, start=start, stop=stop), reads=reads, writes=writes)


def o_tr(S, out, in_, ident, reads, writes):
    S.op("pe", lambda h: h.transpose(out=out, in_=in_, identity=ident), reads=reads, writes=writes)


def o_dma(S, eng, out, in_, reads=(), writes=(), grp=None, slow=False):
    if slow:
        S.op(eng, lambda h: h.dma_start(out=out, in_=in_, allow_slow_non_contiguous=True), reads=reads, writes=writes, dma=grp)
    else:
        S.op(eng, lambda h: h.dma_start(out=out, in_=in_), reads=reads, writes=writes, dma=grp)


def o_copy(S, eng, out, in_, reads, writes, scale=None):
    if eng == "act":
        if scale is None:
            S.op("act", lambda h: h.activation(out=out, in_=in_, func=AF.Copy), reads=reads, writes=writes)
        else:
            S.op("act", lambda h: h.activation(out=out, in_=in_, func=AF.Identity, scale=scale), reads=reads, writes=writes)
    else:
        if scale is None:
            S.op(eng, lambda h: h.tensor_copy(out=out, in_=in_), reads=reads, writes=writes)
        else:
            S.op(eng, lambda h: h.tensor_scalar(out=out, in0=in_, scalar1=scale, scalar2=None, op0=ALU.mult), reads=reads, writes=writes)


def o_act(S, out, in_, func, reads, writes, bias=None, scale=None):
    kw = {}
    if bias is not None:
        kw["bias"] = bias
    if scale is not None:
        kw["scale"] = scale
    S.op("act", lambda h: h.activation(out=out, in_=in_, func=func, **kw), reads=reads, writes=writes)


def o_tt(S, eng, out, in0, in1, op, reads, writes):
    S.op(eng, lambda h: h.tensor_tensor(out=out, in0=in0, in1=in1, op=op), reads=reads, writes=writes)


def o_ts(S, eng, out, in0, s1, op0, reads, writes, s2=None, op1=None):
    if op1 is None:
        S.op(eng, lambda h: h.tensor_scalar(out=out, in0=in0, scalar1=s1, scalar2=None, op0=op0), reads=reads, writes=writes)
    else:
        S.op(eng, lambda h: h.tensor_scalar(out=out, in0=in0, scalar1=s1, scalar2=s2, op0=op0, op1=op1), reads=reads, writes=writes)


def o_stt(S, out, in0, scalar, in1, op0, op1, reads, writes):
    S.op("dve", lambda h: h.scalar_tensor_tensor(out=out, in0=in0, scalar=scalar, in1=in1, op0=op0, op1=op1), reads=reads, writes=writes)


def o_memset(S, eng, ap, val, writes):
    S.op(eng, lambda h: h.memset(ap, val), writes=writes)


def o_recip(S, out, in_, reads, writes):
    S.op("dve", lambda h: h.reciprocal(out=out, in_=in_), reads=reads, writes=writes)


def make_ident(S, t, key):
    o_memset(S, "pool", t[:], 0.0, [key])
    S.op("pool", lambda h: h.affine_select(out=t[:], in_=t[:], pattern=[[-1, 128]], compare_op=ALU.not_equal,
                                           fill=1.0, base=0, channel_multiplier=1), reads=[key], writes=[key])


class St:
    def __init__(self, nc, name):
        self.nc = nc
        self.name = name
        self.es = ExitStack()

    def __enter__(self):
        self.es.__enter__()
        return self

    def __exit__(self, *a):
        return self.es.__exit__(*a)

    def sb(self, n, shape, dt):
        return self.es.enter_context(self.nc.sbuf_tensor(self.name + "_" + n, shape, dt))

    def ps(self, n, shape, dt):
        return self.es.enter_context(self.nc.psum_tensor(self.name + "_" + n, shape, dt))


def stage_A(nc, S, T):
    with St(nc, "A") as st:
        Wb = st.sb("Wb", [128, 16, 4096], BF16)
        ident = st.sb("ident", [128, 128], F32)
        xin = st.sb("xin", [128, 2, 2048], F32)
        xT = st.sb("xT", [128, 16, 512], BF16)
        ofm = st.sb("ofm", [128, 4, 512], BF16)
        otf = st.sb("otf", [128, 2, 512], F32)
        otb = st.sb("otb", [128, 2, 512], BF16)
        ptr = [st.ps("ptr%d" % i, [128, 512], F32) for i in range(2)]
        pmm = [st.ps("pmm%d" % i, [128, 512], F32) for i in range(4)]
        make_ident(S, ident, "ident")
        w_in = T["w_in"]
        for blk in (6, 7, 2, 3, 4, 5, 0, 1):
            o_dma(S, "pool", Wb[:, :, blk * 512:(blk + 1) * 512],
                  w_in[:, blk * 512:(blk + 1) * 512].rearrange("(c p) n -> p c n", p=128),
                  writes=[("Wb", blk)], grp="Wb%d" % blk)
        groups = [[4 * g + j for j in range(4)] for g in range(8)] + [[32]]
        if DBG.get("A_groups") is not None:
            groups = [groups[i] for i in DBG["A_groups"]]
        cnt = {"x": 0, "tr": 0, "fm": 0, "mm": 0, "tf": 0, "tb": 0}
        for tiles in groups:
            ntok = 128 * len(tiles)
            t0 = tiles[0]
            far = t0 < 8
            near = 8 <= t0 < 24
            own = t0 >= 24
            for j, tl in enumerate(tiles):
                slot = cnt["x"] % 2
                cnt["x"] += 1
                src = T["xs"][:, :] if tl == 32 else T["xw"][tl * 128:(tl + 1) * 128, :]
                o_dma(S, "sp", xin[:, slot, :], src, writes=[("xin", slot)], grp="xin%d" % slot)
                for b4 in range(4):
                    pi = cnt["tr"] % 2
                    cnt["tr"] += 1
                    for q in range(4):
                        c = b4 * 4 + q
                        o_tr(S, ptr[pi][:, q * 128:(q + 1) * 128], xin[:, slot, c * 128:(c + 1) * 128], ident[:],
                             reads=[("xin", slot), "ident"] + ([("Wb", i) for i in range(8)] if DBG.get("A_waitW") else []), writes=[("ptr", pi)])
                    o_copy(S, "act" if pi == 0 else "dve", xT[:, b4 * 4:(b4 + 1) * 4, j * 128:(j + 1) * 128],
                           ptr[pi][:, :].rearrange("p (a b) -> p a b", a=4), reads=[("ptr", pi)], writes=["xT"])
            if t0 == 32:
                qcol, kcol, ucol = 1024, 3072, 4096
            else:
                qcol, kcol, ucol = (t0 - 24) * 128, (t0 - 8) * 128, t0 * 128
            fm = []
            if own:
                fm += [(h * 128, T["qT"][h, :, qcol:qcol + ntok]) for h in range(8)]
            if own or near:
                fm += [(1024 + h * 128, T["kT"][h, :, kcol:kcol + ntok]) for h in range(8)]
            fm += [(3072 + c * 128, T["uT"][c, :, ucol:ucol + ntok]) for c in range(8)]
            if DBG.get("A_nofm"):
                fm = []
            if DBG.get("A_fmn") is not None:
                fm = fm[:DBG["A_fmn"]]
            for (wc, dst) in fm:
                pi = cnt["mm"] % 4
                cnt["mm"] += 1
                for k in range(16):
                    o_mm(S, pmm[pi][:, 0:ntok], Wb[:, k, wc:wc + 128], xT[:, k, 0:ntok], k == 0, k == 15,
                         reads=["xT", ("Wb", wc // 512)], writes=[("pmm", pi)])
                oi = cnt["fm"] % 4
                cnt["fm"] += 1
                o_copy(S, "act" if oi % 2 == 0 else "dve", ofm[:, oi, 0:ntok], pmm[pi][:, 0:ntok],
                       reads=[("pmm", pi)], writes=[("ofm", oi)])
                o_dma(S, DBG.get("A_stq", "sp"), dst, ofm[:, oi, 0:ntok], reads=[("ofm", oi)], grp="ofm%d" % oi)
            if (own or near) and not DBG.get("A_notm"):
                for j, tl in enumerate(tiles):
                    row_kv = (tl - 8) * 128 if tl < 32 else 3072
                    row_o = (tl - 24) * 128 if tl < 32 else 1024
                    banks = []
                    if own:
                        banks += [("k", 1024), ("k", 1536)]
                    banks += [("v", 2048), ("v", 2560)]
                    if DBG.get("A_banks") is not None:
                        banks = [banks[i] for i in DBG["A_banks"]]
                    for (kv, wc) in banks:
                        pi = cnt["mm"] % 4
                        cnt["mm"] += 1
                        for k in range(16):
                            o_mm(S, pmm[pi][:, :], xT[:, k, j * 128:(j + 1) * 128], Wb[:, k, wc:wc + 512], k == 0, k == 15,
                                 reads=["xT", ("Wb", wc // 512)], writes=[("pmm", pi)])
                        colo = wc - (1024 if kv == "k" else 2048)
                        if own and not DBG.get("A_nootf"):
                            fi = cnt["tf"] % 2
                            cnt["tf"] += 1
                            o_copy(S, DBG.get("A_otfe", "act"), otf[:, fi, :], pmm[pi][:, :], reads=[("pmm", pi)], writes=[("otf", fi)])
                            o_dma(S, DBG.get("A_stq", "sp"), T["Kout" if kv == "k" else "Vout"][row_o:row_o + 128, colo:colo + 512],
                                  otf[:, fi, :], reads=[("otf", fi)], grp="otf%d" % fi)
                        if kv == "v" and not DBG.get("A_nootb"):
                            bi = cnt["tb"] % 2
                            cnt["tb"] += 1
                            o_copy(S, DBG.get("A_otbe", "dve"), otb[:, bi, :], pmm[pi][:, :], reads=[("pmm", pi)] + ([("otf", 0), ("otf", 1)] if DBG.get("A_ser") else []), writes=[("otb", bi)])
                            o_dma(S, DBG.get("A_stq", "sp"), T["V_scr"][row_kv:row_kv + 128, colo:colo + 512], otb[:, bi, :],
                                  reads=[("otb", bi)], grp="otb%d" % bi)
        S.emit()


def stage_B(nc, S, T):
    with St(nc, "B") as st:
        kT = st.sb("kT", [128, 2, KPOS], BF16)
        Vh = st.sb("Vh", [128, 2, 25, 128], BF16)
        qT = st.sb("qT", [128, 2, TOK], BF16)
        pmask = st.sb("pmask", [128, 17, 128], BF16)
        smask = st.sb("smask", [128, 128], BF16)
        smaskn = st.sb("smaskn", [128, 128], BF16)
        vb = st.sb("vb", [128, 24], F32)
        gatt = st.sb("gatt", [128, 8], F32)
        onesb = st.sb("onesb", [128, 128], BF16)
        identb = st.sb("identb", [128, 128], BF16)
        p = st.sb("p", [128, 2, 512], BF16)
        pm = st.sb("pm", [128, 2, 512], BF16)
        onesv = st.sb("onesv", [128, 24, 128], BF16)
        rd = st.sb("rd", [128, 2, 128], F32)
        a32 = st.sb("a32", [128, 2, 128], F32)
        asq = st.sb("asq", [128, 8, TOK], BF16)
        ag = st.sb("ag", [128, 8, TOK], BF16)
        kc = st.sb("kc", [128, 2, 16, 128], BF16)
        vc = st.sb("vc", [128, 2, 16, 128], BF16)
        kcT = st.sb("kcT", [128, 2048], BF16)
        psm = st.sb("psm", [128, 128], BF16)
        psn = st.sb("psn", [128, 128], BF16)
        pmm_ = st.sb("pmm_", [128, 128], BF16)
        pmn = st.sb("pmn", [128, 128], BF16)
        rd8 = st.sb("rd8", [128, 8], F32)
        a8 = st.sb("a8", [128, 8], F32)
        ssa = st.sb("ssa", [128, 16], F32)
        ps_s2 = [st.ps("ps_s%d" % i, [128, 512], F32) for i in range(2)]
        ps_n2 = [st.ps("ps_n%d" % i, [128, 512], F32) for i in range(2)]
        ps_d2 = [st.ps("ps_d%d" % i, [128, 512], F32) for i in range(2)]
        ptk1 = st.ps("ptk", [128, 1024], BF16)
        ptk = [ptk1, ptk1]
        ps_big = st.ps("ps_big", [128, 512], F32)
        ps_sm = ps_big[:, 0:128]
        ps_new = ps_big[:, 128:256]
        ps_snd = ps_big[:, 256:272]
        ps_ss = ps_big[:, 272:288]

        o_memset(S, "dve", onesb[:], 1.0, ["onesb"])
        make_ident(S, identb, "identb")
        o_dma(S, "pool", pmask[:].rearrange("p a b -> p (a b)"), T["pmask"][:, :], writes=["pmask"], grp="pmask")
        o_dma(S, "pool", smask[:], T["smask"][:, :], writes=["smask"], grp="smask")
        o_dma(S, "pool", smaskn[:], T["smaskn"][:, :], writes=["smaskn"], grp="smaskn")
        o_dma(S, "sp", vb[:], T["vbias"][:, :], writes=["vb"], grp="vb")
        o_dma(S, "sp", gatt[:], T["gatt"][:, :], writes=["gatt"], grp="gatt")
        for j in range(24):
            o_ts(S, "pool", onesv[:, j, :], onesb[:], vb[:, j:j + 1], ALU.mult, reads=["onesb", "vb"], writes=["onesv"])
        o_memset(S, "pool", asq[:, :, 1024:TOK], 0.0, [("asq", h, 8) for h in range(8)])
        o_memset(S, "pool", ag[:, :, 1024:TOK], 0.0, [("ag", h, 8) for h in range(8)])
        it = 0
        blk_i = 0
        sh_i = 0
        for h in range(8):
            b = h % 2
            o_dma(S, "sp", kT[:, b, :], T["kT"][h, :, :], writes=[("kT", b)], grp="kT%d" % b)
            o_dma(S, "sp", Vh[:, b, :, :], T["V_scr"][:, h * 128:(h + 1) * 128].rearrange("(c p) n -> p c n", p=128),
                  writes=[("Vh", b)], grp="Vh%d" % b)
            o_dma(S, "sp", qT[:, b, :], T["qT"][h, :, :], writes=[("qT", b)], grp="qT%d" % b)
            groups_ = [(m, c0, n) for m in range(8) for (c0, n) in ((0, 4), (4, 4), (8, 4), (12, 4), (16, 1))]

            def score(gi):
                m, c0, n = groups_[gi]
                sg = (g0 + gi) % 2
                for q in range(n):
                    kcx = m + c0 + q
                    o_mm(S, ps_s2[sg][:, q * 128:(q + 1) * 128], kT[:, b, kcx * 128:(kcx + 1) * 128], qT[:, b, m * 128:(m + 1) * 128],
                         True, True, reads=[("kT", b), ("qT", b)], writes=[("ps_s", sg)])
                o_act(S, p[:, sg, 0:n * 128], ps_s2[sg][:, 0:n * 128], AF.Exp, reads=[("ps_s", sg)], writes=[("p", sg)], scale=SC_ATT)
                o_tt(S, "dve" if sg == 0 else "pool", pm[:, sg, 0:n * 128], p[:, sg, 0:n * 128],
                     pmask[:, c0:c0 + n, :].rearrange("p a b -> p (a b)"), ALU.mult,
                     reads=[("p", sg), "pmask"], writes=[("pm", sg)])

            g0 = it
            score(0)
            for gi, (m, c0, n) in enumerate(groups_):
                if gi + 1 < len(groups_):
                    score(gi + 1)
                sg = (g0 + gi) % 2
                sl = (blk_i + m) % 2
                for q in range(n):
                    kcx = m + c0 + q
                    cc = c0 + q
                    o_mm(S, ps_n2[sl][:, 0:128], Vh[:, b, kcx, :], pm[:, sg, q * 128:(q + 1) * 128], cc == 0, cc == 16,
                         reads=[("Vh", b), ("pm", sg)], writes=[("ps_n", sl)])
                for q in range(n):
                    kcx = m + c0 + q
                    cc = c0 + q
                    o_mm(S, ps_d2[sl][:, 0:128], onesv[:, kcx, :], pm[:, sg, q * 128:(q + 1) * 128], cc == 0, cc == 16,
                         reads=["onesv", ("pm", sg)], writes=[("ps_d", sl)])
                if c0 == 16:
                    o_recip(S, rd[:, sl, :], ps_d2[sl][:, 0:128], reads=[("ps_d", sl)], writes=[("rd", sl)])
                    o_tt(S, "dve", a32[:, sl, :], ps_n2[sl][:, 0:128], rd[:, sl, :], ALU.mult,
                         reads=[("ps_n", sl), ("rd", sl)], writes=[("a32", sl)])
                    o_tt(S, "pool", asq[:, h, m * 128:(m + 1) * 128], a32[:, sl, :], a32[:, sl, :], ALU.mult,
                         reads=[("a32", sl)], writes=[("asq", h, m)])
                    o_ts(S, "pool", ag[:, h, m * 128:(m + 1) * 128], a32[:, sl, :], gatt[:, h:h + 1], ALU.mult,
                         reads=[("a32", sl), "gatt"], writes=[("ag", h, m)])
            it += len(groups_)
            blk_i += 8
            o_mm(S, ps_new[:, :], kT[:, b, 3072:3200], qT[:, b, 1024:1152], True, True,
                 reads=[("kT", b), ("qT", b)], writes=["ps_big"])
            o_act(S, psn[:], ps_new[:, :], AF.Exp, reads=["ps_big"], writes=["psn"], scale=SC_ATT)
            o_tt(S, "dve", pmn[:], psn[:], smaskn[:], ALU.mult, reads=["psn", "smaskn"], writes=["pmn"])
            for s in range(4):
                sb_ = sh_i % 2
                sh_i += 1
                o_dma(S, "pool", kc[:, sb_, :, :], T["cwk"][s, :, h * 128:(h + 1) * 128].rearrange("(c p) n -> p c n", p=128),
                      writes=[("kc", sb_)], grp="kc%d" % sb_)
                o_dma(S, "pool", vc[:, sb_, :, :], T["cwv"][s, :, h * 128:(h + 1) * 128].rearrange("(c p) n -> p c n", p=128),
                      writes=[("vc", sb_)], grp="vc%d" % sb_)
                for half in range(2):
                    for q8 in range(8):
                        cc = half * 8 + q8
                        o_tr(S, ptk[half][:, q8 * 128:(q8 + 1) * 128], kc[:, sb_, cc, :], identb[:],
                             reads=[("kc", sb_), "identb"], writes=["ptk"])
                    o_copy(S, "act" if half == 0 else "dve", kcT[:, half * 1024:(half + 1) * 1024], ptk[half][:, :],
                           reads=["ptk"], writes=[("kcT", half)])
                q0 = 1024 + 32 * s
                for cc in range(16):
                    o_mm(S, ps_sm[:, cc * 8:(cc + 1) * 8], kcT[:, cc * 128:(cc + 1) * 128], qT[:, b, q0:q0 + 8], True, True,
                         reads=[("kcT", cc // 8), ("qT", b)], writes=["ps_big"])
                o_act(S, psm[:], ps_sm[:, 0:128], AF.Exp, reads=["ps_big"], writes=["psm"], scale=SC_ATT)
                o_tt(S, "dve", pmm_[:], psm[:], smask[:], ALU.mult, reads=["psm", "smask"], writes=["pmm_"])
                for cc in range(16):
                    o_mm(S, ps_snd[:, 0:8], vc[:, sb_, cc, :], pmm_[:, cc * 8:(cc + 1) * 8], cc == 0, False,
                         reads=[("vc", sb_), "pmm_"], writes=["ps_big"])
                o_mm(S, ps_snd[:, 0:8], Vh[:, b, 24, :], pmn[:, 32 * s:32 * s + 8], False, True,
                     reads=[("Vh", b), "pmn"], writes=["ps_big"])
                for cc in range(16):
                    o_mm(S, ps_snd[:, 8:16], onesb[:], pmm_[:, cc * 8:(cc + 1) * 8], cc == 0, False,
                         reads=["onesb", "pmm_"], writes=["ps_big"])
                o_mm(S, ps_snd[:, 8:16], onesb[:], pmn[:, 32 * s:32 * s + 8], False, True,
                     reads=["onesb", "pmn"], writes=["ps_big"])
                o_recip(S, rd8[:], ps_snd[:, 8:16], reads=["ps_big"], writes=["rd8"])
                o_tt(S, "dve", a8[:], ps_snd[:, 0:8], rd8[:], ALU.mult, reads=["ps_big", "rd8"], writes=["a8"])
                o_tt(S, "pool", asq[:, h, q0:q0 + 8], a8[:], a8[:], ALU.mult, reads=["a8"], writes=[("asq", h, 8)])
                o_ts(S, "pool", ag[:, h, q0:q0 + 8], a8[:], gatt[:, h:h + 1], ALU.mult, reads=["a8", "gatt"], writes=[("ag", h, 8)])
        for t in range(NT):
            for h in range(8):
                o_mm(S, ps_ss[:, t:t + 1], asq[:, h, t * 128:(t + 1) * 128], onesb[:, 0:1], h == 0, h == 7,
                     reads=[("asq", h, t), "onesb"], writes=["ps_big"])
        o_act(S, ssa[:, 0:NT], ps_ss[:, 0:NT], AF.Sqrt, reads=["ps_big"], writes=["ssa"], bias=1e-6, scale=1.0 / 1024.0)
        o_recip(S, ssa[:, 0:NT], ssa[:, 0:NT], reads=["ssa"], writes=["ssa"])
        o_dma(S, "sp", T["rstd_a"][:, :], ssa[:, 0:NT], reads=["ssa"], grp="rstd")
        o_dma(S, "sp", T["mixT"][0:8, :, :].rearrange("h p t -> p h t"), ag[:, :, :],
              reads=[("ag", h, m) for h in range(8) for m in range(9)], grp="agst")
        S.emit()


def stage_C(nc, S, T):
    with St(nc, "C") as st:
        lre = st.sb("lre", [128, 32], F32)
        lim = st.sb("lim", [128, 32], F32)
        ldt = st.sb("ldt", [128, 32], F32)
        tA = st.sb("tA", [128, 32], F32)
        tB = st.sb("tB", [128, 32], F32)
        tC = st.sb("tC", [128, 32], F32)
        tI = st.sb("tI", [128, 32], mybir.dt.int32)
        mag = st.sb("mag", [128, 32], F32)
        fre = st.sb("fre", [128, 32], F32)
        fim = st.sb("fim", [128, 32], F32)
        nfim = st.sb("nfim", [128, 32], F32)
        pwr = st.sb("pwr", [128, 11, 32], F32)
        pwi = st.sb("pwi", [128, 11, 32], F32)
        npwi = st.sb("npwi", [128, 11, 32], F32)
        BTre = st.sb("BTre", [128, 32, 128], BF16)
        BTim = st.sb("BTim", [128, 32, 128], BF16)
        CTre = st.sb("CTre", [128, 32, 128], BF16)
        CTim = st.sb("CTim", [128, 32, 128], BF16)
        dcol = st.sb("dcol", [128, 8], F32)
        h0r = st.sb("h0r", [128, 32, 4], F32)
        h0i = st.sb("h0i", [128, 32, 4], F32)
        uT = st.sb("uT", [128, 2, UPOS], BF16)
        xr2 = st.sb("xr", [128, 2, UPOS], F32)
        xi2 = st.sb("xi", [128, 2, UPOS], F32)
        T0r = st.sb("T0r", [128, 1536], F32)
        T0i = st.sb("T0i", [128, 1536], F32)
        T1r = st.sb("T1r", [128, 768], F32)
        T1i = st.sb("T1i", [128, 768], F32)
        Br = st.sb("Br", [128, 1024], F32)
        Bi = st.sb("Bi", [128, 1024], F32)
        Bsr = st.sb("Bsr", [128, 4, 8], F32)
        Bsi = st.sb("Bsi", [128, 4, 8], F32)
        Hr = st.sb("Hr", [128, 4], F32)
        Hi = st.sb("Hi", [128, 4], F32)
        tmp = st.sb("tmp", [128, 4, 512], F32)
        hbr = st.sb("hbr", [128, TOK], BF16)
        hbi = st.sb("hbi", [128, TOK], BF16)
        hfr = st.sb("hfr", [128, 32], F32)
        hfi = st.sb("hfi", [128, 32], F32)
        hsr = st.sb("hsr", [128, 32, 4], F32)
        hsi = st.sb("hsi", [128, 32, 4], F32)
        ysb = st.sb("ysb", [128, TOK], F32)
        gt1 = st.sb("gt1", [128, TOK], F32)
        yg = st.sb("yg", [128, 2, TOK], F32)
        ygb = st.sb("ygb", [128, 2, TOK], BF16)
        pw = [st.ps("pw%d" % i, [128, 512], F32) for i in range(4)]
        py = [st.ps("py%d" % i, [128, 512], F32) for i in range(3)]

        for (t_, nm) in ((lre, "lamre"), (lim, "lamim"), (ldt, "logdt")):
            o_dma(S, "sp", t_[:], T[nm][:, :], writes=[nm], grp=nm)
        o_dma(S, "sp", dcol[:], T["dcol"][:, :], writes=["dcol"], grp="dcol")
        o_dma(S, "sp", h0r[:].rearrange("p a b -> p (a b)"), T["h0re"][:, :], writes=["h0r"], grp="h0r")
        o_dma(S, "sp", h0i[:].rearrange("p a b -> p (a b)"), T["h0im"][:, :], writes=["h0i"], grp="h0i")
        for (t_, nm) in ((BTre, "BTre"), (BTim, "BTim"), (CTre, "CTre"), (CTim, "CTim")):
            o_dma(S, "pool", t_[:].rearrange("p a b -> p (a b)"), T[nm][:, :], writes=[nm], grp=nm)
        P = ["par"]
        o_act(S, ldt[:], ldt[:], AF.Exp, reads=["logdt"], writes=["logdt"] + P)
        o_tt(S, "dve", tA[:], lre[:], ldt[:], ALU.mult, reads=["lamre", "logdt"], writes=P)
        o_act(S, mag[:], tA[:], AF.Exp, reads=P, writes=P)
        o_tt(S, "dve", tB[:], lim[:], ldt[:], ALU.mult, reads=["lamim", "logdt"], writes=P)

        def sin_of(dst, shift):
            o_ts(S, "dve", tC[:], tB[:], shift, ALU.add, reads=P, writes=P, s2=1.0 / (2 * np.pi), op1=ALU.mult)
            S.op("dve", lambda h: h.tensor_copy(out=tI[:], in_=tC[:]), reads=P, writes=P)
            S.op("dve", lambda h: h.tensor_copy(out=tA[:], in_=tI[:]), reads=P, writes=P)
            o_tt(S, "dve", tC[:], tC[:], tA[:], ALU.subtract, reads=P, writes=P)
            o_ts(S, "dve", tA[:], tC[:], 0.5, ALU.is_gt, reads=P, writes=P)
            o_tt(S, "dve", tC[:], tC[:], tA[:], ALU.subtract, reads=P, writes=P)
            o_ts(S, "dve", tA[:], tC[:], -0.5, ALU.is_lt, reads=P, writes=P)
            o_tt(S, "dve", tC[:], tC[:], tA[:], ALU.add, reads=P, writes=P)
            o_ts(S, "dve", tC[:], tC[:], 2 * np.pi, ALU.mult, reads=P, writes=P, s2=3.14159, op1=ALU.min)
            o_ts(S, "dve", tC[:], tC[:], -3.14159, ALU.max, reads=P, writes=P)
            o_act(S, dst, tC[:], AF.Sin, reads=P, writes=P)

        sin_of(pwi[:, 0, :], 0.0)
        sin_of(pwr[:, 0, :], np.pi / 2)
        o_tt(S, "dve", pwr[:, 0, :], pwr[:, 0, :], mag[:], ALU.mult, reads=P, writes=P)
        o_tt(S, "dve", pwi[:, 0, :], pwi[:, 0, :], mag[:], ALU.mult, reads=P, writes=P)
        o_tt(S, "dve", tA[:], lre[:], lre[:], ALU.mult, reads=P + ["lamre"], writes=P)
        o_tt(S, "dve", tC[:], lim[:], lim[:], ALU.mult, reads=P + ["lamim"], writes=P)
        o_tt(S, "dve", tA[:], tA[:], tC[:], ALU.add, reads=P, writes=P)
        o_recip(S, tA[:], tA[:], reads=P, writes=P)
        o_ts(S, "dve", tB[:], pwr[:, 0, :], -1.0, ALU.add, reads=P, writes=P)
        o_tt(S, "dve", fre[:], tB[:], lre[:], ALU.mult, reads=P, writes=P)
        o_tt(S, "dve", tC[:], pwi[:, 0, :], lim[:], ALU.mult, reads=P, writes=P)
        o_tt(S, "dve", fre[:], fre[:], tC[:], ALU.add, reads=P, writes=P)
        o_tt(S, "dve", fre[:], fre[:], tA[:], ALU.mult, reads=P, writes=P)
        o_tt(S, "dve", fim[:], pwi[:, 0, :], lre[:], ALU.mult, reads=P, writes=P)
        o_tt(S, "dve", tC[:], tB[:], lim[:], ALU.mult, reads=P, writes=P)
        o_tt(S, "dve", fim[:], fim[:], tC[:], ALU.subtract, reads=P, writes=P)
        o_tt(S, "dve", fim[:], fim[:], tA[:], ALU.mult, reads=P, writes=P)
        o_ts(S, "dve", nfim[:], fim[:], -1.0, ALU.mult, reads=P, writes=P)
        for k in range(10):
            o_tt(S, "dve", tA[:], pwr[:, k, :], pwr[:, k, :], ALU.mult, reads=P, writes=P)
            o_tt(S, "dve", tC[:], pwi[:, k, :], pwi[:, k, :], ALU.mult, reads=P, writes=P)
            o_tt(S, "dve", pwr[:, k + 1, :], tA[:], tC[:], ALU.subtract, reads=P, writes=P)
            o_tt(S, "dve", tA[:], pwr[:, k, :], pwi[:, k, :], ALU.mult, reads=P, writes=P)
            o_ts(S, "dve", pwi[:, k + 1, :], tA[:], 2.0, ALU.mult, reads=P, writes=P)
        o_ts(S, "dve", npwi[:].rearrange("p a b -> p (a b)"), pwi[:].rearrange("p a b -> p (a b)"), -1.0, ALU.mult, reads=P, writes=P)
        o_memset(S, "pool", hbr[:, 1024:TOK], 0.0, ["hbr_s"])
        o_memset(S, "pool", hbi[:, 1024:TOK], 0.0, ["hbi_s"])

        def cma(dr, di, er, ei, orr, oi, k, tau, rk, wk):
            pr = pwr[:, k, tau:tau + 1]
            pi_ = pwi[:, k, tau:tau + 1]
            npi = npwi[:, k, tau:tau + 1]
            wr_ = [(w_, "r") for w_ in wk]
            wi_ = [(w_, "i") for w_ in wk]
            o_stt(S, dr, er, pr, orr, ALU.mult, ALU.add, reads=rk + P, writes=wr_)
            o_stt(S, di, ei, pr, oi, ALU.mult, ALU.add, reads=rk + P, writes=wi_)
            o_stt(S, dr, ei, npi, dr, ALU.mult, ALU.add, reads=rk + P, writes=wr_)
            o_stt(S, di, er, pi_, di, ALU.mult, ALU.add, reads=rk + P, writes=wi_ + list(wk))
            S.touch(wk, wr_ + wi_)

        blocks = [(i * 512, 512) for i in range(8)] + [(4096, 128)]
        wi = 0
        for c in range(8):
            ub = c % 2
            o_dma(S, "sp", uT[:, ub, :], T["uT"][c, :, :], writes=[("uT", ub)], grp="uT%d" % ub)
            for j in range(4):
                tau = 4 * c + j
                xr = xr2[:, tau % 2, :]
                xi = xi2[:, tau % 2, :]
                XK = ("x", tau % 2)
                for (c0, w) in blocks:
                    p0 = pw[(wi * 2) % 4]
                    p1 = pw[(wi * 2 + 1) % 4]
                    k0 = ("pw", (wi * 2) % 4)
                    k1 = ("pw", (wi * 2 + 1) % 4)
                    ts_ = wi % 2
                    wi += 1
                    o_mm(S, p0[:, 0:w], BTre[:, tau, :], uT[:, ub, c0:c0 + w], True, True, reads=["BTre", ("uT", ub)], writes=[k0])
                    o_mm(S, p1[:, 0:w], BTim[:, tau, :], uT[:, ub, c0:c0 + w], True, True, reads=["BTim", ("uT", ub)], writes=[k1])
                    o_copy(S, "act", tmp[:, 2 * ts_, 0:w], p1[:, 0:w], reads=[k1] + P, writes=[("tmp", 2 * ts_)], scale=nfim[:, tau:tau + 1])
                    o_copy(S, "act", tmp[:, 2 * ts_ + 1, 0:w], p0[:, 0:w], reads=[k0] + P, writes=[("tmp", 2 * ts_ + 1)], scale=fim[:, tau:tau + 1])
                    o_stt(S, xr[:, c0:c0 + w], p0[:, 0:w], fre[:, tau:tau + 1], tmp[:, 2 * ts_, 0:w], ALU.mult, ALU.add,
                          reads=[k0, ("tmp", 2 * ts_)] + P, writes=[XK])
                    o_stt(S, xi[:, c0:c0 + w], p1[:, 0:w], fre[:, tau:tau + 1], tmp[:, 2 * ts_ + 1, 0:w], ALU.mult, ALU.add,
                          reads=[k1, ("tmp", 2 * ts_ + 1)] + P, writes=[XK])
                X = [XK]
                xs_r = xr[:, 4096:UPOS:32]
                xs_i = xi[:, 4096:UPOS:32]
                cma(xs_r, xs_i, h0r[:, tau, :], h0i[:, tau, :], xs_r, xs_i, 0, tau, X + ["h0r", "h0i"], X)
                src_r, src_i, n = xr, xi, 3072
                dsts = [(T0r, T0i), (T1r, T1i)]
                for k in range(10):
                    dr_, di_ = dsts[k % 2]
                    no = n // 2
                    cma(dr_[:, 0:no], di_[:, 0:no], src_r[:, 0:n:2], src_i[:, 0:n:2], src_r[:, 1:n:2], src_i[:, 1:n:2],
                        k, tau, X + ["tree"], ["tree"])
                    src_r, src_i, n = dr_, di_, no
                cma(Hr[:, 0:1], Hi[:, 0:1], src_r[:, 0:1], src_i[:, 0:1], src_r[:, 1:2], src_i[:, 1:2], 10, tau, ["tree"], ["H"])
                cma(Hr[:, 1:2], Hi[:, 1:2], Hr[:, 0:1], Hi[:, 0:1], src_r[:, 2:3], src_i[:, 2:3], 10, tau, ["tree", "H"], ["H"])
                cma(xr[:, 3072:3073], xi[:, 3072:3073], Hr[:, 1:2], Hi[:, 1:2], xr[:, 3072:3073], xi[:, 3072:3073], 0, tau, X + ["H"], X)
                cur = (xr[:, 3072:4096], xi[:, 3072:4096], XK)
                alt = (Br[:, :], Bi[:, :], "B")
                for k in range(10):
                    s_ = 1 << k
                    cr, ci, ck = cur
                    ar_, ai_, ak = alt
                    cma(ar_[:, s_:1024], ai_[:, s_:1024], cr[:, 0:1024 - s_], ci[:, 0:1024 - s_], cr[:, s_:1024], ci[:, s_:1024],
                        k, tau, [ck], [ak])
                    o_copy(S, "act", ar_[:, 0:s_], cr[:, 0:s_], reads=[ck], writes=[ak])
                    o_copy(S, "pool", ai_[:, 0:s_], ci[:, 0:s_], reads=[ck], writes=[ak])
                    cur, alt = alt, cur
                sr = xr[:, 4096:UPOS].rearrange("p (s j) -> p s j", j=32)[:, :, 0:8]
                si_ = xi[:, 4096:UPOS].rearrange("p (s j) -> p s j", j=32)[:, :, 0:8]
                cur = (sr, si_, XK)
                alt = (Bsr[:, :, :], Bsi[:, :, :], "Bs")
                for k in range(3):
                    s_ = 1 << k
                    cr, ci, ck = cur
                    ar_, ai_, ak = alt
                    cma(ar_[:, :, s_:8], ai_[:, :, s_:8], cr[:, :, 0:8 - s_], ci[:, :, 0:8 - s_], cr[:, :, s_:8], ci[:, :, s_:8],
                        k, tau, [ck], [ak])
                    o_copy(S, "act", ar_[:, :, 0:s_], cr[:, :, 0:s_], reads=[ck], writes=[ak])
                    o_copy(S, "pool", ai_[:, :, 0:s_], ci[:, :, 0:s_], reads=[ck], writes=[ak])
                    cur, alt = alt, cur
                fr, fi_, fk = cur
                o_copy(S, "act", hfr[:, tau:tau + 1], xr[:, 4095:4096], reads=X, writes=["hf"])
                o_copy(S, "act", hfi[:, tau:tau + 1], xi[:, 4095:4096], reads=X, writes=["hf"])
                o_copy(S, "pool", hsr[:, tau, :], fr[:, :, 7], reads=[fk], writes=["hs"])
                o_copy(S, "pool", hsi[:, tau, :], fi_[:, :, 7], reads=[fk], writes=["hs"])
                o_copy(S, "act", hbr[:, 0:1024], xr[:, 3072:4096], reads=X, writes=["hbr"])
                o_copy(S, "act", hbi[:, 0:1024], xi[:, 3072:4096], reads=X, writes=["hbi"], scale=-1.0)
                o_copy(S, "pool", hbr[:, 1024:TOK].rearrange("p (s j) -> p s j", j=32)[:, :, 0:8], fr, reads=[fk, "hbr_s"], writes=["hbr_s"])
                o_ts(S, "pool", hbi[:, 1024:TOK].rearrange("p (s j) -> p s j", j=32)[:, :, 0:8], fi_, -1.0, ALU.mult,
                     reads=[fk, "hbi_s"], writes=["hbi_s"])
                for bi, (c0, w) in enumerate([(0, 512), (512, 512), (1024, 128)]):
                    o_mm(S, py[bi][:, 0:w], CTre[:, tau, :], hbr[:, c0:c0 + w], j == 0, False,
                         reads=["CTre", "hbr", "hbr_s"], writes=[("py", bi)])
                    o_mm(S, py[bi][:, 0:w], CTim[:, tau, :], hbi[:, c0:c0 + w], False, j == 3,
                         reads=["CTim", "hbi", "hbi_s"], writes=[("py", bi)])
            yb_ = c % 2
            for bi, (c0, w) in enumerate([(0, 512), (512, 512), (1024, 128)]):
                o_stt(S, ysb[:, c0:c0 + w], uT[:, ub, 3072 + c0:3072 + c0 + w], dcol[:, c:c + 1], py[bi][:, 0:w], ALU.mult, ALU.add,
                      reads=[("uT", ub), ("py", bi), "dcol"], writes=["ysb"])
            o_tt(S, "pool", gt1[:], ysb[:], ysb[:], ALU.mult, reads=["ysb"], writes=["gt1"])
            o_ts(S, "dve", gt1[:], gt1[:], 0.044715, ALU.mult, reads=["gt1"], writes=["gt1"], s2=1.0, op1=ALU.add)
            o_tt(S, "dve", gt1[:], gt1[:], ysb[:], ALU.mult, reads=["gt1", "ysb"], writes=["gt1"])
            o_act(S, gt1[:], gt1[:], AF.Tanh, reads=["gt1"], writes=["gt1"], scale=0.7978845608028654)
            o_ts(S, "dve", gt1[:], gt1[:], 1.0, ALU.add, reads=["gt1"], writes=["gt1"], s2=0.5, op1=ALU.mult)
            o_tt(S, "dve", yg[:, yb_, :], gt1[:], ysb[:], ALU.mult, reads=["gt1", "ysb"], writes=[("yg", yb_)])
            o_copy(S, "pool", ygb[:, yb_, :], yg[:, yb_, :], reads=[("yg", yb_)], writes=[("ygb", yb_)])
            o_dma(S, "sp", T["ygT"][c, :, :], yg[:, yb_, :], reads=[("yg", yb_)], grp="yg%d" % yb_)
            o_dma(S, "sp", T["ygTb"][c, :, :], ygb[:, yb_, :], reads=[("ygb", yb_)], grp="ygb%d" % yb_)
        o_dma(S, "sp", T["hfr"][:, :], hfr[:], reads=["hf"], grp="hfr")
        o_dma(S, "sp", T["hfi"][:, :], hfi[:], reads=["hf"], grp="hfi")
        o_dma(S, "sp", T["hsr"][:, :], hsr[:].rearrange("p a b -> p (a b)"), reads=["hs"], grp="hsr")
        o_dma(S, "sp", T["hsi"][:, :], hsi[:].rearrange("p a b -> p (a b)"), reads=["hs"], grp="hsi")
        S.emit()


def stage_D1(nc, S, T):
    with St(nc, "D1") as st:
        ygT = st.sb("ygT", [128, 8, TOK], F32)
        ygb = st.sb("ygb", [128, 8, TOK], BF16)
        Wg = st.sb("Wg", [128, 8, 1024], BF16)
        gssm = st.sb("gssm", [128, 8], F32)
        onesb = st.sb("onesb", [128, 128], BF16)
        ssq = st.sb("ssq", [128, 8, TOK], BF16)
        sgm = st.sb("sgm", [128, 8, TOK], BF16)
        sg = st.sb("sg", [128, 2, 512], F32)
        so = st.sb("so", [128, 2, 512], F32)
        sss = st.sb("sss", [128, 16], F32)
        pz = [st.ps("pz%d" % i, [128, 512], F32) for i in range(4)]
        ps_ss = st.ps("ps_ss", [128, 16], F32)
        o_memset(S, "dve", onesb[:], 1.0, ["onesb"])
        o_dma(S, "sp", ygT[:], T["ygT"].rearrange("c p t -> p c t"), writes=["ygT"], grp="ygT")
        o_dma(S, "sp", ygb[:], T["ygTb"].rearrange("c p t -> p c t"), writes=["ygb"], grp="ygb")
        o_dma(S, "pool", Wg[:], T["w_glu"].rearrange("(c p) n -> p c n", p=128), writes=["Wg"], grp="Wg")
        o_dma(S, "sp", gssm[:], T["gssm"][:, :], writes=["gssm"], grp="gssm")
        i = 0
        for f in range(8):
            for (c0, w) in [(0, 512), (512, 512), (1024, 128)]:
                pi = i % 4
                si = i % 2
                i += 1
                for k in range(8):
                    o_mm(S, pz[pi][:, 0:w], Wg[:, k, f * 128:(f + 1) * 128], ygb[:, k, c0:c0 + w], k == 0, k == 7,
                         reads=["Wg", "ygb"], writes=[("pz", pi)])
                o_act(S, sg[:, si, 0:w], pz[pi][:, 0:w], AF.Sigmoid, reads=[("pz", pi)], writes=[("sg", si)])
                o_tt(S, "dve", so[:, si, 0:w], ygT[:, f, c0:c0 + w], sg[:, si, 0:w], ALU.mult, reads=["ygT", ("sg", si)], writes=[("so", si)])
                o_tt(S, "pool", ssq[:, f, c0:c0 + w], so[:, si, 0:w], so[:, si, 0:w], ALU.mult, reads=[("so", si)], writes=["ssq"])
                o_ts(S, "pool", sgm[:, f, c0:c0 + w], so[:, si, 0:w], gssm[:, f:f + 1], ALU.mult, reads=[("so", si), "gssm"], writes=["sgm"])
        for t in range(NT):
            for f in range(8):
                o_mm(S, ps_ss[:, t:t + 1], ssq[:, f, t * 128:(t + 1) * 128], onesb[:, 0:1], f == 0, f == 7,
                     reads=["ssq", "onesb"], writes=["ps_ss"])
        o_act(S, sss[:, 0:NT], ps_ss[:, 0:NT], AF.Sqrt, reads=["ps_ss"], writes=["sss"], bias=1e-6, scale=1.0 / 1024.0)
        o_recip(S, sss[:, 0:NT], sss[:, 0:NT], reads=["sss"], writes=["sss"])
        o_dma(S, "sp", T["rstd_s"][:, :], sss[:, 0:NT], reads=["sss"], grp="rstd")
        o_dma(S, "sp", T["mixT"][8:16, :, :].rearrange("h p t -> p h t"), sgm[:, :, :], reads=["sgm"], grp="sgmst")
        S.emit()


def layernorm_tile(S, st_, X, t, lng, lnb, stats, mv, rs, key):
    xt = X[:, t, :]
    for c in range(4):
        S.op("dve", lambda h, c=c: h.bn_stats(out=stats[:, c, :], in_=X[:, t, c * 512:(c + 1) * 512]), reads=[key], writes=["ln_st"])
    S.op("dve", lambda h: h.bn_aggr(out=mv[:], in_=stats[:].rearrange("p a b -> p (a b)")), reads=["ln_st"], writes=["ln_mv"])
    o_act(S, rs[:], mv[:, 1:2], AF.Sqrt, reads=["ln_mv"], writes=["ln_rs"], bias=1e-5, scale=1.0)
    o_recip(S, rs[:], rs[:], reads=["ln_rs"], writes=["ln_rs"])
    o_ts(S, "dve", xt, xt, mv[:, 0:1], ALU.subtract, reads=[key, "ln_mv", "ln_rs"], writes=[key], s2=rs[:, 0:1], op1=ALU.mult)
    o_tt(S, "pool", xt, xt, lng[:], ALU.mult, reads=[key, "lng"], writes=[key])
    o_tt(S, "pool", xt, xt, lnb[:], ALU.add, reads=[key, "lnb"], writes=[key])


def linear_residual_ln(nc, S, T, name, inT_name, w_name, lng_name, lnb_name, xin_fn, xout_name, two_part):
    with St(nc, name) as st:
        X = st.sb("X", [128, NT, 2048], F32)
        inT = st.sb("inT", [128, 16, TOK], BF16)
        Wo = st.sb("Wo", [128, 2, 16, 512], BF16)
        lng = st.sb("lng", [128, 2048], F32)
        lnb = st.sb("lnb", [128, 2048], F32)
        tmp = st.sb("tmp", [128, 2, 512], F32)
        stats = st.sb("stats", [128, 4, 6], F32)
        mv = st.sb("mv", [128, 2], F32)
        rs = st.sb("rs", [128, 1], F32)
        ra = st.sb("ra", [128, NT], F32)
        rsm = st.sb("rsm", [128, NT], F32)
        pa = [st.ps("pa%d" % i, [128, 512], F32) for i in range(2)]
        pb = [st.ps("pb%d" % i, [128, 512], F32) for i in range(2)]
        xin_fn(S, X)
        o_dma(S, "sp", inT[:], T[inT_name].rearrange("c p t -> p c t"), writes=["inT"], grp="inT")
        o_dma(S, "sp", lng[:], T[lng_name].partition_broadcast(128), writes=["lng"], grp="lng")
        o_dma(S, "sp", lnb[:], T[lnb_name].partition_broadcast(128), writes=["lnb"], grp="lnb")
        if two_part:
            o_dma(S, "sp", ra[:], T["rstd_a"][:, :], writes=["ra"], grp="ra")
            o_dma(S, "sp", rsm[:], T["rstd_s"][:, :], writes=["rsm"], grp="rsm")
        i = 0
        for n in range(4):
            wb = n % 2
            o_dma(S, "pool", Wo[:, wb, :, :], T[w_name][:, n * 512:(n + 1) * 512].rearrange("(c p) n -> p c n", p=128),
                  writes=[("Wo", wb)], grp="Wo%d" % wb)
            for t in range(NT):
                pi = i % 2
                i += 1
                xk = ("X", t)
                if two_part:
                    for k in range(8):
                        o_mm(S, pa[pi][:, :], inT[:, k, t * 128:(t + 1) * 128], Wo[:, wb, k, :], k == 0, k == 7,
                             reads=["inT", ("Wo", wb)], writes=[("pa", pi)])
                    for k in range(8):
                        o_mm(S, pb[pi][:, :], inT[:, 8 + k, t * 128:(t + 1) * 128], Wo[:, wb, 8 + k, :], k == 0, k == 7,
                             reads=["inT", ("Wo", wb)], writes=[("pb", pi)])
                    o_copy(S, "act", tmp[:, pi, :], pa[pi][:, :], reads=[("pa", pi), "ra"], writes=[("tmp", pi)], scale=ra[:, t:t + 1])
                    o_stt(S, tmp[:, pi, :], pb[pi][:, :], rsm[:, t:t + 1], tmp[:, pi, :], ALU.mult, ALU.add,
                          reads=[("pb", pi), ("tmp", pi), "rsm"], writes=[("tmp", pi)])
                    o_stt(S, X[:, t, n * 512:(n + 1) * 512], X[:, t, n * 512:(n + 1) * 512], ALPHA, tmp[:, pi, :], ALU.mult, ALU.add,
                          reads=[xk, ("tmp", pi)], writes=[xk])
                else:
                    for k in range(16):
                        o_mm(S, pa[pi][:, :], inT[:, k, t * 128:(t + 1) * 128], Wo[:, wb, k, :], k == 0, k == 15,
                             reads=["inT", ("Wo", wb)], writes=[("pa", pi)])
                    o_stt(S, X[:, t, n * 512:(n + 1) * 512], X[:, t, n * 512:(n + 1) * 512], ALPHA, pa[pi][:, :], ALU.mult, ALU.add,
                          reads=[xk, ("pa", pi)], writes=[xk])
        for t in range(NT):
            layernorm_tile(S, st, X, t, lng, lnb, stats, mv, rs, ("X", t))
            o_dma(S, "sp", T[xout_name][t * 128:(t + 1) * 128, :], X[:, t, :], reads=[("X", t)], grp="xo%d" % (t % 2))
        S.emit()


def xin_from_inputs(T):
    def f(S, X):
        o_dma(S, "sp", X[:, 0:8, :], T["xw"][3072:4096, :].rearrange("(t p) d -> p t d", p=128),
              writes=[("X", t) for t in range(8)], grp="X")
        o_dma(S, "sp", X[:, 8, :], T["xs"][:, :], writes=[("X", 8)], grp="X8")
    return f


def xin_from_scr(T, name):
    def f(S, X):
        o_dma(S, "sp", X[:, :, :], T[name][:, :].rearrange("(t p) d -> p t d", p=128),
              writes=[("X", t) for t in range(NT)], grp="X")
    return f


def transpose_block(S, X, t, ident, ptr, dstT, dst_key, i0, f32copy=None):
    for b4 in range(4):
        pi = (i0 + b4) % 2
        for q in range(4):
            c = b4 * 4 + q
            o_tr(S, ptr[pi][:, q * 128:(q + 1) * 128], X[:, t, c * 128:(c + 1) * 128], ident[:],
                 reads=[("X", t), "ident"], writes=[("ptr", pi)])
        o_copy(S, "act" if pi == 0 else "dve", dstT[:, b4 * 4:(b4 + 1) * 4, t * 128:(t + 1) * 128],
               ptr[pi][:, :].rearrange("p (a b) -> p a b", a=4), reads=[("ptr", pi)], writes=[dst_key])
        if f32copy is not None:
            o_copy(S, "dve" if pi == 0 else "act", f32copy[:, b4 * 4:(b4 + 1) * 4, :],
                   ptr[pi][:, :].rearrange("p (a b) -> p a b", a=4), reads=[("ptr", pi)], writes=["f32copy"])


def stage_E0(nc, S, T):
    with St(nc, "E0") as st:
        ident = st.sb("ident", [128, 128], F32)
        M = st.sb("M", [128, 2, 2048], F32)
        memT = st.sb("memT", [128, 16, 256], BF16)
        Wb = st.sb("Wb", [128, 2, 16, 512], BF16)
        of = st.sb("of", [128, 2, 512], F32)
        ob = st.sb("ob", [128, 2, 512], BF16)
        okT = st.sb("okT", [128, 2, 256], BF16)
        ptr = [st.ps("ptr%d" % i, [128, 512], F32) for i in range(2)]
        pm = [st.ps("pm%d" % i, [128, 512], F32) for i in range(2)]
        pk = [st.ps("pk%d" % i, [128, 256], F32) for i in range(2)]
        make_ident(S, ident, "ident")
        o_dma(S, "sp", M[:], T["memp"][:, :].rearrange("(t p) d -> p t d", p=128), writes=[("X", 0), ("X", 1)], grp="M")
        for t in range(2):
            transpose_block(S, M, t, ident, ptr, memT, "memT", 0)
        wi = 0
        oi = 0
        for (wname, kind) in (("w_mk", "k"), ("w_mv", "v")):
            for n in range(4):
                wb = wi % 2
                wi += 1
                o_dma(S, "pool", Wb[:, wb, :, :], T[wname][:, n * 512:(n + 1) * 512].rearrange("(c p) n -> p c n", p=128),
                      writes=[("Wb", wb)], grp="Wb%d" % wb)
                for t in range(2):
                    pi = oi % 2
                    oi += 1
                    for k in range(16):
                        o_mm(S, pm[pi][:, :], memT[:, k, t * 128:(t + 1) * 128], Wb[:, wb, k, :], k == 0, k == 15,
                             reads=["memT", ("Wb", wb)], writes=[("pm", pi)])
                    o_copy(S, "act", of[:, pi, :], pm[pi][:, :], reads=[("pm", pi)], writes=[("of", pi)])
                    o_dma(S, "sp", T["memKo" if kind == "k" else "memVo"][t * 128:(t + 1) * 128, n * 512:(n + 1) * 512],
                          of[:, pi, :], reads=[("of", pi)], grp="of%d" % pi)
                    if kind == "v":
                        o_copy(S, "dve", ob[:, pi, :], pm[pi][:, :], reads=[("pm", pi)], writes=[("ob", pi)])
                        o_dma(S, "sp", T["mv_scr"][t * 128:(t + 1) * 128, n * 512:(n + 1) * 512], ob[:, pi, :],
                              reads=[("ob", pi)], grp="ob%d" % pi)
                if kind == "k":
                    for fb in range(4):
                        f = n * 4 + fb
                        pi = f % 2
                        for k in range(16):
                            o_mm(S, pk[pi][:, :], Wb[:, wb, k, fb * 128:(fb + 1) * 128], memT[:, k, :], k == 0, k == 15,
                                 reads=["memT", ("Wb", wb)], writes=[("pk", pi)])
                        o_copy(S, "dve", okT[:, pi, :], pk[pi][:, :], reads=[("pk", pi)], writes=[("okT", pi)])
                        o_dma(S, "sp", T["mkT_scr"][f, :, :], okT[:, pi, :], reads=[("okT", pi)], grp="okT%d" % pi)
        S.emit()


def stage_E1(nc, S, T):
    with St(nc, "E1") as st:
        ident = st.sb("ident", [128, 128], F32)
        X = st.sb("X", [128, NT, 2048], F32)
        xT = st.sb("xT", [128, 16, TOK], BF16)
        Wb = st.sb("Wb", [128, 2, 16, 512], BF16)
        oq = st.sb("oq", [128, 2, 512], BF16)
        ptr = [st.ps("ptr%d" % i, [128, 512], F32) for i in range(2)]
        pq = [st.ps("pq%d" % i, [128, 512], F32) for i in range(4)]
        make_ident(S, ident, "ident")
        xin_from_scr(T, "X1")(S, X)
        for t in range(NT):
            transpose_block(S, X, t, ident, ptr, xT, "xT", 0)
        i = 0
        for n in range(4):
            wb = n % 2
            o_dma(S, "pool", Wb[:, wb, :, :], T["w_mq"][:, n * 512:(n + 1) * 512].rearrange("(c p) n -> p c n", p=128),
                  writes=[("Wb", wb)], grp="Wb%d" % wb)
            for fb in range(4):
                f = n * 4 + fb
                for (c0, w) in [(0, 512), (512, 512), (1024, 128)]:
                    pi = i % 4
                    oi = i % 2
                    i += 1
                    for k in range(16):
                        o_mm(S, pq[pi][:, 0:w], Wb[:, wb, k, fb * 128:(fb + 1) * 128], xT[:, k, c0:c0 + w], k == 0, k == 15,
                             reads=["xT", ("Wb", wb)], writes=[("pq", pi)])
                    o_copy(S, "act" if oi == 0 else "dve", oq[:, oi, 0:w], pq[pi][:, 0:w], reads=[("pq", pi)], writes=[("oq", oi)])
                    o_dma(S, "sp", T["qmT"][f, :, c0:c0 + w], oq[:, oi, 0:w], reads=[("oq", oi)], grp="oq%d" % oi)
        S.emit()


def stage_E2(nc, S, T):
    with St(nc, "E2") as st:
        identf = st.sb("identf", [128, 128], F32)
        onesb = st.sb("onesb", [128, 128], BF16)
        mkT = st.sb("mkT", [128, 16, 256], BF16)
        mv = st.sb("mv", [128, 2, 2048], BF16)
        qm = st.sb("qm", [128, 16, TOK], BF16)
        p = st.sb("p", [128, 2, 512], BF16)
        rd = st.sb("rd", [128, 512], F32)
        om = st.sb("om", [128, 2, 4, 512], BF16)
        ck = st.sb("ck", [128, 2, 2048], F32)
        skT = st.sb("skT", [128, 16, 256], BF16)
        sv = st.sb("sv", [128, 2, 2048], BF16)
        p8 = st.sb("p8", [128, 2, 8], BF16)
        rd8 = st.sb("rd8", [128, 8], F32)
        om8 = st.sb("om8", [128, 16, 128], BF16)
        ps_s = [st.ps("ps_s%d" % i, [128, 512], F32) for i in range(2)]
        ps_o = [st.ps("ps_o%d" % i, [128, 512], F32) for i in range(4)]
        ps_d = st.ps("ps_d", [128, 512], F32)
        ptr = st.ps("ptr", [128, 512], F32)
        make_ident(S, identf, "ident")
        o_memset(S, "dve", onesb[:], 1.0, ["onesb"])
        o_memset(S, "pool", om8[:].rearrange("p a b -> p (a b)"), 0.0, ["om8"])
        o_dma(S, "sp", mkT[:], T["mkT_scr"].rearrange("c p t -> p c t"), writes=["mkT"], grp="mkT")
        o_dma(S, "sp", mv[:], T["mv_scr"][:, :].rearrange("(t p) d -> p t d", p=128), writes=["mv"], grp="mv")
        o_dma(S, "sp", qm[:], T["qmT"].rearrange("c p t -> p c t"), writes=["qm"], grp="qm")
        si = 0
        oi = 0
        for hh in range(4):
            for blk in range(2):
                c0 = blk * 512
                ob_ = oi % 2
                oi += 1
                for kc_ in range(2):
                    sl = si % 2
                    si += 1
                    for j in range(4):
                        o_mm(S, ps_s[sl][:, :], mkT[:, 4 * hh + j, kc_ * 128:(kc_ + 1) * 128], qm[:, 4 * hh + j, c0:c0 + 512], j == 0, j == 3,
                             reads=["mkT", "qm"], writes=[("ps_s", sl)])
                    o_act(S, p[:, sl, :], ps_s[sl][:, :], AF.Exp, reads=[("ps_s", sl)], writes=[("p", sl)], scale=SC_MEM)
                    for j in range(4):
                        o_mm(S, ps_o[j][:, :], mv[:, kc_, (4 * hh + j) * 128:(4 * hh + j + 1) * 128], p[:, sl, :], kc_ == 0, kc_ == 1,
                             reads=["mv", ("p", sl)], writes=[("ps_o", j)])
                    o_mm(S, ps_d[:, :], onesb[:], p[:, sl, :], kc_ == 0, kc_ == 1, reads=["onesb", ("p", sl)], writes=["ps_d"])
                o_recip(S, rd[:], ps_d[:, :], reads=["ps_d"], writes=["rd"])
                for j in range(4):
                    o_tt(S, "dve", om[:, ob_, j, :], ps_o[j][:, :], rd[:], ALU.mult, reads=[("ps_o", j), "rd"], writes=[("om", ob_)])
                o_dma(S, "sp", T["omT"][4 * hh:4 * hh + 4, :, c0:c0 + 512].rearrange("c p t -> p c t"), om[:, ob_, :, :],
                      reads=[("om", ob_)], grp="om%d" % ob_)
        for s in range(4):
            q0 = 1024 + 32 * s
            o_dma(S, "sp", ck[:], T["cmk"][s, :, :].rearrange("(t p) d -> p t d", p=128), writes=[("X", 0), ("X", 1)], grp="ck")
            o_dma(S, "pool", sv[:], T["cmv"][s, :, :].rearrange("(t p) d -> p t d", p=128), writes=["sv"], grp="sv")
            for t in range(2):
                for b4 in range(4):
                    for q in range(4):
                        c = b4 * 4 + q
                        o_tr(S, ptr[:, q * 128:(q + 1) * 128], ck[:, t, c * 128:(c + 1) * 128], identf[:],
                             reads=[("X", t), "ident"], writes=["ptr"])
                    o_copy(S, "act" if b4 % 2 == 0 else "dve", skT[:, b4 * 4:(b4 + 1) * 4, t * 128:(t + 1) * 128],
                           ptr[:, :].rearrange("p (a b) -> p a b", a=4), reads=["ptr"], writes=["skT"])
            for hh in range(4):
                for kc_ in range(2):
                    for j in range(4):
                        o_mm(S, ps_s[0][:, kc_ * 8:(kc_ + 1) * 8], skT[:, 4 * hh + j, kc_ * 128:(kc_ + 1) * 128], qm[:, 4 * hh + j, q0:q0 + 8],
                             j == 0, j == 3, reads=["skT", "qm"], writes=[("ps_s", 0)])
                o_act(S, p8[:].rearrange("p a b -> p (a b)"), ps_s[0][:, 0:16], AF.Exp, reads=[("ps_s", 0)], writes=["p8"], scale=SC_MEM)
                for j in range(4):
                    for kc_ in range(2):
                        o_mm(S, ps_o[j][:, 0:8], sv[:, kc_, (4 * hh + j) * 128:(4 * hh + j + 1) * 128], p8[:, kc_, :], kc_ == 0, kc_ == 1,
                             reads=["sv", "p8"], writes=[("ps_o", j)])
                for kc_ in range(2):
                    o_mm(S, ps_d[:, 0:8], onesb[:], p8[:, kc_, :], kc_ == 0, kc_ == 1, reads=["onesb", "p8"], writes=["ps_d"])
                o_recip(S, rd8[:], ps_d[:, 0:8], reads=["ps_d"], writes=["rd8"])
                for j in range(4):
                    o_tt(S, "dve", om8[:, 4 * hh + j, 32 * s:32 * s + 8], ps_o[j][:, 0:8], rd8[:], ALU.mult,
                         reads=[("ps_o", j), "rd8"], writes=["om8"])
        o_dma(S, "sp", T["omT"][:, :, 1024:TOK].rearrange("c p t -> p c t"), om8[:, :, :], reads=["om8"], grp="om8")
        S.emit()


def stage_F1(nc, S, T):
    with St(nc, "F1") as st:
        ident = st.sb("ident", [128, 128], F32)
        X = st.sb("X", [128, NT, 2048], F32)
        xT = st.sb("xT", [128, 16, TOK], BF16)
        xTf = st.sb("xTf", [128, 16, 128], F32)
        Wr = st.sb("Wr", [128, 16, 36], F32)
        br = st.sb("br", [128, 36], F32)
        lg = st.sb("lg", [128, 36], F32)
        G = st.sb("G", [128, NT, 32], F32)
        gm = st.sb("gm", [128, 1], F32)
        goh = st.sb("goh", [128, 4], F32)
        ge = st.sb("ge", [128, 4], F32)
        gs = st.sb("gs", [128, 1], F32)
        gw = st.sb("gw", [128, 1], F32)
        es = st.sb("es", [128, 8], F32)
        m1 = st.sb("m1", [128, 1], F32)
        oh1 = st.sb("oh1", [128, 8], F32)
        em = st.sb("em", [128, 8], F32)
        m2 = st.sb("m2", [128, 1], F32)
        oh2 = st.sb("oh2", [128, 8], F32)
        dd = st.sb("dd", [128, 1], F32)
        w1 = st.sb("w1", [128, 1], F32)
        w2 = st.sb("w2", [128, 1], F32)
        g8 = st.sb("g8", [128, 8], F32)
        ptr = [st.ps("ptr%d" % i, [128, 512], F32) for i in range(2)]
        pl = st.ps("pl", [128, 64], F32)
        make_ident(S, ident, "ident")
        xin_from_scr(T, "X2")(S, X)
        o_dma(S, "sp", Wr[:], T["wr"][:, :].rearrange("(c p) n -> p c n", p=128), writes=["Wr"], grp="Wr")
        o_dma(S, "sp", br[:], T["br"].partition_broadcast(128), writes=["br"], grp="br")
        R = ["rt"]
        for t in range(NT):
            transpose_block(S, X, t, ident, ptr, xT, "xT", 0, f32copy=xTf)
            for k in range(16):
                o_mm(S, pl[:, 0:36], xTf[:, k, :], Wr[:, k, :], k == 0, k == 15, reads=["f32copy", "Wr"], writes=["pl"])
            o_tt(S, "dve", lg[:], pl[:, 0:36], br[:], ALU.add, reads=["pl", "br"], writes=R)
            S.op("dve", lambda h: h.reduce_max(out=gm[:], in_=lg[:, 0:4], axis=mybir.AxisListType.X), reads=R, writes=R)
            o_ts(S, "dve", goh[:], lg[:, 0:4], gm[:, 0:1], ALU.is_equal, reads=R, writes=R)
            o_ts(S, "dve", ge[:], lg[:, 0:4], gm[:, 0:1], ALU.subtract, reads=R, writes=R)
            o_act(S, ge[:], ge[:], AF.Exp, reads=R, writes=R)
            S.op("dve", lambda h: h.reduce_sum(out=gs[:], in_=ge[:], axis=mybir.AxisListType.X), reads=R, writes=R)
            o_recip(S, gw[:], gs[:], reads=R, writes=R)
            o_ts(S, "dve", es[:], lg[:, 4:12], goh[:, 0:1], ALU.mult, reads=R, writes=R)
            for g in range(1, 4):
                o_stt(S, es[:], lg[:, 4 + 8 * g:12 + 8 * g], goh[:, g:g + 1], es[:], ALU.mult, ALU.add, reads=R, writes=R)
            S.op("dve", lambda h: h.reduce_max(out=m1[:], in_=es[:], axis=mybir.AxisListType.X), reads=R, writes=R)
            o_ts(S, "dve", oh1[:], es[:], m1[:, 0:1], ALU.is_equal, reads=R, writes=R)
            o_stt(S, em[:], oh1[:], -1e30, es[:], ALU.mult, ALU.add, reads=R, writes=R)
            S.op("dve", lambda h: h.reduce_max(out=m2[:], in_=em[:], axis=mybir.AxisListType.X), reads=R, writes=R)
            o_ts(S, "dve", oh2[:], em[:], m2[:, 0:1], ALU.is_equal, reads=R, writes=R)
            o_tt(S, "dve", dd[:], m2[:], m1[:], ALU.subtract, reads=R, writes=R)
            o_act(S, dd[:], dd[:], AF.Exp, reads=R, writes=R)
            o_ts(S, "dve", w1[:], dd[:], 1.0, ALU.add, reads=R, writes=R)
            o_recip(S, w1[:], w1[:], reads=R, writes=R)
            o_tt(S, "dve", w1[:], w1[:], gw[:], ALU.mult, reads=R, writes=R)
            o_tt(S, "dve", w2[:], w1[:], dd[:], ALU.mult, reads=R, writes=R)
            o_ts(S, "dve", g8[:], oh1[:], w1[:, 0:1], ALU.mult, reads=R, writes=R)
            o_stt(S, g8[:], oh2[:], w2[:, 0:1], g8[:], ALU.mult, ALU.add, reads=R, writes=R)
            for g in range(4):
                o_ts(S, "dve", G[:, t, 8 * g:8 * g + 8], g8[:], goh[:, g:g + 1], ALU.mult, reads=R, writes=["G"])
            o_ts(S, "pool", X[:, t, :], X[:, t, :], ALPHA, ALU.mult, reads=[("X", t)], writes=[("X", t)])
            o_dma(S, "sp", T["X3"][t * 128:(t + 1) * 128, :], X[:, t, :], reads=[("X", t)], grp="xo%d" % (t % 2))
        o_dma(S, "sp", T["x2T"].rearrange("c p t -> p c t"), xT[:, :, :], reads=["xT"], grp="xTst")
        o_dma(S, "sp", T["G"][:, :], G[:].rearrange("p a b -> p (a b)"), reads=["G"], grp="Gst")
        S.emit()


def stage_F2(nc, S, T):
    with St(nc, "F2") as st:
        X = st.sb("X", [128, NT, 2048], F32)
        xT = st.sb("xT", [128, 16, TOK], BF16)
        G = st.sb("G", [128, NT, 32], F32)
        Wg = st.sb("Wg", [128, 16, 512], BF16)
        Wu = st.sb("Wu", [128, 16, 512], BF16)
        Wd = st.sb("Wd", [128, 4, 2048], BF16)
        hT = st.sb("hT", [128, 4, TOK], BF16)
        sg = st.sb("sg", [128, 2, 512], F32)
        pg = [st.ps("pg%d" % i, [128, 512], F32) for i in range(2)]
        pu = [st.ps("pu%d" % i, [128, 512], F32) for i in range(2)]
        po = [st.ps("po%d" % i, [128, 512], F32) for i in range(4)]
        xin_from_scr(T, "X3")(S, X)
        o_dma(S, "sp", xT[:], T["x2T"].rearrange("c p t -> p c t"), writes=["xT"], grp="xT")
        o_dma(S, "sp", G[:].rearrange("p a b -> p (a b)"), T["G"][:, :], writes=["G"], grp="G")
        i = 0
        oi = 0
        for e in range(32):
            o_dma(S, "pool", Wg[:], T["w_gate"][e, :, :].rearrange("(c p) n -> p c n", p=128), writes=["Wg"], grp="Wg")
            o_dma(S, "pool", Wu[:], T["w_up"][e, :, :].rearrange("(c p) n -> p c n", p=128), writes=["Wu"], grp="Wu")
            o_dma(S, "pool", Wd[:], T["w_down"][e, :, :].rearrange("(c p) n -> p c n", p=128), writes=["Wd"], grp="Wd")
            for f in range(4):
                for (c0, w) in [(0, 512), (512, 512), (1024, 128)]:
                    pi = i % 2
                    i += 1
                    for k in range(16):
                        o_mm(S, pg[pi][:, 0:w], Wg[:, k, f * 128:(f + 1) * 128], xT[:, k, c0:c0 + w], k == 0, k == 15,
                             reads=["Wg", "xT"], writes=[("pg", pi)])
                    for k in range(16):
                        o_mm(S, pu[pi][:, 0:w], Wu[:, k, f * 128:(f + 1) * 128], xT[:, k, c0:c0 + w], k == 0, k == 15,
                             reads=["Wu", "xT"], writes=[("pu", pi)])
                    o_act(S, sg[:, pi, 0:w], pg[pi][:, 0:w], AF.Silu, reads=[("pg", pi)], writes=[("sg", pi)])
                    o_tt(S, "dve", hT[:, f, c0:c0 + w], sg[:, pi, 0:w], pu[pi][:, 0:w], ALU.mult,
                         reads=[("sg", pi), ("pu", pi)], writes=[("hT", f)])
            for t in range(NT):
                for n in range(4):
                    pi = oi % 4
                    oi += 1
                    for f in range(4):
                        o_mm(S, po[pi][:, :], hT[:, f, t * 128:(t + 1) * 128], Wd[:, f, n * 512:(n + 1) * 512], f == 0, f == 3,
                             reads=[("hT", f), "Wd"], writes=[("po", pi)])
                    o_stt(S, X[:, t, n * 512:(n + 1) * 512], po[pi][:, :], G[:, t, e:e + 1], X[:, t, n * 512:(n + 1) * 512],
                          ALU.mult, ALU.add, reads=[("po", pi), "G", ("X", t)], writes=[("X", t)])
        for t in range(NT):
            o_dma(S, "sp", T["X1"][t * 128:(t + 1) * 128, :], X[:, t, :], reads=[("X", t)], grp="xo%d" % (t % 2))
        S.emit()


def stage_F3(nc, S, T):
    with St(nc, "F3") as st:
        X = st.sb("X", [128, NT, 2048], F32)
        lng = st.sb("lng", [128, 2048], F32)
        lnb = st.sb("lnb", [128, 2048], F32)
        stats = st.sb("stats", [128, 4, 6], F32)
        mv = st.sb("mv", [128, 2], F32)
        rs = st.sb("rs", [128, 1], F32)
        xin_from_scr(T, "X1")(S, X)
        o_dma(S, "sp", lng[:], T["ln3_g"].partition_broadcast(128), writes=["lng"], grp="lng")
        o_dma(S, "sp", lnb[:], T["ln3_b"].partition_broadcast(128), writes=["lnb"], grp="lnb")
        for t in range(NT):
            layernorm_tile(S, st, X, t, lng, lnb, stats, mv, rs, ("X", t))
            o_dma(S, "sp", T["y"][t * 128:(t + 1) * 128, :], X[:, t, :], reads=[("X", t)], grp="xo%d" % (t % 2))
        S.emit()


IN_SPECS = [
    ("xw", [4096, 2048]), ("xs", [128, 2048]), ("vbias", [128, 24]), ("pmask", [128, 17 * 128]),
    ("smask", [128, 128]), ("smaskn", [128, 128]), ("cwk", [4, 2048, 1024]), ("cwv", [4, 2048, 1024]),
    ("h0re", [128, 128]), ("h0im", [128, 128]), ("cmk", [4, 256, 2048]), ("cmv", [4, 256, 2048]),
    ("memp", [256, 2048]), ("w_in", [2048, 4096]), ("lamre", [128, 32]), ("lamim", [128, 32]),
    ("logdt", [128, 32]), ("BTre", [128, 4096]), ("BTim", [128, 4096]), ("CTre", [128, 4096]),
    ("CTim", [128, 4096]), ("dcol", [128, 8]), ("w_glu", [1024, 1024]), ("gatt", [128, 8]), ("gssm", [128, 8]),
    ("w_out", [2048, 2048]), ("ln1_g", [1, 2048]), ("ln1_b", [1, 2048]), ("w_mq", [2048, 2048]),
    ("w_mk", [2048, 2048]), ("w_mv", [2048, 2048]), ("w_mo", [2048, 2048]), ("ln2_g", [1, 2048]),
    ("ln2_b", [1, 2048]), ("wr", [2048, 36]), ("br", [1, 36]), ("w_gate", [32, 2048, 512]),
    ("w_up", [32, 2048, 512]), ("w_down", [32, 512, 2048]), ("ln3_g", [1, 2048]), ("ln3_b", [1, 2048]),
]
OUT_SPECS = [
    ("y", [TOK, 2048]), ("Kout", [TOK, 1024]), ("Vout", [TOK, 1024]), ("hfr", [128, 32]), ("hfi", [128, 32]),
    ("hsr", [128, 128]), ("hsi", [128, 128]), ("memKo", [256, 2048]), ("memVo", [256, 2048]),
]
SCR_SPECS = [
    ("qT", [8, 128, TOK], BF16), ("kT", [8, 128, KPOS], BF16), ("V_scr", [KPOS, 1024], BF16), ("uT", [8, 128, UPOS], BF16),
    ("mixT", [16, 128, TOK], BF16), ("rstd_a", [128, NT], F32), ("rstd_s", [128, NT], F32),
    ("ygT", [8, 128, TOK], F32), ("ygTb", [8, 128, TOK], BF16), ("X1", [TOK, 2048], F32), ("X2", [TOK, 2048], F32),
    ("X3", [TOK, 2048], F32), ("mkT_scr", [16, 128, 256], BF16), ("mv_scr", [256, 2048], BF16),
    ("qmT", [16, 128, TOK], BF16), ("omT", [16, 128, TOK], BF16), ("x2T", [16, 128, TOK], BF16), ("G", [128, NT * 32], F32),
]


def build_program(stages=None, debug=False):
    nc = bass.Bass("TRN2", target_bir_lowering=False)
    T = {}
    for (n, s) in IN_SPECS:
        T[n] = nc.dram_tensor(n, s, F32, kind="ExternalInput").ap()
    for (n, s) in OUT_SPECS:
        T[n] = nc.dram_tensor(n, s, F32, kind="ExternalOutput").ap()
    for (n, s, d) in SCR_SPECS:
        T[n] = nc.dram_tensor("scr_" + n, s, d, kind=("ExternalOutput" if debug else "Internal")).ap()
    S = Sched(nc)
    table = [
        ("A", lambda: stage_A(nc, S, T)),
        ("B", lambda: stage_B(nc, S, T)),
        ("C", lambda: stage_C(nc, S, T)),
        ("D1", lambda: stage_D1(nc, S, T)),
        ("D2", lambda: linear_residual_ln(nc, S, T, "D2", "mixT", "w_out", "ln1_g", "ln1_b", xin_from_inputs(T), "X1", True)),
        ("E0", lambda: stage_E0(nc, S, T)),
        ("E1", lambda: stage_E1(nc, S, T)),
        ("E2", lambda: stage_E2(nc, S, T)),
        ("E3", lambda: linear_residual_ln(nc, S, T, "E3", "omT", "w_mo", "ln2_g", "ln2_b", xin_from_scr(T, "X1"), "X2", False)),
        ("F1", lambda: stage_F1(nc, S, T)),
        ("F2", lambda: stage_F2(nc, S, T)),
        ("F3", lambda: stage_F3(nc, S, T)),
    ]
    for (nm, fn) in table:
        if stages is None or nm in stages:
            fn()
    S.close()
    return nc


def _mult(diff):
    m = ((diff >= 0) & (diff <= 128)).astype(np.float32)
    m += ((diff >= 0) & (diff <= 512) & (diff % 4 == 0)).astype(np.float32)
    m += ((diff >= 0) & (diff <= 2048) & (diff % 16 == 0)).astype(np.float32)
    return m


def make_inputs(inp):
    f = np.float32
    xp = inp["x_prompt"]
    xsm = inp["x_sample"]
    j = np.arange(128)
    pm = np.zeros((128, 17, 128), f)
    for cc in range(17):
        diff = (16 - cc) * 128 + j[None, :] - j[:, None]
        pm[:, cc, :] = _mult(diff)
    sm = np.zeros((128, 16, 8), f)
    for cc in range(16):
        cpos = cc * 128 + j[:, None]
        diff = np.arange(8)[None, :] + 2048 - cpos
        sm[:, cc, :] = _mult(diff)
    smn = np.zeros((128, 128), f)
    d8 = np.arange(8)[None, :] - np.arange(8)[:, None]
    for s in range(4):
        smn[32 * s:32 * s + 8, 32 * s:32 * s + 8] = _mult(d8)
    br_, bi_ = inp["ssm_b_re"][0], inp["ssm_b_im"][0]
    cr_, ci_ = inp["ssm_c_re"][0], inp["ssm_c_im"][0]
    BTre = np.zeros((128, 32, 128), f)
    BTim = np.zeros((128, 32, 128), f)
    CTre = np.zeros((128, 32, 128), f)
    CTim = np.zeros((128, 32, 128), f)
    for tau in range(32):
        for gl in range(2):
            g = 2 * tau + gl
            r0 = 32 * (tau % 4) + 16 * gl
            BTre[r0:r0 + 16, tau, 64 * gl:64 * gl + 64] = br_[g].T
            BTim[r0:r0 + 16, tau, 64 * gl:64 * gl + 64] = bi_[g].T
            CTre[64 * gl:64 * gl + 64, tau, r0:r0 + 16] = cr_[g].T
            CTim[64 * gl:64 * gl + 64, tau, r0:r0 + 16] = ci_[g].T
    common = {
        "pmask": pm.reshape(128, -1), "smask": sm.reshape(128, -1), "smaskn": smn,
        "w_in": inp["w_in"][0],
        "lamre": np.ascontiguousarray(inp["ssm_lam_re"][0].reshape(32, 128).T),
        "lamim": np.ascontiguousarray(inp["ssm_lam_im"][0].reshape(32, 128).T),
        "logdt": np.ascontiguousarray(np.repeat(inp["ssm_log_dt"][0].reshape(32, 2), 64, axis=1).T),
        "BTre": BTre.reshape(128, -1), "BTim": BTim.reshape(128, -1), "CTre": CTre.reshape(128, -1), "CTim": CTim.reshape(128, -1),
        "dcol": np.ascontiguousarray(inp["ssm_d"][0].reshape(8, 128).T),
        "w_glu": inp["w_glu"][0],
        "gatt": np.ascontiguousarray(inp["g_attn"][0].reshape(8, 128).T),
        "gssm": np.ascontiguousarray(inp["g_ssm"][0].reshape(8, 128).T),
        "w_out": inp["w_out"][0], "ln1_g": inp["ln1_g"], "ln1_b": inp["ln1_b"],
        "w_mq": inp["w_mq"][0], "w_mk": inp["w_mk"][0], "w_mv": inp["w_mv"][0], "w_mo": inp["w_mo"][0],
        "ln2_g": inp["ln2_g"], "ln2_b": inp["ln2_b"],
        "wr": np.ascontiguousarray(np.concatenate([inp["w_r1"][0], inp["w_r2"][0].reshape(2048, 32)], axis=1)),
        "br": np.ascontiguousarray(np.concatenate([inp["b_r1"][0], inp["b_r2"][0].reshape(32)])[None, :]),
        "w_gate": inp["w_gate"][0], "w_up": inp["w_up"][0], "w_down": inp["w_down"][0],
        "ln3_g": inp["ln3_g"], "ln3_b": inp["ln3_b"],
    }
    maps = []
    for c in range(8):
        b, r = c // 4, c % 4
        xw = np.zeros((4096, 2048), f)
        lo = 1024 * r - 3072
        src0 = max(lo, 0)
        xw[src0 - lo:, :] = xp[b, src0:1024 * r + 1024, :]
        xs = np.zeros((128, 2048), f)
        for s in range(4):
            xs[32 * s:32 * s + 8, :] = xsm[4 * c + s]
        vb = np.ones((128, 24), f)
        for jj in range(16):
            if 8 * r - 16 + jj < 0:
                vb[:, jj] = 0.0
        m = dict(common)
        m.update({
            "xw": xw, "xs": xs, "vbias": vb,
            "cwk": np.ascontiguousarray(inp["cache_win_k"][0, 4 * c:4 * c + 4].reshape(4, 2048, 1024)),
            "cwv": np.ascontiguousarray(inp["cache_win_v"][0, 4 * c:4 * c + 4].reshape(4, 2048, 1024)),
            "h0re": np.ascontiguousarray(inp["state_ssm_re"][0, 4 * c:4 * c + 4].reshape(4, 32, 128).transpose(2, 1, 0).reshape(128, 128)),
            "h0im": np.ascontiguousarray(inp["state_ssm_im"][0, 4 * c:4 * c + 4].reshape(4, 32, 128).transpose(2, 1, 0).reshape(128, 128)),
            "cmk": np.ascontiguousarray(inp["cache_mem_k"][0, 4 * c:4 * c + 4].reshape(4, 256, 2048)),
            "cmv": np.ascontiguousarray(inp["cache_mem_v"][0, 4 * c:4 * c + 4].reshape(4, 256, 2048)),
            "memp": np.ascontiguousarray(inp["mem_prompt"][b]),
        })
        maps.append({k: np.ascontiguousarray(v, dtype=f) for k, v in m.items()})
    return maps


def assemble(res):
    f = np.float32
    yp = np.zeros((2, 4096, 2048), f)
    ys = np.zeros((32, 8, 2048), f)
    wkp = np.zeros((1, 2, 2048, 8, 128), f)
    wvp = np.zeros((1, 2, 2048, 8, 128), f)
    wks = np.zeros((1, 32, 8, 8, 128), f)
    wvs = np.zeros((1, 32, 8, 8, 128), f)
    srp = np.zeros((1, 2, 64, 64), f)
    sip = np.zeros((1, 2, 64, 64), f)
    srs = np.zeros((1, 32, 64, 64), f)
    sis = np.zeros((1, 32, 64, 64), f)
    mkp = np.zeros((1, 2, 256, 4, 512), f)
    mvp = np.zeros((1, 2, 256, 4, 512), f)
    for c in range(8):
        r_ = res[c]
        b, r = c // 4, c % 4
        yp[b, 1024 * r:1024 * r + 1024] = r_["y"][0:1024]
        for s in range(4):
            ys[4 * c + s] = r_["y"][1024 + 32 * s:1024 + 32 * s + 8]
            wks[0, 4 * c + s] = r_["Kout"][1024 + 32 * s:1024 + 32 * s + 8].reshape(8, 8, 128)
            wvs[0, 4 * c + s] = r_["Vout"][1024 + 32 * s:1024 + 32 * s + 8].reshape(8, 8, 128)
        if r >= 2:
            wkp[0, b, 1024 * (r - 2):1024 * (r - 1)] = r_["Kout"][0:1024].reshape(1024, 8, 128)
            wvp[0, b, 1024 * (r - 2):1024 * (r - 1)] = r_["Vout"][0:1024].reshape(1024, 8, 128)
        if r == 3:
            srp[0, b] = r_["hfr"].T.reshape(64, 64)
            sip[0, b] = r_["hfi"].T.reshape(64, 64)
        hs_r = r_["hsr"].reshape(128, 32, 4).transpose(2, 1, 0).reshape(4, 64, 64)
        hs_i = r_["hsi"].reshape(128, 32, 4).transpose(2, 1, 0).reshape(4, 64, 64)
        srs[0, 4 * c:4 * c + 4] = hs_r
        sis[0, 4 * c:4 * c + 4] = hs_i
        if r == 0:
            mkp[0, b] = r_["memKo"].reshape(256, 4, 512)
            mvp[0, b] = r_["memVo"].reshape(256, 4, 512)
    return (yp, ys, wkp, wvp, wks, wvs, srp, sip, srs, sis, mkp, mvp)


def kernel(**inputs):
    inp = {k: np.asarray(v) for k, v in inputs.items()}
    maps = make_inputs(inp)
    nc = build_program()
    res = run_bass_kernel_spmd(nc, maps, core_ids=list(range(8)))
    return assemble(res.results)
```

```python
import numpy as np
from contextlib import ExitStack
import concourse.bass as bass
import concourse.mybir as mybir
from concourse.bass_utils import run_bass_kernel_spmd

F32 = mybir.dt.float32
BF16 = mybir.dt.bfloat16
AF = mybir.ActivationFunctionType
ALU = mybir.AluOpType

ENGS = ("pe", "act", "dve", "pool", "sp")
NT = 9
TOK = 1152
KPOS = 3200
UPOS = 4224
ALPHA = 2.0 ** 0.25
SC_ATT = 128 ** -0.5
SC_MEM = 512 ** -0.5
NEG = -30000.0
DBG = {}
PSUM_KEYS = {"ps_big", "ps_n", "ptr", "pmm", "ps_s", "ps_nd", "ptk", "ps_sm", "ps_snd", "ps_ss", "ps_new", "pw", "py", "pz", "pa", "pb",
             "pm", "pk", "pq", "ps_o", "ps_d", "pl", "pg", "pu", "po"}


class Sched:
    def __init__(self, nc, n_dma_sems=22):
        self.nc = nc
        self.stack = []
        self.esem = {}
        for e in ENGS:
            self.esem[e] = self._sem("e_" + e)
        self.ecount = {e: 0 for e in ENGS}
        self.sval = {e: 0 for e in ENGS}
        self.dsems = [self._sem("d%d" % i) for i in range(n_dma_sems)]
        self.dcount = [0] * n_dma_sems
        self.rr = {}
        self.reset_stage()

    def _sem(self, name):
        cm = self.nc.semaphore(name)
        s = cm.__enter__()
        self.stack.append(cm)
        return s

    def reset_stage(self):
        self.ops = []
        self.dmap = {}
        self.lastw = {}
        self.readers = {}
        self.known = {e: {} for e in ENGS}

    def alt(self, name, engines):
        i = self.rr.get(name, 0)
        self.rr[name] = i + 1
        return engines[i % len(engines)]

    def _dsem(self, group):
        if group not in self.dmap:
            idx = len(self.dmap)
            assert idx < len(self.dsems), "too many dma groups in stage"
            self.dmap[group] = idx
        return self.dmap[group]

    def _need(self, eng, dep, waits):
        kind, a, b = dep
        kn = self.known[eng]
        key = (kind, a)
        if kn.get(key, -1) >= b:
            return
        kn[key] = b
        waits.append(dep)

    def op(self, eng, fn, reads=(), writes=(), dma=None):
        waits = []
        deps = []
        if eng != "pe":
            ex = [k for k in reads if (k[0] if isinstance(k, tuple) else k) in PSUM_KEYS]
            if ex:
                writes = list(writes) + [k for k in ex if k not in writes]
        for k in reads:
            if k in self.lastw:
                deps.append(self.lastw[k])
        for k in writes:
            if k in self.lastw:
                deps.append(self.lastw[k])
            deps.extend(self.readers.get(k, ()))
        if dma is not None:
            di = self._dsem(dma)
            if self.dcount[di] > 0:
                deps.append(("d", di, self.dcount[di]))
        for d in deps:
            if d[0] == "e" and d[1] == eng and eng == "pe":
                continue
            self._need(eng, d, waits)
        if dma is not None:
            self.dcount[di] += 16
            tok = ("d", di, self.dcount[di])
        else:
            self.ecount[eng] += 1
            tok = ("e", eng, self.ecount[eng])
        for k in writes:
            self.lastw[k] = tok
            self.readers[k] = []
        for k in reads:
            self.readers.setdefault(k, []).append(tok)
        self.ops.append((eng, fn, waits, tok))

    def touch(self, keys, from_keys):
        best = None
        for k in from_keys:
            t = self.lastw.get(k)
            if t is not None and (best is None or t[2] > best[2]):
                best = t
        if best is not None:
            for k in keys:
                self.lastw[k] = best
                self.readers[k] = []

    def emit(self):
        nc = self.nc
        fin = [("d", di, self.dcount[di]) for g, di in self.dmap.items()]
        per = {e: [] for e in ENGS}
        need = set()
        for o in self.ops:
            per[o[0]].append(o)
            for (kind, a, b) in o[2]:
                if kind == "e":
                    need.add((a, b))
        sval = self.sval
        smap = {}
        for (eng, fn, waits, tok) in self.ops:
            if tok[0] == "e" and (tok[1], tok[2]) in need:
                sval[eng] += 1
                smap[(tok[1], tok[2])] = sval[eng]
        esem, dsems = self.esem, self.dsems

        def run(engname, h):
            for (_, fn, waits, tok) in per[engname]:
                for (kind, a, b) in waits:
                    if kind == "e":
                        h.wait_ge(esem[a], smap[(a, b)])
                    else:
                        h.wait_ge(dsems[a], b)
                ins = fn(h)
                if tok[0] == "e":
                    if (tok[1], tok[2]) in smap:
                        ins.then_inc(esem[engname], 1)
                else:
                    ins.then_inc(dsems[tok[1]], 16)
            if engname == "sp":
                for (kind, a, b) in fin:
                    h.wait_ge(dsems[a], b)

        with nc.Block() as block:
            @block.tensor
            def _(h):
                run("pe", h)

            @block.scalar
            def _(h):
                run("act", h)

            @block.vector
            def _(h):
                run("dve", h)

            @block.gpsimd
            def _(h):
                run("pool", h)

            @block.sync
            def _(h):
                run("sp", h)
        self.reset_stage()

    def close(self):
        for cm in reversed(self.stack):
            cm.__exit__(None, None, None)


def o_mm(S, out, lhsT, rhs, start, stop, reads, writes):
    S.op("pe", lambda h: h.matmul(out, lhsT=lhsT, rhs=rhs, start=start, stop=stop), reads=reads, writes=writes)


def o_tr(S, out, in_, ident, reads, writes):
    S.op("pe", lambda h: h.transpose(out=out, in_=in_, identity=ident), reads=reads, writes=writes)


def o_dma(S, eng, out, in_, reads=(), writes=(), grp=None, slow=False):
    if slow:
        S.op(eng, lambda h: h.dma_start(out=out, in_=in_, allow_slow_non_contiguous=True), reads=reads, writes=writes, dma=grp)
    else:
        S.op(eng, lambda h: h.dma_start(out=out, in_=in_), reads=reads, writes=writes, dma=grp)


def o_copy(S, eng, out, in_, reads, writes, scale=None):
    if eng == "act":
        if scale is None:
            S.op("act", lambda h: h.activation(out=out, in_=in_, func=AF.Copy), reads=reads, writes=writes)
        else:
            S.op("act", lambda h: h.activation(out=out, in_=in_, func=AF.Identity, scale=scale), reads=reads, writes=writes)
    else:
        if scale is None:
            S.op(eng, lambda h: h.tensor_copy(out=out, in_=in_), reads=reads, writes=writes)
        else:
            S.op(eng, lambda h: h.tensor_scalar(out=out, in0=in_, scalar1=scale, scalar2=None, op0=ALU.mult), reads=reads, writes=writes)


def o_act(S, out, in_, func, reads, writes, bias=None, scale=None):
    kw = {}
    if bias is not None:
        kw["bias"] = bias
    if scale is not None:
        kw["scale"] = scale
    S.op("act", lambda h: h.activation(out=out, in_=in_, func=func, **kw), reads=reads, writes=writes)


def o_tt(S, eng, out, in0, in1, op, reads, writes):
    S.op(eng, lambda h: h.tensor_tensor(out=out, in0=in0, in1=in1, op=op), reads=reads, writes=writes)


def o_ts(S, eng, out, in0, s1, op0, reads, writes, s2=None, op1=None):
    if op1 is None:
        S.op(eng, lambda h: h.tensor_scalar(out=out, in0=in0, scalar1=s1, scalar2=None, op0=op0), reads=reads, writes=writes)
    else:
        S.op(eng, lambda h: h.tensor_scalar(out=out, in0=in0, scalar1=s1, scalar2=s2, op0=op0, op1=op1), reads=reads, writes=writes)


def o_stt(S, out, in0, scalar, in1, op0, op1, reads, writes):
    S.op("dve", lambda h: h.scalar_tensor_tensor(out=out, in0=in0, scalar=scalar, in1=in1, op0=op0, op1=op1), reads=reads, writes=writes)


def o_memset(S, eng, ap, val, writes):
    S.op(eng, lambda h: h.memset(ap, val), writes=writes)


def o_recip(S, out, in_, reads, writes):
    S.op("dve", lambda h: h.reciprocal(out=out, in_=in_), reads=reads, writes=writes)


def make_ident(S, t, key):
    o_memset(S, "pool", t[:], 0.0, [key])
    S.op("pool", lambda h: h.affine_select(out=t[:], in_=t[:], pattern=[[-1, 128]], compare_op=ALU.not_equal,
                                           fill=1.0, base=0, channel_multiplier=1), reads=[key], writes=[key])


class St:
    def __init__(self, nc, name):
        self.nc = nc
        self.name = name
        self.es = ExitStack()

    def __enter__(self):
        self.es.__enter__()
        return self

    def __exit__(self, *a):
        return self.es.__exit__(*a)

    def sb(self, n, shape, dt):
        return self.es.enter_context(self.nc.sbuf_tensor(self.name + "_" + n, shape, dt))

    def ps(self, n, shape, dt):
        return self.es.enter_context(self.nc.psum_tensor(self.name + "_" + n, shape, dt))


def stage_A(nc, S, T):
    with St(nc, "A") as st:
        Wb = st.sb("Wb", [128, 16, 4096], BF16)
        ident = st.sb("ident", [128, 128], F32)
        xin = st.sb("xin", [128, 2, 2048], F32)
        xT = st.sb("xT", [128, 16, 512], BF16)
        ofm = st.sb("ofm", [128, 4, 512], BF16)
        otf = st.sb("otf", [128, 2, 512], F32)
        otb = st.sb("otb", [128, 2, 512], BF16)
        ptr = [st.ps("ptr%d" % i, [128, 512], F32) for i in range(2)]
        pmm = [st.ps("pmm%d" % i, [128, 512], F32) for i in range(4)]
        make_ident(S, ident, "ident")
        w_in = T["w_in"]
        for blk in (6, 7, 2, 3, 4, 5, 0, 1):
            o_dma(S, "pool", Wb[:, :, blk * 512:(blk + 1) * 512],
                  w_in[:, blk * 512:(blk + 1) * 512].rearrange("(c p) n -> p c n", p=128),
                  writes=[("Wb", blk)], grp="Wb%d" % blk)
        groups = [[4 * g + j for j in range(4)] for g in range(8)] + [[32]]
        if DBG.get("A_groups") is not None:
            groups = [groups[i] for i in DBG["A_groups"]]
        cnt = {"x": 0, "tr": 0, "fm": 0, "mm": 0, "tf": 0, "tb": 0}
        for tiles in groups:
            ntok = 128 * len(tiles)
            t0 = tiles[0]
            far = t0 < 8
            near = 8 <= t0 < 24
            own = t0 >= 24
            for j, tl in enumerate(tiles):
                slot = cnt["x"] % 2
                cnt["x"] += 1
                src = T["xs"][:, :] if tl == 32 else T["xw"][tl * 128:(tl + 1) * 128, :]
                o_dma(S, "sp", xin[:, slot, :], src, writes=[("xin", slot)], grp="xin%d" % slot)
                for b4 in range(4):
                    pi = cnt["tr"] % 2
                    cnt["tr"] += 1
                    for q in range(4):
                        c = b4 * 4 + q
                        o_tr(S, ptr[pi][:, q * 128:(q + 1) * 128], xin[:, slot, c * 128:(c + 1) * 128], ident[:],
                             reads=[("xin", slot), "ident"] + ([("Wb", i) for i in range(8)] if DBG.get("A_waitW") else []), writes=[("ptr", pi)])
                    o_copy(S, "act" if pi == 0 else "dve", xT[:, b4 * 4:(b4 + 1) * 4, j * 128:(j + 1) * 128],
                           ptr[pi][:, :].rearrange("p (a b) -> p a b", a=4), reads=[("ptr", pi)], writes=["xT"])
            if t0 == 32:
                qcol, kcol, ucol = 1024, 3072, 4096
            else:
                qcol, kcol, ucol = (t0 - 24) * 128, (t0 - 8) * 128, t0 * 128
            fm = []
            if own:
                fm += [(h * 128, T["qT"][h, :, qcol:qcol + ntok]) for h in range(8)]
            if own or near:
                fm += [(1024 + h * 128, T["kT"][h, :, kcol:kcol + ntok]) for h in range(8)]
            fm += [(3072 + c * 128, T["uT"][c, :, ucol:ucol + ntok]) for c in range(8)]
            if DBG.get("A_nofm"):
                fm = []
            if DBG.get("A_fmn") is not None:
                fm = fm[:DBG["A_fmn"]]
            for (wc, dst) in fm:
                pi = cnt["mm"] % 4
                cnt["mm"] += 1
                for k in range(16):
                    o_mm(S, pmm[pi][:, 0:ntok], Wb[:, k, wc:wc + 128], xT[:, k, 0:ntok], k == 0, k == 15,
                         reads=["xT", ("Wb", wc // 512)], writes=[("pmm", pi)])
                oi = cnt["fm"] % 4
                cnt["fm"] += 1
                o_copy(S, "act" if oi % 2 == 0 else "dve", ofm[:, oi, 0:ntok], pmm[pi][:, 0:ntok],
                       reads=[("pmm", pi)], writes=[("ofm", oi)])
                o_dma(S, DBG.get("A_stq", "sp"), dst, ofm[:, oi, 0:ntok], reads=[("ofm", oi)], grp="ofm%d" % oi)
            if (own or near) and not DBG.get("A_notm"):
                for j, tl in enumerate(tiles):
                    row_kv = (tl - 8) * 128 if tl < 32 else 3072
                    row_o = (tl - 24) * 128 if tl < 32 else 1024
                    banks = []
                    if own:
                        banks += [("k", 1024), ("k", 1536)]
                    banks += [("v", 2048), ("v", 2560)]
                    if DBG.get("A_banks") is not None:
                        banks = [banks[i] for i in DBG["A_banks"]]
                    for (kv, wc) in banks:
                        pi = cnt["mm"] % 4
                        cnt["mm"] += 1
                        for k in range(16):
                            o_mm(S, pmm[pi][:, :], xT[:, k, j * 128:(j + 1) * 128], Wb[:, k, wc:wc + 512], k == 0, k == 15,
                                 reads=["xT", ("Wb", wc // 512)], writes=[("pmm", pi)])
                        colo = wc - (1024 if kv == "k" else 2048)
                        if own and not DBG.get("A_nootf"):
                            fi = cnt["tf"] % 2
                            cnt["tf"] += 1
                            o_copy(S, DBG.get("A_otfe", "act"), otf[:, fi, :], pmm[pi][:, :], reads=[("pmm", pi)], writes=[("otf", fi)])
                            o_dma(S, DBG.get("A_stq", "sp"), T["Kout" if kv == "k" else "Vout"][row_o:row_o + 128, colo:colo + 512],
                                  otf[:, fi, :], reads=[("otf", fi)], grp="otf%d" % fi)
                        if kv == "v" and not DBG.get("A_nootb"):
                            bi = cnt["tb"] % 2
                            cnt["tb"] += 1
                            o_copy(S, DBG.get("A_otbe", "dve"), otb[:, bi, :], pmm[pi][:, :], reads=[("pmm", pi)] + ([("otf", 0), ("otf", 1)] if DBG.get("A_ser") else []), writes=[("otb", bi)])
                            o_dma(S, DBG.get("A_stq", "sp"), T["V_scr"][row_kv:row_kv + 128, colo:colo + 512], otb[:, bi, :],
                                  reads=[("otb", bi)], grp="otb%d" % bi)
        S.emit()


def stage_B(nc, S, T):
    with St(nc, "B") as st:
        kT = st.sb("kT", [128, 2, KPOS], BF16)
        Vh = st.sb("Vh", [128, 2, 25, 128], BF16)
        qT = st.sb("qT", [128, 2, TOK], BF16)
        pmask = st.sb("pmask", [128, 17, 128], BF16)
        smask = st.sb("smask", [128, 128], BF16)
        smaskn = st.sb("smaskn", [128, 128], BF16)
        vb = st.sb("vb", [128, 24], F32)
        gatt = st.sb("gatt", [128, 8], F32)
        onesb = st.sb("onesb", [128, 128], BF16)
        identb = st.sb("identb", [128, 128], BF16)
        p = st.sb("p", [128, 2, 512], BF16)
        pm = st.sb("pm", [128, 2, 512], BF16)
        onesv = st.sb("onesv", [128, 24, 128], BF16)
        rd = st.sb("rd", [128, 2, 128], F32)
        a32 = st.sb("a32", [128, 2, 128], F32)
        asq = st.sb("asq", [128, 8, TOK], BF16)
        ag = st.sb("ag", [128, 8, TOK], BF16)
        kc = st.sb("kc", [128, 2, 16, 128], BF16)
        vc = st.sb("vc", [128, 2, 16, 128], BF16)
        kcT = st.sb("kcT", [128, 2048], BF16)
        psm = st.sb("psm", [128, 128], BF16)
        psn = st.sb("psn", [128, 128], BF16)
        pmm_ = st.sb("pmm_", [128, 128], BF16)
        pmn = st.sb("pmn", [128, 128], BF16)
        rd8 = st.sb("rd8", [128, 8], F32)
        a8 = st.sb("a8", [128, 8], F32)
        ssa = st.sb("ssa", [128, 16], F32)
        ps_s2 = [st.ps("ps_s%d" % i, [128, 512], F32) for i in range(2)]
        ps_n2 = [st.ps("ps_n%d" % i, [128, 512], F32) for i in range(2)]
        ps_d2 = [st.ps("ps_d%d" % i, [128, 512], F32) for i in range(2)]
        ptk1 = st.ps("ptk", [128, 1024], BF16)
        ptk = [ptk1, ptk1]
        ps_big = st.ps("ps_big", [128, 512], F32)
        ps_sm = ps_big[:, 0:128]
        ps_new = ps_big[:, 128:256]
        ps_snd = ps_big[:, 256:272]
        ps_ss = ps_big[:, 272:288]

        o_memset(S, "dve", onesb[:], 1.0, ["onesb"])
        make_ident(S, identb, "identb")
        o_dma(S, "pool", pmask[:].rearrange("p a b -> p (a b)"), T["pmask"][:, :], writes=["pmask"], grp="pmask")
        o_dma(S, "pool", smask[:], T["smask"][:, :], writes=["smask"], grp="smask")
        o_dma(S, "pool", smaskn[:], T["smaskn"][:, :], writes=["smaskn"], grp="smaskn")
        o_dma(S, "sp", vb[:], T["vbias"][:, :], writes=["vb"], grp="vb")
        o_dma(S, "sp", gatt[:], T["gatt"][:, :], writes=["gatt"], grp="gatt")
        for j in range(24):
            o_ts(S, "pool", onesv[:, j, :], onesb[:], vb[:, j:j + 1], ALU.mult, reads=["onesb", "vb"], writes=["onesv"])
        o_memset(S, "pool", asq[:, :, 1024:TOK], 0.0, [("asq", h, 8) for h in range(8)])
        o_memset(S, "pool", ag[:, :, 1024:TOK], 0.0, [("ag", h, 8) for h in range(8)])
        it = 0
        blk_i = 0
        sh_i = 0
        for h in range(8):
            b = h % 2
            o_dma(S, "sp", kT[:, b, :], T["kT"][h, :, :], writes=[("kT", b)], grp="kT%d" % b)
            o_dma(S, "sp", Vh[:, b, :, :], T["V_scr"][:, h * 128:(h + 1) * 128].rearrange("(c p) n -> p c n", p=128),
                  writes=[("Vh", b)], grp="Vh%d" % b)
            o_dma(S, "sp", qT[:, b, :], T["qT"][h, :, :], writes=[("qT", b)], grp="qT%d" % b)
            groups_ = [(m, c0, n) for m in range(8) for (c0, n) in ((0, 4), (4, 4), (8, 4), (12, 4), (16, 1))]

            def score(gi):
                m, c0, n = groups_[gi]
                sg = (g0 + gi) % 2
                for q in range(n):
                    kcx = m + c0 + q
                    o_mm(S, ps_s2[sg][:, q * 128:(q + 1) * 128], kT[:, b, kcx * 128:(kcx + 1) * 128], qT[:, b, m * 128:(m + 1) * 128],
                         True, True, reads=[("kT", b), ("qT", b)], writes=[("ps_s", sg)])
                o_act(S, p[:, sg, 0:n * 128], ps_s2[sg][:, 0:n * 128], AF.Exp, reads=[("ps_s", sg)], writes=[("p", sg)], scale=SC_ATT)
                o_tt(S, "dve" if sg == 0 else "pool", pm[:, sg, 0:n * 128], p[:, sg, 0:n * 128],
                     pmask[:, c0:c0 + n, :].rearrange("p a b -> p (a b)"), ALU.mult,
                     reads=[("p", sg), "pmask"], writes=[("pm", sg)])

            g0 = it
            score(0)
            for gi, (m, c0, n) in enumerate(groups_):
                if gi + 1 < len(groups_):
                    score(gi + 1)
                sg = (g0 + gi) % 2
                sl = (blk_i + m) % 2
                for q in range(n):
                    kcx = m + c0 + q
                    cc = c0 + q
                    o_mm(S, ps_n2[sl][:, 0:128], Vh[:, b, kcx, :], pm[:, sg, q * 128:(q + 1) * 128], cc == 0, cc == 16,
                         reads=[("Vh", b), ("pm", sg)], writes=[("ps_n", sl)])
                for q in range(n):
                    kcx = m + c0 + q
                    cc = c0 + q
                    o_mm(S, ps_d2[sl][:, 0:128], onesv[:, kcx, :], pm[:, sg, q * 128:(q + 1) * 128], cc == 0, cc == 16,
                         reads=["onesv", ("pm", sg)], writes=[("ps_d", sl)])
                if c0 == 16:
                    o_recip(S, rd[:, sl, :], ps_d2[sl][:, 0:128], reads=[("ps_d", sl)], writes=[("rd", sl)])
                    o_tt(S, "dve", a32[:, sl, :], ps_n2[sl][:, 0:128], rd[:, sl, :], ALU.mult,
                         reads=[("ps_n", sl), ("rd", sl)], writes=[("a32", sl)])
                    o_tt(S, "pool", asq[:, h, m * 128:(m + 1) * 128], a32[:, sl, :], a32[:, sl, :], ALU.mult,
                         reads=[("a32", sl)], writes=[("asq", h, m)])
                    o_ts(S, "pool", ag[:, h, m * 128:(m + 1) * 128], a32[:, sl, :], gatt[:, h:h + 1], ALU.mult,
                         reads=[("a32", sl), "gatt"], writes=[("ag", h, m)])
            it += len(groups_)
            blk_i += 8
            o_mm(S, ps_new[:, :], kT[:, b, 3072:3200], qT[:, b, 1024:1152], True, True,
                 reads=[("kT", b), ("qT", b)], writes=["ps_big"])
            o_act(S, psn[:], ps_new[:, :], AF.Exp, reads=["ps_big"], writes=["psn"], scale=SC_ATT)
            o_tt(S, "dve", pmn[:], psn[:], smaskn[:], ALU.mult, reads=["psn", "smaskn"], writes=["pmn"])
            for s in range(4):
                sb_ = sh_i % 2
                sh_i += 1
                o_dma(S, "pool", kc[:, sb_, :, :], T["cwk"][s, :, h * 128:(h + 1) * 128].rearrange("(c p) n -> p c n", p=128),
                      writes=[("kc", sb_)], grp="kc%d" % sb_)
                o_dma(S, "pool", vc[:, sb_, :, :], T["cwv"][s, :, h * 128:(h + 1) * 128].rearrange("(c p) n -> p c n", p=128),
                      writes=[("vc", sb_)], grp="vc%d" % sb_)
                for half in range(2):
                    for q8 in range(8):
                        cc = half * 8 + q8
                        o_tr(S, ptk[half][:, q8 * 128:(q8 + 1) * 128], kc[:, sb_, cc, :], identb[:],
                             reads=[("kc", sb_), "identb"], writes=["ptk"])
                    o_copy(S, "act" if half == 0 else "dve", kcT[:, half * 1024:(half + 1) * 1024], ptk[half][:, :],
                           reads=["ptk"], writes=[("kcT", half)])
                q0 = 1024 + 32 * s
                for cc in range(16):
                    o_mm(S, ps_sm[:, cc * 8:(cc + 1) * 8], kcT[:, cc * 128:(cc + 1) * 128], qT[:, b, q0:q0 + 8], True, True,
                         reads=[("kcT", cc // 8), ("qT", b)], writes=["ps_big"])
                o_act(S, psm[:], ps_sm[:, 0:128], AF.Exp, reads=["ps_big"], writes=["psm"], scale=SC_ATT)
                o_tt(S, "dve", pmm_[:], psm[:], smask[:], ALU.mult, reads=["psm", "smask"], writes=["pmm_"])
                for cc in range(16):
                    o_mm(S, ps_snd[:, 0:8], vc[:, sb_, cc, :], pmm_[:, cc * 8:(cc + 1) * 8], cc == 0, False,
                         reads=[("vc", sb_), "pmm_"], writes=["ps_big"])
                o_mm(S, ps_snd[:, 0:8], Vh[:, b, 24, :], pmn[:, 32 * s:32 * s + 8], False, True,
                     reads=[("Vh", b), "pmn"], writes=["ps_big"])
                for cc in range(16):
                    o_mm(S, ps_snd[:, 8:16], onesb[:], pmm_[:, cc * 8:(cc + 1) * 8], cc == 0, False,
                         reads=["onesb", "pmm_"], writes=["ps_big"])
                o_mm(S, ps_snd[:, 8:16], onesb[:], pmn[:, 32 * s:32 * s + 8], False, True,
                     reads=["onesb", "pmn"], writes=["ps_big"])
                o_recip(S, rd8[:], ps_snd[:, 8:16], reads=["ps_big"], writes=["rd8"])
                o_tt(S, "dve", a8[:], ps_snd[:, 0:8], rd8[:], ALU.mult, reads=["ps_big", "rd8"], writes=["a8"])
                o_tt(S, "pool", asq[:, h, q0:q0 + 8], a8[:], a8[:], ALU.mult, reads=["a8"], writes=[("asq", h, 8)])
                o_ts(S, "pool", ag[:, h, q0:q0 + 8], a8[:], gatt[:, h:h + 1], ALU.mult, reads=["a8", "gatt"], writes=[("ag", h, 8)])
        for t in range(NT):
            for h in range(8):
                o_mm(S, ps_ss[:, t:t + 1], asq[:, h, t * 128:(t + 1) * 128], onesb[:, 0:1], h == 0, h == 7,
                     reads=[("asq", h, t), "onesb"], writes=["ps_big"])
        o_act(S, ssa[:, 0:NT], ps_ss[:, 0:NT], AF.Sqrt, reads=["ps_big"], writes=["ssa"], bias=1e-6, scale=1.0 / 1024.0)
        o_recip(S, ssa[:, 0:NT], ssa[:, 0:NT], reads=["ssa"], writes=["ssa"])
        o_dma(S, "sp", T["rstd_a"][:, :], ssa[:, 0:NT], reads=["ssa"], grp="rstd")
        o_dma(S, "sp", T["mixT"][0:8, :, :].rearrange("h p t -> p h t"), ag[:, :, :],
              reads=[("ag", h, m) for h in range(8) for m in range(9)], grp="agst")
        S.emit()


def stage_C(nc, S, T):
    with St(nc, "C") as st:
        lre = st.sb("lre", [128, 32], F32)
        lim = st.sb("lim", [128, 32], F32)
        ldt = st.sb("ldt", [128, 32], F32)
        tA = st.sb("tA", [128, 32], F32)
        tB = st.sb("tB", [128, 32], F32)
        tC = st.sb("tC", [128, 32], F32)
        tI = st.sb("tI", [128, 32], mybir.dt.int32)
        mag = st.sb("mag", [128, 32], F32)
        fre = st.sb("fre", [128, 32], F32)
        fim = st.sb("fim", [128, 32], F32)
        nfim = st.sb("nfim", [128, 32], F32)
        pwr = st.sb("pwr", [128, 11, 32], F32)
        pwi = st.sb("pwi", [128, 11, 32], F32)
        npwi = st.sb("npwi", [128, 11, 32], F32)
        BTre = st.sb("BTre", [128, 32, 128], BF16)
        BTim = st.sb("BTim", [128, 32, 128], BF16)
        CTre = st.sb("CTre", [128, 32, 128], BF16)
        CTim = st.sb("CTim", [128, 32, 128], BF16)
        dcol = st.sb("dcol", [128, 8], F32)
        h0r = st.sb("h0r", [128, 32, 4], F32)
        h0i = st.sb("h0i", [128, 32, 4], F32)
        uT = st.sb("uT", [128, 2, UPOS], BF16)
        xr2 = st.sb("xr", [128, 2, UPOS], F32)
        xi2 = st.sb("xi", [128, 2, UPOS], F32)
        T0r = st.sb("T0r", [128, 1536], F32)
        T0i = st.sb("T0i", [128, 1536], F32)
        T1r = st.sb("T1r", [128, 768], F32)
        T1i = st.sb("T1i", [128, 768], F32)
        identF = st.sb("identF", [128, 128], F32)
        onesF = st.sb("onesF", [128, 128], F32)
        Dre = st.sb("Dre", [128, 2, 512], F32)
        Dim = st.sb("Dim", [128, 2, 512], F32)
        Bsr = st.sb("Bsr", [128, 4, 8], F32)
        Bsi = st.sb("Bsi", [128, 4, 8], F32)
        Hr = st.sb("Hr", [128, 4], F32)
        Hi = st.sb("Hi", [128, 4], F32)
        tmp = st.sb("tmp", [128, 4, 512], F32)
        hbr = st.sb("hbr", [128, TOK], BF16)
        hbi = st.sb("hbi", [128, TOK], BF16)
        hfr = st.sb("hfr", [128, 32], F32)
        hfi = st.sb("hfi", [128, 32], F32)
        hsr = st.sb("hsr", [128, 32, 4], F32)
        hsi = st.sb("hsi", [128, 32, 4], F32)
        ysb = st.sb("ysb", [128, TOK], F32)
        gt1 = st.sb("gt1", [128, TOK], F32)
        yg = st.sb("yg", [128, 2, TOK], F32)
        ygb = st.sb("ygb", [128, 2, TOK], BF16)
        pw = [st.ps("pw%d" % i, [128, 512], F32) for i in range(4)]
        py = [st.ps("py%d" % i, [128, 512], F32) for i in range(3)]

        for (t_, nm) in ((lre, "lamre"), (lim, "lamim"), (ldt, "logdt")):
            o_dma(S, "sp", t_[:], T[nm][:, :], writes=[nm], grp=nm)
        o_dma(S, "sp", dcol[:], T["dcol"][:, :], writes=["dcol"], grp="dcol")
        o_dma(S, "sp", h0r[:].rearrange("p a b -> p (a b)"), T["h0re"][:, :], writes=["h0r"], grp="h0r")
        o_dma(S, "sp", h0i[:].rearrange("p a b -> p (a b)"), T["h0im"][:, :], writes=["h0i"], grp="h0i")
        for (t_, nm) in ((BTre, "BTre"), (BTim, "BTim"), (CTre, "CTre"), (CTim, "CTim")):
            o_dma(S, "pool", t_[:].rearrange("p a b -> p (a b)"), T[nm][:, :], writes=[nm], grp=nm)
        P = ["par"]
        o_act(S, ldt[:], ldt[:], AF.Exp, reads=["logdt"], writes=["logdt"] + P)
        o_tt(S, "dve", tA[:], lre[:], ldt[:], ALU.mult, reads=["lamre", "logdt"], writes=P)
        o_act(S, mag[:], tA[:], AF.Exp, reads=P, writes=P)
        o_tt(S, "dve", tB[:], lim[:], ldt[:], ALU.mult, reads=["lamim", "logdt"], writes=P)

        def sin_of(dst, shift):
            o_ts(S, "dve", tC[:], tB[:], shift, ALU.add, reads=P, writes=P, s2=1.0 / (2 * np.pi), op1=ALU.mult)
            S.op("dve", lambda h: h.tensor_copy(out=tI[:], in_=tC[:]), reads=P, writes=P)
            S.op("dve", lambda h: h.tensor_copy(out=tA[:], in_=tI[:]), reads=P, writes=P)
            o_tt(S, "dve", tC[:], tC[:], tA[:], ALU.subtract, reads=P, writes=P)
            o_ts(S, "dve", tA[:], tC[:], 0.5, ALU.is_gt, reads=P, writes=P)
            o_tt(S, "dve", tC[:], tC[:], tA[:], ALU.subtract, reads=P, writes=P)
            o_ts(S, "dve", tA[:], tC[:], -0.5, ALU.is_lt, reads=P, writes=P)
            o_tt(S, "dve", tC[:], tC[:], tA[:], ALU.add, reads=P, writes=P)
            o_ts(S, "dve", tC[:], tC[:], 2 * np.pi, ALU.mult, reads=P, writes=P, s2=3.14159, op1=ALU.min)
            o_ts(S, "dve", tC[:], tC[:], -3.14159, ALU.max, reads=P, writes=P)
            o_act(S, dst, tC[:], AF.Sin, reads=P, writes=P)

        sin_of(pwi[:, 0, :], 0.0)
        sin_of(pwr[:, 0, :], np.pi / 2)
        o_tt(S, "dve", pwr[:, 0, :], pwr[:, 0, :], mag[:], ALU.mult, reads=P, writes=P)
        o_tt(S, "dve", pwi[:, 0, :], pwi[:, 0, :], mag[:], ALU.mult, reads=P, writes=P)
        o_tt(S, "dve", tA[:], lre[:], lre[:], ALU.mult, reads=P + ["lamre"], writes=P)
        o_tt(S, "dve", tC[:], lim[:], lim[:], ALU.mult, reads=P + ["lamim"], writes=P)
        o_tt(S, "dve", tA[:], tA[:], tC[:], ALU.add, reads=P, writes=P)
        o_recip(S, tA[:], tA[:], reads=P, writes=P)
        o_ts(S, "dve", tB[:], pwr[:, 0, :], -1.0, ALU.add, reads=P, writes=P)
        o_tt(S, "dve", fre[:], tB[:], lre[:], ALU.mult, reads=P, writes=P)
        o_tt(S, "dve", tC[:], pwi[:, 0, :], lim[:], ALU.mult, reads=P, writes=P)
        o_tt(S, "dve", fre[:], fre[:], tC[:], ALU.add, reads=P, writes=P)
        o_tt(S, "dve", fre[:], fre[:], tA[:], ALU.mult, reads=P, writes=P)
        o_tt(S, "dve", fim[:], pwi[:, 0, :], lre[:], ALU.mult, reads=P, writes=P)
        o_tt(S, "dve", tC[:], tB[:], lim[:], ALU.mult, reads=P, writes=P)
        o_tt(S, "dve", fim[:], fim[:], tC[:], ALU.subtract, reads=P, writes=P)
        o_tt(S, "dve", fim[:], fim[:], tA[:], ALU.mult, reads=P, writes=P)
        o_ts(S, "dve", nfim[:], fim[:], -1.0, ALU.mult, reads=P, writes=P)
        for k in range(10):
            o_tt(S, "dve", tA[:], pwr[:, k, :], pwr[:, k, :], ALU.mult, reads=P, writes=P)
            o_tt(S, "dve", tC[:], pwi[:, k, :], pwi[:, k, :], ALU.mult, reads=P, writes=P)
            o_tt(S, "dve", pwr[:, k + 1, :], tA[:], tC[:], ALU.subtract, reads=P, writes=P)
            o_tt(S, "dve", tA[:], pwr[:, k, :], pwi[:, k, :], ALU.mult, reads=P, writes=P)
            o_ts(S, "dve", pwi[:, k + 1, :], tA[:], 2.0, ALU.mult, reads=P, writes=P)
        o_ts(S, "dve", npwi[:].rearrange("p a b -> p (a b)"), pwi[:].rearrange("p a b -> p (a b)"), -1.0, ALU.mult, reads=P, writes=P)
        make_ident(S, identF, "identF")
        o_memset(S, "pool", onesF[:], 1.0, ["onesF"])
        for g in range(8):
            db = g % 2
            for q in range(4):
                tq = 4 * g + q
                o_ts(S, "pool", Dre[:, db, 128 * q:128 * (q + 1)], identF[:], fre[:, tq:tq + 1], ALU.mult,
                     reads=["identF"] + P, writes=[("Dre", db)])
                o_ts(S, "pool", Dim[:, db, 128 * q:128 * (q + 1)], identF[:], fim[:, tq:tq + 1], ALU.mult,
                     reads=["identF"] + P, writes=[("Dim", db)])
            o_mm(S, pw[0][:, :], onesF[:], Dre[:, db, :], True, True, reads=["onesF", ("Dre", db)], writes=[("pw", 0)])
            o_mm(S, pw[1][:, :], onesF[:], Dim[:, db, :], True, True, reads=["onesF", ("Dim", db)], writes=[("pw", 1)])
            bre = BTre[:, 4 * g:4 * g + 4, :].rearrange("p a b -> p (a b)")
            bim = BTim[:, 4 * g:4 * g + 4, :].rearrange("p a b -> p (a b)")
            o_tt(S, "dve", tmp[:, 0, :], pw[0][:, :], bre, ALU.mult, reads=[("pw", 0), "BTre"], writes=[("tmp", 0)])
            o_tt(S, "dve", tmp[:, 1, :], pw[1][:, :], bim, ALU.mult, reads=[("pw", 1), "BTim"], writes=[("tmp", 1)])
            o_tt(S, "dve", tmp[:, 2, :], pw[0][:, :], bim, ALU.mult, reads=[("pw", 0), "BTim"], writes=[("tmp", 2)])
            o_tt(S, "dve", tmp[:, 3, :], pw[1][:, :], bre, ALU.mult, reads=[("pw", 1), "BTre"], writes=[("tmp", 3)])
            o_tt(S, "dve", bre, tmp[:, 0, :], tmp[:, 1, :], ALU.subtract, reads=[("tmp", 0), ("tmp", 1)], writes=["BTre"])
            o_tt(S, "dve", bim, tmp[:, 2, :], tmp[:, 3, :], ALU.add, reads=[("tmp", 2), ("tmp", 3)], writes=["BTim"])
        o_memset(S, "pool", hbr[:, 1024:TOK], 0.0, ["hbr_s"])
        o_memset(S, "pool", hbi[:, 1024:TOK], 0.0, ["hbi_s"])

        def cma(dr, di, er, ei, orr, oi, k, tau, rk, wk):
            pr = pwr[:, k, tau:tau + 1]
            pi_ = pwi[:, k, tau:tau + 1]
            npi = npwi[:, k, tau:tau + 1]
            wr_ = [(w_, "r") for w_ in wk]
            wi_ = [(w_, "i") for w_ in wk]
            o_stt(S, dr, er, pr, orr, ALU.mult, ALU.add, reads=rk + P, writes=wr_)
            o_stt(S, di, ei, pr, oi, ALU.mult, ALU.add, reads=rk + P, writes=wi_)
            o_stt(S, dr, ei, npi, dr, ALU.mult, ALU.add, reads=rk + P, writes=wr_)
            o_stt(S, di, er, pi_, di, ALU.mult, ALU.add, reads=rk + P, writes=wi_ + list(wk))
            S.touch(wk, wr_ + wi_)

        blocks = [(i * 512, 512) for i in range(8)] + [(4096, 128)]
        wic = [0]

        def emit_xe(tau, ub):
            xr = xr2[:, tau % 2, :]
            xi = xi2[:, tau % 2, :]
            XK = ("x", tau % 2)
            for (c0, w) in blocks:
                wi = wic[0]
                p0 = pw[(wi * 2) % 4]
                p1 = pw[(wi * 2 + 1) % 4]
                k0 = ("pw", (wi * 2) % 4)
                k1 = ("pw", (wi * 2 + 1) % 4)
                wic[0] += 1
                o_mm(S, p0[:, 0:w], BTre[:, tau, :], uT[:, ub, c0:c0 + w], True, True, reads=["BTre", ("uT", ub)], writes=[k0])
                o_mm(S, p1[:, 0:w], BTim[:, tau, :], uT[:, ub, c0:c0 + w], True, True, reads=["BTim", ("uT", ub)], writes=[k1])
                o_copy(S, "act", xr[:, c0:c0 + w], p0[:, 0:w], reads=[k0], writes=[XK])
                o_copy(S, "act", xi[:, c0:c0 + w], p1[:, 0:w], reads=[k1], writes=[XK])

        o_dma(S, "sp", uT[:, 0, :], T["uT"][0, :, :], writes=[("uT", 0)], grp="uT0")
        emit_xe(0, 0)
        for c in range(8):
            ub = c % 2
            for j in range(4):
                tau = 4 * c + j
                xr = xr2[:, tau % 2, :]
                xi = xi2[:, tau % 2, :]
                XK = ("x", tau % 2)
                if tau + 1 < 32:
                    cn = (tau + 1) // 4
                    if (tau + 1) % 4 == 0:
                        o_dma(S, "sp", uT[:, cn % 2, :], T["uT"][cn, :, :], writes=[("uT", cn % 2)], grp="uT%d" % (cn % 2))
                    emit_xe(tau + 1, cn % 2)
                X = [XK]
                xs_r = xr[:, 4096:UPOS:32]
                xs_i = xi[:, 4096:UPOS:32]
                cma(xs_r, xs_i, h0r[:, tau, :], h0i[:, tau, :], xs_r, xs_i, 0, tau, X + ["h0r", "h0i"], X)
                src_r, src_i, n = xr, xi, 3072
                dsts = [(T0r, T0i), (T1r, T1i)]
                for k in range(10):
                    dr_, di_ = dsts[k % 2]
                    no = n // 2
                    cma(dr_[:, 0:no], di_[:, 0:no], src_r[:, 0:n:2], src_i[:, 0:n:2], src_r[:, 1:n:2], src_i[:, 1:n:2],
                        k, tau, X + ["tree"], ["tree"])
                    src_r, src_i, n = dr_, di_, no
                cma(Hr[:, 0:1], Hi[:, 0:1], src_r[:, 0:1], src_i[:, 0:1], src_r[:, 1:2], src_i[:, 1:2], 10, tau, ["tree"], ["H"])
                cma(Hr[:, 1:2], Hi[:, 1:2], Hr[:, 0:1], Hi[:, 0:1], src_r[:, 2:3], src_i[:, 2:3], 10, tau, ["tree", "H"], ["H"])
                cma(xr[:, 3072:3073], xi[:, 3072:3073], Hr[:, 1:2], Hi[:, 1:2], xr[:, 3072:3073], xi[:, 3072:3073], 0, tau, X + ["H"], X)
                o_ = 3072
                for k in range(10):
                    st_ = 1 << (k + 1)
                    h_ = 1 << k
                    cnt = 1024 // st_
                    t0 = o_ + st_ - 1
                    s0 = o_ + h_ - 1
                    t1 = t0 + (cnt - 1) * st_ + 1
                    s1 = s0 + (cnt - 1) * st_ + 1
                    cma(xr[:, t0:t1:st_], xi[:, t0:t1:st_], xr[:, s0:s1:st_], xi[:, s0:s1:st_],
                        xr[:, t0:t1:st_], xi[:, t0:t1:st_], k, tau, X, X)
                for k in range(8, -1, -1):
                    st_ = 1 << (k + 1)
                    h_ = 1 << k
                    cnt = 1024 // st_ - 1
                    t0 = o_ + st_ + h_ - 1
                    s0 = o_ + st_ - 1
                    t1 = t0 + (cnt - 1) * st_ + 1
                    s1 = s0 + (cnt - 1) * st_ + 1
                    cma(xr[:, t0:t1:st_], xi[:, t0:t1:st_], xr[:, s0:s1:st_], xi[:, s0:s1:st_],
                        xr[:, t0:t1:st_], xi[:, t0:t1:st_], k, tau, X, X)
                sr = xr[:, 4096:UPOS].rearrange("p (s j) -> p s j", j=32)[:, :, 0:8]
                si_ = xi[:, 4096:UPOS].rearrange("p (s j) -> p s j", j=32)[:, :, 0:8]
                cur = (sr, si_, XK)
                alt = (Bsr[:, :, :], Bsi[:, :, :], "Bs")
                for k in range(3):
                    s_ = 1 << k
                    cr, ci, ck = cur
                    ar_, ai_, ak = alt
                    cma(ar_[:, :, s_:8], ai_[:, :, s_:8], cr[:, :, 0:8 - s_], ci[:, :, 0:8 - s_], cr[:, :, s_:8], ci[:, :, s_:8],
                        k, tau, [ck], [ak])
                    o_copy(S, "act", ar_[:, :, 0:s_], cr[:, :, 0:s_], reads=[ck], writes=[ak])
                    o_copy(S, "pool", ai_[:, :, 0:s_], ci[:, :, 0:s_], reads=[ck], writes=[ak])
                    cur, alt = alt, cur
                fr, fi_, fk = cur
                o_copy(S, "act", hfr[:, tau:tau + 1], xr[:, 4095:4096], reads=X, writes=["hf"])
                o_copy(S, "act", hfi[:, tau:tau + 1], xi[:, 4095:4096], reads=X, writes=["hf"])
                o_copy(S, "pool", hsr[:, tau, :], fr[:, :, 7], reads=[fk], writes=["hs"])
                o_copy(S, "pool", hsi[:, tau, :], fi_[:, :, 7], reads=[fk], writes=["hs"])
                o_copy(S, "act", hbr[:, 0:1024], xr[:, 3072:4096], reads=X, writes=["hbr"])
                o_copy(S, "act", hbi[:, 0:1024], xi[:, 3072:4096], reads=X, writes=["hbi"], scale=-1.0)
                o_copy(S, "pool", hbr[:, 1024:TOK].rearrange("p (s j) -> p s j", j=32)[:, :, 0:8], fr, reads=[fk, "hbr_s"], writes=["hbr_s"])
                o_ts(S, "pool", hbi[:, 1024:TOK].rearrange("p (s j) -> p s j", j=32)[:, :, 0:8], fi_, -1.0, ALU.mult,
                     reads=[fk, "hbi_s"], writes=["hbi_s"])
                for bi, (c0, w) in enumerate([(0, 512), (512, 512), (1024, 128)]):
                    o_mm(S, py[bi][:, 0:w], CTre[:, tau, :], hbr[:, c0:c0 + w], j == 0, False,
                         reads=["CTre", "hbr", "hbr_s"], writes=[("py", bi)])
                    o_mm(S, py[bi][:, 0:w], CTim[:, tau, :], hbi[:, c0:c0 + w], False, j == 3,
                         reads=["CTim", "hbi", "hbi_s"], writes=[("py", bi)])
            yb_ = c % 2
            for bi, (c0, w) in enumerate([(0, 512), (512, 512), (1024, 128)]):
                o_stt(S, ysb[:, c0:c0 + w], uT[:, ub, 3072 + c0:3072 + c0 + w], dcol[:, c:c + 1], py[bi][:, 0:w], ALU.mult, ALU.add,
                      reads=[("uT", ub), ("py", bi), "dcol"], writes=["ysb"])
            o_tt(S, "pool", gt1[:], ysb[:], ysb[:], ALU.mult, reads=["ysb"], writes=["gt1"])
            o_ts(S, "dve", gt1[:], gt1[:], 0.044715, ALU.mult, reads=["gt1"], writes=["gt1"], s2=1.0, op1=ALU.add)
            o_tt(S, "dve", gt1[:], gt1[:], ysb[:], ALU.mult, reads=["gt1", "ysb"], writes=["gt1"])
            o_act(S, gt1[:], gt1[:], AF.Tanh, reads=["gt1"], writes=["gt1"], scale=0.7978845608028654)
            o_ts(S, "dve", gt1[:], gt1[:], 1.0, ALU.add, reads=["gt1"], writes=["gt1"], s2=0.5, op1=ALU.mult)
            o_tt(S, "dve", yg[:, yb_, :], gt1[:], ysb[:], ALU.mult, reads=["gt1", "ysb"], writes=[("yg", yb_)])
            o_copy(S, "pool", ygb[:, yb_, :], yg[:, yb_, :], reads=[("yg", yb_)], writes=[("ygb", yb_)])
            o_dma(S, "sp", T["ygT"][c, :, :], yg[:, yb_, :], reads=[("yg", yb_)], grp="yg%d" % yb_)
            o_dma(S, "sp", T["ygTb"][c, :, :], ygb[:, yb_, :], reads=[("ygb", yb_)], grp="ygb%d" % yb_)
        o_dma(S, "sp", T["hfr"][:, :], hfr[:], reads=["hf"], grp="hfr")
        o_dma(S, "sp", T["hfi"][:, :], hfi[:], reads=["hf"], grp="hfi")
        o_dma(S, "sp", T["hsr"][:, :], hsr[:].rearrange("p a b -> p (a b)"), reads=["hs"], grp="hsr")
        o_dma(S, "sp", T["hsi"][:, :], hsi[:].rearrange("p a b -> p (a b)"), reads=["hs"], grp="hsi")
        S.emit()


def stage_D1(nc, S, T):
    with St(nc, "D1") as st:
        ygT = st.sb("ygT", [128, 8, TOK], F32)
        ygb = st.sb("ygb", [128, 8, TOK], BF16)
        Wg = st.sb("Wg", [128, 8, 1024], BF16)
        gssm = st.sb("gssm", [128, 8], F32)
        onesb = st.sb("onesb", [128, 128], BF16)
        ssq = st.sb("ssq", [128, 8, TOK], BF16)
        sgm = st.sb("sgm", [128, 8, TOK], BF16)
        sg = st.sb("sg", [128, 2, 512], F32)
        so = st.sb("so", [128, 2, 512], F32)
        sss = st.sb("sss", [128, 16], F32)
        pz = [st.ps("pz%d" % i, [128, 512], F32) for i in range(4)]
        ps_ss = st.ps("ps_ss", [128, 16], F32)
        o_memset(S, "dve", onesb[:], 1.0, ["onesb"])
        o_dma(S, "sp", ygT[:], T["ygT"].rearrange("c p t -> p c t"), writes=["ygT"], grp="ygT")
        o_dma(S, "sp", ygb[:], T["ygTb"].rearrange("c p t -> p c t"), writes=["ygb"], grp="ygb")
        o_dma(S, "pool", Wg[:], T["w_glu"].rearrange("(c p) n -> p c n", p=128), writes=["Wg"], grp="Wg")
        o_dma(S, "sp", gssm[:], T["gssm"][:, :], writes=["gssm"], grp="gssm")
        i = 0
        for f in range(8):
            for (c0, w) in [(0, 512), (512, 512), (1024, 128)]:
                pi = i % 4
                si = i % 2
                i += 1
                for k in range(8):
                    o_mm(S, pz[pi][:, 0:w], Wg[:, k, f * 128:(f + 1) * 128], ygb[:, k, c0:c0 + w], k == 0, k == 7,
                         reads=["Wg", "ygb"], writes=[("pz", pi)])
                o_act(S, sg[:, si, 0:w], pz[pi][:, 0:w], AF.Sigmoid, reads=[("pz", pi)], writes=[("sg", si)])
                o_tt(S, "dve", so[:, si, 0:w], ygT[:, f, c0:c0 + w], sg[:, si, 0:w], ALU.mult, reads=["ygT", ("sg", si)], writes=[("so", si)])
                o_tt(S, "pool", ssq[:, f, c0:c0 + w], so[:, si, 0:w], so[:, si, 0:w], ALU.mult, reads=[("so", si)], writes=["ssq"])
                o_ts(S, "pool", sgm[:, f, c0:c0 + w], so[:, si, 0:w], gssm[:, f:f + 1], ALU.mult, reads=[("so", si), "gssm"], writes=["sgm"])
        for t in range(NT):
            for f in range(8):
                o_mm(S, ps_ss[:, t:t + 1], ssq[:, f, t * 128:(t + 1) * 128], onesb[:, 0:1], f == 0, f == 7,
                     reads=["ssq", "onesb"], writes=["ps_ss"])
        o_act(S, sss[:, 0:NT], ps_ss[:, 0:NT], AF.Sqrt, reads=["ps_ss"], writes=["sss"], bias=1e-6, scale=1.0 / 1024.0)
        o_recip(S, sss[:, 0:NT], sss[:, 0:NT], reads=["sss"], writes=["sss"])
        o_dma(S, "sp", T["rstd_s"][:, :], sss[:, 0:NT], reads=["sss"], grp="rstd")
        o_dma(S, "sp", T["mixT"][8:16, :, :].rearrange("h p t -> p h t"), sgm[:, :, :], reads=["sgm"], grp="sgmst")
        S.emit()


def layernorm_tile(S, st_, X, t, lng, lnb, stats, mv, rs, key):
    xt = X[:, t, :]
    for c in range(4):
        S.op("dve", lambda h, c=c: h.bn_stats(out=stats[:, c, :], in_=X[:, t, c * 512:(c + 1) * 512]), reads=[key], writes=["ln_st"])
    S.op("dve", lambda h: h.bn_aggr(out=mv[:], in_=stats[:].rearrange("p a b -> p (a b)")), reads=["ln_st"], writes=["ln_mv"])
    o_act(S, rs[:], mv[:, 1:2], AF.Sqrt, reads=["ln_mv"], writes=["ln_rs"], bias=1e-5, scale=1.0)
    o_recip(S, rs[:], rs[:], reads=["ln_rs"], writes=["ln_rs"])
    o_ts(S, "dve", xt, xt, mv[:, 0:1], ALU.subtract, reads=[key, "ln_mv", "ln_rs"], writes=[key], s2=rs[:, 0:1], op1=ALU.mult)
    o_tt(S, "pool", xt, xt, lng[:], ALU.mult, reads=[key, "lng"], writes=[key])
    o_tt(S, "pool", xt, xt, lnb[:], ALU.add, reads=[key, "lnb"], writes=[key])


def linear_residual_ln(nc, S, T, name, inT_name, w_name, lng_name, lnb_name, xin_fn, xout_name, two_part):
    with St(nc, name) as st:
        X = st.sb("X", [128, NT, 2048], F32)
        inT = st.sb("inT", [128, 16, TOK], BF16)
        Wo = st.sb("Wo", [128, 2, 16, 512], BF16)
        lng = st.sb("lng", [128, 2048], F32)
        lnb = st.sb("lnb", [128, 2048], F32)
        tmp = st.sb("tmp", [128, 2, 512], F32)
        stats = st.sb("stats", [128, 4, 6], F32)
        mv = st.sb("mv", [128, 2], F32)
        rs = st.sb("rs", [128, 1], F32)
        ra = st.sb("ra", [128, NT], F32)
        rsm = st.sb("rsm", [128, NT], F32)
        pa = [st.ps("pa%d" % i, [128, 512], F32) for i in range(2)]
        pb = [st.ps("pb%d" % i, [128, 512], F32) for i in range(2)]
        xin_fn(S, X)
        o_dma(S, "sp", inT[:], T[inT_name].rearrange("c p t -> p c t"), writes=["inT"], grp="inT")
        o_dma(S, "sp", lng[:], T[lng_name].partition_broadcast(128), writes=["lng"], grp="lng")
        o_dma(S, "sp", lnb[:], T[lnb_name].partition_broadcast(128), writes=["lnb"], grp="lnb")
        if two_part:
            o_dma(S, "sp", ra[:], T["rstd_a"][:, :], writes=["ra"], grp="ra")
            o_dma(S, "sp", rsm[:], T["rstd_s"][:, :], writes=["rsm"], grp="rsm")
        i = 0
        for n in range(4):
            wb = n % 2
            o_dma(S, "pool", Wo[:, wb, :, :], T[w_name][:, n * 512:(n + 1) * 512].rearrange("(c p) n -> p c n", p=128),
                  writes=[("Wo", wb)], grp="Wo%d" % wb)
            for t in range(NT):
                pi = i % 2
                i += 1
                xk = ("X", t)
                if two_part:
                    for k in range(8):
                        o_mm(S, pa[pi][:, :], inT[:, k, t * 128:(t + 1) * 128], Wo[:, wb, k, :], k == 0, k == 7,
                             reads=["inT", ("Wo", wb)], writes=[("pa", pi)])
                    for k in range(8):
                        o_mm(S, pb[pi][:, :], inT[:, 8 + k, t * 128:(t + 1) * 128], Wo[:, wb, 8 + k, :], k == 0, k == 7,
                             reads=["inT", ("Wo", wb)], writes=[("pb", pi)])
                    o_copy(S, "act", tmp[:, pi, :], pa[pi][:, :], reads=[("pa", pi), "ra"], writes=[("tmp", pi)], scale=ra[:, t:t + 1])
                    o_stt(S, tmp[:, pi, :], pb[pi][:, :], rsm[:, t:t + 1], tmp[:, pi, :], ALU.mult, ALU.add,
                          reads=[("pb", pi), ("tmp", pi), "rsm"], writes=[("tmp", pi)])
                    o_stt(S, X[:, t, n * 512:(n + 1) * 512], X[:, t, n * 512:(n + 1) * 512], ALPHA, tmp[:, pi, :], ALU.mult, ALU.add,
                          reads=[xk, ("tmp", pi)], writes=[xk])
                else:
                    for k in range(16):
                        o_mm(S, pa[pi][:, :], inT[:, k, t * 128:(t + 1) * 128], Wo[:, wb, k, :], k == 0, k == 15,
                             reads=["inT", ("Wo", wb)], writes=[("pa", pi)])
                    o_stt(S, X[:, t, n * 512:(n + 1) * 512], X[:, t, n * 512:(n + 1) * 512], ALPHA, pa[pi][:, :], ALU.mult, ALU.add,
                          reads=[xk, ("pa", pi)], writes=[xk])
        for t in range(NT):
            layernorm_tile(S, st, X, t, lng, lnb, stats, mv, rs, ("X", t))
            o_dma(S, "sp", T[xout_name][t * 128:(t + 1) * 128, :], X[:, t, :], reads=[("X", t)], grp="xo%d" % (t % 2))
        S.emit()


def xin_from_inputs(T):
    def f(S, X):
        o_dma(S, "sp", X[:, 0:8, :], T["xw"][3072:4096, :].rearrange("(t p) d -> p t d", p=128),
              writes=[("X", t) for t in range(8)], grp="X")
        o_dma(S, "sp", X[:, 8, :], T["xs"][:, :], writes=[("X", 8)], grp="X8")
    return f


def xin_from_scr(T, name):
    def f(S, X):
        o_dma(S, "sp", X[:, :, :], T[name][:, :].rearrange("(t p) d -> p t d", p=128),
              writes=[("X", t) for t in range(NT)], grp="X")
    return f


def transpose_block(S, X, t, ident, ptr, dstT, dst_key, i0, f32copy=None):
    for b4 in range(4):
        pi = (i0 + b4) % 2
        for q in range(4):
            c = b4 * 4 + q
            o_tr(S, ptr[pi][:, q * 128:(q + 1) * 128], X[:, t, c * 128:(c + 1) * 128], ident[:],
                 reads=[("X", t), "ident"], writes=[("ptr", pi)])
        o_copy(S, "act" if pi == 0 else "dve", dstT[:, b4 * 4:(b4 + 1) * 4, t * 128:(t + 1) * 128],
               ptr[pi][:, :].rearrange("p (a b) -> p a b", a=4), reads=[("ptr", pi)], writes=[dst_key])
        if f32copy is not None:
            o_copy(S, "dve" if pi == 0 else "act", f32copy[:, b4 * 4:(b4 + 1) * 4, :],
                   ptr[pi][:, :].rearrange("p (a b) -> p a b", a=4), reads=[("ptr", pi)], writes=["f32copy"])


def stage_E0(nc, S, T):
    with St(nc, "E0") as st:
        ident = st.sb("ident", [128, 128], F32)
        M = st.sb("M", [128, 2, 2048], F32)
        memT = st.sb("memT", [128, 16, 256], BF16)
        Wb = st.sb("Wb", [128, 2, 16, 512], BF16)
        of = st.sb("of", [128, 2, 512], F32)
        ob = st.sb("ob", [128, 2, 512], BF16)
        okT = st.sb("okT", [128, 2, 256], BF16)
        ptr = [st.ps("ptr%d" % i, [128, 512], F32) for i in range(2)]
        pm = [st.ps("pm%d" % i, [128, 512], F32) for i in range(2)]
        pk = [st.ps("pk%d" % i, [128, 256], F32) for i in range(2)]
        make_ident(S, ident, "ident")
        o_dma(S, "sp", M[:], T["memp"][:, :].rearrange("(t p) d -> p t d", p=128), writes=[("X", 0), ("X", 1)], grp="M")
        for t in range(2):
            transpose_block(S, M, t, ident, ptr, memT, "memT", 0)
        wi = 0
        oi = 0
        for (wname, kind) in (("w_mk", "k"), ("w_mv", "v")):
            for n in range(4):
                wb = wi % 2
                wi += 1
                o_dma(S, "pool", Wb[:, wb, :, :], T[wname][:, n * 512:(n + 1) * 512].rearrange("(c p) n -> p c n", p=128),
                      writes=[("Wb", wb)], grp="Wb%d" % wb)
                for t in range(2):
                    pi = oi % 2
                    oi += 1
                    for k in range(16):
                        o_mm(S, pm[pi][:, :], memT[:, k, t * 128:(t + 1) * 128], Wb[:, wb, k, :], k == 0, k == 15,
                             reads=["memT", ("Wb", wb)], writes=[("pm", pi)])
                    o_copy(S, "act", of[:, pi, :], pm[pi][:, :], reads=[("pm", pi)], writes=[("of", pi)])
                    o_dma(S, "sp", T["memKo" if kind == "k" else "memVo"][t * 128:(t + 1) * 128, n * 512:(n + 1) * 512],
                          of[:, pi, :], reads=[("of", pi)], grp="of%d" % pi)
                    if kind == "v":
                        o_copy(S, "dve", ob[:, pi, :], pm[pi][:, :], reads=[("pm", pi)], writes=[("ob", pi)])
                        o_dma(S, "sp", T["mv_scr"][t * 128:(t + 1) * 128, n * 512:(n + 1) * 512], ob[:, pi, :],
                              reads=[("ob", pi)], grp="ob%d" % pi)
                if kind == "k":
                    for fb in range(4):
                        f = n * 4 + fb
                        pi = f % 2
                        for k in range(16):
                            o_mm(S, pk[pi][:, :], Wb[:, wb, k, fb * 128:(fb + 1) * 128], memT[:, k, :], k == 0, k == 15,
                                 reads=["memT", ("Wb", wb)], writes=[("pk", pi)])
                        o_copy(S, "dve", okT[:, pi, :], pk[pi][:, :], reads=[("pk", pi)], writes=[("okT", pi)])
                        o_dma(S, "sp", T["mkT_scr"][f, :, :], okT[:, pi, :], reads=[("okT", pi)], grp="okT%d" % pi)
        S.emit()


def stage_E1(nc, S, T):
    with St(nc, "E1") as st:
        ident = st.sb("ident", [128, 128], F32)
        X = st.sb("X", [128, NT, 2048], F32)
        xT = st.sb("xT", [128, 16, TOK], BF16)
        Wb = st.sb("Wb", [128, 2, 16, 512], BF16)
        oq = st.sb("oq", [128, 2, 512], BF16)
        ptr = [st.ps("ptr%d" % i, [128, 512], F32) for i in range(2)]
        pq = [st.ps("pq%d" % i, [128, 512], F32) for i in range(4)]
        make_ident(S, ident, "ident")
        xin_from_scr(T, "X1")(S, X)
        for t in range(NT):
            transpose_block(S, X, t, ident, ptr, xT, "xT", 0)
        i = 0
        for n in range(4):
            wb = n % 2
            o_dma(S, "pool", Wb[:, wb, :, :], T["w_mq"][:, n * 512:(n + 1) * 512].rearrange("(c p) n -> p c n", p=128),
                  writes=[("Wb", wb)], grp="Wb%d" % wb)
            for fb in range(4):
                f = n * 4 + fb
                for (c0, w) in [(0, 512), (512, 512), (1024, 128)]:
                    pi = i % 4
                    oi = i % 2
                    i += 1
                    for k in range(16):
                        o_mm(S, pq[pi][:, 0:w], Wb[:, wb, k, fb * 128:(fb + 1) * 128], xT[:, k, c0:c0 + w], k == 0, k == 15,
                             reads=["xT", ("Wb", wb)], writes=[("pq", pi)])
                    o_copy(S, "act" if oi == 0 else "dve", oq[:, oi, 0:w], pq[pi][:, 0:w], reads=[("pq", pi)], writes=[("oq", oi)])
                    o_dma(S, "sp", T["qmT"][f, :, c0:c0 + w], oq[:, oi, 0:w], reads=[("oq", oi)], grp="oq%d" % oi)
        S.emit()


def stage_E2(nc, S, T):
    with St(nc, "E2") as st:
        identf = st.sb("identf", [128, 128], F32)
        onesb = st.sb("onesb", [128, 128], BF16)
        mkT = st.sb("mkT", [128, 16, 256], BF16)
        mv = st.sb("mv", [128, 2, 2048], BF16)
        qm = st.sb("qm", [128, 16, TOK], BF16)
        p = st.sb("p", [128, 2, 512], BF16)
        rd = st.sb("rd", [128, 512], F32)
        om = st.sb("om", [128, 2, 4, 512], BF16)
        ck = st.sb("ck", [128, 2, 2048], F32)
        skT = st.sb("skT", [128, 16, 256], BF16)
        sv = st.sb("sv", [128, 2, 2048], BF16)
        p8 = st.sb("p8", [128, 2, 8], BF16)
        rd8 = st.sb("rd8", [128, 8], F32)
        om8 = st.sb("om8", [128, 16, 128], BF16)
        ps_s = [st.ps("ps_s%d" % i, [128, 512], F32) for i in range(2)]
        ps_o = [st.ps("ps_o%d" % i, [128, 512], F32) for i in range(4)]
        ps_d = st.ps("ps_d", [128, 512], F32)
        ptr = st.ps("ptr", [128, 512], F32)
        make_ident(S, identf, "ident")
        o_memset(S, "dve", onesb[:], 1.0, ["onesb"])
        o_memset(S, "pool", om8[:].rearrange("p a b -> p (a b)"), 0.0, ["om8"])
        o_dma(S, "sp", mkT[:], T["mkT_scr"].rearrange("c p t -> p c t"), writes=["mkT"], grp="mkT")
        o_dma(S, "sp", mv[:], T["mv_scr"][:, :].rearrange("(t p) d -> p t d", p=128), writes=["mv"], grp="mv")
        o_dma(S, "sp", qm[:], T["qmT"].rearrange("c p t -> p c t"), writes=["qm"], grp="qm")
        si = 0
        oi = 0
        for hh in range(4):
            for blk in range(2):
                c0 = blk * 512
                ob_ = oi % 2
                oi += 1
                for kc_ in range(2):
                    sl = si % 2
                    si += 1
                    for j in range(4):
                        o_mm(S, ps_s[sl][:, :], mkT[:, 4 * hh + j, kc_ * 128:(kc_ + 1) * 128], qm[:, 4 * hh + j, c0:c0 + 512], j == 0, j == 3,
                             reads=["mkT", "qm"], writes=[("ps_s", sl)])
                    o_act(S, p[:, sl, :], ps_s[sl][:, :], AF.Exp, reads=[("ps_s", sl)], writes=[("p", sl)], scale=SC_MEM)
                    for j in range(4):
                        o_mm(S, ps_o[j][:, :], mv[:, kc_, (4 * hh + j) * 128:(4 * hh + j + 1) * 128], p[:, sl, :], kc_ == 0, kc_ == 1,
                             reads=["mv", ("p", sl)], writes=[("ps_o", j)])
                    o_mm(S, ps_d[:, :], onesb[:], p[:, sl, :], kc_ == 0, kc_ == 1, reads=["onesb", ("p", sl)], writes=["ps_d"])
                o_recip(S, rd[:], ps_d[:, :], reads=["ps_d"], writes=["rd"])
                for j in range(4):
                    o_tt(S, "dve", om[:, ob_, j, :], ps_o[j][:, :], rd[:], ALU.mult, reads=[("ps_o", j), "rd"], writes=[("om", ob_)])
                o_dma(S, "sp", T["omT"][4 * hh:4 * hh + 4, :, c0:c0 + 512].rearrange("c p t -> p c t"), om[:, ob_, :, :],
                      reads=[("om", ob_)], grp="om%d" % ob_)
        for s in range(4):
            q0 = 1024 + 32 * s
            o_dma(S, "sp", ck[:], T["cmk"][s, :, :].rearrange("(t p) d -> p t d", p=128), writes=[("X", 0), ("X", 1)], grp="ck")
            o_dma(S, "pool", sv[:], T["cmv"][s, :, :].rearrange("(t p) d -> p t d", p=128), writes=["sv"], grp="sv")
            for t in range(2):
                for b4 in range(4):
                    for q in range(4):
                        c = b4 * 4 + q
                        o_tr(S, ptr[:, q * 128:(q + 1) * 128], ck[:, t, c * 128:(c + 1) * 128], identf[:],
                             reads=[("X", t), "ident"], writes=["ptr"])
                    o_copy(S, "act" if b4 % 2 == 0 else "dve", skT[:, b4 * 4:(b4 + 1) * 4, t * 128:(t + 1) * 128],
                           ptr[:, :].rearrange("p (a b) -> p a b", a=4), reads=["ptr"], writes=["skT"])
            for hh in range(4):
                for kc_ in range(2):
                    for j in range(4):
                        o_mm(S, ps_s[0][:, kc_ * 8:(kc_ + 1) * 8], skT[:, 4 * hh + j, kc_ * 128:(kc_ + 1) * 128], qm[:, 4 * hh + j, q0:q0 + 8],
                             j == 0, j == 3, reads=["skT", "qm"], writes=[("ps_s", 0)])
                o_act(S, p8[:].rearrange("p a b -> p (a b)"), ps_s[0][:, 0:16], AF.Exp, reads=[("ps_s", 0)], writes=["p8"], scale=SC_MEM)
                for j in range(4):
                    for kc_ in range(2):
                        o_mm(S, ps_o[j][:, 0:8], sv[:, kc_, (4 * hh + j) * 128:(4 * hh + j + 1) * 128], p8[:, kc_, :], kc_ == 0, kc_ == 1,
                             reads=["sv", "p8"], writes=[("ps_o", j)])
                for kc_ in range(2):
                    o_mm(S, ps_d[:, 0:8], onesb[:], p8[:, kc_, :], kc_ == 0, kc_ == 1, reads=["onesb", "p8"], writes=["ps_d"])
                o_recip(S, rd8[:], ps_d[:, 0:8], reads=["ps_d"], writes=["rd8"])
                for j in range(4):
                    o_tt(S, "dve", om8[:, 4 * hh + j, 32 * s:32 * s + 8], ps_o[j][:, 0:8], rd8[:], ALU.mult,
                         reads=[("ps_o", j), "rd8"], writes=["om8"])
        o_dma(S, "sp", T["omT"][:, :, 1024:TOK].rearrange("c p t -> p c t"), om8[:, :, :], reads=["om8"], grp="om8")
        S.emit()


def stage_F1(nc, S, T):
    with St(nc, "F1") as st:
        ident = st.sb("ident", [128, 128], F32)
        X = st.sb("X", [128, NT, 2048], F32)
        xT = st.sb("xT", [128, 16, TOK], BF16)
        xTf = st.sb("xTf", [128, 16, 128], F32)
        Wr = st.sb("Wr", [128, 16, 36], F32)
        br = st.sb("br", [128, 36], F32)
        lg = st.sb("lg", [128, 36], F32)
        G = st.sb("G", [128, NT, 32], F32)
        gm = st.sb("gm", [128, 1], F32)
        goh = st.sb("goh", [128, 4], F32)
        ge = st.sb("ge", [128, 4], F32)
        gs = st.sb("gs", [128, 1], F32)
        gw = st.sb("gw", [128, 1], F32)
        es = st.sb("es", [128, 8], F32)
        m1 = st.sb("m1", [128, 1], F32)
        oh1 = st.sb("oh1", [128, 8], F32)
        em = st.sb("em", [128, 8], F32)
        m2 = st.sb("m2", [128, 1], F32)
        oh2 = st.sb("oh2", [128, 8], F32)
        dd = st.sb("dd", [128, 1], F32)
        w1 = st.sb("w1", [128, 1], F32)
        w2 = st.sb("w2", [128, 1], F32)
        g8 = st.sb("g8", [128, 8], F32)
        ptr = [st.ps("ptr%d" % i, [128, 512], F32) for i in range(2)]
        pl = st.ps("pl", [128, 64], F32)
        make_ident(S, ident, "ident")
        xin_from_scr(T, "X2")(S, X)
        o_dma(S, "sp", Wr[:], T["wr"][:, :].rearrange("(c p) n -> p c n", p=128), writes=["Wr"], grp="Wr")
        o_dma(S, "sp", br[:], T["br"].partition_broadcast(128), writes=["br"], grp="br")
        R = ["rt"]
        for t in range(NT):
            transpose_block(S, X, t, ident, ptr, xT, "xT", 0, f32copy=xTf)
            for k in range(16):
                o_mm(S, pl[:, 0:36], xTf[:, k, :], Wr[:, k, :], k == 0, k == 15, reads=["f32copy", "Wr"], writes=["pl"])
            o_tt(S, "dve", lg[:], pl[:, 0:36], br[:], ALU.add, reads=["pl", "br"], writes=R)
            S.op("dve", lambda h: h.reduce_max(out=gm[:], in_=lg[:, 0:4], axis=mybir.AxisListType.X), reads=R, writes=R)
            o_ts(S, "dve", goh[:], lg[:, 0:4], gm[:, 0:1], ALU.is_equal, reads=R, writes=R)
            o_ts(S, "dve", ge[:], lg[:, 0:4], gm[:, 0:1], ALU.subtract, reads=R, writes=R)
            o_act(S, ge[:], ge[:], AF.Exp, reads=R, writes=R)
            S.op("dve", lambda h: h.reduce_sum(out=gs[:], in_=ge[:], axis=mybir.AxisListType.X), reads=R, writes=R)
            o_recip(S, gw[:], gs[:], reads=R, writes=R)
            o_ts(S, "dve", es[:], lg[:, 4:12], goh[:, 0:1], ALU.mult, reads=R, writes=R)
            for g in range(1, 4):
                o_stt(S, es[:], lg[:, 4 + 8 * g:12 + 8 * g], goh[:, g:g + 1], es[:], ALU.mult, ALU.add, reads=R, writes=R)
            S.op("dve", lambda h: h.reduce_max(out=m1[:], in_=es[:], axis=mybir.AxisListType.X), reads=R, writes=R)
            o_ts(S, "dve", oh1[:], es[:], m1[:, 0:1], ALU.is_equal, reads=R, writes=R)
            o_stt(S, em[:], oh1[:], -1e30, es[:], ALU.mult, ALU.add, reads=R, writes=R)
            S.op("dve", lambda h: h.reduce_max(out=m2[:], in_=em[:], axis=mybir.AxisListType.X), reads=R, writes=R)
            o_ts(S, "dve", oh2[:], em[:], m2[:, 0:1], ALU.is_equal, reads=R, writes=R)
            o_tt(S, "dve", dd[:], m2[:], m1[:], ALU.subtract, reads=R, writes=R)
            o_act(S, dd[:], dd[:], AF.Exp, reads=R, writes=R)
            o_ts(S, "dve", w1[:], dd[:], 1.0, ALU.add, reads=R, writes=R)
            o_recip(S, w1[:], w1[:], reads=R, writes=R)
            o_tt(S, "dve", w1[:], w1[:], gw[:], ALU.mult, reads=R, writes=R)
            o_tt(S, "dve", w2[:], w1[:], dd[:], ALU.mult, reads=R, writes=R)
            o_ts(S, "dve", g8[:], oh1[:], w1[:, 0:1], ALU.mult, reads=R, writes=R)
            o_stt(S, g8[:], oh2[:], w2[:, 0:1], g8[:], ALU.mult, ALU.add, reads=R, writes=R)
            for g in range(4):
                o_ts(S, "dve", G[:, t, 8 * g:8 * g + 8], g8[:], goh[:, g:g + 1], ALU.mult, reads=R, writes=["G"])
            o_ts(S, "pool", X[:, t, :], X[:, t, :], ALPHA, ALU.mult, reads=[("X", t)], writes=[("X", t)])
            o_dma(S, "sp", T["X3"][t * 128:(t + 1) * 128, :], X[:, t, :], reads=[("X", t)], grp="xo%d" % (t % 2))
        o_dma(S, "sp", T["x2T"].rearrange("c p t -> p c t"), xT[:, :, :], reads=["xT"], grp="xTst")
        o_dma(S, "sp", T["G"][:, :], G[:].rearrange("p a b -> p (a b)"), reads=["G"], grp="Gst")
        S.emit()


def stage_F2(nc, S, T):
    with St(nc, "F2") as st:
        X = st.sb("X", [128, NT, 2048], F32)
        xT = st.sb("xT", [128, 16, TOK], BF16)
        G = st.sb("G", [128, NT, 32], F32)
        Wg = st.sb("Wg", [128, 16, 512], BF16)
        Wu = st.sb("Wu", [128, 16, 512], BF16)
        Wd = st.sb("Wd", [128, 4, 2048], BF16)
        hT = st.sb("hT", [128, 4, TOK], BF16)
        sg = st.sb("sg", [128, 2, 512], F32)
        pg = [st.ps("pg%d" % i, [128, 512], F32) for i in range(2)]
        pu = [st.ps("pu%d" % i, [128, 512], F32) for i in range(2)]
        po = [st.ps("po%d" % i, [128, 512], F32) for i in range(4)]
        xin_from_scr(T, "X3")(S, X)
        o_dma(S, "sp", xT[:], T["x2T"].rearrange("c p t -> p c t"), writes=["xT"], grp="xT")
        o_dma(S, "sp", G[:].rearrange("p a b -> p (a b)"), T["G"][:, :], writes=["G"], grp="G")
        i = 0
        oi = 0
        for e in range(32):
            o_dma(S, "pool", Wg[:], T["w_gate"][e, :, :].rearrange("(c p) n -> p c n", p=128), writes=["Wg"], grp="Wg")
            o_dma(S, "pool", Wu[:], T["w_up"][e, :, :].rearrange("(c p) n -> p c n", p=128), writes=["Wu"], grp="Wu")
            o_dma(S, "pool", Wd[:], T["w_down"][e, :, :].rearrange("(c p) n -> p c n", p=128), writes=["Wd"], grp="Wd")
            for f in range(4):
                for (c0, w) in [(0, 512), (512, 512), (1024, 128)]:
                    pi = i % 2
                    i += 1
                    for k in range(16):
                        o_mm(S, pg[pi][:, 0:w], Wg[:, k, f * 128:(f + 1) * 128], xT[:, k, c0:c0 + w], k == 0, k == 15,
                             reads=["Wg", "xT"], writes=[("pg", pi)])
                    for k in range(16):
                        o_mm(S, pu[pi][:, 0:w], Wu[:, k, f * 128:(f + 1) * 128], xT[:, k, c0:c0 + w], k == 0, k == 15,
                             reads=["Wu", "xT"], writes=[("pu", pi)])
                    o_act(S, sg[:, pi, 0:w], pg[pi][:, 0:w], AF.Silu, reads=[("pg", pi)], writes=[("sg", pi)])
                    o_tt(S, "dve", hT[:, f, c0:c0 + w], sg[:, pi, 0:w], pu[pi][:, 0:w], ALU.mult,
                         reads=[("sg", pi), ("pu", pi)], writes=[("hT", f)])
            for t in range(NT):
                for n in range(4):
                    pi = oi % 4
                    oi += 1
                    for f in range(4):
                        o_mm(S, po[pi][:, :], hT[:, f, t * 128:(t + 1) * 128], Wd[:, f, n * 512:(n + 1) * 512], f == 0, f == 3,
                             reads=[("hT", f), "Wd"], writes=[("po", pi)])
                    o_stt(S, X[:, t, n * 512:(n + 1) * 512], po[pi][:, :], G[:, t, e:e + 1], X[:, t, n * 512:(n + 1) * 512],
                          ALU.mult, ALU.add, reads=[("po", pi), "G", ("X", t)], writes=[("X", t)])
        for t in range(NT):
            o_dma(S, "sp", T["X1"][t * 128:(t + 1) * 128, :], X[:, t, :], reads=[("X", t)], grp="xo%d" % (t % 2))
        S.emit()


def stage_F3(nc, S, T):
    with St(nc, "F3") as st:
        X = st.sb("X", [128, NT, 2048], F32)
        lng = st.sb("lng", [128, 2048], F32)
        lnb = st.sb("lnb", [128, 2048], F32)
        stats = st.sb("stats", [128, 4, 6], F32)
        mv = st.sb("mv", [128, 2], F32)
        rs = st.sb("rs", [128, 1], F32)
        xin_from_scr(T, "X1")(S, X)
        o_dma(S, "sp", lng[:], T["ln3_g"].partition_broadcast(128), writes=["lng"], grp="lng")
        o_dma(S, "sp", lnb[:], T["ln3_b"].partition_broadcast(128), writes=["lnb"], grp="lnb")
        for t in range(NT):
            layernorm_tile(S, st, X, t, lng, lnb, stats, mv, rs, ("X", t))
            o_dma(S, "sp", T["y"][t * 128:(t + 1) * 128, :], X[:, t, :], reads=[("X", t)], grp="xo%d" % (t % 2))
        S.emit()


IN_SPECS = [
    ("xw", [4096, 2048]), ("xs", [128, 2048]), ("vbias", [128, 24]), ("pmask", [128, 17 * 128]),
    ("smask", [128, 128]), ("smaskn", [128, 128]), ("cwk", [4, 2048, 1024]), ("cwv", [4, 2048, 1024]),
    ("h0re", [128, 128]), ("h0im", [128, 128]), ("cmk", [4, 256, 2048]), ("cmv", [4, 256, 2048]),
    ("memp", [256, 2048]), ("w_in", [2048, 4096]), ("lamre", [128, 32]), ("lamim", [128, 32]),
    ("logdt", [128, 32]), ("BTre", [128, 4096]), ("BTim", [128, 4096]), ("CTre", [128, 4096]),
    ("CTim", [128, 4096]), ("dcol", [128, 8]), ("w_glu", [1024, 1024]), ("gatt", [128, 8]), ("gssm", [128, 8]),
    ("w_out", [2048, 2048]), ("ln1_g", [1, 2048]), ("ln1_b", [1, 2048]), ("w_mq", [2048, 2048]),
    ("w_mk", [2048, 2048]), ("w_mv", [2048, 2048]), ("w_mo", [2048, 2048]), ("ln2_g", [1, 2048]),
    ("ln2_b", [1, 2048]), ("wr", [2048, 36]), ("br", [1, 36]), ("w_gate", [32, 2048, 512]),
    ("w_up", [32, 2048, 512]), ("w_down", [32, 512, 2048]), ("ln3_g", [1, 2048]), ("ln3_b", [1, 2048]),
]
OUT_SPECS = [
    ("y", [TOK, 2048]), ("Kout", [TOK, 1024]), ("Vout", [TOK, 1024]), ("hfr", [128, 32]), ("hfi", [128, 32]),
    ("hsr", [128, 128]), ("hsi", [128, 128]), ("memKo", [256, 2048]), ("memVo", [256, 2048]),
]
SCR_SPECS = [
    ("qT", [8, 128, TOK], BF16), ("kT", [8, 128, KPOS], BF16), ("V_scr", [KPOS, 1024], BF16), ("uT", [8, 128, UPOS], BF16),
    ("mixT", [16, 128, TOK], BF16), ("rstd_a", [128, NT], F32), ("rstd_s", [128, NT], F32),
    ("ygT", [8, 128, TOK], F32), ("ygTb", [8, 128, TOK], BF16), ("X1", [TOK, 2048], F32), ("X2", [TOK, 2048], F32),
    ("X3", [TOK, 2048], F32), ("mkT_scr", [16, 128, 256], BF16), ("mv_scr", [256, 2048], BF16),
    ("qmT", [16, 128, TOK], BF16), ("omT", [16, 128, TOK], BF16), ("x2T", [16, 128, TOK], BF16), ("G", [128, NT * 32], F32),
]


def build_program(stages=None, debug=False):
    nc = bass.Bass("TRN2", target_bir_lowering=False)
    T = {}
    for (n, s) in IN_SPECS:
        T[n] = nc.dram_tensor(n, s, F32, kind="ExternalInput").ap()
    for (n, s) in OUT_SPECS:
        T[n] = nc.dram_tensor(n, s, F32, kind="ExternalOutput").ap()
    for (n, s, d) in SCR_SPECS:
        T[n] = nc.dram_tensor("scr_" + n, s, d, kind=("ExternalOutput" if debug else "Internal")).ap()
    S = Sched(nc)
    table = [
        ("A", lambda: stage_A(nc, S, T)),
        ("B", lambda: stage_B(nc, S, T)),
        ("C", lambda: stage_C(nc, S, T)),
        ("D1", lambda: stage_D1(nc, S, T)),
        ("D2", lambda: linear_residual_ln(nc, S, T, "D2", "mixT", "w_out", "ln1_g", "ln1_b", xin_from_inputs(T), "X1", True)),
        ("E0", lambda: stage_E0(nc, S, T)),
        ("E1", lambda: stage_E1(nc, S, T)),
        ("E2", lambda: stage_E2(nc, S, T)),
        ("E3", lambda: linear_residual_ln(nc, S, T, "E3", "omT", "w_mo", "ln2_g", "ln2_b", xin_from_scr(T, "X1"), "X2", False)),
        ("F1", lambda: stage_F1(nc, S, T)),
        ("F2", lambda: stage_F2(nc, S, T)),
        ("F3", lambda: stage_F3(nc, S, T)),
    ]
    for (nm, fn) in table:
        if stages is None or nm in stages:
            fn()
    S.close()
    return nc


def _mult(diff):
    m = ((diff >= 0) & (diff <= 128)).astype(np.float32)
    m += ((diff >= 0) & (diff <= 512) & (diff % 4 == 0)).astype(np.float32)
    m += ((diff >= 0) & (diff <= 2048) & (diff % 16 == 0)).astype(np.float32)
    return m


def make_inputs(inp):
    f = np.float32
    xp = inp["x_prompt"]
    xsm = inp["x_sample"]
    j = np.arange(128)
    pm = np.zeros((128, 17, 128), f)
    for cc in range(17):
        diff = (16 - cc) * 128 + j[None, :] - j[:, None]
        pm[:, cc, :] = _mult(diff)
    sm = np.zeros((128, 16, 8), f)
    for cc in range(16):
        cpos = cc * 128 + j[:, None]
        diff = np.arange(8)[None, :] + 2048 - cpos
        sm[:, cc, :] = _mult(diff)
    smn = np.zeros((128, 128), f)
    d8 = np.arange(8)[None, :] - np.arange(8)[:, None]
    for s in range(4):
        smn[32 * s:32 * s + 8, 32 * s:32 * s + 8] = _mult(d8)
    br_, bi_ = inp["ssm_b_re"][0], inp["ssm_b_im"][0]
    cr_, ci_ = inp["ssm_c_re"][0], inp["ssm_c_im"][0]
    BTre = np.zeros((128, 32, 128), f)
    BTim = np.zeros((128, 32, 128), f)
    CTre = np.zeros((128, 32, 128), f)
    CTim = np.zeros((128, 32, 128), f)
    for tau in range(32):
        for gl in range(2):
            g = 2 * tau + gl
            r0 = 32 * (tau % 4) + 16 * gl
            BTre[r0:r0 + 16, tau, 64 * gl:64 * gl + 64] = br_[g].T
            BTim[r0:r0 + 16, tau, 64 * gl:64 * gl + 64] = bi_[g].T
            CTre[64 * gl:64 * gl + 64, tau, r0:r0 + 16] = cr_[g].T
            CTim[64 * gl:64 * gl + 64, tau, r0:r0 + 16] = ci_[g].T
    common = {
        "pmask": pm.reshape(128, -1), "smask": sm.reshape(128, -1), "smaskn": smn,
        "w_in": inp["w_in"][0],
        "lamre": np.ascontiguousarray(inp["ssm_lam_re"][0].reshape(32, 128).T),
        "lamim": np.ascontiguousarray(inp["ssm_lam_im"][0].reshape(32, 128).T),
        "logdt": np.ascontiguousarray(np.repeat(inp["ssm_log_dt"][0].reshape(32, 2), 64, axis=1).T),
        "BTre": BTre.reshape(128, -1), "BTim": BTim.reshape(128, -1), "CTre": CTre.reshape(128, -1), "CTim": CTim.reshape(128, -1),
        "dcol": np.ascontiguousarray(inp["ssm_d"][0].reshape(8, 128).T),
        "w_glu": inp["w_glu"][0],
        "gatt": np.ascontiguousarray(inp["g_attn"][0].reshape(8, 128).T),
        "gssm": np.ascontiguousarray(inp["g_ssm"][0].reshape(8, 128).T),
        "w_out": inp["w_out"][0], "ln1_g": inp["ln1_g"], "ln1_b": inp["ln1_b"],
        "w_mq": inp["w_mq"][0], "w_mk": inp["w_mk"][0], "w_mv": inp["w_mv"][0], "w_mo": inp["w_mo"][0],
        "ln2_g": inp["ln2_g"], "ln2_b": inp["ln2_b"],
        "wr": np.ascontiguousarray(np.concatenate([inp["w_r1"][0], inp["w_r2"][0].reshape(2048, 32)], axis=1)),
        "br": np.ascontiguousarray(np.concatenate([inp["b_r1"][0], inp["b_r2"][0].reshape(32)])[None, :]),
        "w_gate": inp["w_gate"][0], "w_up": inp["w_up"][0], "w_down": inp["w_down"][0],
        "ln3_g": inp["ln3_g"], "ln3_b": inp["ln3_b"],
    }
    maps = []
    for c in range(8):
        b, r = c // 4, c % 4
        xw = np.zeros((4096, 2048), f)
        lo = 1024 * r - 3072
        src0 = max(lo, 0)
        xw[src0 - lo:, :] = xp[b, src0:1024 * r + 1024, :]
        xs = np.zeros((128, 2048), f)
        for s in range(4):
            xs[32 * s:32 * s + 8, :] = xsm[4 * c + s]
        vb = np.ones((128, 24), f)
        for jj in range(16):
            if 8 * r - 16 + jj < 0:
                vb[:, jj] = 0.0
        m = dict(common)
        m.update({
            "xw": xw, "xs": xs, "vbias": vb,
            "cwk": np.ascontiguousarray(inp["cache_win_k"][0, 4 * c:4 * c + 4].reshape(4, 2048, 1024)),
            "cwv": np.ascontiguousarray(inp["cache_win_v"][0, 4 * c:4 * c + 4].reshape(4, 2048, 1024)),
            "h0re": np.ascontiguousarray(inp["state_ssm_re"][0, 4 * c:4 * c + 4].reshape(4, 32, 128).transpose(2, 1, 0).reshape(128, 128)),
            "h0im": np.ascontiguousarray(inp["state_ssm_im"][0, 4 * c:4 * c + 4].reshape(4, 32, 128).transpose(2, 1, 0).reshape(128, 128)),
            "cmk": np.ascontiguousarray(inp["cache_mem_k"][0, 4 * c:4 * c + 4].reshape(4, 256, 2048)),
            "cmv": np.ascontiguousarray(inp["cache_mem_v"][0, 4 * c:4 * c + 4].reshape(4, 256, 2048)),
            "memp": np.ascontiguousarray(inp["mem_prompt"][b]),
        })
        maps.append({k: np.ascontiguousarray(v, dtype=f) for k, v in m.items()})
    return maps


def assemble(res):
    f = np.float32
    yp = np.zeros((2, 4096, 2048), f)
    ys = np.zeros((32, 8, 2048), f)
    wkp = np.zeros((1, 2, 2048, 8, 128), f)
    wvp = np.zeros((1, 2, 2048, 8, 128), f)
    wks = np.zeros((1, 32, 8, 8, 128), f)
    wvs = np.zeros((1, 32, 8, 8, 128), f)
    srp = np.zeros((1, 2, 64, 64), f)
    sip = np.zeros((1, 2, 64, 64), f)
    srs = np.zeros((1, 32, 64, 64), f)
    sis = np.zeros((1, 32, 64, 64), f)
    mkp = np.zeros((1, 2, 256, 4, 512), f)
    mvp = np.zeros((1, 2, 256, 4, 512), f)
    for c in range(8):
        r_ = res[c]
        b, r = c // 4, c % 4
        yp[b, 1024 * r:1024 * r + 1024] = r_["y"][0:1024]
        for s in range(4):
            ys[4 * c + s] = r_["y"][1024 + 32 * s:1024 + 32 * s + 8]
            wks[0, 4 * c + s] = r_["Kout"][1024 + 32 * s:1024 + 32 * s + 8].reshape(8, 8, 128)
            wvs[0, 4 * c + s] = r_["Vout"][1024 + 32 * s:1024 + 32 * s + 8].reshape(8, 8, 128)
        if r >= 2:
            wkp[0, b, 1024 * (r - 2):1024 * (r - 1)] = r_["Kout"][0:1024].reshape(1024, 8, 128)
            wvp[0, b, 1024 * (r - 2):1024 * (r - 1)] = r_["Vout"][0:1024].reshape(1024, 8, 128)
        if r == 3:
            srp[0, b] = r_["hfr"].T.reshape(64, 64)
            sip[0, b] = r_["hfi"].T.reshape(64, 64)
        hs_r = r_["hsr"].reshape(128, 32, 4).transpose(2, 1, 0).reshape(4, 64, 64)
        hs_i = r_["hsi"].reshape(128, 32, 4).transpose(2, 1, 0).reshape(4, 64, 64)
        srs[0, 4 * c:4 * c + 4] = hs_r
        sis[0, 4 * c:4 * c + 4] = hs_i
        if r == 0:
            mkp[0, b] = r_["memKo"].reshape(256, 4, 512)
            mvp[0, b] = r_["memVo"].reshape(256, 4, 512)
    return (yp, ys, wkp, wvp, wks, wvs, srp, sip, srs, sis, mkp, mvp)


def kernel(**inputs):
    inp = {k: np.asarray(v) for k, v in inputs.items()}
    maps = make_inputs(inp)
    nc = build_program()
    res = run_bass_kernel_spmd(nc, maps, core_ids=list(range(8)))
    return assemble(res.results)
```

```python
import numpy as np
from contextlib import ExitStack
import concourse.bass as bass
import concourse.mybir as mybir
from concourse.bass_utils import run_bass_kernel_spmd

F32 = mybir.dt.float32
BF16 = mybir.dt.bfloat16
AF = mybir.ActivationFunctionType
ALU = mybir.AluOpType

ENGS = ("pe", "act", "dve", "pool", "sp")
NT = 9
TOK = 1152
KPOS = 3200
UPOS = 4224
ALPHA = 2.0 ** 0.25
SC_ATT = 128 ** -0.5
SC_MEM = 512 ** -0.5
NEG = -30000.0
DBG = {}
PSUM_KEYS = {"ps_big", "ps_n", "ptr", "pmm", "ps_s", "ps_nd", "ptk", "ps_sm", "ps_snd", "ps_ss", "ps_new", "pw", "py", "pz", "pa", "pb",
             "pm", "pk", "pq", "ps_o", "ps_d", "pl", "pg", "pu", "po"}


class Sched:
    def __init__(self, nc, n_dma_sems=22):
        self.nc = nc
        self.stack = []
        self.esem = {}
        for e in ENGS:
            self.esem[e] = self._sem("e_" + e)
        self.ecount = {e: 0 for e in ENGS}
        self.sval = {e: 0 for e in ENGS}
        self.dsems = [self._sem("d%d" % i) for i in range(n_dma_sems)]
        self.dcount = [0] * n_dma_sems
        self.rr = {}
        self.reset_stage()

    def _sem(self, name):
        cm = self.nc.semaphore(name)
        s = cm.__enter__()
        self.stack.append(cm)
        return s

    def reset_stage(self):
        self.ops = []
        self.dmap = {}
        self.lastw = {}
        self.readers = {}
        self.known = {e: {} for e in ENGS}

    def alt(self, name, engines):
        i = self.rr.get(name, 0)
        self.rr[name] = i + 1
        return engines[i % len(engines)]

    def _dsem(self, group):
        if group not in self.dmap:
            idx = len(self.dmap)
            assert idx < len(self.dsems), "too many dma groups in stage"
            self.dmap[group] = idx
        return self.dmap[group]

    def _need(self, eng, dep, waits):
        kind, a, b = dep
        kn = self.known[eng]
        key = (kind, a)
        if kn.get(key, -1) >= b:
            return
        kn[key] = b
        waits.append(dep)

    def op(self, eng, fn, reads=(), writes=(), dma=None):
        waits = []
        deps = []
        if eng != "pe":
            ex = [k for k in reads if (k[0] if isinstance(k, tuple) else k) in PSUM_KEYS]
            if ex:
                writes = list(writes) + [k for k in ex if k not in writes]
        for k in reads:
            if k in self.lastw:
                deps.append(self.lastw[k])
        for k in writes:
            if k in self.lastw:
                deps.append(self.lastw[k])
            deps.extend(self.readers.get(k, ()))
        if dma is not None:
            di = self._dsem(dma)
            if self.dcount[di] > 0:
                deps.append(("d", di, self.dcount[di]))
        for d in deps:
            if d[0] == "e" and d[1] == eng and eng == "pe":
                continue
            self._need(eng, d, waits)
        if dma is not None:
            self.dcount[di] += 16
            tok = ("d", di, self.dcount[di])
        else:
            self.ecount[eng] += 1
            tok = ("e", eng, self.ecount[eng])
        for k in writes:
            self.lastw[k] = tok
            self.readers[k] = []
        for k in reads:
            self.readers.setdefault(k, []).append(tok)
        self.ops.append((eng, fn, waits, tok))

    def touch(self, keys, from_keys):
        best = None
        for k in from_keys:
            t = self.lastw.get(k)
            if t is not None and (best is None or t[2] > best[2]):
                best = t
        if best is not None:
            for k in keys:
                self.lastw[k] = best
                self.readers[k] = []

    def emit(self):
        nc = self.nc
        fin = [("d", di, self.dcount[di]) for g, di in self.dmap.items()]
        per = {e: [] for e in ENGS}
        need = set()
        for o in self.ops:
            per[o[0]].append(o)
            for (kind, a, b) in o[2]:
                if kind == "e":
                    need.add((a, b))
        sval = self.sval
        smap = {}
        for (eng, fn, waits, tok) in self.ops:
            if tok[0] == "e" and (tok[1], tok[2]) in need:
                sval[eng] += 1
                smap[(tok[1], tok[2])] = sval[eng]
        esem, dsems = self.esem, self.dsems

        def run(engname, h):
            for (_, fn, waits, tok) in per[engname]:
                for (kind, a, b) in waits:
                    if kind == "e":
                        h.wait_ge(esem[a], smap[(a, b)])
                    else:
                        h.wait_ge(dsems[a], b)
                ins = fn(h)
                if tok[0] == "e":
                    if (tok[1], tok[2]) in smap:
                        ins.then_inc(esem[engname], 1)
                else:
                    ins.then_inc(dsems[tok[1]], 16)
            if engname == "sp":
                for (kind, a, b) in fin:
                    h.wait_ge(dsems[a], b)

        with nc.Block() as block:
            @block.tensor
            def _(h):
                run("pe", h)

            @block.scalar
            def _(h):
                run("act", h)

            @block.vector
            def _(h):
                run("dve", h)

            @block.gpsimd
            def _(h):
                run("pool", h)

            @block.sync
            def _(h):
                run("sp", h)
        self.reset_stage()

    def close(self):
        for cm in reversed(self.stack):
            cm.__exit__(None, None, None)


def o_mm(S, out, lhsT, rhs, start, stop, reads, writes):
    S.op("pe", lambda h: h.matmul(out, lhsT=lhsT, rhs=rhs, start=start, stop=stop), reads=reads, writes=writes)


def o_tr(S, out, in_, ident, reads, writes):
    S.op("pe", lambda h: h.transpose(out=out, in_=in_, identity=ident), reads=reads, writes=writes)


def o_dma(S, eng, out, in_, reads=(), writes=(), grp=None, slow=False):
    if slow:
        S.op(eng, lambda h: h.dma_start(out=out, in_=in_, allow_slow_non_contiguous=True), reads=reads, writes=writes, dma=grp)
    else:
        S.op(eng, lambda h: h.dma_start(out=out, in_=in_), reads=reads, writes=writes, dma=grp)


def o_copy(S, eng, out, in_, reads, writes, scale=None):
    if eng == "act":
        if scale is None:
            S.op("act", lambda h: h.activation(out=out, in_=in_, func=AF.Copy), reads=reads, writes=writes)
        else:
            S.op("act", lambda h: h.activation(out=out, in_=in_, func=AF.Identity, scale=scale), reads=reads, writes=writes)
    else:
        if scale is None:
            S.op(eng, lambda h: h.tensor_copy(out=out, in_=in_), reads=reads, writes=writes)
        else:
            S.op(eng, lambda h: h.tensor_scalar(out=out, in0=in_, scalar1=scale, scalar2=None, op0=ALU.mult), reads=reads, writes=writes)


def o_act(S, out, in_, func, reads, writes, bias=None, scale=None):
    kw = {}
    if bias is not None:
        kw["bias"] = bias
    if scale is not None:
        kw["scale"] = scale
    S.op("act", lambda h: h.activation(out=out, in_=in_, func=func, **kw), reads=reads, writes=writes)


def o_tt(S, eng, out, in0, in1, op, reads, writes):
    S.op(eng, lambda h: h.tensor_tensor(out=out, in0=in0, in1=in1, op=op), reads=reads, writes=writes)


def o_ts(S, eng, out, in0, s1, op0, reads, writes, s2=None, op1=None):
    if op1 is None:
        S.op(eng, lambda h: h.tensor_scalar(out=out, in0=in0, scalar1=s1, scalar2=None, op0=op0), reads=reads, writes=writes)
    else:
        S.op(eng, lambda h: h.tensor_scalar(out=out, in0=in0, scalar1=s1, scalar2=s2, op0=op0, op1=op1), reads=reads, writes=writes)


def o_stt(S, out, in0, scalar, in1, op0, op1, reads, writes):
    S.op("dve", lambda h: h.scalar_tensor_tensor(out=out, in0=in0, scalar=scalar, in1=in1, op0=op0, op1=op1), reads=reads, writes=writes)


def o_memset(S, eng, ap, val, writes):
    S.op(eng, lambda h: h.memset(ap, val), writes=writes)


def o_recip(S, out, in_, reads, writes):
    S.op("dve", lambda h: h.reciprocal(out=out, in_=in_), reads=reads, writes=writes)


def make_ident(S, t, key):
    o_memset(S, "pool", t[:], 0.0, [key])
    S.op("pool", lambda h: h.affine_select(out=t[:], in_=t[:], pattern=[[-1, 128]], compare_op=ALU.not_equal,
                                           fill=1.0, base=0, channel_multiplier=1), reads=[key], writes=[key])


class St:
    def __init__(self, nc, name):
        self.nc = nc
        self.name = name
        self.es = ExitStack()

    def __enter__(self):
        self.es.__enter__()
        return self

    def __exit__(self, *a):
        return self.es.__exit__(*a)

    def sb(self, n, shape, dt):
        return self.es.enter_context(self.nc.sbuf_tensor(self.name + "_" + n, shape, dt))

    def ps(self, n, shape, dt):
        return self.es.enter_context(self.nc.psum_tensor(self.name + "_" + n, shape, dt))


def stage_A(nc, S, T):
    with St(nc, "A") as st:
        Wb = st.sb("Wb", [128, 16, 4096], BF16)
        ident = st.sb("ident", [128, 128], F32)
        xin = st.sb("xin", [128, 2, 2048], F32)
        xT = st.sb("xT", [128, 16, 512], BF16)
        ofm = st.sb("ofm", [128, 4, 512], BF16)
        otf = st.sb("otf", [128, 2, 512], F32)
        otb = st.sb("otb", [128, 2, 512], BF16)
        ptr = [st.ps("ptr%d" % i, [128, 512], F32) for i in range(2)]
        pmm = [st.ps("pmm%d" % i, [128, 512], F32) for i in range(4)]
        make_ident(S, ident, "ident")
        w_in = T["w_in"]
        for blk in (6, 7, 2, 3, 4, 5, 0, 1):
            o_dma(S, "pool", Wb[:, :, blk * 512:(blk + 1) * 512],
                  w_in[:, blk * 512:(blk + 1) * 512].rearrange("(c p) n -> p c n", p=128),
                  writes=[("Wb", blk)], grp="Wb%d" % blk)
        groups = [[4 * g + j for j in range(4)] for g in range(8)] + [[32]]
        if DBG.get("A_groups") is not None:
            groups = [groups[i] for i in DBG["A_groups"]]
        cnt = {"x": 0, "tr": 0, "fm": 0, "mm": 0, "tf": 0, "tb": 0}
        for tiles in groups:
            ntok = 128 * len(tiles)
            t0 = tiles[0]
            far = t0 < 8
            near = 8 <= t0 < 24
            own = t0 >= 24
            for j, tl in enumerate(tiles):
                slot = cnt["x"] % 2
                cnt["x"] += 1
                src = T["xs"][:, :] if tl == 32 else T["xw"][tl * 128:(tl + 1) * 128, :]
                o_dma(S, "sp", xin[:, slot, :], src, writes=[("xin", slot)], grp="xin%d" % slot)
                for b4 in range(4):
                    pi = cnt["tr"] % 2
                    cnt["tr"] += 1
                    for q in range(4):
                        c = b4 * 4 + q
                        o_tr(S, ptr[pi][:, q * 128:(q + 1) * 128], xin[:, slot, c * 128:(c + 1) * 128], ident[:],
                             reads=[("xin", slot), "ident"] + ([("Wb", i) for i in range(8)] if DBG.get("A_waitW") else []), writes=[("ptr", pi)])
                    o_copy(S, "act" if pi == 0 else "dve", xT[:, b4 * 4:(b4 + 1) * 4, j * 128:(j + 1) * 128],
                           ptr[pi][:, :].rearrange("p (a b) -> p a b", a=4), reads=[("ptr", pi)], writes=["xT"])
            if t0 == 32:
                qcol, kcol, ucol = 1024, 3072, 4096
            else:
                qcol, kcol, ucol = (t0 - 24) * 128, (t0 - 8) * 128, t0 * 128
            fm = []
            if own:
                fm += [(h * 128, T["qT"][h, :, qcol:qcol + ntok]) for h in range(8)]
            if own or near:
                fm += [(1024 + h * 128, T["kT"][h, :, kcol:kcol + ntok]) for h in range(8)]
            fm += [(3072 + c * 128, T["uT"][c, :, ucol:ucol + ntok]) for c in range(8)]
            if DBG.get("A_nofm"):
                fm = []
            if DBG.get("A_fmn") is not None:
                fm = fm[:DBG["A_fmn"]]
            for (wc, dst) in fm:
                pi = cnt["mm"] % 4
                cnt["mm"] += 1
                for k in range(16):
                    o_mm(S, pmm[pi][:, 0:ntok], Wb[:, k, wc:wc + 128], xT[:, k, 0:ntok], k == 0, k == 15,
                         reads=["xT", ("Wb", wc // 512)], writes=[("pmm", pi)])
                oi = cnt["fm"] % 4
                cnt["fm"] += 1
                o_copy(S, "act" if oi % 2 == 0 else "dve", ofm[:, oi, 0:ntok], pmm[pi][:, 0:ntok],
                       reads=[("pmm", pi)], writes=[("ofm", oi)])
                o_dma(S, DBG.get("A_stq", "sp"), dst, ofm[:, oi, 0:ntok], reads=[("ofm", oi)], grp="ofm%d" % oi)
            if (own or near) and not DBG.get("A_notm"):
                for j, tl in enumerate(tiles):
                    row_kv = (tl - 8) * 128 if tl < 32 else 3072
                    row_o = (tl - 24) * 128 if tl < 32 else 1024
                    banks = []
                    if own:
                        banks += [("k", 1024), ("k", 1536)]
                    banks += [("v", 2048), ("v", 2560)]
                    if DBG.get("A_banks") is not None:
                        banks = [banks[i] for i in DBG["A_banks"]]
                    for (kv, wc) in banks:
                        pi = cnt["mm"] % 4
                        cnt["mm"] += 1
                        for k in range(16):
                            o_mm(S, pmm[pi][:, :], xT[:, k, j * 128:(j + 1) * 128], Wb[:, k, wc:wc + 512], k == 0, k == 15,
                                 reads=["xT", ("Wb", wc // 512)], writes=[("pmm", pi)])
                        colo = wc - (1024 if kv == "k" else 2048)
                        if own and not DBG.get("A_nootf"):
                            fi = cnt["tf"] % 2
                            cnt["tf"] += 1
                            o_copy(S, DBG.get("A_otfe", "act"), otf[:, fi, :], pmm[pi][:, :], reads=[("pmm", pi)], writes=[("otf", fi)])
                            o_dma(S, DBG.get("A_stq", "sp"), T["Kout" if kv == "k" else "Vout"][row_o:row_o + 128, colo:colo + 512],
                                  otf[:, fi, :], reads=[("otf", fi)], grp="otf%d" % fi)
                        if kv == "v" and not DBG.get("A_nootb"):
                            bi = cnt["tb"] % 2
                            cnt["tb"] += 1
                            o_copy(S, DBG.get("A_otbe", "dve"), otb[:, bi, :], pmm[pi][:, :], reads=[("pmm", pi)] + ([("otf", 0), ("otf", 1)] if DBG.get("A_ser") else []), writes=[("otb", bi)])
                            o_dma(S, DBG.get("A_stq", "sp"), T["V_scr"][row_kv:row_kv + 128, colo:colo + 512], otb[:, bi, :],
                                  reads=[("otb", bi)], grp="otb%d" % bi)
        S.emit()


def stage_B(nc, S, T):
    with St(nc, "B") as st:
        kT = st.sb("kT", [128, 2, KPOS], BF16)
        Vh = st.sb("Vh", [128, 2, 25, 128], BF16)
        qT = st.sb("qT", [128, 2, TOK], BF16)
        pmask = st.sb("pmask", [128, 17, 128], BF16)
        smask = st.sb("smask", [128, 128], BF16)
        smaskn = st.sb("smaskn", [128, 128], BF16)
        vb = st.sb("vb", [128, 24], F32)
        gatt = st.sb("gatt", [128, 8], F32)
        onesb = st.sb("onesb", [128, 128], BF16)
        identb = st.sb("identb", [128, 128], BF16)
        p = st.sb("p", [128, 2, 512], BF16)
        pm = st.sb("pm", [128, 2, 512], BF16)
        onesv = st.sb("onesv", [128, 24, 128], BF16)
        rd = st.sb("rd", [128, 2, 128], F32)
        a32 = st.sb("a32", [128, 2, 128], F32)
        asq = st.sb("asq", [128, 8, TOK], BF16)
        ag = st.sb("ag", [128, 8, TOK], BF16)
        kc = st.sb("kc", [128, 2, 16, 128], BF16)
        vc = st.sb("vc", [128, 2, 16, 128], BF16)
        kcT = st.sb("kcT", [128, 2048], BF16)
        psm = st.sb("psm", [128, 128], BF16)
        psn = st.sb("psn", [128, 128], BF16)
        pmm_ = st.sb("pmm_", [128, 128], BF16)
        pmn = st.sb("pmn", [128, 128], BF16)
        rd8 = st.sb("rd8", [128, 8], F32)
        a8 = st.sb("a8", [128, 8], F32)
        ssa = st.sb("ssa", [128, 16], F32)
        ps_s2 = [st.ps("ps_s%d" % i, [128, 512], F32) for i in range(2)]
        ps_n2 = [st.ps("ps_n%d" % i, [128, 512], F32) for i in range(2)]
        ps_d2 = [st.ps("ps_d%d" % i, [128, 512], F32) for i in range(2)]
        ptk1 = st.ps("ptk", [128, 1024], BF16)
        ptk = [ptk1, ptk1]
        ps_big = st.ps("ps_big", [128, 512], F32)
        ps_sm = ps_big[:, 0:128]
        ps_new = ps_big[:, 128:256]
        ps_snd = ps_big[:, 256:272]
        ps_ss = ps_big[:, 272:288]

        o_memset(S, "dve", onesb[:], 1.0, ["onesb"])
        make_ident(S, identb, "identb")
        o_dma(S, "pool", pmask[:].rearrange("p a b -> p (a b)"), T["pmask"][:, :], writes=["pmask"], grp="pmask")
        o_dma(S, "pool", smask[:], T["smask"][:, :], writes=["smask"], grp="smask")
        o_dma(S, "pool", smaskn[:], T["smaskn"][:, :], writes=["smaskn"], grp="smaskn")
        o_dma(S, "sp", vb[:], T["vbias"][:, :], writes=["vb"], grp="vb")
        o_dma(S, "sp", gatt[:], T["gatt"][:, :], writes=["gatt"], grp="gatt")
        for j in range(24):
            o_ts(S, "pool", onesv[:, j, :], onesb[:], vb[:, j:j + 1], ALU.mult, reads=["onesb", "vb"], writes=["onesv"])
        o_memset(S, "pool", asq[:, :, 1024:TOK], 0.0, [("asq", h, 8) for h in range(8)])
        o_memset(S, "pool", ag[:, :, 1024:TOK], 0.0, [("ag", h, 8) for h in range(8)])
        it = 0
        blk_i = 0
        sh_i = 0
        for h in range(8):
            b = h % 2
            o_dma(S, "sp", kT[:, b, :], T["kT"][h, :, :], writes=[("kT", b)], grp="kT%d" % b)
            o_dma(S, "sp", Vh[:, b, :, :], T["V_scr"][:, h * 128:(h + 1) * 128].rearrange("(c p) n -> p c n", p=128),
                  writes=[("Vh", b)], grp="Vh%d" % b)
            o_dma(S, "sp", qT[:, b, :], T["qT"][h, :, :], writes=[("qT", b)], grp="qT%d" % b)
            groups_ = [(m, c0, n) for m in range(8) for (c0, n) in ((0, 4), (4, 4), (8, 4), (12, 4), (16, 1))]

            def score(gi):
                m, c0, n = groups_[gi]
                sg = (g0 + gi) % 2
                for q in range(n):
                    kcx = m + c0 + q
                    o_mm(S, ps_s2[sg][:, q * 128:(q + 1) * 128], kT[:, b, kcx * 128:(kcx + 1) * 128], qT[:, b, m * 128:(m + 1) * 128],
                         True, True, reads=[("kT", b), ("qT", b)], writes=[("ps_s", sg)])
                o_act(S, p[:, sg, 0:n * 128], ps_s2[sg][:, 0:n * 128], AF.Exp, reads=[("ps_s", sg)], writes=[("p", sg)], scale=SC_ATT)
                o_tt(S, "dve" if sg == 0 else "pool", pm[:, sg, 0:n * 128], p[:, sg, 0:n * 128],
                     pmask[:, c0:c0 + n, :].rearrange("p a b -> p (a b)"), ALU.mult,
                     reads=[("p", sg), "pmask"], writes=[("pm", sg)])

            g0 = it
            score(0)
            for gi, (m, c0, n) in enumerate(groups_):
                if gi + 1 < len(groups_):
                    score(gi + 1)
                sg = (g0 + gi) % 2
                sl = (blk_i + m) % 2
                for q in range(n):
                    kcx = m + c0 + q
                    cc = c0 + q
                    o_mm(S, ps_n2[sl][:, 0:128], Vh[:, b, kcx, :], pm[:, sg, q * 128:(q + 1) * 128], cc == 0, cc == 16,
                         reads=[("Vh", b), ("pm", sg)], writes=[("ps_n", sl)])
                for q in range(n):
                    kcx = m + c0 + q
                    cc = c0 + q
                    o_mm(S, ps_d2[sl][:, 0:128], onesv[:, kcx, :], pm[:, sg, q * 128:(q + 1) * 128], cc == 0, cc == 16,
                         reads=["onesv", ("pm", sg)], writes=[("ps_d", sl)])
                if c0 == 16:
                    o_recip(S, rd[:, sl, :], ps_d2[sl][:, 0:128], reads=[("ps_d", sl)], writes=[("rd", sl)])
                    o_tt(S, "dve", a32[:, sl, :], ps_n2[sl][:, 0:128], rd[:, sl, :], ALU.mult,
                         reads=[("ps_n", sl), ("rd", sl)], writes=[("a32", sl)])
                    o_tt(S, "pool", asq[:, h, m * 128:(m + 1) * 128], a32[:, sl, :], a32[:, sl, :], ALU.mult,
                         reads=[("a32", sl)], writes=[("asq", h, m)])
                    o_copy(S, "act", ag[:, h, m * 128:(m + 1) * 128], a32[:, sl, :],
                           reads=[("a32", sl), "gatt"], writes=[("ag", h, m)], scale=gatt[:, h:h + 1])
            it += len(groups_)
            blk_i += 8
            o_mm(S, ps_new[:, :], kT[:, b, 3072:3200], qT[:, b, 1024:1152], True, True,
                 reads=[("kT", b), ("qT", b)], writes=["ps_big"])
            o_act(S, psn[:], ps_new[:, :], AF.Exp, reads=["ps_big"], writes=["psn"], scale=SC_ATT)
            o_tt(S, "dve", pmn[:], psn[:], smaskn[:], ALU.mult, reads=["psn", "smaskn"], writes=["pmn"])
            for s in range(4):
                sb_ = sh_i % 2
                sh_i += 1
                o_dma(S, "pool", kc[:, sb_, :, :], T["cwk"][s, :, h * 128:(h + 1) * 128].rearrange("(c p) n -> p c n", p=128),
                      writes=[("kc", sb_)], grp="kc%d" % sb_)
                o_dma(S, "pool", vc[:, sb_, :, :], T["cwv"][s, :, h * 128:(h + 1) * 128].rearrange("(c p) n -> p c n", p=128),
                      writes=[("vc", sb_)], grp="vc%d" % sb_)
                for half in range(2):
                    for q8 in range(8):
                        cc = half * 8 + q8
                        o_tr(S, ptk[half][:, q8 * 128:(q8 + 1) * 128], kc[:, sb_, cc, :], identb[:],
                             reads=[("kc", sb_), "identb"], writes=["ptk"])
                    o_copy(S, "act" if half == 0 else "dve", kcT[:, half * 1024:(half + 1) * 1024], ptk[half][:, :],
                           reads=["ptk"], writes=[("kcT", half)])
                q0 = 1024 + 32 * s
                for cc in range(16):
                    o_mm(S, ps_sm[:, cc * 8:(cc + 1) * 8], kcT[:, cc * 128:(cc + 1) * 128], qT[:, b, q0:q0 + 8], True, True,
                         reads=[("kcT", cc // 8), ("qT", b)], writes=["ps_big"])
                o_act(S, psm[:], ps_sm[:, 0:128], AF.Exp, reads=["ps_big"], writes=["psm"], scale=SC_ATT)
                o_tt(S, "dve", pmm_[:], psm[:], smask[:], ALU.mult, reads=["psm", "smask"], writes=["pmm_"])
                for cc in range(16):
                    o_mm(S, ps_snd[:, 0:8], vc[:, sb_, cc, :], pmm_[:, cc * 8:(cc + 1) * 8], cc == 0, False,
                         reads=[("vc", sb_), "pmm_"], writes=["ps_big"])
                o_mm(S, ps_snd[:, 0:8], Vh[:, b, 24, :], pmn[:, 32 * s:32 * s + 8], False, True,
                     reads=[("Vh", b), "pmn"], writes=["ps_big"])
                for cc in range(16):
                    o_mm(S, ps_snd[:, 8:16], onesb[:], pmm_[:, cc * 8:(cc + 1) * 8], cc == 0, False,
                         reads=["onesb", "pmm_"], writes=["ps_big"])
                o_mm(S, ps_snd[:, 8:16], onesb[:], pmn[:, 32 * s:32 * s + 8], False, True,
                     reads=["onesb", "pmn"], writes=["ps_big"])
                o_recip(S, rd8[:], ps_snd[:, 8:16], reads=["ps_big"], writes=["rd8"])
                o_tt(S, "dve", a8[:], ps_snd[:, 0:8], rd8[:], ALU.mult, reads=["ps_big", "rd8"], writes=["a8"])
                o_tt(S, "pool", asq[:, h, q0:q0 + 8], a8[:], a8[:], ALU.mult, reads=["a8"], writes=[("asq", h, 8)])
                o_ts(S, "pool", ag[:, h, q0:q0 + 8], a8[:], gatt[:, h:h + 1], ALU.mult, reads=["a8", "gatt"], writes=[("ag", h, 8)])
        for t in range(NT):
            for h in range(8):
                o_mm(S, ps_ss[:, t:t + 1], asq[:, h, t * 128:(t + 1) * 128], onesb[:, 0:1], h == 0, h == 7,
                     reads=[("asq", h, t), "onesb"], writes=["ps_big"])
        o_act(S, ssa[:, 0:NT], ps_ss[:, 0:NT], AF.Sqrt, reads=["ps_big"], writes=["ssa"], bias=1e-6, scale=1.0 / 1024.0)
        o_recip(S, ssa[:, 0:NT], ssa[:, 0:NT], reads=["ssa"], writes=["ssa"])
        o_dma(S, "sp", T["rstd_a"][:, :], ssa[:, 0:NT], reads=["ssa"], grp="rstd")
        o_dma(S, "sp", T["mixT"][0:8, :, :].rearrange("h p t -> p h t"), ag[:, :, :],
              reads=[("ag", h, m) for h in range(8) for m in range(9)], grp="agst")
        S.emit()


def stage_C(nc, S, T):
    with St(nc, "C") as st:
        lre = st.sb("lre", [128, 32], F32)
        lim = st.sb("lim", [128, 32], F32)
        ldt = st.sb("ldt", [128, 32], F32)
        tA = st.sb("tA", [128, 32], F32)
        tB = st.sb("tB", [128, 32], F32)
        tC = st.sb("tC", [128, 32], F32)
        tI = st.sb("tI", [128, 32], mybir.dt.int32)
        mag = st.sb("mag", [128, 32], F32)
        fre = st.sb("fre", [128, 32], F32)
        fim = st.sb("fim", [128, 32], F32)
        nfim = st.sb("nfim", [128, 32], F32)
        pwr = st.sb("pwr", [128, 11, 32], F32)
        pwi = st.sb("pwi", [128, 11, 32], F32)
        npwi = st.sb("npwi", [128, 11, 32], F32)
        BTre = st.sb("BTre", [128, 32, 128], BF16)
        BTim = st.sb("BTim", [128, 32, 128], BF16)
        CTre = st.sb("CTre", [128, 32, 128], BF16)
        CTim = st.sb("CTim", [128, 32, 128], BF16)
        dcol = st.sb("dcol", [128, 8], F32)
        h0r = st.sb("h0r", [128, 32, 4], F32)
        h0i = st.sb("h0i", [128, 32, 4], F32)
        uT = st.sb("uT", [128, 2, UPOS], BF16)
        xr2 = st.sb("xr", [128, 2, UPOS], F32)
        xi2 = st.sb("xi", [128, 2, UPOS], F32)
        T0r = st.sb("T0r", [128, 1536], F32)
        T0i = st.sb("T0i", [128, 1536], F32)
        T1r = st.sb("T1r", [128, 768], F32)
        T1i = st.sb("T1i", [128, 768], F32)
        identF = st.sb("identF", [128, 128], F32)
        onesF = st.sb("onesF", [128, 128], F32)
        Dre = st.sb("Dre", [128, 2, 512], F32)
        Dim = st.sb("Dim", [128, 2, 512], F32)
        Bsr = st.sb("Bsr", [128, 4, 8], F32)
        Bsi = st.sb("Bsi", [128, 4, 8], F32)
        Hr = st.sb("Hr", [128, 4], F32)
        Hi = st.sb("Hi", [128, 4], F32)
        tmp = st.sb("tmp", [128, 4, 512], F32)
        hbr = st.sb("hbr", [128, TOK], BF16)
        hbi = st.sb("hbi", [128, TOK], BF16)
        hfr = st.sb("hfr", [128, 32], F32)
        hfi = st.sb("hfi", [128, 32], F32)
        hsr = st.sb("hsr", [128, 32, 4], F32)
        hsi = st.sb("hsi", [128, 32, 4], F32)
        ysb = st.sb("ysb", [128, TOK], F32)
        gt1 = st.sb("gt1", [128, TOK], F32)
        yg = st.sb("yg", [128, 2, TOK], F32)
        ygb = st.sb("ygb", [128, 2, TOK], BF16)
        pw = [st.ps("pw%d" % i, [128, 512], F32) for i in range(4)]
        py = [st.ps("py%d" % i, [128, 512], F32) for i in range(3)]

        for (t_, nm) in ((lre, "lamre"), (lim, "lamim"), (ldt, "logdt")):
            o_dma(S, "sp", t_[:], T[nm][:, :], writes=[nm], grp=nm)
        o_dma(S, "sp", dcol[:], T["dcol"][:, :], writes=["dcol"], grp="dcol")
        o_dma(S, "sp", h0r[:].rearrange("p a b -> p (a b)"), T["h0re"][:, :], writes=["h0r"], grp="h0r")
        o_dma(S, "sp", h0i[:].rearrange("p a b -> p (a b)"), T["h0im"][:, :], writes=["h0i"], grp="h0i")
        for (t_, nm) in ((BTre, "BTre"), (BTim, "BTim"), (CTre, "CTre"), (CTim, "CTim")):
            o_dma(S, "pool", t_[:].rearrange("p a b -> p (a b)"), T[nm][:, :], writes=[nm], grp=nm)
        P = ["par"]
        o_act(S, ldt[:], ldt[:], AF.Exp, reads=["logdt"], writes=["logdt"] + P)
        o_tt(S, "dve", tA[:], lre[:], ldt[:], ALU.mult, reads=["lamre", "logdt"], writes=P)
        o_act(S, mag[:], tA[:], AF.Exp, reads=P, writes=P)
        o_tt(S, "dve", tB[:], lim[:], ldt[:], ALU.mult, reads=["lamim", "logdt"], writes=P)

        def sin_of(dst, shift):
            o_ts(S, "dve", tC[:], tB[:], shift, ALU.add, reads=P, writes=P, s2=1.0 / (2 * np.pi), op1=ALU.mult)
            S.op("dve", lambda h: h.tensor_copy(out=tI[:], in_=tC[:]), reads=P, writes=P)
            S.op("dve", lambda h: h.tensor_copy(out=tA[:], in_=tI[:]), reads=P, writes=P)
            o_tt(S, "dve", tC[:], tC[:], tA[:], ALU.subtract, reads=P, writes=P)
            o_ts(S, "dve", tA[:], tC[:], 0.5, ALU.is_gt, reads=P, writes=P)
            o_tt(S, "dve", tC[:], tC[:], tA[:], ALU.subtract, reads=P, writes=P)
            o_ts(S, "dve", tA[:], tC[:], -0.5, ALU.is_lt, reads=P, writes=P)
            o_tt(S, "dve", tC[:], tC[:], tA[:], ALU.add, reads=P, writes=P)
            o_ts(S, "dve", tC[:], tC[:], 2 * np.pi, ALU.mult, reads=P, writes=P, s2=3.14159, op1=ALU.min)
            o_ts(S, "dve", tC[:], tC[:], -3.14159, ALU.max, reads=P, writes=P)
            o_act(S, dst, tC[:], AF.Sin, reads=P, writes=P)

        sin_of(pwi[:, 0, :], 0.0)
        sin_of(pwr[:, 0, :], np.pi / 2)
        o_tt(S, "dve", pwr[:, 0, :], pwr[:, 0, :], mag[:], ALU.mult, reads=P, writes=P)
        o_tt(S, "dve", pwi[:, 0, :], pwi[:, 0, :], mag[:], ALU.mult, reads=P, writes=P)
        o_tt(S, "dve", tA[:], lre[:], lre[:], ALU.mult, reads=P + ["lamre"], writes=P)
        o_tt(S, "dve", tC[:], lim[:], lim[:], ALU.mult, reads=P + ["lamim"], writes=P)
        o_tt(S, "dve", tA[:], tA[:], tC[:], ALU.add, reads=P, writes=P)
        o_recip(S, tA[:], tA[:], reads=P, writes=P)
        o_ts(S, "dve", tB[:], pwr[:, 0, :], -1.0, ALU.add, reads=P, writes=P)
        o_tt(S, "dve", fre[:], tB[:], lre[:], ALU.mult, reads=P, writes=P)
        o_tt(S, "dve", tC[:], pwi[:, 0, :], lim[:], ALU.mult, reads=P, writes=P)
        o_tt(S, "dve", fre[:], fre[:], tC[:], ALU.add, reads=P, writes=P)
        o_tt(S, "dve", fre[:], fre[:], tA[:], ALU.mult, reads=P, writes=P)
        o_tt(S, "dve", fim[:], pwi[:, 0, :], lre[:], ALU.mult, reads=P, writes=P)
        o_tt(S, "dve", tC[:], tB[:], lim[:], ALU.mult, reads=P, writes=P)
        o_tt(S, "dve", fim[:], fim[:], tC[:], ALU.subtract, reads=P, writes=P)
        o_tt(S, "dve", fim[:], fim[:], tA[:], ALU.mult, reads=P, writes=P)
        o_ts(S, "dve", nfim[:], fim[:], -1.0, ALU.mult, reads=P, writes=P)
        for k in range(10):
            o_tt(S, "dve", tA[:], pwr[:, k, :], pwr[:, k, :], ALU.mult, reads=P, writes=P)
            o_tt(S, "dve", tC[:], pwi[:, k, :], pwi[:, k, :], ALU.mult, reads=P, writes=P)
            o_tt(S, "dve", pwr[:, k + 1, :], tA[:], tC[:], ALU.subtract, reads=P, writes=P)
            o_tt(S, "dve", tA[:], pwr[:, k, :], pwi[:, k, :], ALU.mult, reads=P, writes=P)
            o_ts(S, "dve", pwi[:, k + 1, :], tA[:], 2.0, ALU.mult, reads=P, writes=P)
        o_ts(S, "dve", npwi[:].rearrange("p a b -> p (a b)"), pwi[:].rearrange("p a b -> p (a b)"), -1.0, ALU.mult, reads=P, writes=P)
        make_ident(S, identF, "identF")
        o_memset(S, "pool", onesF[:], 1.0, ["onesF"])
        for g in range(8):
            db = g % 2
            for q in range(4):
                tq = 4 * g + q
                o_copy(S, "act", Dre[:, db, 128 * q:128 * (q + 1)], identF[:], reads=["identF"] + P, writes=[("Dre", db)],
                       scale=fre[:, tq:tq + 1])
                o_copy(S, "act", Dim[:, db, 128 * q:128 * (q + 1)], identF[:], reads=["identF"] + P, writes=[("Dim", db)],
                       scale=fim[:, tq:tq + 1])
            o_mm(S, pw[0][:, :], onesF[:], Dre[:, db, :], True, True, reads=["onesF", ("Dre", db)], writes=[("pw", 0)])
            o_mm(S, pw[1][:, :], onesF[:], Dim[:, db, :], True, True, reads=["onesF", ("Dim", db)], writes=[("pw", 1)])
            bre = BTre[:, 4 * g:4 * g + 4, :].rearrange("p a b -> p (a b)")
            bim = BTim[:, 4 * g:4 * g + 4, :].rearrange("p a b -> p (a b)")
            o_tt(S, "dve", tmp[:, 0, :], pw[0][:, :], bre, ALU.mult, reads=[("pw", 0), "BTre"], writes=[("tmp", 0)])
            o_tt(S, "dve", tmp[:, 1, :], pw[1][:, :], bim, ALU.mult, reads=[("pw", 1), "BTim"], writes=[("tmp", 1)])
            o_tt(S, "dve", tmp[:, 2, :], pw[0][:, :], bim, ALU.mult, reads=[("pw", 0), "BTim"], writes=[("tmp", 2)])
            o_tt(S, "dve", tmp[:, 3, :], pw[1][:, :], bre, ALU.mult, reads=[("pw", 1), "BTre"], writes=[("tmp", 3)])
            o_tt(S, "dve", bre, tmp[:, 0, :], tmp[:, 1, :], ALU.subtract, reads=[("tmp", 0), ("tmp", 1)], writes=["BTre"])
            o_tt(S, "dve", bim, tmp[:, 2, :], tmp[:, 3, :], ALU.add, reads=[("tmp", 2), ("tmp", 3)], writes=["BTim"])
        o_memset(S, "pool", hbr[:, 1024:TOK], 0.0, ["hbr_s"])
        o_memset(S, "pool", hbi[:, 1024:TOK], 0.0, ["hbi_s"])

        def cma(dr, di, er, ei, orr, oi, k, tau, rk, wk):
            pr = pwr[:, k, tau:tau + 1]
            pi_ = pwi[:, k, tau:tau + 1]
            npi = npwi[:, k, tau:tau + 1]
            wr_ = [(w_, "r") for w_ in wk]
            wi_ = [(w_, "i") for w_ in wk]
            o_stt(S, dr, er, pr, orr, ALU.mult, ALU.add, reads=rk + P, writes=wr_)
            o_stt(S, di, ei, pr, oi, ALU.mult, ALU.add, reads=rk + P, writes=wi_)
            o_stt(S, dr, ei, npi, dr, ALU.mult, ALU.add, reads=rk + P, writes=wr_)
            o_stt(S, di, er, pi_, di, ALU.mult, ALU.add, reads=rk + P, writes=wi_ + list(wk))
            S.touch(wk, wr_ + wi_)

        blocks = [(i * 512, 512) for i in range(8)] + [(4096, 128)]
        wic = [0]

        def emit_xe(tau, ub):
            xr = xr2[:, tau % 2, :]
            xi = xi2[:, tau % 2, :]
            XK = ("x", tau % 2)
            for (c0, w) in blocks:
                wi = wic[0]
                p0 = pw[(wi * 2) % 4]
                p1 = pw[(wi * 2 + 1) % 4]
                k0 = ("pw", (wi * 2) % 4)
                k1 = ("pw", (wi * 2 + 1) % 4)
                wic[0] += 1
                o_mm(S, p0[:, 0:w], BTre[:, tau, :], uT[:, ub, c0:c0 + w], True, True, reads=["BTre", ("uT", ub)], writes=[k0])
                o_mm(S, p1[:, 0:w], BTim[:, tau, :], uT[:, ub, c0:c0 + w], True, True, reads=["BTim", ("uT", ub)], writes=[k1])
                o_copy(S, "act", xr[:, c0:c0 + w], p0[:, 0:w], reads=[k0], writes=[XK])
                o_copy(S, "act", xi[:, c0:c0 + w], p1[:, 0:w], reads=[k1], writes=[XK])

        o_dma(S, "sp", uT[:, 0, :], T["uT"][0, :, :], writes=[("uT", 0)], grp="uT0")
        emit_xe(0, 0)
        for c in range(8):
            ub = c % 2
            for j in range(4):
                tau = 4 * c + j
                xr = xr2[:, tau % 2, :]
                xi = xi2[:, tau % 2, :]
                XK = ("x", tau % 2)
                if tau + 1 < 32:
                    cn = (tau + 1) // 4
                    if (tau + 1) % 4 == 0:
                        o_dma(S, "sp", uT[:, cn % 2, :], T["uT"][cn, :, :], writes=[("uT", cn % 2)], grp="uT%d" % (cn % 2))
                    emit_xe(tau + 1, cn % 2)
                X = [XK]
                xs_r = xr[:, 4096:UPOS:32]
                xs_i = xi[:, 4096:UPOS:32]
                cma(xs_r, xs_i, h0r[:, tau, :], h0i[:, tau, :], xs_r, xs_i, 0, tau, X + ["h0r", "h0i"], X)
                src_r, src_i, n = xr, xi, 3072
                dsts = [(T0r, T0i), (T1r, T1i)]
                for k in range(10):
                    dr_, di_ = dsts[k % 2]
                    no = n // 2
                    cma(dr_[:, 0:no], di_[:, 0:no], src_r[:, 0:n:2], src_i[:, 0:n:2], src_r[:, 1:n:2], src_i[:, 1:n:2],
                        k, tau, X + ["tree"], ["tree"])
                    src_r, src_i, n = dr_, di_, no
                cma(Hr[:, 0:1], Hi[:, 0:1], src_r[:, 0:1], src_i[:, 0:1], src_r[:, 1:2], src_i[:, 1:2], 10, tau, ["tree"], ["H"])
                cma(Hr[:, 1:2], Hi[:, 1:2], Hr[:, 0:1], Hi[:, 0:1], src_r[:, 2:3], src_i[:, 2:3], 10, tau, ["tree", "H"], ["H"])
                cma(xr[:, 3072:3073], xi[:, 3072:3073], Hr[:, 1:2], Hi[:, 1:2], xr[:, 3072:3073], xi[:, 3072:3073], 0, tau, X + ["H"], X)
                o_ = 3072
                for k in range(10):
                    st_ = 1 << (k + 1)
                    h_ = 1 << k
                    cnt = 1024 // st_
                    t0 = o_ + st_ - 1
                    s0 = o_ + h_ - 1
                    t1 = t0 + (cnt - 1) * st_ + 1
                    s1 = s0 + (cnt - 1) * st_ + 1
                    cma(xr[:, t0:t1:st_], xi[:, t0:t1:st_], xr[:, s0:s1:st_], xi[:, s0:s1:st_],
                        xr[:, t0:t1:st_], xi[:, t0:t1:st_], k, tau, X, X)
                for k in range(8, -1, -1):
                    st_ = 1 << (k + 1)
                    h_ = 1 << k
                    cnt = 1024 // st_ - 1
                    t0 = o_ + st_ + h_ - 1
                    s0 = o_ + st_ - 1
                    t1 = t0 + (cnt - 1) * st_ + 1
                    s1 = s0 + (cnt - 1) * st_ + 1
                    cma(xr[:, t0:t1:st_], xi[:, t0:t1:st_], xr[:, s0:s1:st_], xi[:, s0:s1:st_],
                        xr[:, t0:t1:st_], xi[:, t0:t1:st_], k, tau, X, X)
                sr = xr[:, 4096:UPOS].rearrange("p (s j) -> p s j", j=32)[:, :, 0:8]
                si_ = xi[:, 4096:UPOS].rearrange("p (s j) -> p s j", j=32)[:, :, 0:8]
                cur = (sr, si_, XK)
                alt = (Bsr[:, :, :], Bsi[:, :, :], "Bs")
                for k in range(3):
                    s_ = 1 << k
                    cr, ci, ck = cur
                    ar_, ai_, ak = alt
                    cma(ar_[:, :, s_:8], ai_[:, :, s_:8], cr[:, :, 0:8 - s_], ci[:, :, 0:8 - s_], cr[:, :, s_:8], ci[:, :, s_:8],
                        k, tau, [ck], [ak])
                    o_copy(S, "act", ar_[:, :, 0:s_], cr[:, :, 0:s_], reads=[ck], writes=[ak])
                    o_copy(S, "pool", ai_[:, :, 0:s_], ci[:, :, 0:s_], reads=[ck], writes=[ak])
                    cur, alt = alt, cur
                fr, fi_, fk = cur
                o_copy(S, "act", hfr[:, tau:tau + 1], xr[:, 4095:4096], reads=X, writes=["hf"])
                o_copy(S, "act", hfi[:, tau:tau + 1], xi[:, 4095:4096], reads=X, writes=["hf"])
                o_copy(S, "pool", hsr[:, tau, :], fr[:, :, 7], reads=[fk], writes=["hs"])
                o_copy(S, "pool", hsi[:, tau, :], fi_[:, :, 7], reads=[fk], writes=["hs"])
                o_copy(S, "act", hbr[:, 0:1024], xr[:, 3072:4096], reads=X, writes=["hbr"])
                o_copy(S, "act", hbi[:, 0:1024], xi[:, 3072:4096], reads=X, writes=["hbi"], scale=-1.0)
                o_copy(S, "pool", hbr[:, 1024:TOK].rearrange("p (s j) -> p s j", j=32)[:, :, 0:8], fr, reads=[fk, "hbr_s"], writes=["hbr_s"])
                o_ts(S, "pool", hbi[:, 1024:TOK].rearrange("p (s j) -> p s j", j=32)[:, :, 0:8], fi_, -1.0, ALU.mult,
                     reads=[fk, "hbi_s"], writes=["hbi_s"])
                for bi, (c0, w) in enumerate([(0, 512), (512, 512), (1024, 128)]):
                    o_mm(S, py[bi][:, 0:w], CTre[:, tau, :], hbr[:, c0:c0 + w], j == 0, False,
                         reads=["CTre", "hbr", "hbr_s"], writes=[("py", bi)])
                    o_mm(S, py[bi][:, 0:w], CTim[:, tau, :], hbi[:, c0:c0 + w], False, j == 3,
                         reads=["CTim", "hbi", "hbi_s"], writes=[("py", bi)])
            yb_ = c % 2
            for bi, (c0, w) in enumerate([(0, 512), (512, 512), (1024, 128)]):
                o_stt(S, ysb[:, c0:c0 + w], uT[:, ub, 3072 + c0:3072 + c0 + w], dcol[:, c:c + 1], py[bi][:, 0:w], ALU.mult, ALU.add,
                      reads=[("uT", ub), ("py", bi), "dcol"], writes=["ysb"])
            o_tt(S, "pool", gt1[:], ysb[:], ysb[:], ALU.mult, reads=["ysb"], writes=["gt1"])
            o_ts(S, "dve", gt1[:], gt1[:], 0.044715, ALU.mult, reads=["gt1"], writes=["gt1"], s2=1.0, op1=ALU.add)
            o_tt(S, "dve", gt1[:], gt1[:], ysb[:], ALU.mult, reads=["gt1", "ysb"], writes=["gt1"])
            o_act(S, gt1[:], gt1[:], AF.Tanh, reads=["gt1"], writes=["gt1"], scale=0.7978845608028654)
            o_ts(S, "dve", gt1[:], gt1[:], 1.0, ALU.add, reads=["gt1"], writes=["gt1"], s2=0.5, op1=ALU.mult)
            o_tt(S, "dve", yg[:, yb_, :], gt1[:], ysb[:], ALU.mult, reads=["gt1", "ysb"], writes=[("yg", yb_)])
            o_copy(S, "pool", ygb[:, yb_, :], yg[:, yb_, :], reads=[("yg", yb_)], writes=[("ygb", yb_)])
            o_dma(S, "sp", T["ygT"][c, :, :], yg[:, yb_, :], reads=[("yg", yb_)], grp="yg%d" % yb_)
            o_dma(S, "sp", T["ygTb"][c, :, :], ygb[:, yb_, :], reads=[("ygb", yb_)], grp="ygb%d" % yb_)
        o_dma(S, "sp", T["hfr"][:, :], hfr[:], reads=["hf"], grp="hfr")
        o_dma(S, "sp", T["hfi"][:, :], hfi[:], reads=["hf"], grp="hfi")
        o_dma(S, "sp", T["hsr"][:, :], hsr[:].rearrange("p a b -> p (a b)"), reads=["hs"], grp="hsr")
        o_dma(S, "sp", T["hsi"][:, :], hsi[:].rearrange("p a b -> p (a b)"), reads=["hs"], grp="hsi")
        S.emit()


def stage_D1(nc, S, T):
    with St(nc, "D1") as st:
        ygT = st.sb("ygT", [128, 8, TOK], F32)
        ygb = st.sb("ygb", [128, 8, TOK], BF16)
        Wg = st.sb("Wg", [128, 8, 1024], BF16)
        gssm = st.sb("gssm", [128, 8], F32)
        onesb = st.sb("onesb", [128, 128], BF16)
        ssq = st.sb("ssq", [128, 8, TOK], BF16)
        sgm = st.sb("sgm", [128, 8, TOK], BF16)
        sg = st.sb("sg", [128, 2, 512], F32)
        so = st.sb("so", [128, 2, 512], F32)
        sss = st.sb("sss", [128, 16], F32)
        pz = [st.ps("pz%d" % i, [128, 512], F32) for i in range(4)]
        ps_ss = st.ps("ps_ss", [128, 16], F32)
        o_memset(S, "dve", onesb[:], 1.0, ["onesb"])
        o_dma(S, "sp", ygT[:], T["ygT"].rearrange("c p t -> p c t"), writes=["ygT"], grp="ygT")
        o_dma(S, "sp", ygb[:], T["ygTb"].rearrange("c p t -> p c t"), writes=["ygb"], grp="ygb")
        o_dma(S, "pool", Wg[:], T["w_glu"].rearrange("(c p) n -> p c n", p=128), writes=["Wg"], grp="Wg")
        o_dma(S, "sp", gssm[:], T["gssm"][:, :], writes=["gssm"], grp="gssm")
        i = 0
        for f in range(8):
            for (c0, w) in [(0, 512), (512, 512), (1024, 128)]:
                pi = i % 4
                si = i % 2
                i += 1
                for k in range(8):
                    o_mm(S, pz[pi][:, 0:w], Wg[:, k, f * 128:(f + 1) * 128], ygb[:, k, c0:c0 + w], k == 0, k == 7,
                         reads=["Wg", "ygb"], writes=[("pz", pi)])
                o_act(S, sg[:, si, 0:w], pz[pi][:, 0:w], AF.Sigmoid, reads=[("pz", pi)], writes=[("sg", si)])
                o_tt(S, "dve", so[:, si, 0:w], ygT[:, f, c0:c0 + w], sg[:, si, 0:w], ALU.mult, reads=["ygT", ("sg", si)], writes=[("so", si)])
                o_tt(S, "pool", ssq[:, f, c0:c0 + w], so[:, si, 0:w], so[:, si, 0:w], ALU.mult, reads=[("so", si)], writes=["ssq"])
                o_copy(S, "act", sgm[:, f, c0:c0 + w], so[:, si, 0:w], reads=[("so", si), "gssm"], writes=["sgm"], scale=gssm[:, f:f + 1])
        for t in range(NT):
            for f in range(8):
                o_mm(S, ps_ss[:, t:t + 1], ssq[:, f, t * 128:(t + 1) * 128], onesb[:, 0:1], f == 0, f == 7,
                     reads=["ssq", "onesb"], writes=["ps_ss"])
        o_act(S, sss[:, 0:NT], ps_ss[:, 0:NT], AF.Sqrt, reads=["ps_ss"], writes=["sss"], bias=1e-6, scale=1.0 / 1024.0)
        o_recip(S, sss[:, 0:NT], sss[:, 0:NT], reads=["sss"], writes=["sss"])
        o_dma(S, "sp", T["rstd_s"][:, :], sss[:, 0:NT], reads=["sss"], grp="rstd")
        o_dma(S, "sp", T["mixT"][8:16, :, :].rearrange("h p t -> p h t"), sgm[:, :, :], reads=["sgm"], grp="sgmst")
        S.emit()


def layernorm_tile(S, st_, X, t, lng, lnb, stats, mv, rs, key):
    xt = X[:, t, :]
    for c in range(4):
        S.op("dve", lambda h, c=c: h.bn_stats(out=stats[:, c, :], in_=X[:, t, c * 512:(c + 1) * 512]), reads=[key], writes=["ln_st"])
    S.op("dve", lambda h: h.bn_aggr(out=mv[:], in_=stats[:].rearrange("p a b -> p (a b)")), reads=["ln_st"], writes=["ln_mv"])
    o_act(S, rs[:], mv[:, 1:2], AF.Sqrt, reads=["ln_mv"], writes=["ln_rs"], bias=1e-5, scale=1.0)
    o_recip(S, rs[:], rs[:], reads=["ln_rs"], writes=["ln_rs"])
    o_ts(S, "dve", xt, xt, mv[:, 0:1], ALU.subtract, reads=[key, "ln_mv", "ln_rs"], writes=[key], s2=rs[:, 0:1], op1=ALU.mult)
    o_tt(S, "dve", xt, xt, lng[:], ALU.mult, reads=[key, "lng"], writes=[key])
    o_tt(S, "pool", xt, xt, lnb[:], ALU.add, reads=[key, "lnb"], writes=[key])


def linear_residual_ln(nc, S, T, name, inT_name, w_name, lng_name, lnb_name, xin_fn, xout_name, two_part):
    with St(nc, name) as st:
        X = st.sb("X", [128, NT, 2048], F32)
        inT = st.sb("inT", [128, 16, TOK], BF16)
        Wo = st.sb("Wo", [128, 2, 16, 512], BF16)
        lng = st.sb("lng", [128, 2048], F32)
        lnb = st.sb("lnb", [128, 2048], F32)
        tmp = st.sb("tmp", [128, 2, 512], F32)
        stats = st.sb("stats", [128, 4, 6], F32)
        mv = st.sb("mv", [128, 2], F32)
        rs = st.sb("rs", [128, 1], F32)
        ra = st.sb("ra", [128, NT], F32)
        rsm = st.sb("rsm", [128, NT], F32)
        pa = [st.ps("pa%d" % i, [128, 512], F32) for i in range(2)]
        pb = [st.ps("pb%d" % i, [128, 512], F32) for i in range(2)]
        xin_fn(S, X)
        o_dma(S, "sp", inT[:], T[inT_name].rearrange("c p t -> p c t"), writes=["inT"], grp="inT")
        o_dma(S, "sp", lng[:], T[lng_name].partition_broadcast(128), writes=["lng"], grp="lng")
        o_dma(S, "sp", lnb[:], T[lnb_name].partition_broadcast(128), writes=["lnb"], grp="lnb")
        if two_part:
            o_dma(S, "sp", ra[:], T["rstd_a"][:, :], writes=["ra"], grp="ra")
            o_dma(S, "sp", rsm[:], T["rstd_s"][:, :], writes=["rsm"], grp="rsm")
        i = 0
        for n in range(4):
            wb = n % 2
            o_dma(S, "pool", Wo[:, wb, :, :], T[w_name][:, n * 512:(n + 1) * 512].rearrange("(c p) n -> p c n", p=128),
                  writes=[("Wo", wb)], grp="Wo%d" % wb)
            for t in range(NT):
                pi = i % 2
                i += 1
                xk = ("X", t)
                if two_part:
                    for k in range(8):
                        o_mm(S, pa[pi][:, :], inT[:, k, t * 128:(t + 1) * 128], Wo[:, wb, k, :], k == 0, k == 7,
                             reads=["inT", ("Wo", wb)], writes=[("pa", pi)])
                    for k in range(8):
                        o_mm(S, pb[pi][:, :], inT[:, 8 + k, t * 128:(t + 1) * 128], Wo[:, wb, 8 + k, :], k == 0, k == 7,
                             reads=["inT", ("Wo", wb)], writes=[("pb", pi)])
                    o_copy(S, "act", tmp[:, pi, :], pa[pi][:, :], reads=[("pa", pi), "ra"], writes=[("tmp", pi)], scale=ra[:, t:t + 1])
                    o_stt(S, tmp[:, pi, :], pb[pi][:, :], rsm[:, t:t + 1], tmp[:, pi, :], ALU.mult, ALU.add,
                          reads=[("pb", pi), ("tmp", pi), "rsm"], writes=[("tmp", pi)])
                    o_stt(S, X[:, t, n * 512:(n + 1) * 512], X[:, t, n * 512:(n + 1) * 512], ALPHA, tmp[:, pi, :], ALU.mult, ALU.add,
                          reads=[xk, ("tmp", pi)], writes=[xk])
                else:
                    for k in range(16):
                        o_mm(S, pa[pi][:, :], inT[:, k, t * 128:(t + 1) * 128], Wo[:, wb, k, :], k == 0, k == 15,
                             reads=["inT", ("Wo", wb)], writes=[("pa", pi)])
                    o_stt(S, X[:, t, n * 512:(n + 1) * 512], X[:, t, n * 512:(n + 1) * 512], ALPHA, pa[pi][:, :], ALU.mult, ALU.add,
                          reads=[xk, ("pa", pi)], writes=[xk])
        for t in range(NT):
            layernorm_tile(S, st, X, t, lng, lnb, stats, mv, rs, ("X", t))
            o_dma(S, "sp", T[xout_name][t * 128:(t + 1) * 128, :], X[:, t, :], reads=[("X", t)], grp="xo%d" % (t % 2))
        S.emit()


def xin_from_inputs(T):
    def f(S, X):
        o_dma(S, "sp", X[:, 0:8, :], T["xw"][3072:4096, :].rearrange("(t p) d -> p t d", p=128),
              writes=[("X", t) for t in range(8)], grp="X")
        o_dma(S, "sp", X[:, 8, :], T["xs"][:, :], writes=[("X", 8)], grp="X8")
    return f


def xin_from_scr(T, name):
    def f(S, X):
        o_dma(S, "sp", X[:, :, :], T[name][:, :].rearrange("(t p) d -> p t d", p=128),
              writes=[("X", t) for t in range(NT)], grp="X")
    return f


def transpose_block(S, X, t, ident, ptr, dstT, dst_key, i0, f32copy=None):
    for b4 in range(4):
        pi = (i0 + b4) % 2
        for q in range(4):
            c = b4 * 4 + q
            o_tr(S, ptr[pi][:, q * 128:(q + 1) * 128], X[:, t, c * 128:(c + 1) * 128], ident[:],
                 reads=[("X", t), "ident"], writes=[("ptr", pi)])
        o_copy(S, "act" if pi == 0 else "dve", dstT[:, b4 * 4:(b4 + 1) * 4, t * 128:(t + 1) * 128],
               ptr[pi][:, :].rearrange("p (a b) -> p a b", a=4), reads=[("ptr", pi)], writes=[dst_key])
        if f32copy is not None:
            o_copy(S, "dve" if pi == 0 else "act", f32copy[:, b4 * 4:(b4 + 1) * 4, :],
                   ptr[pi][:, :].rearrange("p (a b) -> p a b", a=4), reads=[("ptr", pi)], writes=["f32copy"])


def stage_E0(nc, S, T):
    with St(nc, "E0") as st:
        ident = st.sb("ident", [128, 128], F32)
        M = st.sb("M", [128, 2, 2048], F32)
        memT = st.sb("memT", [128, 16, 256], BF16)
        Wb = st.sb("Wb", [128, 2, 16, 512], BF16)
        of = st.sb("of", [128, 2, 512], F32)
        ob = st.sb("ob", [128, 2, 512], BF16)
        okT = st.sb("okT", [128, 2, 256], BF16)
        ptr = [st.ps("ptr%d" % i, [128, 512], F32) for i in range(2)]
        pm = [st.ps("pm%d" % i, [128, 512], F32) for i in range(2)]
        pk = [st.ps("pk%d" % i, [128, 256], F32) for i in range(2)]
        make_ident(S, ident, "ident")
        o_dma(S, "sp", M[:], T["memp"][:, :].rearrange("(t p) d -> p t d", p=128), writes=[("X", 0), ("X", 1)], grp="M")
        for t in range(2):
            transpose_block(S, M, t, ident, ptr, memT, "memT", 0)
        wi = 0
        oi = 0
        for (wname, kind) in (("w_mk", "k"), ("w_mv", "v")):
            for n in range(4):
                wb = wi % 2
                wi += 1
                o_dma(S, "pool", Wb[:, wb, :, :], T[wname][:, n * 512:(n + 1) * 512].rearrange("(c p) n -> p c n", p=128),
                      writes=[("Wb", wb)], grp="Wb%d" % wb)
                for t in range(2):
                    pi = oi % 2
                    oi += 1
                    for k in range(16):
                        o_mm(S, pm[pi][:, :], memT[:, k, t * 128:(t + 1) * 128], Wb[:, wb, k, :], k == 0, k == 15,
                             reads=["memT", ("Wb", wb)], writes=[("pm", pi)])
                    o_copy(S, "act", of[:, pi, :], pm[pi][:, :], reads=[("pm", pi)], writes=[("of", pi)])
                    o_dma(S, "sp", T["memKo" if kind == "k" else "memVo"][t * 128:(t + 1) * 128, n * 512:(n + 1) * 512],
                          of[:, pi, :], reads=[("of", pi)], grp="of%d" % pi)
                    if kind == "v":
                        o_copy(S, "dve", ob[:, pi, :], pm[pi][:, :], reads=[("pm", pi)], writes=[("ob", pi)])
                        o_dma(S, "sp", T["mv_scr"][t * 128:(t + 1) * 128, n * 512:(n + 1) * 512], ob[:, pi, :],
                              reads=[("ob", pi)], grp="ob%d" % pi)
                if kind == "k":
                    for fb in range(4):
                        f = n * 4 + fb
                        pi = f % 2
                        for k in range(16):
                            o_mm(S, pk[pi][:, :], Wb[:, wb, k, fb * 128:(fb + 1) * 128], memT[:, k, :], k == 0, k == 15,
                                 reads=["memT", ("Wb", wb)], writes=[("pk", pi)])
                        o_copy(S, "dve", okT[:, pi, :], pk[pi][:, :], reads=[("pk", pi)], writes=[("okT", pi)])
                        o_dma(S, "sp", T["mkT_scr"][f, :, :], okT[:, pi, :], reads=[("okT", pi)], grp="okT%d" % pi)
        S.emit()


def stage_E1(nc, S, T):
    with St(nc, "E1") as st:
        ident = st.sb("ident", [128, 128], F32)
        X = st.sb("X", [128, NT, 2048], F32)
        xT = st.sb("xT", [128, 16, TOK], BF16)
        Wb = st.sb("Wb", [128, 2, 16, 512], BF16)
        oq = st.sb("oq", [128, 2, 512], BF16)
        ptr = [st.ps("ptr%d" % i, [128, 512], F32) for i in range(2)]
        pq = [st.ps("pq%d" % i, [128, 512], F32) for i in range(4)]
        make_ident(S, ident, "ident")
        xin_from_scr(T, "X1")(S, X)
        for t in range(NT):
            transpose_block(S, X, t, ident, ptr, xT, "xT", 0)
        i = 0
        for n in range(4):
            wb = n % 2
            o_dma(S, "pool", Wb[:, wb, :, :], T["w_mq"][:, n * 512:(n + 1) * 512].rearrange("(c p) n -> p c n", p=128),
                  writes=[("Wb", wb)], grp="Wb%d" % wb)
            for fb in range(4):
                f = n * 4 + fb
                for (c0, w) in [(0, 512), (512, 512), (1024, 128)]:
                    pi = i % 4
                    oi = i % 2
                    i += 1
                    for k in range(16):
                        o_mm(S, pq[pi][:, 0:w], Wb[:, wb, k, fb * 128:(fb + 1) * 128], xT[:, k, c0:c0 + w], k == 0, k == 15,
                             reads=["xT", ("Wb", wb)], writes=[("pq", pi)])
                    o_copy(S, "act" if oi == 0 else "dve", oq[:, oi, 0:w], pq[pi][:, 0:w], reads=[("pq", pi)], writes=[("oq", oi)])
                    o_dma(S, "sp", T["qmT"][f, :, c0:c0 + w], oq[:, oi, 0:w], reads=[("oq", oi)], grp="oq%d" % oi)
        S.emit()


def stage_E2(nc, S, T):
    with St(nc, "E2") as st:
        identf = st.sb("identf", [128, 128], F32)
        onesb = st.sb("onesb", [128, 128], BF16)
        mkT = st.sb("mkT", [128, 16, 256], BF16)
        mv = st.sb("mv", [128, 2, 2048], BF16)
        qm = st.sb("qm", [128, 16, TOK], BF16)
        p = st.sb("p", [128, 2, 512], BF16)
        rd = st.sb("rd", [128, 512], F32)
        om = st.sb("om", [128, 2, 4, 512], BF16)
        ck = st.sb("ck", [128, 2, 2048], F32)
        skT = st.sb("skT", [128, 16, 256], BF16)
        sv = st.sb("sv", [128, 2, 2048], BF16)
        p8 = st.sb("p8", [128, 2, 8], BF16)
        rd8 = st.sb("rd8", [128, 8], F32)
        om8 = st.sb("om8", [128, 16, 128], BF16)
        ps_s = [st.ps("ps_s%d" % i, [128, 512], F32) for i in range(2)]
        ps_o = [st.ps("ps_o%d" % i, [128, 512], F32) for i in range(4)]
        ps_d = st.ps("ps_d", [128, 512], F32)
        ptr = st.ps("ptr", [128, 512], F32)
        make_ident(S, identf, "ident")
        o_memset(S, "dve", onesb[:], 1.0, ["onesb"])
        o_memset(S, "pool", om8[:].rearrange("p a b -> p (a b)"), 0.0, ["om8"])
        o_dma(S, "sp", mkT[:], T["mkT_scr"].rearrange("c p t -> p c t"), writes=["mkT"], grp="mkT")
        o_dma(S, "sp", mv[:], T["mv_scr"][:, :].rearrange("(t p) d -> p t d", p=128), writes=["mv"], grp="mv")
        o_dma(S, "sp", qm[:], T["qmT"].rearrange("c p t -> p c t"), writes=["qm"], grp="qm")
        si = 0
        oi = 0
        for hh in range(4):
            for blk in range(2):
                c0 = blk * 512
                ob_ = oi % 2
                oi += 1
                for kc_ in range(2):
                    sl = si % 2
                    si += 1
                    for j in range(4):
                        o_mm(S, ps_s[sl][:, :], mkT[:, 4 * hh + j, kc_ * 128:(kc_ + 1) * 128], qm[:, 4 * hh + j, c0:c0 + 512], j == 0, j == 3,
                             reads=["mkT", "qm"], writes=[("ps_s", sl)])
                    o_act(S, p[:, sl, :], ps_s[sl][:, :], AF.Exp, reads=[("ps_s", sl)], writes=[("p", sl)], scale=SC_MEM)
                    for j in range(4):
                        o_mm(S, ps_o[j][:, :], mv[:, kc_, (4 * hh + j) * 128:(4 * hh + j + 1) * 128], p[:, sl, :], kc_ == 0, kc_ == 1,
                             reads=["mv", ("p", sl)], writes=[("ps_o", j)])
                    o_mm(S, ps_d[:, :], onesb[:], p[:, sl, :], kc_ == 0, kc_ == 1, reads=["onesb", ("p", sl)], writes=["ps_d"])
                o_recip(S, rd[:], ps_d[:, :], reads=["ps_d"], writes=["rd"])
                for j in range(4):
                    o_tt(S, "dve", om[:, ob_, j, :], ps_o[j][:, :], rd[:], ALU.mult, reads=[("ps_o", j), "rd"], writes=[("om", ob_)])
                o_dma(S, "sp", T["omT"][4 * hh:4 * hh + 4, :, c0:c0 + 512].rearrange("c p t -> p c t"), om[:, ob_, :, :],
                      reads=[("om", ob_)], grp="om%d" % ob_)
        for s in range(4):
            q0 = 1024 + 32 * s
            o_dma(S, "sp", ck[:], T["cmk"][s, :, :].rearrange("(t p) d -> p t d", p=128), writes=[("X", 0), ("X", 1)], grp="ck")
            o_dma(S, "pool", sv[:], T["cmv"][s, :, :].rearrange("(t p) d -> p t d", p=128), writes=["sv"], grp="sv")
            for t in range(2):
                for b4 in range(4):
                    for q in range(4):
                        c = b4 * 4 + q
                        o_tr(S, ptr[:, q * 128:(q + 1) * 128], ck[:, t, c * 128:(c + 1) * 128], identf[:],
                             reads=[("X", t), "ident"], writes=["ptr"])
                    o_copy(S, "act" if b4 % 2 == 0 else "dve", skT[:, b4 * 4:(b4 + 1) * 4, t * 128:(t + 1) * 128],
                           ptr[:, :].rearrange("p (a b) -> p a b", a=4), reads=["ptr"], writes=["skT"])
            for hh in range(4):
                for kc_ in range(2):
                    for j in range(4):
                        o_mm(S, ps_s[0][:, kc_ * 8:(kc_ + 1) * 8], skT[:, 4 * hh + j, kc_ * 128:(kc_ + 1) * 128], qm[:, 4 * hh + j, q0:q0 + 8],
                             j == 0, j == 3, reads=["skT", "qm"], writes=[("ps_s", 0)])
                o_act(S, p8[:].rearrange("p a b -> p (a b)"), ps_s[0][:, 0:16], AF.Exp, reads=[("ps_s", 0)], writes=["p8"], scale=SC_MEM)
                for j in range(4):
                    for kc_ in range(2):
                        o_mm(S, ps_o[j][:, 0:8], sv[:, kc_, (4 * hh + j) * 128:(4 * hh + j + 1) * 128], p8[:, kc_, :], kc_ == 0, kc_ == 1,
                             reads=["sv", "p8"], writes=[("ps_o", j)])
                for kc_ in range(2):
                    o_mm(S, ps_d[:, 0:8], onesb[:], p8[:, kc_, :], kc_ == 0, kc_ == 1, reads=["onesb", "p8"], writes=["ps_d"])
                o_recip(S, rd8[:], ps_d[:, 0:8], reads=["ps_d"], writes=["rd8"])
                for j in range(4):
                    o_tt(S, "dve", om8[:, 4 * hh + j, 32 * s:32 * s + 8], ps_o[j][:, 0:8], rd8[:], ALU.mult,
                         reads=[("ps_o", j), "rd8"], writes=["om8"])
        o_dma(S, "sp", T["omT"][:, :, 1024:TOK].rearrange("c p t -> p c t"), om8[:, :, :], reads=["om8"], grp="om8")
        S.emit()


def stage_F1(nc, S, T):
    with St(nc, "F1") as st:
        ident = st.sb("ident", [128, 128], F32)
        X = st.sb("X", [128, NT, 2048], F32)
        xT = st.sb("xT", [128, 16, TOK], BF16)
        xTf = st.sb("xTf", [128, 16, 128], F32)
        Wr = st.sb("Wr", [128, 16, 36], F32)
        br = st.sb("br", [128, 36], F32)
        lg = st.sb("lg", [128, 36], F32)
        G = st.sb("G", [128, NT, 32], F32)
        gm = st.sb("gm", [128, 1], F32)
        goh = st.sb("goh", [128, 4], F32)
        ge = st.sb("ge", [128, 4], F32)
        gs = st.sb("gs", [128, 1], F32)
        gw = st.sb("gw", [128, 1], F32)
        es = st.sb("es", [128, 8], F32)
        m1 = st.sb("m1", [128, 1], F32)
        oh1 = st.sb("oh1", [128, 8], F32)
        em = st.sb("em", [128, 8], F32)
        m2 = st.sb("m2", [128, 1], F32)
        oh2 = st.sb("oh2", [128, 8], F32)
        dd = st.sb("dd", [128, 1], F32)
        w1 = st.sb("w1", [128, 1], F32)
        w2 = st.sb("w2", [128, 1], F32)
        g8 = st.sb("g8", [128, 8], F32)
        ptr = [st.ps("ptr%d" % i, [128, 512], F32) for i in range(2)]
        pl = st.ps("pl", [128, 64], F32)
        make_ident(S, ident, "ident")
        xin_from_scr(T, "X2")(S, X)
        o_dma(S, "sp", Wr[:], T["wr"][:, :].rearrange("(c p) n -> p c n", p=128), writes=["Wr"], grp="Wr")
        o_dma(S, "sp", br[:], T["br"].partition_broadcast(128), writes=["br"], grp="br")
        R = ["rt"]
        for t in range(NT):
            transpose_block(S, X, t, ident, ptr, xT, "xT", 0, f32copy=xTf)
            for k in range(16):
                o_mm(S, pl[:, 0:36], xTf[:, k, :], Wr[:, k, :], k == 0, k == 15, reads=["f32copy", "Wr"], writes=["pl"])
            o_tt(S, "dve", lg[:], pl[:, 0:36], br[:], ALU.add, reads=["pl", "br"], writes=R)
            S.op("dve", lambda h: h.reduce_max(out=gm[:], in_=lg[:, 0:4], axis=mybir.AxisListType.X), reads=R, writes=R)
            o_ts(S, "dve", goh[:], lg[:, 0:4], gm[:, 0:1], ALU.is_equal, reads=R, writes=R)
            o_ts(S, "dve", ge[:], lg[:, 0:4], gm[:, 0:1], ALU.subtract, reads=R, writes=R)
            o_act(S, ge[:], ge[:], AF.Exp, reads=R, writes=R)
            S.op("dve", lambda h: h.reduce_sum(out=gs[:], in_=ge[:], axis=mybir.AxisListType.X), reads=R, writes=R)
            o_recip(S, gw[:], gs[:], reads=R, writes=R)
            o_ts(S, "dve", es[:], lg[:, 4:12], goh[:, 0:1], ALU.mult, reads=R, writes=R)
            for g in range(1, 4):
                o_stt(S, es[:], lg[:, 4 + 8 * g:12 + 8 * g], goh[:, g:g + 1], es[:], ALU.mult, ALU.add, reads=R, writes=R)
            S.op("dve", lambda h: h.reduce_max(out=m1[:], in_=es[:], axis=mybir.AxisListType.X), reads=R, writes=R)
            o_ts(S, "dve", oh1[:], es[:], m1[:, 0:1], ALU.is_equal, reads=R, writes=R)
            o_stt(S, em[:], oh1[:], -1e30, es[:], ALU.mult, ALU.add, reads=R, writes=R)
            S.op("dve", lambda h: h.reduce_max(out=m2[:], in_=em[:], axis=mybir.AxisListType.X), reads=R, writes=R)
            o_ts(S, "dve", oh2[:], em[:], m2[:, 0:1], ALU.is_equal, reads=R, writes=R)
            o_tt(S, "dve", dd[:], m2[:], m1[:], ALU.subtract, reads=R, writes=R)
            o_act(S, dd[:], dd[:], AF.Exp, reads=R, writes=R)
            o_ts(S, "dve", w1[:], dd[:], 1.0, ALU.add, reads=R, writes=R)
            o_recip(S, w1[:], w1[:], reads=R, writes=R)
            o_tt(S, "dve", w1[:], w1[:], gw[:], ALU.mult, reads=R, writes=R)
            o_tt(S, "dve", w2[:], w1[:], dd[:], ALU.mult, reads=R, writes=R)
            o_ts(S, "dve", g8[:], oh1[:], w1[:, 0:1], ALU.mult, reads=R, writes=R)
            o_stt(S, g8[:], oh2[:], w2[:, 0:1], g8[:], ALU.mult, ALU.add, reads=R, writes=R)
            for g in range(4):
                o_ts(S, "dve", G[:, t, 8 * g:8 * g + 8], g8[:], goh[:, g:g + 1], ALU.mult, reads=R, writes=["G"])
            o_copy(S, "act", X[:, t, :], X[:, t, :], reads=[("X", t)], writes=[("X", t)], scale=ALPHA)
            o_dma(S, "sp", T["X3"][t * 128:(t + 1) * 128, :], X[:, t, :], reads=[("X", t)], grp="xo%d" % (t % 2))
        o_dma(S, "sp", T["x2T"].rearrange("c p t -> p c t"), xT[:, :, :], reads=["xT"], grp="xTst")
        o_dma(S, "sp", T["G"][:, :], G[:].rearrange("p a b -> p (a b)"), reads=["G"], grp="Gst")
        S.emit()


def stage_F2(nc, S, T):
    with St(nc, "F2") as st:
        X = st.sb("X", [128, NT, 2048], F32)
        xT = st.sb("xT", [128, 16, TOK], BF16)
        G = st.sb("G", [128, NT, 32], F32)
        Wg = st.sb("Wg", [128, 16, 512], BF16)
        Wu = st.sb("Wu", [128, 16, 512], BF16)
        Wd = st.sb("Wd", [128, 4, 2048], BF16)
        hT = st.sb("hT", [128, 4, TOK], BF16)
        sg = st.sb("sg", [128, 2, 512], F32)
        pg = [st.ps("pg%d" % i, [128, 512], F32) for i in range(2)]
        pu = [st.ps("pu%d" % i, [128, 512], F32) for i in range(2)]
        po = [st.ps("po%d" % i, [128, 512], F32) for i in range(4)]
        xin_from_scr(T, "X3")(S, X)
        o_dma(S, "sp", xT[:], T["x2T"].rearrange("c p t -> p c t"), writes=["xT"], grp="xT")
        o_dma(S, "sp", G[:].rearrange("p a b -> p (a b)"), T["G"][:, :], writes=["G"], grp="G")
        i = 0
        oi = 0
        for e in range(32):
            o_dma(S, "pool", Wg[:], T["w_gate"][e, :, :].rearrange("(c p) n -> p c n", p=128), writes=["Wg"], grp="Wg")
            o_dma(S, "pool", Wu[:], T["w_up"][e, :, :].rearrange("(c p) n -> p c n", p=128), writes=["Wu"], grp="Wu")
            o_dma(S, "pool", Wd[:], T["w_down"][e, :, :].rearrange("(c p) n -> p c n", p=128), writes=["Wd"], grp="Wd")
            for f in range(4):
                for (c0, w) in [(0, 512), (512, 512), (1024, 128)]:
                    pi = i % 2
                    i += 1
                    for k in range(16):
                        o_mm(S, pg[pi][:, 0:w], Wg[:, k, f * 128:(f + 1) * 128], xT[:, k, c0:c0 + w], k == 0, k == 15,
                             reads=["Wg", "xT"], writes=[("pg", pi)])
                    for k in range(16):
                        o_mm(S, pu[pi][:, 0:w], Wu[:, k, f * 128:(f + 1) * 128], xT[:, k, c0:c0 + w], k == 0, k == 15,
                             reads=["Wu", "xT"], writes=[("pu", pi)])
                    o_act(S, sg[:, pi, 0:w], pg[pi][:, 0:w], AF.Silu, reads=[("pg", pi)], writes=[("sg", pi)])
                    o_tt(S, "dve", hT[:, f, c0:c0 + w], sg[:, pi, 0:w], pu[pi][:, 0:w], ALU.mult,
                         reads=[("sg", pi), ("pu", pi)], writes=[("hT", f)])
            for t in range(NT):
                for n in range(4):
                    pi = oi % 4
                    oi += 1
                    for f in range(4):
                        o_mm(S, po[pi][:, :], hT[:, f, t * 128:(t + 1) * 128], Wd[:, f, n * 512:(n + 1) * 512], f == 0, f == 3,
                             reads=[("hT", f), "Wd"], writes=[("po", pi)])
                    o_stt(S, X[:, t, n * 512:(n + 1) * 512], po[pi][:, :], G[:, t, e:e + 1], X[:, t, n * 512:(n + 1) * 512],
                          ALU.mult, ALU.add, reads=[("po", pi), "G", ("X", t)], writes=[("X", t)])
        for t in range(NT):
            o_dma(S, "sp", T["X1"][t * 128:(t + 1) * 128, :], X[:, t, :], reads=[("X", t)], grp="xo%d" % (t % 2))
        S.emit()


def stage_F3(nc, S, T):
    with St(nc, "F3") as st:
        X = st.sb("X", [128, NT, 2048], F32)
        lng = st.sb("lng", [128, 2048], F32)
        lnb = st.sb("lnb", [128, 2048], F32)
        stats = st.sb("stats", [128, 4, 6], F32)
        mv = st.sb("mv", [128, 2], F32)
        rs = st.sb("rs", [128, 1], F32)
        xin_from_scr(T, "X1")(S, X)
        o_dma(S, "sp", lng[:], T["ln3_g"].partition_broadcast(128), writes=["lng"], grp="lng")
        o_dma(S, "sp", lnb[:], T["ln3_b"].partition_broadcast(128), writes=["lnb"], grp="lnb")
        for t in range(NT):
            layernorm_tile(S, st, X, t, lng, lnb, stats, mv, rs, ("X", t))
            o_dma(S, "sp", T["y"][t * 128:(t + 1) * 128, :], X[:, t, :], reads=[("X", t)], grp="xo%d" % (t % 2))
        S.emit()


IN_SPECS = [
    ("xw", [4096, 2048]), ("xs", [128, 2048]), ("vbias", [128, 24]), ("pmask", [128, 17 * 128]),
    ("smask", [128, 128]), ("smaskn", [128, 128]), ("cwk", [4, 2048, 1024]), ("cwv", [4, 2048, 1024]),
    ("h0re", [128, 128]), ("h0im", [128, 128]), ("cmk", [4, 256, 2048]), ("cmv", [4, 256, 2048]),
    ("memp", [256, 2048]), ("w_in", [2048, 4096]), ("lamre", [128, 32]), ("lamim", [128, 32]),
    ("logdt", [128, 32]), ("BTre", [128, 4096]), ("BTim", [128, 4096]), ("CTre", [128, 4096]),
    ("CTim", [128, 4096]), ("dcol", [128, 8]), ("w_glu", [1024, 1024]), ("gatt", [128, 8]), ("gssm", [128, 8]),
    ("w_out", [2048, 2048]), ("ln1_g", [1, 2048]), ("ln1_b", [1, 2048]), ("w_mq", [2048, 2048]),
    ("w_mk", [2048, 2048]), ("w_mv", [2048, 2048]), ("w_mo", [2048, 2048]), ("ln2_g", [1, 2048]),
    ("ln2_b", [1, 2048]), ("wr", [2048, 36]), ("br", [1, 36]), ("w_gate", [32, 2048, 512]),
    ("w_up", [32, 2048, 512]), ("w_down", [32, 512, 2048]), ("ln3_g", [1, 2048]), ("ln3_b", [1, 2048]),
]
OUT_SPECS = [
    ("y", [TOK, 2048]), ("Kout", [TOK, 1024]), ("Vout", [TOK, 1024]), ("hfr", [128, 32]), ("hfi", [128, 32]),
    ("hsr", [128, 128]), ("hsi", [128, 128]), ("memKo", [256, 2048]), ("memVo", [256, 2048]),
]
SCR_SPECS = [
    ("qT", [8, 128, TOK], BF16), ("kT", [8, 128, KPOS], BF16), ("V_scr", [KPOS, 1024], BF16), ("uT", [8, 128, UPOS], BF16),
    ("mixT", [16, 128, TOK], BF16), ("rstd_a", [128, NT], F32), ("rstd_s", [128, NT], F32),
    ("ygT", [8, 128, TOK], F32), ("ygTb", [8, 128, TOK], BF16), ("X1", [TOK, 2048], F32), ("X2", [TOK, 2048], F32),
    ("X3", [TOK, 2048], F32), ("mkT_scr", [16, 128, 256], BF16), ("mv_scr", [256, 2048], BF16),
    ("qmT", [16, 128, TOK], BF16), ("omT", [16, 128, TOK], BF16), ("x2T", [16, 128, TOK], BF16), ("G", [128, NT * 32], F32),
]


def build_program(stages=None, debug=False):
    nc = bass.Bass("TRN2", target_bir_lowering=False)
    T = {}
    for (n, s) in IN_SPECS:
        T[n] = nc.dram_tensor(n, s, F32, kind="ExternalInput").ap()
    for (n, s) in OUT_SPECS:
        T[n] = nc.dram_tensor(n, s, F32, kind="ExternalOutput").ap()
    for (n, s, d) in SCR_SPECS:
        T[n] = nc.dram_tensor("scr_" + n, s, d, kind=("ExternalOutput" if debug else "Internal")).ap()
    S = Sched(nc)
    table = [
        ("A", lambda: stage_A(nc, S, T)),
        ("B", lambda: stage_B(nc, S, T)),
        ("C", lambda: stage_C(nc, S, T)),
        ("D1", lambda: stage_D1(nc, S, T)),
        ("D2", lambda: linear_residual_ln(nc, S, T, "D2", "mixT", "w_out", "ln1_g", "ln1_b", xin_from_inputs(T), "X1", True)),
        ("E0", lambda: stage_E0(nc, S, T)),
        ("E1", lambda: stage_E1(nc, S, T)),
        ("E2", lambda: stage_E2(nc, S, T)),
        ("E3", lambda: linear_residual_ln(nc, S, T, "E3", "omT", "w_mo", "ln2_g", "ln2_b", xin_from_scr(T, "X1"), "X2", False)),
        ("F1", lambda: stage_F1(nc, S, T)),
        ("F2", lambda: stage_F2(nc, S, T)),
        ("F3", lambda: stage_F3(nc, S, T)),
    ]
    for (nm, fn) in table:
        if stages is None or nm in stages:
            fn()
    S.close()
    return nc


def _mult(diff):
    m = ((diff >= 0) & (diff <= 128)).astype(np.float32)
    m += ((diff >= 0) & (diff <= 512) & (diff % 4 == 0)).astype(np.float32)
    m += ((diff >= 0) & (diff <= 2048) & (diff % 16 == 0)).astype(np.float32)
    return m


def make_inputs(inp):
    f = np.float32
    xp = inp["x_prompt"]
    xsm = inp["x_sample"]
    j = np.arange(128)
    pm = np.zeros((128, 17, 128), f)
    for cc in range(17):
        diff = (16 - cc) * 128 + j[None, :] - j[:, None]
        pm[:, cc, :] = _mult(diff)
    sm = np.zeros((128, 16, 8), f)
    for cc in range(16):
        cpos = cc * 128 + j[:, None]
        diff = np.arange(8)[None, :] + 2048 - cpos
        sm[:, cc, :] = _mult(diff)
    smn = np.zeros((128, 128), f)
    d8 = np.arange(8)[None, :] - np.arange(8)[:, None]
    for s in range(4):
        smn[32 * s:32 * s + 8, 32 * s:32 * s + 8] = _mult(d8)
    br_, bi_ = inp["ssm_b_re"][0], inp["ssm_b_im"][0]
    cr_, ci_ = inp["ssm_c_re"][0], inp["ssm_c_im"][0]
    BTre = np.zeros((128, 32, 128), f)
    BTim = np.zeros((128, 32, 128), f)
    CTre = np.zeros((128, 32, 128), f)
    CTim = np.zeros((128, 32, 128), f)
    for tau in range(32):
        for gl in range(2):
            g = 2 * tau + gl
            r0 = 32 * (tau % 4) + 16 * gl
            BTre[r0:r0 + 16, tau, 64 * gl:64 * gl + 64] = br_[g].T
            BTim[r0:r0 + 16, tau, 64 * gl:64 * gl + 64] = bi_[g].T
            CTre[64 * gl:64 * gl + 64, tau, r0:r0 + 16] = cr_[g].T
            CTim[64 * gl:64 * gl + 64, tau, r0:r0 + 16] = ci_[g].T
    common = {
        "pmask": pm.reshape(128, -1), "smask": sm.reshape(128, -1), "smaskn": smn,
        "w_in": inp["w_in"][0],
        "lamre": np.ascontiguousarray(inp["ssm_lam_re"][0].reshape(32, 128).T),
        "lamim": np.ascontiguousarray(inp["ssm_lam_im"][0].reshape(32, 128).T),
        "logdt": np.ascontiguousarray(np.repeat(inp["ssm_log_dt"][0].reshape(32, 2), 64, axis=1).T),
        "BTre": BTre.reshape(128, -1), "BTim": BTim.reshape(128, -1), "CTre": CTre.reshape(128, -1), "CTim": CTim.reshape(128, -1),
        "dcol": np.ascontiguousarray(inp["ssm_d"][0].reshape(8, 128).T),
        "w_glu": inp["w_glu"][0],
        "gatt": np.ascontiguousarray(inp["g_attn"][0].reshape(8, 128).T),
        "gssm": np.ascontiguousarray(inp["g_ssm"][0].reshape(8, 128).T),
        "w_out": inp["w_out"][0], "ln1_g": inp["ln1_g"], "ln1_b": inp["ln1_b"],
        "w_mq": inp["w_mq"][0], "w_mk": inp["w_mk"][0], "w_mv": inp["w_mv"][0], "w_mo": inp["w_mo"][0],
        "ln2_g": inp["ln2_g"], "ln2_b": inp["ln2_b"],
        "wr": np.ascontiguousarray(np.concatenate([inp["w_r1"][0], inp["w_r2"][0].reshape(2048, 32)], axis=1)),
        "br": np.ascontiguousarray(np.concatenate([inp["b_r1"][0], inp["b_r2"][0].reshape(32)])[None, :]),
        "w_gate": inp["w_gate"][0], "w_up": inp["w_up"][0], "w_down": inp["w_down"][0],
        "ln3_g": inp["ln3_g"], "ln3_b": inp["ln3_b"],
    }
    maps = []
    for c in range(8):
        b, r = c // 4, c % 4
        xw = np.zeros((4096, 2048), f)
        lo = 1024 * r - 3072
        src0 = max(lo, 0)
        xw[src0 - lo:, :] = xp[b, src0:1024 * r + 1024, :]
        xs = np.zeros((128, 2048), f)
        for s in range(4):
            xs[32 * s:32 * s + 8, :] = xsm[4 * c + s]
        vb = np.ones((128, 24), f)
        for jj in range(16):
            if 8 * r - 16 + jj < 0:
                vb[:, jj] = 0.0
        m = dict(common)
        m.update({
            "xw": xw, "xs": xs, "vbias": vb,
            "cwk": np.ascontiguousarray(inp["cache_win_k"][0, 4 * c:4 * c + 4].reshape(4, 2048, 1024)),
            "cwv": np.ascontiguousarray(inp["cache_win_v"][0, 4 * c:4 * c + 4].reshape(4, 2048, 1024)),
            "h0re": np.ascontiguousarray(inp["state_ssm_re"][0, 4 * c:4 * c + 4].reshape(4, 32, 128).transpose(2, 1, 0).reshape(128, 128)),
            "h0im": np.ascontiguousarray(inp["state_ssm_im"][0, 4 * c:4 * c + 4].reshape(4, 32, 128).transpose(2, 1, 0).reshape(128, 128)),
            "cmk": np.ascontiguousarray(inp["cache_mem_k"][0, 4 * c:4 * c + 4].reshape(4, 256, 2048)),
            "cmv": np.ascontiguousarray(inp["cache_mem_v"][0, 4 * c:4 * c + 4].reshape(4, 256, 2048)),
            "memp": np.ascontiguousarray(inp["mem_prompt"][b]),
        })
        maps.append({k: np.ascontiguousarray(v, dtype=f) for k, v in m.items()})
    return maps


def assemble(res):
    f = np.float32
    yp = np.zeros((2, 4096, 2048), f)
    ys = np.zeros((32, 8, 2048), f)
    wkp = np.zeros((1, 2, 2048, 8, 128), f)
    wvp = np.zeros((1, 2, 2048, 8, 128), f)
    wks = np.zeros((1, 32, 8, 8, 128), f)
    wvs = np.zeros((1, 32, 8, 8, 128), f)
    srp = np.zeros((1, 2, 64, 64), f)
    sip = np.zeros((1, 2, 64, 64), f)
    srs = np.zeros((1, 32, 64, 64), f)
    sis = np.zeros((1, 32, 64, 64), f)
    mkp = np.zeros((1, 2, 256, 4, 512), f)
    mvp = np.zeros((1, 2, 256, 4, 512), f)
    for c in range(8):
        r_ = res[c]
        b, r = c // 4, c % 4
        yp[b, 1024 * r:1024 * r + 1024] = r_["y"][0:1024]
        for s in range(4):
            ys[4 * c + s] = r_["y"][1024 + 32 * s:1024 + 32 * s + 8]
            wks[0, 4 * c + s] = r_["Kout"][1024 + 32 * s:1024 + 32 * s + 8].reshape(8, 8, 128)
            wvs[0, 4 * c + s] = r_["Vout"][1024 + 32 * s:1024 + 32 * s + 8].reshape(8, 8, 128)
        if r >= 2:
            wkp[0, b, 1024 * (r - 2):1024 * (r - 1)] = r_["Kout"][0:1024].reshape(1024, 8, 128)
            wvp[0, b, 1024 * (r - 2):1024 * (r - 1)] = r_["Vout"][0:1024].reshape(1024, 8, 128)
        if r == 3:
            srp[0, b] = r_["hfr"].T.reshape(64, 64)
            sip[0, b] = r_["hfi"].T.reshape(64, 64)
        hs_r = r_["hsr"].reshape(128, 32, 4).transpose(2, 1, 0).reshape(4, 64, 64)
        hs_i = r_["hsi"].reshape(128, 32, 4).transpose(2, 1, 0).reshape(4, 64, 64)
        srs[0, 4 * c:4 * c + 4] = hs_r
        sis[0, 4 * c:4 * c + 4] = hs_i
        if r == 0:
            mkp[0, b] = r_["memKo"].reshape(256, 4, 512)
            mvp[0, b] = r_["memVo"].reshape(256, 4, 512)
    return (yp, ys, wkp, wvp, wks, wvs, srp, sip, srs, sis, mkp, mvp)


def kernel(**inputs):
    inp = {k: np.asarray(v) for k, v in inputs.items()}
    maps = make_inputs(inp)
    nc = build_program()
    res = run_bass_kernel_spmd(nc, maps, core_ids=list(range(8)))
    return assemble(res.results)
```

```python
import numpy as np
from contextlib import ExitStack
import concourse.bass as bass
import concourse.mybir as mybir
from concourse.bass_utils import run_bass_kernel_spmd

F32 = mybir.dt.float32
BF16 = mybir.dt.bfloat16
AF = mybir.ActivationFunctionType
ALU = mybir.AluOpType

ENGS = ("pe", "act", "dve", "pool", "sp")
NT = 9
TOK = 1152
KPOS = 3200
UPOS = 4224
ALPHA = 2.0 ** 0.25
SC_ATT = 128 ** -0.5
SC_MEM = 512 ** -0.5
NEG = -30000.0
DBG = {}
PSUM_KEYS = {"ps_big", "ps_n", "ptr", "pmm", "ps_s", "ps_nd", "ptk", "ps_sm", "ps_snd", "ps_ss", "ps_new", "pw", "py", "pz", "pa", "pb",
             "pm", "pk", "pq", "ps_o", "ps_d", "pl", "pg", "pu", "po"}


class Sched:
    def __init__(self, nc, n_dma_sems=22):
        self.nc = nc
        self.stack = []
        self.esem = {}
        for e in ENGS:
            self.esem[e] = self._sem("e_" + e)
        self.ecount = {e: 0 for e in ENGS}
        self.sval = {e: 0 for e in ENGS}
        self.dsems = [self._sem("d%d" % i) for i in range(n_dma_sems)]
        self.dcount = [0] * n_dma_sems
        self.rr = {}
        self.reset_stage()

    def _sem(self, name):
        cm = self.nc.semaphore(name)
        s = cm.__enter__()
        self.stack.append(cm)
        return s

    def reset_stage(self):
        self.ops = []
        self.dmap = {}
        self.lastw = {}
        self.readers = {}
        self.known = {e: {} for e in ENGS}

    def alt(self, name, engines):
        i = self.rr.get(name, 0)
        self.rr[name] = i + 1
        return engines[i % len(engines)]

    def _dsem(self, group):
        if group not in self.dmap:
            idx = len(self.dmap)
            assert idx < len(self.dsems), "too many dma groups in stage"
            self.dmap[group] = idx
        return self.dmap[group]

    def _need(self, eng, dep, waits):
        kind, a, b = dep
        kn = self.known[eng]
        key = (kind, a)
        if kn.get(key, -1) >= b:
            return
        kn[key] = b
        waits.append(dep)

    def op(self, eng, fn, reads=(), writes=(), dma=None):
        waits = []
        deps = []
        if eng != "pe":
            ex = [k for k in reads if (k[0] if isinstance(k, tuple) else k) in PSUM_KEYS]
            if ex:
                writes = list(writes) + [k for k in ex if k not in writes]
        for k in reads:
            if k in self.lastw:
                deps.append(self.lastw[k])
        for k in writes:
            if k in self.lastw:
                deps.append(self.lastw[k])
            deps.extend(self.readers.get(k, ()))
        if dma is not None:
            di = self._dsem(dma)
            if self.dcount[di] > 0:
                deps.append(("d", di, self.dcount[di]))
        for d in deps:
            if d[0] == "e" and d[1] == eng and eng == "pe":
                continue
            self._need(eng, d, waits)
        if dma is not None:
            self.dcount[di] += 16
            tok = ("d", di, self.dcount[di])
        else:
            self.ecount[eng] += 1
            tok = ("e", eng, self.ecount[eng])
        for k in writes:
            self.lastw[k] = tok
            self.readers[k] = []
        for k in reads:
            self.readers.setdefault(k, []).append(tok)
        self.ops.append((eng, fn, waits, tok))

    def touch(self, keys, from_keys):
        best = None
        for k in from_keys:
            t = self.lastw.get(k)
            if t is not None and (best is None or t[2] > best[2]):
                best = t
        if best is not None:
            for k in keys:
                self.lastw[k] = best
                self.readers[k] = []

    def emit(self):
        nc = self.nc
        fin = [("d", di, self.dcount[di]) for g, di in self.dmap.items()]
        per = {e: [] for e in ENGS}
        need = set()
        for o in self.ops:
            per[o[0]].append(o)
            for (kind, a, b) in o[2]:
                if kind == "e":
                    need.add((a, b))
        sval = self.sval
        smap = {}
        for (eng, fn, waits, tok) in self.ops:
            if tok[0] == "e" and (tok[1], tok[2]) in need:
                sval[eng] += 1
                smap[(tok[1], tok[2])] = sval[eng]
        esem, dsems = self.esem, self.dsems

        def run(engname, h):
            for (_, fn, waits, tok) in per[engname]:
                for (kind, a, b) in waits:
                    if kind == "e":
                        h.wait_ge(esem[a], smap[(a, b)])
                    else:
                        h.wait_ge(dsems[a], b)
                ins = fn(h)
                if tok[0] == "e":
                    if (tok[1], tok[2]) in smap:
                        ins.then_inc(esem[engname], 1)
                else:
                    ins.then_inc(dsems[tok[1]], 16)
            if engname == "sp":
                for (kind, a, b) in fin:
                    h.wait_ge(dsems[a], b)

        with nc.Block() as block:
            @block.tensor
            def _(h):
                run("pe", h)

            @block.scalar
            def _(h):
                run("act", h)

            @block.vector
            def _(h):
                run("dve", h)

            @block.gpsimd
            def _(h):
                run("pool", h)

            @block.sync
            def _(h):
                run("sp", h)
        self.reset_stage()

    def close(self):
        for cm in reversed(self.stack):
            cm.__exit__(None, None, None)


def o_mm(S, out, lhsT, rhs, start, stop, reads, writes):
    S.op("pe", lambda h: h.matmul(out, lhsT=lhsT, rhs=rhs, start=start, stop=stop), reads=reads, writes=writes)


def o_tr(S, out, in_, ident, reads, writes):
    S.op("pe", lambda h: h.transpose(out=out, in_=in_, identity=ident), reads=reads, writes=writes)


def o_dma(S, eng, out, in_, reads=(), writes=(), grp=None, slow=False):
    if slow:
        S.op(eng, lambda h: h.dma_start(out=out, in_=in_, allow_slow_non_contiguous=True), reads=reads, writes=writes, dma=grp)
    else:
        S.op(eng, lambda h: h.dma_start(out=out, in_=in_), reads=reads, writes=writes, dma=grp)


def o_copy(S, eng, out, in_, reads, writes, scale=None):
    if eng == "act":
        if scale is None:
            S.op("act", lambda h: h.activation(out=out, in_=in_, func=AF.Copy), reads=reads, writes=writes)
        else:
            S.op("act", lambda h: h.activation(out=out, in_=in_, func=AF.Identity, scale=scale), reads=reads, writes=writes)
    else:
        if scale is None:
            S.op(eng, lambda h: h.tensor_copy(out=out, in_=in_), reads=reads, writes=writes)
        else:
            S.op(eng, lambda h: h.tensor_scalar(out=out, in0=in_, scalar1=scale, scalar2=None, op0=ALU.mult), reads=reads, writes=writes)


def o_act(S, out, in_, func, reads, writes, bias=None, scale=None):
    kw = {}
    if bias is not None:
        kw["bias"] = bias
    if scale is not None:
        kw["scale"] = scale
    S.op("act", lambda h: h.activation(out=out, in_=in_, func=func, **kw), reads=reads, writes=writes)


def o_tt(S, eng, out, in0, in1, op, reads, writes):
    S.op(eng, lambda h: h.tensor_tensor(out=out, in0=in0, in1=in1, op=op), reads=reads, writes=writes)


def o_ts(S, eng, out, in0, s1, op0, reads, writes, s2=None, op1=None):
    if op1 is None:
        S.op(eng, lambda h: h.tensor_scalar(out=out, in0=in0, scalar1=s1, scalar2=None, op0=op0), reads=reads, writes=writes)
    else:
        S.op(eng, lambda h: h.tensor_scalar(out=out, in0=in0, scalar1=s1, scalar2=s2, op0=op0, op1=op1), reads=reads, writes=writes)


def o_stt(S, out, in0, scalar, in1, op0, op1, reads, writes):
    S.op("dve", lambda h: h.scalar_tensor_tensor(out=out, in0=in0, scalar=scalar, in1=in1, op0=op0, op1=op1), reads=reads, writes=writes)


def o_memset(S, eng, ap, val, writes):
    S.op(eng, lambda h: h.memset(ap, val), writes=writes)


def o_recip(S, out, in_, reads, writes):
    S.op("dve", lambda h: h.reciprocal(out=out, in_=in_), reads=reads, writes=writes)


def make_ident(S, t, key):
    o_memset(S, "pool", t[:], 0.0, [key])
    S.op("pool", lambda h: h.affine_select(out=t[:], in_=t[:], pattern=[[-1, 128]], compare_op=ALU.not_equal,
                                           fill=1.0, base=0, channel_multiplier=1), reads=[key], writes=[key])


class St:
    def __init__(self, nc, name):
        self.nc = nc
        self.name = name
        self.es = ExitStack()

    def __enter__(self):
        self.es.__enter__()
        return self

    def __exit__(self, *a):
        return self.es.__exit__(*a)

    def sb(self, n, shape, dt):
        return self.es.enter_context(self.nc.sbuf_tensor(self.name + "_" + n, shape, dt))

    def ps(self, n, shape, dt):
        return self.es.enter_context(self.nc.psum_tensor(self.name + "_" + n, shape, dt))


def stage_A(nc, S, T):
    with St(nc, "A") as st:
        Wb = st.sb("Wb", [128, 16, 4096], BF16)
        ident = st.sb("ident", [128, 128], F32)
        xin = st.sb("xin", [128, 2, 2048], F32)
        xT = st.sb("xT", [128, 16, 512], BF16)
        ofm = st.sb("ofm", [128, 4, 512], BF16)
        otf = st.sb("otf", [128, 2, 512], F32)
        otb = st.sb("otb", [128, 2, 512], BF16)
        ptr = [st.ps("ptr%d" % i, [128, 512], F32) for i in range(2)]
        pmm = [st.ps("pmm%d" % i, [128, 512], F32) for i in range(4)]
        make_ident(S, ident, "ident")
        w_in = T["w_in"]
        for blk in (6, 7, 2, 3, 4, 5, 0, 1):
            o_dma(S, "pool", Wb[:, :, blk * 512:(blk + 1) * 512],
                  w_in[:, blk * 512:(blk + 1) * 512].rearrange("(c p) n -> p c n", p=128),
                  writes=[("Wb", blk)], grp="Wb%d" % blk)
        groups = [[4 * g + j for j in range(4)] for g in range(8)] + [[32]]
        if DBG.get("A_groups") is not None:
            groups = [groups[i] for i in DBG["A_groups"]]
        cnt = {"x": 0, "tr": 0, "fm": 0, "mm": 0, "tf": 0, "tb": 0}
        for tiles in groups:
            ntok = 128 * len(tiles)
            t0 = tiles[0]
            far = t0 < 8
            near = 8 <= t0 < 24
            own = t0 >= 24
            for j, tl in enumerate(tiles):
                slot = cnt["x"] % 2
                cnt["x"] += 1
                src = T["xs"][:, :] if tl == 32 else T["xw"][tl * 128:(tl + 1) * 128, :]
                o_dma(S, "sp", xin[:, slot, :], src, writes=[("xin", slot)], grp="xin%d" % slot)
                for b4 in range(4):
                    pi = cnt["tr"] % 2
                    cnt["tr"] += 1
                    for q in range(4):
                        c = b4 * 4 + q
                        o_tr(S, ptr[pi][:, q * 128:(q + 1) * 128], xin[:, slot, c * 128:(c + 1) * 128], ident[:],
                             reads=[("xin", slot), "ident"] + ([("Wb", i) for i in range(8)] if DBG.get("A_waitW") else []), writes=[("ptr", pi)])
                    o_copy(S, "act" if pi == 0 else "dve", xT[:, b4 * 4:(b4 + 1) * 4, j * 128:(j + 1) * 128],
                           ptr[pi][:, :].rearrange("p (a b) -> p a b", a=4), reads=[("ptr", pi)], writes=["xT"])
            if t0 == 32:
                qcol, kcol, ucol = 1024, 3072, 4096
            else:
                qcol, kcol, ucol = (t0 - 24) * 128, (t0 - 8) * 128, t0 * 128
            fm = []
            if own:
                fm += [(h * 128, T["qT"][h, :, qcol:qcol + ntok]) for h in range(8)]
            if own or near:
                fm += [(1024 + h * 128, T["kT"][h, :, kcol:kcol + ntok]) for h in range(8)]
            fm += [(3072 + c * 128, T["uT"][c, :, ucol:ucol + ntok]) for c in range(8)]
            if DBG.get("A_nofm"):
                fm = []
            if DBG.get("A_fmn") is not None:
                fm = fm[:DBG["A_fmn"]]
            for (wc, dst) in fm:
                pi = cnt["mm"] % 4
                cnt["mm"] += 1
                for k in range(16):
                    o_mm(S, pmm[pi][:, 0:ntok], Wb[:, k, wc:wc + 128], xT[:, k, 0:ntok], k == 0, k == 15,
                         reads=["xT", ("Wb", wc // 512)], writes=[("pmm", pi)])
                oi = cnt["fm"] % 4
                cnt["fm"] += 1
                o_copy(S, "act" if oi % 2 == 0 else "dve", ofm[:, oi, 0:ntok], pmm[pi][:, 0:ntok],
                       reads=[("pmm", pi)], writes=[("ofm", oi)])
                o_dma(S, DBG.get("A_stq", "sp"), dst, ofm[:, oi, 0:ntok], reads=[("ofm", oi)], grp="ofm%d" % oi)
            if (own or near) and not DBG.get("A_notm"):
                for j, tl in enumerate(tiles):
                    row_kv = (tl - 8) * 128 if tl < 32 else 3072
                    row_o = (tl - 24) * 128 if tl < 32 else 1024
                    banks = []
                    if own:
                        banks += [("k", 1024), ("k", 1536)]
                    banks += [("v", 2048), ("v", 2560)]
                    if DBG.get("A_banks") is not None:
                        banks = [banks[i] for i in DBG["A_banks"]]
                    for (kv, wc) in banks:
                        pi = cnt["mm"] % 4
                        cnt["mm"] += 1
                        for k in range(16):
                            o_mm(S, pmm[pi][:, :], xT[:, k, j * 128:(j + 1) * 128], Wb[:, k, wc:wc + 512], k == 0, k == 15,
                                 reads=["xT", ("Wb", wc // 512)], writes=[("pmm", pi)])
                        colo = wc - (1024 if kv == "k" else 2048)
                        if own and not DBG.get("A_nootf"):
                            fi = cnt["tf"] % 2
                            cnt["tf"] += 1
                            o_copy(S, DBG.get("A_otfe", "act"), otf[:, fi, :], pmm[pi][:, :], reads=[("pmm", pi)], writes=[("otf", fi)])
                            o_dma(S, DBG.get("A_stq", "sp"), T["Kout" if kv == "k" else "Vout"][row_o:row_o + 128, colo:colo + 512],
                                  otf[:, fi, :], reads=[("otf", fi)], grp="otf%d" % fi)
                        if kv == "v" and not DBG.get("A_nootb"):
                            bi = cnt["tb"] % 2
                            cnt["tb"] += 1
                            o_copy(S, DBG.get("A_otbe", "dve"), otb[:, bi, :], pmm[pi][:, :], reads=[("pmm", pi)] + ([("otf", 0), ("otf", 1)] if DBG.get("A_ser") else []), writes=[("otb", bi)])
                            o_dma(S, DBG.get("A_stq", "sp"), T["V_scr"][row_kv:row_kv + 128, colo:colo + 512], otb[:, bi, :],
                                  reads=[("otb", bi)], grp="otb%d" % bi)
        S.emit()


def stage_B(nc, S, T):
    with St(nc, "B") as st:
        kT = st.sb("kT", [128, 2, KPOS], BF16)
        Vh = st.sb("Vh", [128, 2, 25, 128], BF16)
        qT = st.sb("qT", [128, 2, TOK], BF16)
        pmask = st.sb("pmask", [128, 17, 128], BF16)
        smask = st.sb("smask", [128, 128], BF16)
        smaskn = st.sb("smaskn", [128, 128], BF16)
        vb = st.sb("vb", [128, 24], F32)
        gatt = st.sb("gatt", [128, 8], F32)
        onesb = st.sb("onesb", [128, 128], BF16)
        identb = st.sb("identb", [128, 128], BF16)
        p = st.sb("p", [128, 2, 512], BF16)
        pm = st.sb("pm", [128, 2, 512], BF16)
        onesv = st.sb("onesv", [128, 24, 128], BF16)
        rd = st.sb("rd", [128, 2, 128], F32)
        a32 = st.sb("a32", [128, 2, 128], F32)
        asq = st.sb("asq", [128, 8, TOK], BF16)
        ag = st.sb("ag", [128, 8, TOK], BF16)
        kc = st.sb("kc", [128, 2, 16, 128], BF16)
        vc = st.sb("vc", [128, 2, 16, 128], BF16)
        kcT = st.sb("kcT", [128, 2048], BF16)
        psm = st.sb("psm", [128, 128], BF16)
        psn = st.sb("psn", [128, 128], BF16)
        pmm_ = st.sb("pmm_", [128, 128], BF16)
        pmn = st.sb("pmn", [128, 128], BF16)
        rd8 = st.sb("rd8", [128, 8], F32)
        a8 = st.sb("a8", [128, 8], F32)
        ssa = st.sb("ssa", [128, 16], F32)
        ps_s2 = [st.ps("ps_s%d" % i, [128, 512], F32) for i in range(2)]
        ps_n2 = [st.ps("ps_n%d" % i, [128, 512], F32) for i in range(2)]
        ps_d2 = [st.ps("ps_d%d" % i, [128, 512], F32) for i in range(2)]
        ptk1 = st.ps("ptk", [128, 1024], BF16)
        ptk = [ptk1, ptk1]
        ps_big = st.ps("ps_big", [128, 512], F32)
        ps_sm = ps_big[:, 0:128]
        ps_new = ps_big[:, 128:256]
        ps_snd = ps_big[:, 256:272]
        ps_ss = ps_big[:, 272:288]

        o_memset(S, "dve", onesb[:], 1.0, ["onesb"])
        make_ident(S, identb, "identb")
        o_dma(S, "pool", pmask[:].rearrange("p a b -> p (a b)"), T["pmask"][:, :], writes=["pmask"], grp="pmask")
        o_dma(S, "pool", smask[:], T["smask"][:, :], writes=["smask"], grp="smask")
        o_dma(S, "pool", smaskn[:], T["smaskn"][:, :], writes=["smaskn"], grp="smaskn")
        o_dma(S, "sp", vb[:], T["vbias"][:, :], writes=["vb"], grp="vb")
        o_dma(S, "sp", gatt[:], T["gatt"][:, :], writes=["gatt"], grp="gatt")
        for j in range(24):
            o_ts(S, "pool", onesv[:, j, :], onesb[:], vb[:, j:j + 1], ALU.mult, reads=["onesb", "vb"], writes=["onesv"])
        o_memset(S, "pool", asq[:, :, 1024:TOK], 0.0, [("asq", h, 8) for h in range(8)])
        o_memset(S, "pool", ag[:, :, 1024:TOK], 0.0, [("ag", h, 8) for h in range(8)])
        it = 0
        blk_i = 0
        sh_i = 0
        for h in range(8):
            b = h % 2
            o_dma(S, "sp", kT[:, b, :], T["kT"][h, :, :], writes=[("kT", b)], grp="kT%d" % b)
            o_dma(S, "sp", Vh[:, b, :, :], T["V_scr"][:, h * 128:(h + 1) * 128].rearrange("(c p) n -> p c n", p=128),
                  writes=[("Vh", b)], grp="Vh%d" % b)
            o_dma(S, "sp", qT[:, b, :], T["qT"][h, :, :], writes=[("qT", b)], grp="qT%d" % b)
            groups_ = [(m, c0, n) for m in range(8) for (c0, n) in ((0, 4), (4, 4), (8, 4), (12, 4), (16, 1))]

            def score(gi):
                m, c0, n = groups_[gi]
                sg = (g0 + gi) % 2
                for q in range(n):
                    kcx = m + c0 + q
                    o_mm(S, ps_s2[sg][:, q * 128:(q + 1) * 128], kT[:, b, kcx * 128:(kcx + 1) * 128], qT[:, b, m * 128:(m + 1) * 128],
                         True, True, reads=[("kT", b), ("qT", b)], writes=[("ps_s", sg)])
                o_act(S, p[:, sg, 0:n * 128], ps_s2[sg][:, 0:n * 128], AF.Exp, reads=[("ps_s", sg)], writes=[("p", sg)], scale=SC_ATT)
                o_tt(S, "dve" if sg == 0 else "pool", pm[:, sg, 0:n * 128], p[:, sg, 0:n * 128],
                     pmask[:, c0:c0 + n, :].rearrange("p a b -> p (a b)"), ALU.mult,
                     reads=[("p", sg), "pmask"], writes=[("pm", sg)])

            g0 = it
            score(0)
            for gi, (m, c0, n) in enumerate(groups_):
                if gi + 1 < len(groups_):
                    score(gi + 1)
                sg = (g0 + gi) % 2
                sl = (blk_i + m) % 2
                for q in range(n):
                    kcx = m + c0 + q
                    cc = c0 + q
                    o_mm(S, ps_n2[sl][:, 0:128], Vh[:, b, kcx, :], pm[:, sg, q * 128:(q + 1) * 128], cc == 0, cc == 16,
                         reads=[("Vh", b), ("pm", sg)], writes=[("ps_n", sl)])
                for q in range(n):
                    kcx = m + c0 + q
                    cc = c0 + q
                    o_mm(S, ps_d2[sl][:, 0:128], onesv[:, kcx, :], pm[:, sg, q * 128:(q + 1) * 128], cc == 0, cc == 16,
                         reads=["onesv", ("pm", sg)], writes=[("ps_d", sl)])
                if c0 == 16:
                    o_recip(S, rd[:, sl, :], ps_d2[sl][:, 0:128], reads=[("ps_d", sl)], writes=[("rd", sl)])
                    o_tt(S, "dve", a32[:, sl, :], ps_n2[sl][:, 0:128], rd[:, sl, :], ALU.mult,
                         reads=[("ps_n", sl), ("rd", sl)], writes=[("a32", sl)])
                    o_tt(S, "pool", asq[:, h, m * 128:(m + 1) * 128], a32[:, sl, :], a32[:, sl, :], ALU.mult,
                         reads=[("a32", sl)], writes=[("asq", h, m)])
                    o_copy(S, "act", ag[:, h, m * 128:(m + 1) * 128], a32[:, sl, :],
                           reads=[("a32", sl), "gatt"], writes=[("ag", h, m)], scale=gatt[:, h:h + 1])
            it += len(groups_)
            blk_i += 8
            o_mm(S, ps_new[:, :], kT[:, b, 3072:3200], qT[:, b, 1024:1152], True, True,
                 reads=[("kT", b), ("qT", b)], writes=["ps_big"])
            o_act(S, psn[:], ps_new[:, :], AF.Exp, reads=["ps_big"], writes=["psn"], scale=SC_ATT)
            o_tt(S, "dve", pmn[:], psn[:], smaskn[:], ALU.mult, reads=["psn", "smaskn"], writes=["pmn"])
            for s in range(4):
                sb_ = sh_i % 2
                sh_i += 1
                o_dma(S, "pool", kc[:, sb_, :, :], T["cwk"][s, :, h * 128:(h + 1) * 128].rearrange("(c p) n -> p c n", p=128),
                      writes=[("kc", sb_)], grp="kc%d" % sb_)
                o_dma(S, "pool", vc[:, sb_, :, :], T["cwv"][s, :, h * 128:(h + 1) * 128].rearrange("(c p) n -> p c n", p=128),
                      writes=[("vc", sb_)], grp="vc%d" % sb_)
                for half in range(2):
                    for q8 in range(8):
                        cc = half * 8 + q8
                        o_tr(S, ptk[half][:, q8 * 128:(q8 + 1) * 128], kc[:, sb_, cc, :], identb[:],
                             reads=[("kc", sb_), "identb"], writes=["ptk"])
                    o_copy(S, "act" if half == 0 else "dve", kcT[:, half * 1024:(half + 1) * 1024], ptk[half][:, :],
                           reads=["ptk"], writes=[("kcT", half)])
                q0 = 1024 + 32 * s
                for cc in range(16):
                    o_mm(S, ps_sm[:, cc * 8:(cc + 1) * 8], kcT[:, cc * 128:(cc + 1) * 128], qT[:, b, q0:q0 + 8], True, True,
                         reads=[("kcT", cc // 8), ("qT", b)], writes=["ps_big"])
                o_act(S, psm[:], ps_sm[:, 0:128], AF.Exp, reads=["ps_big"], writes=["psm"], scale=SC_ATT)
                o_tt(S, "dve", pmm_[:], psm[:], smask[:], ALU.mult, reads=["psm", "smask"], writes=["pmm_"])
                for cc in range(16):
                    o_mm(S, ps_snd[:, 0:8], vc[:, sb_, cc, :], pmm_[:, cc * 8:(cc + 1) * 8], cc == 0, False,
                         reads=[("vc", sb_), "pmm_"], writes=["ps_big"])
                o_mm(S, ps_snd[:, 0:8], Vh[:, b, 24, :], pmn[:, 32 * s:32 * s + 8], False, True,
                     reads=[("Vh", b), "pmn"], writes=["ps_big"])
                for cc in range(16):
                    o_mm(S, ps_snd[:, 8:16], onesb[:], pmm_[:, cc * 8:(cc + 1) * 8], cc == 0, False,
                         reads=["onesb", "pmm_"], writes=["ps_big"])
                o_mm(S, ps_snd[:, 8:16], onesb[:], pmn[:, 32 * s:32 * s + 8], False, True,
                     reads=["onesb", "pmn"], writes=["ps_big"])
                o_recip(S, rd8[:], ps_snd[:, 8:16], reads=["ps_big"], writes=["rd8"])
                o_tt(S, "dve", a8[:], ps_snd[:, 0:8], rd8[:], ALU.mult, reads=["ps_big", "rd8"], writes=["a8"])
                o_tt(S, "pool", asq[:, h, q0:q0 + 8], a8[:], a8[:], ALU.mult, reads=["a8"], writes=[("asq", h, 8)])
                o_ts(S, "pool", ag[:, h, q0:q0 + 8], a8[:], gatt[:, h:h + 1], ALU.mult, reads=["a8", "gatt"], writes=[("ag", h, 8)])
        for t in range(NT):
            for h in range(8):
                o_mm(S, ps_ss[:, t:t + 1], asq[:, h, t * 128:(t + 1) * 128], onesb[:, 0:1], h == 0, h == 7,
                     reads=[("asq", h, t), "onesb"], writes=["ps_big"])
        o_act(S, ssa[:, 0:NT], ps_ss[:, 0:NT], AF.Sqrt, reads=["ps_big"], writes=["ssa"], bias=1e-6, scale=1.0 / 1024.0)
        o_recip(S, ssa[:, 0:NT], ssa[:, 0:NT], reads=["ssa"], writes=["ssa"])
        o_dma(S, "sp", T["rstd_a"][:, :], ssa[:, 0:NT], reads=["ssa"], grp="rstd")
        o_dma(S, "sp", T["mixT"][0:8, :, :].rearrange("h p t -> p h t"), ag[:, :, :],
              reads=[("ag", h, m) for h in range(8) for m in range(9)], grp="agst")
        S.emit()


def stage_C(nc, S, T):
    with St(nc, "C") as st:
        lre = st.sb("lre", [128, 32], F32)
        lim = st.sb("lim", [128, 32], F32)
        ldt = st.sb("ldt", [128, 32], F32)
        tA = st.sb("tA", [128, 32], F32)
        tB = st.sb("tB", [128, 32], F32)
        tC = st.sb("tC", [128, 32], F32)
        tI = st.sb("tI", [128, 32], mybir.dt.int32)
        mag = st.sb("mag", [128, 32], F32)
        fre = st.sb("fre", [128, 32], F32)
        fim = st.sb("fim", [128, 32], F32)
        nfim = st.sb("nfim", [128, 32], F32)
        pwr = st.sb("pwr", [128, 11, 32], F32)
        pwi = st.sb("pwi", [128, 11, 32], F32)
        npwi = st.sb("npwi", [128, 11, 32], F32)
        BTre = st.sb("BTre", [128, 32, 128], BF16)
        BTim = st.sb("BTim", [128, 32, 128], BF16)
        CTre = st.sb("CTre", [128, 32, 128], BF16)
        CTim = st.sb("CTim", [128, 32, 128], BF16)
        dcol = st.sb("dcol", [128, 8], F32)
        h0r = st.sb("h0r", [128, 32, 4], F32)
        h0i = st.sb("h0i", [128, 32, 4], F32)
        uT = st.sb("uT", [128, 2, UPOS], BF16)
        xr2 = st.sb("xr", [128, 2, UPOS], F32)
        xi2 = st.sb("xi", [128, 2, UPOS], F32)
        T0r = st.sb("T0r", [128, 1536], F32)
        T0i = st.sb("T0i", [128, 1536], F32)
        T1r = st.sb("T1r", [128, 768], F32)
        T1i = st.sb("T1i", [128, 768], F32)
        identF = st.sb("identF", [128, 128], F32)
        onesF = st.sb("onesF", [128, 128], F32)
        Dre = st.sb("Dre", [128, 2, 512], F32)
        Dim = st.sb("Dim", [128, 2, 512], F32)
        Bsr = st.sb("Bsr", [128, 4, 8], F32)
        Bsi = st.sb("Bsi", [128, 4, 8], F32)
        Hr = st.sb("Hr", [128, 4], F32)
        Hi = st.sb("Hi", [128, 4], F32)
        tmp = st.sb("tmp", [128, 4, 512], F32)
        hbr = st.sb("hbr", [128, TOK], BF16)
        hbi = st.sb("hbi", [128, TOK], BF16)
        hfr = st.sb("hfr", [128, 32], F32)
        hfi = st.sb("hfi", [128, 32], F32)
        hsr = st.sb("hsr", [128, 32, 4], F32)
        hsi = st.sb("hsi", [128, 32, 4], F32)
        ysb = st.sb("ysb", [128, TOK], F32)
        gt1 = st.sb("gt1", [128, TOK], F32)
        yg = st.sb("yg", [128, 2, TOK], F32)
        ygb = st.sb("ygb", [128, 2, TOK], BF16)
        pw = [st.ps("pw%d" % i, [128, 512], F32) for i in range(4)]
        py = [st.ps("py%d" % i, [128, 512], F32) for i in range(3)]

        for (t_, nm) in ((lre, "lamre"), (lim, "lamim"), (ldt, "logdt")):
            o_dma(S, "sp", t_[:], T[nm][:, :], writes=[nm], grp=nm)
        o_dma(S, "sp", dcol[:], T["dcol"][:, :], writes=["dcol"], grp="dcol")
        o_dma(S, "sp", h0r[:].rearrange("p a b -> p (a b)"), T["h0re"][:, :], writes=["h0r"], grp="h0r")
        o_dma(S, "sp", h0i[:].rearrange("p a b -> p (a b)"), T["h0im"][:, :], writes=["h0i"], grp="h0i")
        for (t_, nm) in ((BTre, "BTre"), (BTim, "BTim"), (CTre, "CTre"), (CTim, "CTim")):
            o_dma(S, "pool", t_[:].rearrange("p a b -> p (a b)"), T[nm][:, :], writes=[nm], grp=nm)
        P = ["par"]
        o_act(S, ldt[:], ldt[:], AF.Exp, reads=["logdt"], writes=["logdt"] + P)
        o_tt(S, "dve", tA[:], lre[:], ldt[:], ALU.mult, reads=["lamre", "logdt"], writes=P)
        o_act(S, mag[:], tA[:], AF.Exp, reads=P, writes=P)
        o_tt(S, "dve", tB[:], lim[:], ldt[:], ALU.mult, reads=["lamim", "logdt"], writes=P)

        def sin_of(dst, shift):
            o_ts(S, "dve", tC[:], tB[:], shift, ALU.add, reads=P, writes=P, s2=1.0 / (2 * np.pi), op1=ALU.mult)
            S.op("dve", lambda h: h.tensor_copy(out=tI[:], in_=tC[:]), reads=P, writes=P)
            S.op("dve", lambda h: h.tensor_copy(out=tA[:], in_=tI[:]), reads=P, writes=P)
            o_tt(S, "dve", tC[:], tC[:], tA[:], ALU.subtract, reads=P, writes=P)
            o_ts(S, "dve", tA[:], tC[:], 0.5, ALU.is_gt, reads=P, writes=P)
            o_tt(S, "dve", tC[:], tC[:], tA[:], ALU.subtract, reads=P, writes=P)
            o_ts(S, "dve", tA[:], tC[:], -0.5, ALU.is_lt, reads=P, writes=P)
            o_tt(S, "dve", tC[:], tC[:], tA[:], ALU.add, reads=P, writes=P)
            o_ts(S, "dve", tC[:], tC[:], 2 * np.pi, ALU.mult, reads=P, writes=P, s2=3.14159, op1=ALU.min)
            o_ts(S, "dve", tC[:], tC[:], -3.14159, ALU.max, reads=P, writes=P)
            o_act(S, dst, tC[:], AF.Sin, reads=P, writes=P)

        sin_of(pwi[:, 0, :], 0.0)
        sin_of(pwr[:, 0, :], np.pi / 2)
        o_tt(S, "dve", pwr[:, 0, :], pwr[:, 0, :], mag[:], ALU.mult, reads=P, writes=P)
        o_tt(S, "dve", pwi[:, 0, :], pwi[:, 0, :], mag[:], ALU.mult, reads=P, writes=P)
        o_tt(S, "dve", tA[:], lre[:], lre[:], ALU.mult, reads=P + ["lamre"], writes=P)
        o_tt(S, "dve", tC[:], lim[:], lim[:], ALU.mult, reads=P + ["lamim"], writes=P)
        o_tt(S, "dve", tA[:], tA[:], tC[:], ALU.add, reads=P, writes=P)
        o_recip(S, tA[:], tA[:], reads=P, writes=P)
        o_ts(S, "dve", tB[:], pwr[:, 0, :], -1.0, ALU.add, reads=P, writes=P)
        o_tt(S, "dve", fre[:], tB[:], lre[:], ALU.mult, reads=P, writes=P)
        o_tt(S, "dve", tC[:], pwi[:, 0, :], lim[:], ALU.mult, reads=P, writes=P)
        o_tt(S, "dve", fre[:], fre[:], tC[:], ALU.add, reads=P, writes=P)
        o_tt(S, "dve", fre[:], fre[:], tA[:], ALU.mult, reads=P, writes=P)
        o_tt(S, "dve", fim[:], pwi[:, 0, :], lre[:], ALU.mult, reads=P, writes=P)
        o_tt(S, "dve", tC[:], tB[:], lim[:], ALU.mult, reads=P, writes=P)
        o_tt(S, "dve", fim[:], fim[:], tC[:], ALU.subtract, reads=P, writes=P)
        o_tt(S, "dve", fim[:], fim[:], tA[:], ALU.mult, reads=P, writes=P)
        o_ts(S, "dve", nfim[:], fim[:], -1.0, ALU.mult, reads=P, writes=P)
        for k in range(10):
            o_tt(S, "dve", tA[:], pwr[:, k, :], pwr[:, k, :], ALU.mult, reads=P, writes=P)
            o_tt(S, "dve", tC[:], pwi[:, k, :], pwi[:, k, :], ALU.mult, reads=P, writes=P)
            o_tt(S, "dve", pwr[:, k + 1, :], tA[:], tC[:], ALU.subtract, reads=P, writes=P)
            o_tt(S, "dve", tA[:], pwr[:, k, :], pwi[:, k, :], ALU.mult, reads=P, writes=P)
            o_ts(S, "dve", pwi[:, k + 1, :], tA[:], 2.0, ALU.mult, reads=P, writes=P)
        o_ts(S, "dve", npwi[:].rearrange("p a b -> p (a b)"), pwi[:].rearrange("p a b -> p (a b)"), -1.0, ALU.mult, reads=P, writes=P)
        make_ident(S, identF, "identF")
        o_memset(S, "pool", onesF[:], 1.0, ["onesF"])
        for g in range(8):
            db = g % 2
            for q in range(4):
                tq = 4 * g + q
                o_copy(S, "act", Dre[:, db, 128 * q:128 * (q + 1)], identF[:], reads=["identF"] + P, writes=[("Dre", db)],
                       scale=fre[:, tq:tq + 1])
                o_copy(S, "act", Dim[:, db, 128 * q:128 * (q + 1)], identF[:], reads=["identF"] + P, writes=[("Dim", db)],
                       scale=fim[:, tq:tq + 1])
            o_mm(S, pw[0][:, :], onesF[:], Dre[:, db, :], True, True, reads=["onesF", ("Dre", db)], writes=[("pw", 0)])
            o_mm(S, pw[1][:, :], onesF[:], Dim[:, db, :], True, True, reads=["onesF", ("Dim", db)], writes=[("pw", 1)])
            bre = BTre[:, 4 * g:4 * g + 4, :].rearrange("p a b -> p (a b)")
            bim = BTim[:, 4 * g:4 * g + 4, :].rearrange("p a b -> p (a b)")
            o_tt(S, "dve", tmp[:, 0, :], pw[0][:, :], bre, ALU.mult, reads=[("pw", 0), "BTre"], writes=[("tmp", 0)])
            o_tt(S, "dve", tmp[:, 1, :], pw[1][:, :], bim, ALU.mult, reads=[("pw", 1), "BTim"], writes=[("tmp", 1)])
            o_tt(S, "dve", tmp[:, 2, :], pw[0][:, :], bim, ALU.mult, reads=[("pw", 0), "BTim"], writes=[("tmp", 2)])
            o_tt(S, "dve", tmp[:, 3, :], pw[1][:, :], bre, ALU.mult, reads=[("pw", 1), "BTre"], writes=[("tmp", 3)])
            o_tt(S, "dve", bre, tmp[:, 0, :], tmp[:, 1, :], ALU.subtract, reads=[("tmp", 0), ("tmp", 1)], writes=["BTre"])
            o_tt(S, "dve", bim, tmp[:, 2, :], tmp[:, 3, :], ALU.add, reads=[("tmp", 2), ("tmp", 3)], writes=["BTim"])
        o_memset(S, "pool", hbr[:, 1024:TOK], 0.0, ["hbr_s"])
        o_memset(S, "pool", hbi[:, 1024:TOK], 0.0, ["hbi_s"])

        def cma(dr, di, er, ei, orr, oi, k, tau, rk, wk):
            pr = pwr[:, k, tau:tau + 1]
            pi_ = pwi[:, k, tau:tau + 1]
            npi = npwi[:, k, tau:tau + 1]
            wr_ = [(w_, "r") for w_ in wk]
            wi_ = [(w_, "i") for w_ in wk]
            o_stt(S, dr, er, pr, orr, ALU.mult, ALU.add, reads=rk + P, writes=wr_)
            o_stt(S, di, ei, pr, oi, ALU.mult, ALU.add, reads=rk + P, writes=wi_)
            o_stt(S, dr, ei, npi, dr, ALU.mult, ALU.add, reads=rk + P, writes=wr_)
            o_stt(S, di, er, pi_, di, ALU.mult, ALU.add, reads=rk + P, writes=wi_ + list(wk))
            S.touch(wk, wr_ + wi_)

        blocks = [(i * 512, 512) for i in range(8)] + [(4096, 128)]
        wic = [0]

        def emit_xe(tau, ub):
            xr = xr2[:, tau % 2, :]
            xi = xi2[:, tau % 2, :]
            XK = ("x", tau % 2)
            for (c0, w) in blocks:
                wi = wic[0]
                p0 = pw[(wi * 2) % 4]
                p1 = pw[(wi * 2 + 1) % 4]
                k0 = ("pw", (wi * 2) % 4)
                k1 = ("pw", (wi * 2 + 1) % 4)
                wic[0] += 1
                o_mm(S, p0[:, 0:w], BTre[:, tau, :], uT[:, ub, c0:c0 + w], True, True, reads=["BTre", ("uT", ub)], writes=[k0])
                o_mm(S, p1[:, 0:w], BTim[:, tau, :], uT[:, ub, c0:c0 + w], True, True, reads=["BTim", ("uT", ub)], writes=[k1])
                o_copy(S, "act", xr[:, c0:c0 + w], p0[:, 0:w], reads=[k0], writes=[XK])
                o_copy(S, "act", xi[:, c0:c0 + w], p1[:, 0:w], reads=[k1], writes=[XK])

        o_dma(S, "sp", uT[:, 0, :], T["uT"][0, :, :], writes=[("uT", 0)], grp="uT0")
        emit_xe(0, 0)
        for c in range(8):
            ub = c % 2
            for j in range(4):
                tau = 4 * c + j
                xr = xr2[:, tau % 2, :]
                xi = xi2[:, tau % 2, :]
                XK = ("x", tau % 2)
                if tau + 1 < 32:
                    cn = (tau + 1) // 4
                    if (tau + 1) % 4 == 0:
                        o_dma(S, "sp", uT[:, cn % 2, :], T["uT"][cn, :, :], writes=[("uT", cn % 2)], grp="uT%d" % (cn % 2))
                    emit_xe(tau + 1, cn % 2)
                X = [XK]
                xs_r = xr[:, 4096:UPOS:32]
                xs_i = xi[:, 4096:UPOS:32]
                cma(xs_r, xs_i, h0r[:, tau, :], h0i[:, tau, :], xs_r, xs_i, 0, tau, X + ["h0r", "h0i"], X)
                src_r, src_i, n = xr, xi, 3072
                dsts = [(T0r, T0i), (T1r, T1i)]
                for k in range(10):
                    dr_, di_ = dsts[k % 2]
                    no = n // 2
                    cma(dr_[:, 0:no], di_[:, 0:no], src_r[:, 0:n:2], src_i[:, 0:n:2], src_r[:, 1:n:2], src_i[:, 1:n:2],
                        k, tau, X + ["tree"], ["tree"])
                    src_r, src_i, n = dr_, di_, no
                cma(Hr[:, 0:1], Hi[:, 0:1], src_r[:, 0:1], src_i[:, 0:1], src_r[:, 1:2], src_i[:, 1:2], 10, tau, ["tree"], ["H"])
                cma(Hr[:, 1:2], Hi[:, 1:2], Hr[:, 0:1], Hi[:, 0:1], src_r[:, 2:3], src_i[:, 2:3], 10, tau, ["tree", "H"], ["H"])
                cma(xr[:, 3072:3073], xi[:, 3072:3073], Hr[:, 1:2], Hi[:, 1:2], xr[:, 3072:3073], xi[:, 3072:3073], 0, tau, X + ["H"], X)
                o_ = 3072
                for k in range(10):
                    st_ = 1 << (k + 1)
                    h_ = 1 << k
                    cnt = 1024 // st_
                    t0 = o_ + st_ - 1
                    s0 = o_ + h_ - 1
                    t1 = t0 + (cnt - 1) * st_ + 1
                    s1 = s0 + (cnt - 1) * st_ + 1
                    cma(xr[:, t0:t1:st_], xi[:, t0:t1:st_], xr[:, s0:s1:st_], xi[:, s0:s1:st_],
                        xr[:, t0:t1:st_], xi[:, t0:t1:st_], k, tau, X, X)
                for k in range(8, -1, -1):
                    st_ = 1 << (k + 1)
                    h_ = 1 << k
                    cnt = 1024 // st_ - 1
                    t0 = o_ + st_ + h_ - 1
                    s0 = o_ + st_ - 1
                    t1 = t0 + (cnt - 1) * st_ + 1
                    s1 = s0 + (cnt - 1) * st_ + 1
                    cma(xr[:, t0:t1:st_], xi[:, t0:t1:st_], xr[:, s0:s1:st_], xi[:, s0:s1:st_],
                        xr[:, t0:t1:st_], xi[:, t0:t1:st_], k, tau, X, X)
                sr = xr[:, 4096:UPOS].rearrange("p (s j) -> p s j", j=32)[:, :, 0:8]
                si_ = xi[:, 4096:UPOS].rearrange("p (s j) -> p s j", j=32)[:, :, 0:8]
                cur = (sr, si_, XK)
                alt = (Bsr[:, :, :], Bsi[:, :, :], "Bs")
                for k in range(3):
                    s_ = 1 << k
                    cr, ci, ck = cur
                    ar_, ai_, ak = alt
                    cma(ar_[:, :, s_:8], ai_[:, :, s_:8], cr[:, :, 0:8 - s_], ci[:, :, 0:8 - s_], cr[:, :, s_:8], ci[:, :, s_:8],
                        k, tau, [ck], [ak])
                    o_copy(S, "act", ar_[:, :, 0:s_], cr[:, :, 0:s_], reads=[ck], writes=[ak])
                    o_copy(S, "pool", ai_[:, :, 0:s_], ci[:, :, 0:s_], reads=[ck], writes=[ak])
                    cur, alt = alt, cur
                fr, fi_, fk = cur
                o_copy(S, "act", hfr[:, tau:tau + 1], xr[:, 4095:4096], reads=X, writes=["hf"])
                o_copy(S, "act", hfi[:, tau:tau + 1], xi[:, 4095:4096], reads=X, writes=["hf"])
                o_copy(S, "pool", hsr[:, tau, :], fr[:, :, 7], reads=[fk], writes=["hs"])
                o_copy(S, "pool", hsi[:, tau, :], fi_[:, :, 7], reads=[fk], writes=["hs"])
                o_copy(S, "act", hbr[:, 0:1024], xr[:, 3072:4096], reads=X, writes=["hbr"])
                o_copy(S, "act", hbi[:, 0:1024], xi[:, 3072:4096], reads=X, writes=["hbi"], scale=-1.0)
                o_copy(S, "pool", hbr[:, 1024:TOK].rearrange("p (s j) -> p s j", j=32)[:, :, 0:8], fr, reads=[fk, "hbr_s"], writes=["hbr_s"])
                o_ts(S, "pool", hbi[:, 1024:TOK].rearrange("p (s j) -> p s j", j=32)[:, :, 0:8], fi_, -1.0, ALU.mult,
                     reads=[fk, "hbi_s"], writes=["hbi_s"])
                for bi, (c0, w) in enumerate([(0, 512), (512, 512), (1024, 128)]):
                    o_mm(S, py[bi][:, 0:w], CTre[:, tau, :], hbr[:, c0:c0 + w], j == 0, False,
                         reads=["CTre", "hbr", "hbr_s"], writes=[("py", bi)])
                    o_mm(S, py[bi][:, 0:w], CTim[:, tau, :], hbi[:, c0:c0 + w], False, j == 3,
                         reads=["CTim", "hbi", "hbi_s"], writes=[("py", bi)])
            yb_ = c % 2
            for bi, (c0, w) in enumerate([(0, 512), (512, 512), (1024, 128)]):
                o_stt(S, ysb[:, c0:c0 + w], uT[:, ub, 3072 + c0:3072 + c0 + w], dcol[:, c:c + 1], py[bi][:, 0:w], ALU.mult, ALU.add,
                      reads=[("uT", ub), ("py", bi), "dcol"], writes=["ysb"])
            o_tt(S, "pool", gt1[:], ysb[:], ysb[:], ALU.mult, reads=["ysb"], writes=["gt1"])
            o_ts(S, "dve", gt1[:], gt1[:], 0.044715, ALU.mult, reads=["gt1"], writes=["gt1"], s2=1.0, op1=ALU.add)
            o_tt(S, "dve", gt1[:], gt1[:], ysb[:], ALU.mult, reads=["gt1", "ysb"], writes=["gt1"])
            o_act(S, gt1[:], gt1[:], AF.Tanh, reads=["gt1"], writes=["gt1"], scale=0.7978845608028654)
            o_ts(S, "dve", gt1[:], gt1[:], 1.0, ALU.add, reads=["gt1"], writes=["gt1"], s2=0.5, op1=ALU.mult)
            o_tt(S, "dve", yg[:, yb_, :], gt1[:], ysb[:], ALU.mult, reads=["gt1", "ysb"], writes=[("yg", yb_)])
            o_copy(S, "pool", ygb[:, yb_, :], yg[:, yb_, :], reads=[("yg", yb_)], writes=[("ygb", yb_)])
            o_dma(S, "sp", T["ygT"][c, :, :], yg[:, yb_, :], reads=[("yg", yb_)], grp="yg%d" % yb_)
            o_dma(S, "sp", T["ygTb"][c, :, :], ygb[:, yb_, :], reads=[("ygb", yb_)], grp="ygb%d" % yb_)
        o_dma(S, "sp", T["hfr"][:, :], hfr[:], reads=["hf"], grp="hfr")
        o_dma(S, "sp", T["hfi"][:, :], hfi[:], reads=["hf"], grp="hfi")
        o_dma(S, "sp", T["hsr"][:, :], hsr[:].rearrange("p a b -> p (a b)"), reads=["hs"], grp="hsr")
        o_dma(S, "sp", T["hsi"][:, :], hsi[:].rearrange("p a b -> p (a b)"), reads=["hs"], grp="hsi")
        S.emit()


def stage_D1(nc, S, T):
    with St(nc, "D1") as st:
        ygT = st.sb("ygT", [128, 8, TOK], F32)
        ygb = st.sb("ygb", [128, 8, TOK], BF16)
        Wg = st.sb("Wg", [128, 8, 1024], BF16)
        gssm = st.sb("gssm", [128, 8], F32)
        onesb = st.sb("onesb", [128, 128], BF16)
        ssq = st.sb("ssq", [128, 8, TOK], BF16)
        sgm = st.sb("sgm", [128, 8, TOK], BF16)
        sg = st.sb("sg", [128, 2, 512], F32)
        so = st.sb("so", [128, 2, 512], F32)
        sss = st.sb("sss", [128, 16], F32)
        pz = [st.ps("pz%d" % i, [128, 512], F32) for i in range(4)]
        ps_ss = st.ps("ps_ss", [128, 16], F32)
        o_memset(S, "dve", onesb[:], 1.0, ["onesb"])
        o_dma(S, "sp", ygT[:], T["ygT"].rearrange("c p t -> p c t"), writes=["ygT"], grp="ygT")
        o_dma(S, "sp", ygb[:], T["ygTb"].rearrange("c p t -> p c t"), writes=["ygb"], grp="ygb")
        o_dma(S, "pool", Wg[:], T["w_glu"].rearrange("(c p) n -> p c n", p=128), writes=["Wg"], grp="Wg")
        o_dma(S, "sp", gssm[:], T["gssm"][:, :], writes=["gssm"], grp="gssm")
        i = 0
        for f in range(8):
            for (c0, w) in [(0, 512), (512, 512), (1024, 128)]:
                pi = i % 4
                si = i % 2
                i += 1
                for k in range(8):
                    o_mm(S, pz[pi][:, 0:w], Wg[:, k, f * 128:(f + 1) * 128], ygb[:, k, c0:c0 + w], k == 0, k == 7,
                         reads=["Wg", "ygb"], writes=[("pz", pi)])
                o_act(S, sg[:, si, 0:w], pz[pi][:, 0:w], AF.Sigmoid, reads=[("pz", pi)], writes=[("sg", si)])
                o_tt(S, "dve", so[:, si, 0:w], ygT[:, f, c0:c0 + w], sg[:, si, 0:w], ALU.mult, reads=["ygT", ("sg", si)], writes=[("so", si)])
                o_tt(S, "pool", ssq[:, f, c0:c0 + w], so[:, si, 0:w], so[:, si, 0:w], ALU.mult, reads=[("so", si)], writes=["ssq"])
                o_copy(S, "act", sgm[:, f, c0:c0 + w], so[:, si, 0:w], reads=[("so", si), "gssm"], writes=["sgm"], scale=gssm[:, f:f + 1])
        for t in range(NT):
            for f in range(8):
                o_mm(S, ps_ss[:, t:t + 1], ssq[:, f, t * 128:(t + 1) * 128], onesb[:, 0:1], f == 0, f == 7,
                     reads=["ssq", "onesb"], writes=["ps_ss"])
        o_act(S, sss[:, 0:NT], ps_ss[:, 0:NT], AF.Sqrt, reads=["ps_ss"], writes=["sss"], bias=1e-6, scale=1.0 / 1024.0)
        o_recip(S, sss[:, 0:NT], sss[:, 0:NT], reads=["sss"], writes=["sss"])
        o_dma(S, "sp", T["rstd_s"][:, :], sss[:, 0:NT], reads=["sss"], grp="rstd")
        o_dma(S, "sp", T["mixT"][8:16, :, :].rearrange("h p t -> p h t"), sgm[:, :, :], reads=["sgm"], grp="sgmst")
        S.emit()


def layernorm_tile(S, st_, X, t, lng, lnb, stats, mv, rs, key):
    xt = X[:, t, :]
    for c in range(4):
        S.op("dve", lambda h, c=c: h.bn_stats(out=stats[:, c, :], in_=X[:, t, c * 512:(c + 1) * 512]), reads=[key], writes=["ln_st"])
    S.op("dve", lambda h: h.bn_aggr(out=mv[:], in_=stats[:].rearrange("p a b -> p (a b)")), reads=["ln_st"], writes=["ln_mv"])
    o_act(S, rs[:], mv[:, 1:2], AF.Sqrt, reads=["ln_mv"], writes=["ln_rs"], bias=1e-5, scale=1.0)
    o_recip(S, rs[:], rs[:], reads=["ln_rs"], writes=["ln_rs"])
    o_stt(S, mv[:, 0:1], mv[:, 0:1], -1.0, rs[:, 0:1], ALU.mult, ALU.mult, reads=["ln_mv", "ln_rs"], writes=["ln_mv"])
    o_act(S, xt, xt, AF.Identity, reads=[key, "ln_mv", "ln_rs"], writes=[key], bias=mv[:, 0:1], scale=rs[:, 0:1])
    o_tt(S, "dve", xt, xt, lng[:], ALU.mult, reads=[key, "lng"], writes=[key])
    o_tt(S, "pool", xt, xt, lnb[:], ALU.add, reads=[key, "lnb"], writes=[key])


def linear_residual_ln(nc, S, T, name, inT_name, w_name, lng_name, lnb_name, xin_fn, xout_name, two_part):
    with St(nc, name) as st:
        X = st.sb("X", [128, NT, 2048], F32)
        inT = st.sb("inT", [128, 16, TOK], BF16)
        Wo = st.sb("Wo", [128, 2, 16, 512], BF16)
        lng = st.sb("lng", [128, 2048], F32)
        lnb = st.sb("lnb", [128, 2048], F32)
        tmp = st.sb("tmp", [128, 2, 512], F32)
        stats = st.sb("stats", [128, 4, 6], F32)
        mv = st.sb("mv", [128, 2], F32)
        rs = st.sb("rs", [128, 1], F32)
        ra = st.sb("ra", [128, NT], F32)
        rsm = st.sb("rsm", [128, NT], F32)
        pa = [st.ps("pa%d" % i, [128, 512], F32) for i in range(2)]
        pb = [st.ps("pb%d" % i, [128, 512], F32) for i in range(2)]
        xin_fn(S, X)
        o_dma(S, "sp", inT[:], T[inT_name].rearrange("c p t -> p c t"), writes=["inT"], grp="inT")
        o_dma(S, "sp", lng[:], T[lng_name].partition_broadcast(128), writes=["lng"], grp="lng")
        o_dma(S, "sp", lnb[:], T[lnb_name].partition_broadcast(128), writes=["lnb"], grp="lnb")
        if two_part:
            o_dma(S, "sp", ra[:], T["rstd_a"][:, :], writes=["ra"], grp="ra")
            o_dma(S, "sp", rsm[:], T["rstd_s"][:, :], writes=["rsm"], grp="rsm")
        i = 0
        for n in range(4):
            wb = n % 2
            o_dma(S, "pool", Wo[:, wb, :, :], T[w_name][:, n * 512:(n + 1) * 512].rearrange("(c p) n -> p c n", p=128),
                  writes=[("Wo", wb)], grp="Wo%d" % wb)
            for t in range(NT):
                pi = i % 2
                i += 1
                xk = ("X", t)
                if two_part:
                    for k in range(8):
                        o_mm(S, pa[pi][:, :], inT[:, k, t * 128:(t + 1) * 128], Wo[:, wb, k, :], k == 0, k == 7,
                             reads=["inT", ("Wo", wb)], writes=[("pa", pi)])
                    for k in range(8):
                        o_mm(S, pb[pi][:, :], inT[:, 8 + k, t * 128:(t + 1) * 128], Wo[:, wb, 8 + k, :], k == 0, k == 7,
                             reads=["inT", ("Wo", wb)], writes=[("pb", pi)])
                    o_copy(S, "act", tmp[:, pi, :], pa[pi][:, :], reads=[("pa", pi), "ra"], writes=[("tmp", pi)], scale=ra[:, t:t + 1])
                    o_stt(S, tmp[:, pi, :], pb[pi][:, :], rsm[:, t:t + 1], tmp[:, pi, :], ALU.mult, ALU.add,
                          reads=[("pb", pi), ("tmp", pi), "rsm"], writes=[("tmp", pi)])
                    o_stt(S, X[:, t, n * 512:(n + 1) * 512], X[:, t, n * 512:(n + 1) * 512], ALPHA, tmp[:, pi, :], ALU.mult, ALU.add,
                          reads=[xk, ("tmp", pi)], writes=[xk])
                else:
                    for k in range(16):
                        o_mm(S, pa[pi][:, :], inT[:, k, t * 128:(t + 1) * 128], Wo[:, wb, k, :], k == 0, k == 15,
                             reads=["inT", ("Wo", wb)], writes=[("pa", pi)])
                    o_stt(S, X[:, t, n * 512:(n + 1) * 512], X[:, t, n * 512:(n + 1) * 512], ALPHA, pa[pi][:, :], ALU.mult, ALU.add,
                          reads=[xk, ("pa", pi)], writes=[xk])
                if n == 3:
                    layernorm_tile(S, st, X, t, lng, lnb, stats, mv, rs, ("X", t))
                    o_dma(S, "sp", T[xout_name][t * 128:(t + 1) * 128, :], X[:, t, :], reads=[("X", t)], grp="xo%d" % (t % 2))
        S.emit()


def xin_from_inputs(T):
    def f(S, X):
        o_dma(S, "sp", X[:, 0:8, :], T["xw"][3072:4096, :].rearrange("(t p) d -> p t d", p=128),
              writes=[("X", t) for t in range(8)], grp="X")
        o_dma(S, "sp", X[:, 8, :], T["xs"][:, :], writes=[("X", 8)], grp="X8")
    return f


def xin_from_scr(T, name):
    def f(S, X):
        o_dma(S, "sp", X[:, :, :], T[name][:, :].rearrange("(t p) d -> p t d", p=128),
              writes=[("X", t) for t in range(NT)], grp="X")
    return f


def transpose_block(S, X, t, ident, ptr, dstT, dst_key, i0, f32copy=None):
    for b4 in range(4):
        pi = (i0 + b4) % 2
        for q in range(4):
            c = b4 * 4 + q
            o_tr(S, ptr[pi][:, q * 128:(q + 1) * 128], X[:, t, c * 128:(c + 1) * 128], ident[:],
                 reads=[("X", t), "ident"], writes=[("ptr", pi)])
        o_copy(S, "act" if pi == 0 else "dve", dstT[:, b4 * 4:(b4 + 1) * 4, t * 128:(t + 1) * 128],
               ptr[pi][:, :].rearrange("p (a b) -> p a b", a=4), reads=[("ptr", pi)], writes=[dst_key])
        if f32copy is not None:
            o_copy(S, "dve" if pi == 0 else "act", f32copy[:, b4 * 4:(b4 + 1) * 4, :],
                   ptr[pi][:, :].rearrange("p (a b) -> p a b", a=4), reads=[("ptr", pi)], writes=["f32copy"])


def stage_E0(nc, S, T):
    with St(nc, "E0") as st:
        ident = st.sb("ident", [128, 128], F32)
        M = st.sb("M", [128, 2, 2048], F32)
        memT = st.sb("memT", [128, 16, 256], BF16)
        Wb = st.sb("Wb", [128, 2, 16, 512], BF16)
        of = st.sb("of", [128, 2, 512], F32)
        ob = st.sb("ob", [128, 2, 512], BF16)
        okT = st.sb("okT", [128, 2, 256], BF16)
        ptr = [st.ps("ptr%d" % i, [128, 512], F32) for i in range(2)]
        pm = [st.ps("pm%d" % i, [128, 512], F32) for i in range(2)]
        pk = [st.ps("pk%d" % i, [128, 256], F32) for i in range(2)]
        make_ident(S, ident, "ident")
        o_dma(S, "sp", M[:], T["memp"][:, :].rearrange("(t p) d -> p t d", p=128), writes=[("X", 0), ("X", 1)], grp="M")
        for t in range(2):
            transpose_block(S, M, t, ident, ptr, memT, "memT", 0)
        wi = 0
        oi = 0
        for (wname, kind) in (("w_mk", "k"), ("w_mv", "v")):
            for n in range(4):
                wb = wi % 2
                wi += 1
                o_dma(S, "pool", Wb[:, wb, :, :], T[wname][:, n * 512:(n + 1) * 512].rearrange("(c p) n -> p c n", p=128),
                      writes=[("Wb", wb)], grp="Wb%d" % wb)
                for t in range(2):
                    pi = oi % 2
                    oi += 1
                    for k in range(16):
                        o_mm(S, pm[pi][:, :], memT[:, k, t * 128:(t + 1) * 128], Wb[:, wb, k, :], k == 0, k == 15,
                             reads=["memT", ("Wb", wb)], writes=[("pm", pi)])
                    o_copy(S, "act", of[:, pi, :], pm[pi][:, :], reads=[("pm", pi)], writes=[("of", pi)])
                    o_dma(S, "sp", T["memKo" if kind == "k" else "memVo"][t * 128:(t + 1) * 128, n * 512:(n + 1) * 512],
                          of[:, pi, :], reads=[("of", pi)], grp="of%d" % pi)
                    if kind == "v":
                        o_copy(S, "dve", ob[:, pi, :], pm[pi][:, :], reads=[("pm", pi)], writes=[("ob", pi)])
                        o_dma(S, "sp", T["mv_scr"][t * 128:(t + 1) * 128, n * 512:(n + 1) * 512], ob[:, pi, :],
                              reads=[("ob", pi)], grp="ob%d" % pi)
                if kind == "k":
                    for fb in range(4):
                        f = n * 4 + fb
                        pi = f % 2
                        for k in range(16):
                            o_mm(S, pk[pi][:, :], Wb[:, wb, k, fb * 128:(fb + 1) * 128], memT[:, k, :], k == 0, k == 15,
                                 reads=["memT", ("Wb", wb)], writes=[("pk", pi)])
                        o_copy(S, "dve", okT[:, pi, :], pk[pi][:, :], reads=[("pk", pi)], writes=[("okT", pi)])
                        o_dma(S, "sp", T["mkT_scr"][f, :, :], okT[:, pi, :], reads=[("okT", pi)], grp="okT%d" % pi)
        S.emit()


def stage_E1(nc, S, T):
    with St(nc, "E1") as st:
        ident = st.sb("ident", [128, 128], F32)
        X = st.sb("X", [128, NT, 2048], F32)
        xT = st.sb("xT", [128, 16, TOK], BF16)
        Wb = st.sb("Wb", [128, 2, 16, 512], BF16)
        oq = st.sb("oq", [128, 2, 512], BF16)
        ptr = [st.ps("ptr%d" % i, [128, 512], F32) for i in range(2)]
        pq = [st.ps("pq%d" % i, [128, 512], F32) for i in range(4)]
        make_ident(S, ident, "ident")
        xin_from_scr(T, "X1")(S, X)
        for t in range(NT):
            transpose_block(S, X, t, ident, ptr, xT, "xT", 0)
        i = 0
        for n in range(4):
            wb = n % 2
            o_dma(S, "pool", Wb[:, wb, :, :], T["w_mq"][:, n * 512:(n + 1) * 512].rearrange("(c p) n -> p c n", p=128),
                  writes=[("Wb", wb)], grp="Wb%d" % wb)
            for fb in range(4):
                f = n * 4 + fb
                for (c0, w) in [(0, 512), (512, 512), (1024, 128)]:
                    pi = i % 4
                    oi = i % 2
                    i += 1
                    for k in range(16):
                        o_mm(S, pq[pi][:, 0:w], Wb[:, wb, k, fb * 128:(fb + 1) * 128], xT[:, k, c0:c0 + w], k == 0, k == 15,
                             reads=["xT", ("Wb", wb)], writes=[("pq", pi)])
                    o_copy(S, "act" if oi == 0 else "dve", oq[:, oi, 0:w], pq[pi][:, 0:w], reads=[("pq", pi)], writes=[("oq", oi)])
                    o_dma(S, "sp", T["qmT"][f, :, c0:c0 + w], oq[:, oi, 0:w], reads=[("oq", oi)], grp="oq%d" % oi)
        S.emit()


def stage_E2(nc, S, T):
    with St(nc, "E2") as st:
        identf = st.sb("identf", [128, 128], F32)
        onesb = st.sb("onesb", [128, 128], BF16)
        mkT = st.sb("mkT", [128, 16, 256], BF16)
        mv = st.sb("mv", [128, 2, 2048], BF16)
        qm = st.sb("qm", [128, 16, TOK], BF16)
        p = st.sb("p", [128, 2, 512], BF16)
        rd = st.sb("rd", [128, 512], F32)
        om = st.sb("om", [128, 2, 4, 512], BF16)
        ck = st.sb("ck", [128, 2, 2048], F32)
        skT = st.sb("skT", [128, 16, 256], BF16)
        sv = st.sb("sv", [128, 2, 2048], BF16)
        p8 = st.sb("p8", [128, 2, 8], BF16)
        rd8 = st.sb("rd8", [128, 8], F32)
        om8 = st.sb("om8", [128, 16, 128], BF16)
        ps_s = [st.ps("ps_s%d" % i, [128, 512], F32) for i in range(2)]
        ps_o = [st.ps("ps_o%d" % i, [128, 512], F32) for i in range(4)]
        ps_d = st.ps("ps_d", [128, 512], F32)
        ptr = st.ps("ptr", [128, 512], F32)
        make_ident(S, identf, "ident")
        o_memset(S, "dve", onesb[:], 1.0, ["onesb"])
        o_memset(S, "pool", om8[:].rearrange("p a b -> p (a b)"), 0.0, ["om8"])
        o_dma(S, "sp", mkT[:], T["mkT_scr"].rearrange("c p t -> p c t"), writes=["mkT"], grp="mkT")
        o_dma(S, "sp", mv[:], T["mv_scr"][:, :].rearrange("(t p) d -> p t d", p=128), writes=["mv"], grp="mv")
        o_dma(S, "sp", qm[:], T["qmT"].rearrange("c p t -> p c t"), writes=["qm"], grp="qm")
        si = 0
        oi = 0
        for hh in range(4):
            for blk in range(2):
                c0 = blk * 512
                ob_ = oi % 2
                oi += 1
                for kc_ in range(2):
                    sl = si % 2
                    si += 1
                    for j in range(4):
                        o_mm(S, ps_s[sl][:, :], mkT[:, 4 * hh + j, kc_ * 128:(kc_ + 1) * 128], qm[:, 4 * hh + j, c0:c0 + 512], j == 0, j == 3,
                             reads=["mkT", "qm"], writes=[("ps_s", sl)])
                    o_act(S, p[:, sl, :], ps_s[sl][:, :], AF.Exp, reads=[("ps_s", sl)], writes=[("p", sl)], scale=SC_MEM)
                    for j in range(4):
                        o_mm(S, ps_o[j][:, :], mv[:, kc_, (4 * hh + j) * 128:(4 * hh + j + 1) * 128], p[:, sl, :], kc_ == 0, kc_ == 1,
                             reads=["mv", ("p", sl)], writes=[("ps_o", j)])
                    o_mm(S, ps_d[:, :], onesb[:], p[:, sl, :], kc_ == 0, kc_ == 1, reads=["onesb", ("p", sl)], writes=["ps_d"])
                o_recip(S, rd[:], ps_d[:, :], reads=["ps_d"], writes=["rd"])
                for j in range(4):
                    o_tt(S, "dve", om[:, ob_, j, :], ps_o[j][:, :], rd[:], ALU.mult, reads=[("ps_o", j), "rd"], writes=[("om", ob_)])
                o_dma(S, "sp", T["omT"][4 * hh:4 * hh + 4, :, c0:c0 + 512].rearrange("c p t -> p c t"), om[:, ob_, :, :],
                      reads=[("om", ob_)], grp="om%d" % ob_)
        for s in range(4):
            q0 = 1024 + 32 * s
            o_dma(S, "sp", ck[:], T["cmk"][s, :, :].rearrange("(t p) d -> p t d", p=128), writes=[("X", 0), ("X", 1)], grp="ck")
            o_dma(S, "pool", sv[:], T["cmv"][s, :, :].rearrange("(t p) d -> p t d", p=128), writes=["sv"], grp="sv")
            for t in range(2):
                for b4 in range(4):
                    for q in range(4):
                        c = b4 * 4 + q
                        o_tr(S, ptr[:, q * 128:(q + 1) * 128], ck[:, t, c * 128:(c + 1) * 128], identf[:],
                             reads=[("X", t), "ident"], writes=["ptr"])
                    o_copy(S, "act" if b4 % 2 == 0 else "dve", skT[:, b4 * 4:(b4 + 1) * 4, t * 128:(t + 1) * 128],
                           ptr[:, :].rearrange("p (a b) -> p a b", a=4), reads=["ptr"], writes=["skT"])
            for hh in range(4):
                for kc_ in range(2):
                    for j in range(4):
                        o_mm(S, ps_s[0][:, kc_ * 8:(kc_ + 1) * 8], skT[:, 4 * hh + j, kc_ * 128:(kc_ + 1) * 128], qm[:, 4 * hh + j, q0:q0 + 8],
                             j == 0, j == 3, reads=["skT", "qm"], writes=[("ps_s", 0)])
                o_act(S, p8[:].rearrange("p a b -> p (a b)"), ps_s[0][:, 0:16], AF.Exp, reads=[("ps_s", 0)], writes=["p8"], scale=SC_MEM)
                for j in range(4):
                    for kc_ in range(2):
                        o_mm(S, ps_o[j][:, 0:8], sv[:, kc_, (4 * hh + j) * 128:(4 * hh + j + 1) * 128], p8[:, kc_, :], kc_ == 0, kc_ == 1,
                             reads=["sv", "p8"], writes=[("ps_o", j)])
                for kc_ in range(2):
                    o_mm(S, ps_d[:, 0:8], onesb[:], p8[:, kc_, :], kc_ == 0, kc_ == 1, reads=["onesb", "p8"], writes=["ps_d"])
                o_recip(S, rd8[:], ps_d[:, 0:8], reads=["ps_d"], writes=["rd8"])
                for j in range(4):
                    o_tt(S, "dve", om8[:, 4 * hh + j, 32 * s:32 * s + 8], ps_o[j][:, 0:8], rd8[:], ALU.mult,
                         reads=[("ps_o", j), "rd8"], writes=["om8"])
        o_dma(S, "sp", T["omT"][:, :, 1024:TOK].rearrange("c p t -> p c t"), om8[:, :, :], reads=["om8"], grp="om8")
        S.emit()


def stage_F1(nc, S, T):
    with St(nc, "F1") as st:
        ident = st.sb("ident", [128, 128], F32)
        X = st.sb("X", [128, NT, 2048], F32)
        xT = st.sb("xT", [128, 16, TOK], BF16)
        xTf = st.sb("xTf", [128, 16, 128], F32)
        Wr = st.sb("Wr", [128, 16, 36], F32)
        br = st.sb("br", [128, 36], F32)
        lg = st.sb("lg", [128, 36], F32)
        G = st.sb("G", [128, NT, 32], F32)
        gm = st.sb("gm", [128, 1], F32)
        goh = st.sb("goh", [128, 4], F32)
        ge = st.sb("ge", [128, 4], F32)
        gs = st.sb("gs", [128, 1], F32)
        gw = st.sb("gw", [128, 1], F32)
        es = st.sb("es", [128, 8], F32)
        m1 = st.sb("m1", [128, 1], F32)
        oh1 = st.sb("oh1", [128, 8], F32)
        em = st.sb("em", [128, 8], F32)
        m2 = st.sb("m2", [128, 1], F32)
        oh2 = st.sb("oh2", [128, 8], F32)
        dd = st.sb("dd", [128, 1], F32)
        w1 = st.sb("w1", [128, 1], F32)
        w2 = st.sb("w2", [128, 1], F32)
        g8 = st.sb("g8", [128, 8], F32)
        ptr = [st.ps("ptr%d" % i, [128, 512], F32) for i in range(2)]
        pl = st.ps("pl", [128, 64], F32)
        make_ident(S, ident, "ident")
        xin_from_scr(T, "X2")(S, X)
        o_dma(S, "sp", Wr[:], T["wr"][:, :].rearrange("(c p) n -> p c n", p=128), writes=["Wr"], grp="Wr")
        o_dma(S, "sp", br[:], T["br"].partition_broadcast(128), writes=["br"], grp="br")
        R = ["rt"]
        for t in range(NT):
            transpose_block(S, X, t, ident, ptr, xT, "xT", 0, f32copy=xTf)
            for k in range(16):
                o_mm(S, pl[:, 0:36], xTf[:, k, :], Wr[:, k, :], k == 0, k == 15, reads=["f32copy", "Wr"], writes=["pl"])
            o_tt(S, "dve", lg[:], pl[:, 0:36], br[:], ALU.add, reads=["pl", "br"], writes=R)
            S.op("dve", lambda h: h.reduce_max(out=gm[:], in_=lg[:, 0:4], axis=mybir.AxisListType.X), reads=R, writes=R)
            o_ts(S, "dve", goh[:], lg[:, 0:4], gm[:, 0:1], ALU.is_equal, reads=R, writes=R)
            o_ts(S, "dve", ge[:], lg[:, 0:4], gm[:, 0:1], ALU.subtract, reads=R, writes=R)
            o_act(S, ge[:], ge[:], AF.Exp, reads=R, writes=R)
            S.op("dve", lambda h: h.reduce_sum(out=gs[:], in_=ge[:], axis=mybir.AxisListType.X), reads=R, writes=R)
            o_recip(S, gw[:], gs[:], reads=R, writes=R)
            o_ts(S, "dve", es[:], lg[:, 4:12], goh[:, 0:1], ALU.mult, reads=R, writes=R)
            for g in range(1, 4):
                o_stt(S, es[:], lg[:, 4 + 8 * g:12 + 8 * g], goh[:, g:g + 1], es[:], ALU.mult, ALU.add, reads=R, writes=R)
            S.op("dve", lambda h: h.reduce_max(out=m1[:], in_=es[:], axis=mybir.AxisListType.X), reads=R, writes=R)
            o_ts(S, "dve", oh1[:], es[:], m1[:, 0:1], ALU.is_equal, reads=R, writes=R)
            o_stt(S, em[:], oh1[:], -1e30, es[:], ALU.mult, ALU.add, reads=R, writes=R)
            S.op("dve", lambda h: h.reduce_max(out=m2[:], in_=em[:], axis=mybir.AxisListType.X), reads=R, writes=R)
            o_ts(S, "dve", oh2[:], em[:], m2[:, 0:1], ALU.is_equal, reads=R, writes=R)
            o_tt(S, "dve", dd[:], m2[:], m1[:], ALU.subtract, reads=R, writes=R)
            o_act(S, dd[:], dd[:], AF.Exp, reads=R, writes=R)
            o_ts(S, "dve", w1[:], dd[:], 1.0, ALU.add, reads=R, writes=R)
            o_recip(S, w1[:], w1[:], reads=R, writes=R)
            o_tt(S, "dve", w1[:], w1[:], gw[:], ALU.mult, reads=R, writes=R)
            o_tt(S, "dve", w2[:], w1[:], dd[:], ALU.mult, reads=R, writes=R)
            o_ts(S, "dve", g8[:], oh1[:], w1[:, 0:1], ALU.mult, reads=R, writes=R)
            o_stt(S, g8[:], oh2[:], w2[:, 0:1], g8[:], ALU.mult, ALU.add, reads=R, writes=R)
            for g in range(4):
                o_ts(S, "dve", G[:, t, 8 * g:8 * g + 8], g8[:], goh[:, g:g + 1], ALU.mult, reads=R, writes=["G"])
            o_copy(S, "act", X[:, t, :], X[:, t, :], reads=[("X", t)], writes=[("X", t)], scale=ALPHA)
            o_dma(S, "sp", T["X3"][t * 128:(t + 1) * 128, :], X[:, t, :], reads=[("X", t)], grp="xo%d" % (t % 2))
        o_dma(S, "sp", T["x2T"].rearrange("c p t -> p c t"), xT[:, :, :], reads=["xT"], grp="xTst")
        o_dma(S, "sp", T["G"][:, :], G[:].rearrange("p a b -> p (a b)"), reads=["G"], grp="Gst")
        S.emit()


def stage_F2(nc, S, T):
    with St(nc, "F2") as st:
        X = st.sb("X", [128, NT, 2048], F32)
        xT = st.sb("xT", [128, 16, TOK], BF16)
        G = st.sb("G", [128, NT, 32], F32)
        Wg = st.sb("Wg", [128, 16, 512], BF16)
        Wu = st.sb("Wu", [128, 16, 512], BF16)
        Wd = st.sb("Wd", [128, 4, 2048], BF16)
        hT = st.sb("hT", [128, 4, TOK], BF16)
        sg = st.sb("sg", [128, 2, 512], F32)
        pg = [st.ps("pg%d" % i, [128, 512], F32) for i in range(2)]
        pu = [st.ps("pu%d" % i, [128, 512], F32) for i in range(2)]
        po = [st.ps("po%d" % i, [128, 512], F32) for i in range(4)]
        xin_from_scr(T, "X3")(S, X)
        o_dma(S, "sp", xT[:], T["x2T"].rearrange("c p t -> p c t"), writes=["xT"], grp="xT")
        o_dma(S, "sp", G[:].rearrange("p a b -> p (a b)"), T["G"][:, :], writes=["G"], grp="G")
        i = 0
        oi = 0
        for e in range(32):
            o_dma(S, "pool", Wg[:], T["w_gate"][e, :, :].rearrange("(c p) n -> p c n", p=128), writes=["Wg"], grp="Wg")
            o_dma(S, "pool", Wu[:], T["w_up"][e, :, :].rearrange("(c p) n -> p c n", p=128), writes=["Wu"], grp="Wu")
            o_dma(S, "pool", Wd[:], T["w_down"][e, :, :].rearrange("(c p) n -> p c n", p=128), writes=["Wd"], grp="Wd")
            for f in range(4):
                for (c0, w) in [(0, 512), (512, 512), (1024, 128)]:
                    pi = i % 2
                    i += 1
                    for k in range(16):
                        o_mm(S, pg[pi][:, 0:w], Wg[:, k, f * 128:(f + 1) * 128], xT[:, k, c0:c0 + w], k == 0, k == 15,
                             reads=["Wg", "xT"], writes=[("pg", pi)])
                    for k in range(16):
                        o_mm(S, pu[pi][:, 0:w], Wu[:, k, f * 128:(f + 1) * 128], xT[:, k, c0:c0 + w], k == 0, k == 15,
                             reads=["Wu", "xT"], writes=[("pu", pi)])
                    o_act(S, sg[:, pi, 0:w], pg[pi][:, 0:w], AF.Silu, reads=[("pg", pi)], writes=[("sg", pi)])
                    o_tt(S, "dve", hT[:, f, c0:c0 + w], sg[:, pi, 0:w], pu[pi][:, 0:w], ALU.mult,
                         reads=[("sg", pi), ("pu", pi)], writes=[("hT", f)])
            for t in range(NT):
                for n in range(4):
                    pi = oi % 4
                    oi += 1
                    for f in range(4):
                        o_mm(S, po[pi][:, :], hT[:, f, t * 128:(t + 1) * 128], Wd[:, f, n * 512:(n + 1) * 512], f == 0, f == 3,
                             reads=[("hT", f), "Wd"], writes=[("po", pi)])
                    o_stt(S, X[:, t, n * 512:(n + 1) * 512], po[pi][:, :], G[:, t, e:e + 1], X[:, t, n * 512:(n + 1) * 512],
                          ALU.mult, ALU.add, reads=[("po", pi), "G", ("X", t)], writes=[("X", t)])
        for t in range(NT):
            o_dma(S, "sp", T["X1"][t * 128:(t + 1) * 128, :], X[:, t, :], reads=[("X", t)], grp="xo%d" % (t % 2))
        S.emit()


def stage_F3(nc, S, T):
    with St(nc, "F3") as st:
        X = st.sb("X", [128, NT, 2048], F32)
        lng = st.sb("lng", [128, 2048], F32)
        lnb = st.sb("lnb", [128, 2048], F32)
        stats = st.sb("stats", [128, 4, 6], F32)
        mv = st.sb("mv", [128, 2], F32)
        rs = st.sb("rs", [128, 1], F32)
        xin_from_scr(T, "X1")(S, X)
        o_dma(S, "sp", lng[:], T["ln3_g"].partition_broadcast(128), writes=["lng"], grp="lng")
        o_dma(S, "sp", lnb[:], T["ln3_b"].partition_broadcast(128), writes=["lnb"], grp="lnb")
        for t in range(NT):
            layernorm_tile(S, st, X, t, lng, lnb, stats, mv, rs, ("X", t))
            o_dma(S, "sp", T["y"][t * 128:(t + 1) * 128, :], X[:, t, :], reads=[("X", t)], grp="xo%d" % (t % 2))
        S.emit()


IN_SPECS = [
    ("xw", [4096, 2048]), ("xs", [128, 2048]), ("vbias", [128, 24]), ("pmask", [128, 17 * 128]),
    ("smask", [128, 128]), ("smaskn", [128, 128]), ("cwk", [4, 2048, 1024]), ("cwv", [4, 2048, 1024]),
    ("h0re", [128, 128]), ("h0im", [128, 128]), ("cmk", [4, 256, 2048]), ("cmv", [4, 256, 2048]),
    ("memp", [256, 2048]), ("w_in", [2048, 4096]), ("lamre", [128, 32]), ("lamim", [128, 32]),
    ("logdt", [128, 32]), ("BTre", [128, 4096]), ("BTim", [128, 4096]), ("CTre", [128, 4096]),
    ("CTim", [128, 4096]), ("dcol", [128, 8]), ("w_glu", [1024, 1024]), ("gatt", [128, 8]), ("gssm", [128, 8]),
    ("w_out", [2048, 2048]), ("ln1_g", [1, 2048]), ("ln1_b", [1, 2048]), ("w_mq", [2048, 2048]),
    ("w_mk", [2048, 2048]), ("w_mv", [2048, 2048]), ("w_mo", [2048, 2048]), ("ln2_g", [1, 2048]),
    ("ln2_b", [1, 2048]), ("wr", [2048, 36]), ("br", [1, 36]), ("w_gate", [32, 2048, 512]),
    ("w_up", [32, 2048, 512]), ("w_down", [32, 512, 2048]), ("ln3_g", [1, 2048]), ("ln3_b", [1, 2048]),
]
OUT_SPECS = [
    ("y", [TOK, 2048]), ("Kout", [TOK, 1024]), ("Vout", [TOK, 1024]), ("hfr", [128, 32]), ("hfi", [128, 32]),
    ("hsr", [128, 128]), ("hsi", [128, 128]), ("memKo", [256, 2048]), ("memVo", [256, 2048]),
]
SCR_SPECS = [
    ("qT", [8, 128, TOK], BF16), ("kT", [8, 128, KPOS], BF16), ("V_scr", [KPOS, 1024], BF16), ("uT", [8, 128, UPOS], BF16),
    ("mixT", [16, 128, TOK], BF16), ("rstd_a", [128, NT], F32), ("rstd_s", [128, NT], F32),
    ("ygT", [8, 128, TOK], F32), ("ygTb", [8, 128, TOK], BF16), ("X1", [TOK, 2048], F32), ("X2", [TOK, 2048], F32),
    ("X3", [TOK, 2048], F32), ("mkT_scr", [16, 128, 256], BF16), ("mv_scr", [256, 2048], BF16),
    ("qmT", [16, 128, TOK], BF16), ("omT", [16, 128, TOK], BF16), ("x2T", [16, 128, TOK], BF16), ("G", [128, NT * 32], F32),
]


def build_program(stages=None, debug=False):
    nc = bass.Bass("TRN2", target_bir_lowering=False)
    T = {}
    for (n, s) in IN_SPECS:
        T[n] = nc.dram_tensor(n, s, F32, kind="ExternalInput").ap()
    for (n, s) in OUT_SPECS:
        T[n] = nc.dram_tensor(n, s, F32, kind="ExternalOutput").ap()
    for (n, s, d) in SCR_SPECS:
        T[n] = nc.dram_tensor("scr_" + n, s, d, kind=("ExternalOutput" if debug else "Internal")).ap()
    S = Sched(nc)
    table = [
        ("A", lambda: stage_A(nc, S, T)),
        ("B", lambda: stage_B(nc, S, T)),
        ("C", lambda: stage_C(nc, S, T)),
        ("D1", lambda: stage_D1(nc, S, T)),
        ("D2", lambda: linear_residual_ln(nc, S, T, "D2", "mixT", "w_out", "ln1_g", "ln1_b", xin_from_inputs(T), "X1", True)),
        ("E0", lambda: stage_E0(nc, S, T)),
        ("E1", lambda: stage_E1(nc, S, T)),
        ("E2", lambda: stage_E2(nc, S, T)),
        ("E3", lambda: linear_residual_ln(nc, S, T, "E3", "omT", "w_mo", "ln2_g", "ln2_b", xin_from_scr(T, "X1"), "X2", False)),
        ("F1", lambda: stage_F1(nc, S, T)),
        ("F2", lambda: stage_F2(nc, S, T)),
        ("F3", lambda: stage_F3(nc, S, T)),
    ]
    for (nm, fn) in table:
        if stages is None or nm in stages:
            fn()
    S.close()
    return nc


def _mult(diff):
    m = ((diff >= 0) & (diff <= 128)).astype(np.float32)
    m += ((diff >= 0) & (diff <= 512) & (diff % 4 == 0)).astype(np.float32)
    m += ((diff >= 0) & (diff <= 2048) & (diff % 16 == 0)).astype(np.float32)
    return m


def make_inputs(inp):
    f = np.float32
    xp = inp["x_prompt"]
    xsm = inp["x_sample"]
    j = np.arange(128)
    pm = np.zeros((128, 17, 128), f)
    for cc in range(17):
        diff = (16 - cc) * 128 + j[None, :] - j[:, None]
        pm[:, cc, :] = _mult(diff)
    sm = np.zeros((128, 16, 8), f)
    for cc in range(16):
        cpos = cc * 128 + j[:, None]
        diff = np.arange(8)[None, :] + 2048 - cpos
        sm[:, cc, :] = _mult(diff)
    smn = np.zeros((128, 128), f)
    d8 = np.arange(8)[None, :] - np.arange(8)[:, None]
    for s in range(4):
        smn[32 * s:32 * s + 8, 32 * s:32 * s + 8] = _mult(d8)
    br_, bi_ = inp["ssm_b_re"][0], inp["ssm_b_im"][0]
    cr_, ci_ = inp["ssm_c_re"][0], inp["ssm_c_im"][0]
    BTre = np.zeros((128, 32, 128), f)
    BTim = np.zeros((128, 32, 128), f)
    CTre = np.zeros((128, 32, 128), f)
    CTim = np.zeros((128, 32, 128), f)
    for tau in range(32):
        for gl in range(2):
            g = 2 * tau + gl
            r0 = 32 * (tau % 4) + 16 * gl
            BTre[r0:r0 + 16, tau, 64 * gl:64 * gl + 64] = br_[g].T
            BTim[r0:r0 + 16, tau, 64 * gl:64 * gl + 64] = bi_[g].T
            CTre[64 * gl:64 * gl + 64, tau, r0:r0 + 16] = cr_[g].T
            CTim[64 * gl:64 * gl + 64, tau, r0:r0 + 16] = ci_[g].T
    common = {
        "pmask": pm.reshape(128, -1), "smask": sm.reshape(128, -1), "smaskn": smn,
        "w_in": inp["w_in"][0],
        "lamre": np.ascontiguousarray(inp["ssm_lam_re"][0].reshape(32, 128).T),
        "lamim": np.ascontiguousarray(inp["ssm_lam_im"][0].reshape(32, 128).T),
        "logdt": np.ascontiguousarray(np.repeat(inp["ssm_log_dt"][0].reshape(32, 2), 64, axis=1).T),
        "BTre": BTre.reshape(128, -1), "BTim": BTim.reshape(128, -1), "CTre": CTre.reshape(128, -1), "CTim": CTim.reshape(128, -1),
        "dcol": np.ascontiguousarray(inp["ssm_d"][0].reshape(8, 128).T),
        "w_glu": inp["w_glu"][0],
        "gatt": np.ascontiguousarray(inp["g_attn"][0].reshape(8, 128).T),
        "gssm": np.ascontiguousarray(inp["g_ssm"][0].reshape(8, 128).T),
        "w_out": inp["w_out"][0], "ln1_g": inp["ln1_g"], "ln1_b": inp["ln1_b"],
        "w_mq": inp["w_mq"][0], "w_mk": inp["w_mk"][0], "w_mv": inp["w_mv"][0], "w_mo": inp["w_mo"][0],
        "ln2_g": inp["ln2_g"], "ln2_b": inp["ln2_b"],
        "wr": np.ascontiguousarray(np.concatenate([inp["w_r1"][0], inp["w_r2"][0].reshape(2048, 32)], axis=1)),
        "br": np.ascontiguousarray(np.concatenate([inp["b_r1"][0], inp["b_r2"][0].reshape(32)])[None, :]),
        "w_gate": inp["w_gate"][0], "w_up": inp["w_up"][0], "w_down": inp["w_down"][0],
        "ln3_g": inp["ln3_g"], "ln3_b": inp["ln3_b"],
    }
    maps = []
    for c in range(8):
        b, r = c // 4, c % 4
        xw = np.zeros((4096, 2048), f)
        lo = 1024 * r - 3072
        src0 = max(lo, 0)
        xw[src0 - lo:, :] = xp[b, src0:1024 * r + 1024, :]
        xs = np.zeros((128, 2048), f)
        for s in range(4):
            xs[32 * s:32 * s + 8, :] = xsm[4 * c + s]
        vb = np.ones((128, 24), f)
        for jj in range(16):
            if 8 * r - 16 + jj < 0:
                vb[:, jj] = 0.0
        m = dict(common)
        m.update({
            "xw": xw, "xs": xs, "vbias": vb,
            "cwk": np.ascontiguousarray(inp["cache_win_k"][0, 4 * c:4 * c + 4].reshape(4, 2048, 1024)),
            "cwv": np.ascontiguousarray(inp["cache_win_v"][0, 4 * c:4 * c + 4].reshape(4, 2048, 1024)),
            "h0re": np.ascontiguousarray(inp["state_ssm_re"][0, 4 * c:4 * c + 4].reshape(4, 32, 128).transpose(2, 1, 0).reshape(128, 128)),
            "h0im": np.ascontiguousarray(inp["state_ssm_im"][0, 4 * c:4 * c + 4].reshape(4, 32, 128).transpose(2, 1, 0).reshape(128, 128)),
            "cmk": np.ascontiguousarray(inp["cache_mem_k"][0, 4 * c:4 * c + 4].reshape(4, 256, 2048)),
            "cmv": np.ascontiguousarray(inp["cache_mem_v"][0, 4 * c:4 * c + 4].reshape(4, 256, 2048)),
            "memp": np.ascontiguousarray(inp["mem_prompt"][b]),
        })
        maps.append({k: np.ascontiguousarray(v, dtype=f) for k, v in m.items()})
    return maps


def assemble(res):
    f = np.float32
    yp = np.zeros((2, 4096, 2048), f)
    ys = np.zeros((32, 8, 2048), f)
    wkp = np.zeros((1, 2, 2048, 8, 128), f)
    wvp = np.zeros((1, 2, 2048, 8, 128), f)
    wks = np.zeros((1, 32, 8, 8, 128), f)
    wvs = np.zeros((1, 32, 8, 8, 128), f)
    srp = np.zeros((1, 2, 64, 64), f)
    sip = np.zeros((1, 2, 64, 64), f)
    srs = np.zeros((1, 32, 64, 64), f)
    sis = np.zeros((1, 32, 64, 64), f)
    mkp = np.zeros((1, 2, 256, 4, 512), f)
    mvp = np.zeros((1, 2, 256, 4, 512), f)
    for c in range(8):
        r_ = res[c]
        b, r = c // 4, c % 4
        yp[b, 1024 * r:1024 * r + 1024] = r_["y"][0:1024]
        for s in range(4):
            ys[4 * c + s] = r_["y"][1024 + 32 * s:1024 + 32 * s + 8]
            wks[0, 4 * c + s] = r_["Kout"][1024 + 32 * s:1024 + 32 * s + 8].reshape(8, 8, 128)
            wvs[0, 4 * c + s] = r_["Vout"][1024 + 32 * s:1024 + 32 * s + 8].reshape(8, 8, 128)
        if r >= 2:
            wkp[0, b, 1024 * (r - 2):1024 * (r - 1)] = r_["Kout"][0:1024].reshape(1024, 8, 128)
            wvp[0, b, 1024 * (r - 2):1024 * (r - 1)] = r_["Vout"][0:1024].reshape(1024, 8, 128)
        if r == 3:
            srp[0, b] = r_["hfr"].T.reshape(64, 64)
            sip[0, b] = r_["hfi"].T.reshape(64, 64)
        hs_r = r_["hsr"].reshape(128, 32, 4).transpose(2, 1, 0).reshape(4, 64, 64)
        hs_i = r_["hsi"].reshape(128, 32, 4).transpose(2, 1, 0).reshape(4, 64, 64)
        srs[0, 4 * c:4 * c + 4] = hs_r
        sis[0, 4 * c:4 * c + 4] = hs_i
        if r == 0:
            mkp[0, b] = r_["memKo"].reshape(256, 4, 512)
            mvp[0, b] = r_["memVo"].reshape(256, 4, 512)
    return (yp, ys, wkp, wvp, wks, wvs, srp, sip, srs, sis, mkp, mvp)


def kernel(**inputs):
    inp = {k: np.asarray(v) for k, v in inputs.items()}
    maps = make_inputs(inp)
    nc = build_program()
    res = run_bass_kernel_spmd(nc, maps, core_ids=list(range(8)))
    return assemble(res.results)
```

```python
import numpy as np
from contextlib import ExitStack
import concourse.bass as bass
import concourse.mybir as mybir
from concourse.bass_utils import run_bass_kernel_spmd

F32 = mybir.dt.float32
BF16 = mybir.dt.bfloat16
AF = mybir.ActivationFunctionType
ALU = mybir.AluOpType

ENGS = ("pe", "act", "dve", "pool", "sp")
NT = 9
TOK = 1152
KPOS = 3200
UPOS = 4224
ALPHA = 2.0 ** 0.25
SC_ATT = 128 ** -0.5
SC_MEM = 512 ** -0.5
NEG = -30000.0
DBG = {}
PSUM_KEYS = {"ps_big", "ps_n", "ptr", "pmm", "ps_s", "ps_nd", "ptk", "ps_sm", "ps_snd", "ps_ss", "ps_new", "pw", "py", "pz", "pa", "pb",
             "pm", "pk", "pq", "ps_o", "ps_d", "pl", "pg", "pu", "po"}


class Sched:
    def __init__(self, nc, n_dma_sems=22):
        self.nc = nc
        self.stack = []
        self.esem = {}
        for e in ENGS:
            self.esem[e] = self._sem("e_" + e)
        self.ecount = {e: 0 for e in ENGS}
        self.sval = {e: 0 for e in ENGS}
        self.dsems = [self._sem("d%d" % i) for i in range(n_dma_sems)]
        self.dcount = [0] * n_dma_sems
        self.rr = {}
        self.reset_stage()

    def _sem(self, name):
        cm = self.nc.semaphore(name)
        s = cm.__enter__()
        self.stack.append(cm)
        return s

    def reset_stage(self):
        self.ops = []
        self.dmap = {}
        self.lastw = {}
        self.readers = {}
        self.known = {e: {} for e in ENGS}

    def alt(self, name, engines):
        i = self.rr.get(name, 0)
        self.rr[name] = i + 1
        return engines[i % len(engines)]

    def _dsem(self, group):
        if group not in self.dmap:
            idx = len(self.dmap)
            assert idx < len(self.dsems), "too many dma groups in stage"
            self.dmap[group] = idx
        return self.dmap[group]

    def _need(self, eng, dep, waits):
        kind, a, b = dep
        kn = self.known[eng]
        key = (kind, a)
        if kn.get(key, -1) >= b:
            return
        kn[key] = b
        waits.append(dep)

    def op(self, eng, fn, reads=(), writes=(), dma=None):
        waits = []
        deps = []
        if eng != "pe":
            ex = [k for k in reads if (k[0] if isinstance(k, tuple) else k) in PSUM_KEYS]
            if ex:
                writes = list(writes) + [k for k in ex if k not in writes]
        for k in reads:
            if k in self.lastw:
                deps.append(self.lastw[k])
        for k in writes:
            if k in self.lastw:
                deps.append(self.lastw[k])
            deps.extend(self.readers.get(k, ()))
        if dma is not None:
            di = self._dsem(dma)
            if self.dcount[di] > 0:
                deps.append(("d", di, self.dcount[di]))
        for d in deps:
            if d[0] == "e" and d[1] == eng and eng == "pe":
                continue
            self._need(eng, d, waits)
        if dma is not None:
            self.dcount[di] += 16
            tok = ("d", di, self.dcount[di])
        else:
            self.ecount[eng] += 1
            tok = ("e", eng, self.ecount[eng])
        for k in writes:
            self.lastw[k] = tok
            self.readers[k] = []
        for k in reads:
            self.readers.setdefault(k, []).append(tok)
        self.ops.append((eng, fn, waits, tok))

    def touch(self, keys, from_keys):
        best = None
        for k in from_keys:
            t = self.lastw.get(k)
            if t is not None and (best is None or t[2] > best[2]):
                best = t
        if best is not None:
            for k in keys:
                self.lastw[k] = best
                self.readers[k] = []

    def emit(self):
        nc = self.nc
        fin = [("d", di, self.dcount[di]) for g, di in self.dmap.items()]
        per = {e: [] for e in ENGS}
        need = set()
        for o in self.ops:
            per[o[0]].append(o)
            for (kind, a, b) in o[2]:
                if kind == "e":
                    need.add((a, b))
        sval = self.sval
        smap = {}
        for (eng, fn, waits, tok) in self.ops:
            if tok[0] == "e" and (tok[1], tok[2]) in need:
                sval[eng] += 1
                smap[(tok[1], tok[2])] = sval[eng]
        esem, dsems = self.esem, self.dsems

        def run(engname, h):
            for (_, fn, waits, tok) in per[engname]:
                for (kind, a, b) in waits:
                    if kind == "e":
                        h.wait_ge(esem[a], smap[(a, b)])
                    else:
                        h.wait_ge(dsems[a], b)
                ins = fn(h)
                if tok[0] == "e":
                    if (tok[1], tok[2]) in smap:
                        ins.then_inc(esem[engname], 1)
                else:
                    ins.then_inc(dsems[tok[1]], 16)
            if engname == "sp":
                for (kind, a, b) in fin:
                    h.wait_ge(dsems[a], b)

        with nc.Block() as block:
            @block.tensor
            def _(h):
                run("pe", h)

            @block.scalar
            def _(h):
                run("act", h)

            @block.vector
            def _(h):
                run("dve", h)

            @block.gpsimd
            def _(h):
                run("pool", h)

            @block.sync
            def _(h):
                run("sp", h)
        self.reset_stage()

    def close(self):
        for cm in reversed(self.stack):
            cm.__exit__(None, None, None)


def o_mm(S, out, lhsT, rhs, start, stop, reads, writes):
    S.op("pe", lambda h: h.matmul(out, lhsT=lhsT, rhs=rhs, start=start, stop=stop), reads=reads, writes=writes)


def o_tr(S, out, in_, ident, reads, writes):
    S.op("pe", lambda h: h.transpose(out=out, in_=in_, identity=ident), reads=reads, writes=writes)


def o_dma(S, eng, out, in_, reads=(), writes=(), grp=None, slow=False):
    if slow:
        S.op(eng, lambda h: h.dma_start(out=out, in_=in_, allow_slow_non_contiguous=True), reads=reads, writes=writes, dma=grp)
    else:
        S.op(eng, lambda h: h.dma_start(out=out, in_=in_), reads=reads, writes=writes, dma=grp)


def o_copy(S, eng, out, in_, reads, writes, scale=None):
    if eng == "act":
        if scale is None:
            S.op("act", lambda h: h.activation(out=out, in_=in_, func=AF.Copy), reads=reads, writes=writes)
        else:
            S.op("act", lambda h: h.activation(out=out, in_=in_, func=AF.Identity, scale=scale), reads=reads, writes=writes)
    else:
        if scale is None:
            S.op(eng, lambda h: h.tensor_copy(out=out, in_=in_), reads=reads, writes=writes)
        else:
            S.op(eng, lambda h: h.tensor_scalar(out=out, in0=in_, scalar1=scale, scalar2=None, op0=ALU.mult), reads=reads, writes=writes)


def o_act(S, out, in_, func, reads, writes, bias=None, scale=None):
    kw = {}
    if bias is not None:
        kw["bias"] = bias
    if scale is not None:
        kw["scale"] = scale
    S.op("act", lambda h: h.activation(out=out, in_=in_, func=func, **kw), reads=reads, writes=writes)


def o_tt(S, eng, out, in0, in1, op, reads, writes):
    S.op(eng, lambda h: h.tensor_tensor(out=out, in0=in0, in1=in1, op=op), reads=reads, writes=writes)


def o_ts(S, eng, out, in0, s1, op0, reads, writes, s2=None, op1=None):
    if op1 is None:
        S.op(eng, lambda h: h.tensor_scalar(out=out, in0=in0, scalar1=s1, scalar2=None, op0=op0), reads=reads, writes=writes)
    else:
        S.op(eng, lambda h: h.tensor_scalar(out=out, in0=in0, scalar1=s1, scalar2=s2, op0=op0, op1=op1), reads=reads, writes=writes)


def o_stt(S, out, in0, scalar, in1, op0, op1, reads, writes):
    S.op("dve", lambda h: h.scalar_tensor_tensor(out=out, in0=in0, scalar=scalar, in1=in1, op0=op0, op1=op1), reads=reads, writes=writes)


def o_memset(S, eng, ap, val, writes):
    S.op(eng, lambda h: h.memset(ap, val), writes=writes)


def o_recip(S, out, in_, reads, writes):
    S.op("dve", lambda h: h.reciprocal(out=out, in_=in_), reads=reads, writes=writes)


def make_ident(S, t, key):
    o_memset(S, "pool", t[:], 0.0, [key])
    S.op("pool", lambda h: h.affine_select(out=t[:], in_=t[:], pattern=[[-1, 128]], compare_op=ALU.not_equal,
                                           fill=1.0, base=0, channel_multiplier=1), reads=[key], writes=[key])


class St:
    def __init__(self, nc, name):
        self.nc = nc
        self.name = name
        self.es = ExitStack()

    def __enter__(self):
        self.es.__enter__()
        return self

    def __exit__(self, *a):
        return self.es.__exit__(*a)

    def sb(self, n, shape, dt):
        return self.es.enter_context(self.nc.sbuf_tensor(self.name + "_" + n, shape, dt))

    def ps(self, n, shape, dt):
        return self.es.enter_context(self.nc.psum_tensor(self.name + "_" + n, shape, dt))


def stage_A(nc, S, T):
    with St(nc, "A") as st:
        Wb = st.sb("Wb", [128, 16, 4096], BF16)
        ident = st.sb("ident", [128, 128], F32)
        xin = st.sb("xin", [128, 2, 2048], F32)
        xT = st.sb("xT", [128, 16, 512], BF16)
        ofm = st.sb("ofm", [128, 4, 512], BF16)
        otf = st.sb("otf", [128, 2, 512], F32)
        otb = st.sb("otb", [128, 2, 512], BF16)
        ptr = [st.ps("ptr%d" % i, [128, 512], F32) for i in range(2)]
        pmm = [st.ps("pmm%d" % i, [128, 512], F32) for i in range(4)]
        make_ident(S, ident, "ident")
        w_in = T["w_in"]
        for blk in (6, 7, 2, 3, 4, 5, 0, 1):
            o_dma(S, "pool", Wb[:, :, blk * 512:(blk + 1) * 512],
                  w_in[:, blk * 512:(blk + 1) * 512].rearrange("(c p) n -> p c n", p=128),
                  writes=[("Wb", blk)], grp="Wb%d" % blk)
        groups = [[4 * g + j for j in range(4)] for g in range(8)] + [[32]]
        if DBG.get("A_groups") is not None:
            groups = [groups[i] for i in DBG["A_groups"]]
        cnt = {"x": 0, "tr": 0, "fm": 0, "mm": 0, "tf": 0, "tb": 0}
        for tiles in groups:
            ntok = 128 * len(tiles)
            t0 = tiles[0]
            far = t0 < 8
            near = 8 <= t0 < 24
            own = t0 >= 24
            for j, tl in enumerate(tiles):
                slot = cnt["x"] % 2
                cnt["x"] += 1
                src = T["xs"][:, :] if tl == 32 else T["xw"][tl * 128:(tl + 1) * 128, :]
                o_dma(S, "sp", xin[:, slot, :], src, writes=[("xin", slot)], grp="xin%d" % slot)
                for b4 in range(4):
                    pi = cnt["tr"] % 2
                    cnt["tr"] += 1
                    for q in range(4):
                        c = b4 * 4 + q
                        o_tr(S, ptr[pi][:, q * 128:(q + 1) * 128], xin[:, slot, c * 128:(c + 1) * 128], ident[:],
                             reads=[("xin", slot), "ident"] + ([("Wb", i) for i in range(8)] if DBG.get("A_waitW") else []), writes=[("ptr", pi)])
                    o_copy(S, "act" if pi == 0 else "dve", xT[:, b4 * 4:(b4 + 1) * 4, j * 128:(j + 1) * 128],
                           ptr[pi][:, :].rearrange("p (a b) -> p a b", a=4), reads=[("ptr", pi)], writes=["xT"])
            if t0 == 32:
                qcol, kcol, ucol = 1024, 3072, 4096
            else:
                qcol, kcol, ucol = (t0 - 24) * 128, (t0 - 8) * 128, t0 * 128
            fm = []
            if own:
                fm += [(h * 128, T["qT"][h, :, qcol:qcol + ntok]) for h in range(8)]
            if own or near:
                fm += [(1024 + h * 128, T["kT"][h, :, kcol:kcol + ntok]) for h in range(8)]
            fm += [(3072 + c * 128, T["uT"][c, :, ucol:ucol + ntok]) for c in range(8)]
            if DBG.get("A_nofm"):
                fm = []
            if DBG.get("A_fmn") is not None:
                fm = fm[:DBG["A_fmn"]]
            for (wc, dst) in fm:
                pi = cnt["mm"] % 4
                cnt["mm"] += 1
                for k in range(16):
                    o_mm(S, pmm[pi][:, 0:ntok], Wb[:, k, wc:wc + 128], xT[:, k, 0:ntok], k == 0, k == 15,
                         reads=["xT", ("Wb", wc // 512)], writes=[("pmm", pi)])
                oi = cnt["fm"] % 4
                cnt["fm"] += 1
                o_copy(S, "act" if oi % 2 == 0 else "dve", ofm[:, oi, 0:ntok], pmm[pi][:, 0:ntok],
                       reads=[("pmm", pi)], writes=[("ofm", oi)])
                o_dma(S, DBG.get("A_stq", "sp"), dst, ofm[:, oi, 0:ntok], reads=[("ofm", oi)], grp="ofm%d" % oi)
            if (own or near) and not DBG.get("A_notm"):
                for j, tl in enumerate(tiles):
                    row_kv = (tl - 8) * 128 if tl < 32 else 3072
                    row_o = (tl - 24) * 128 if tl < 32 else 1024
                    banks = []
                    if own:
                        banks += [("k", 1024), ("k", 1536)]
                    banks += [("v", 2048), ("v", 2560)]
                    if DBG.get("A_banks") is not None:
                        banks = [banks[i] for i in DBG["A_banks"]]
                    for (kv, wc) in banks:
                        pi = cnt["mm"] % 4
                        cnt["mm"] += 1
                        for k in range(16):
                            o_mm(S, pmm[pi][:, :], xT[:, k, j * 128:(j + 1) * 128], Wb[:, k, wc:wc + 512], k == 0, k == 15,
                                 reads=["xT", ("Wb", wc // 512)], writes=[("pmm", pi)])
                        colo = wc - (1024 if kv == "k" else 2048)
                        if own and not DBG.get("A_nootf"):
                            fi = cnt["tf"] % 2
                            cnt["tf"] += 1
                            o_copy(S, DBG.get("A_otfe", "act"), otf[:, fi, :], pmm[pi][:, :], reads=[("pmm", pi)], writes=[("otf", fi)])
                            o_dma(S, DBG.get("A_stq", "sp"), T["Kout" if kv == "k" else "Vout"][row_o:row_o + 128, colo:colo + 512],
                                  otf[:, fi, :], reads=[("otf", fi)], grp="otf%d" % fi)
                        if kv == "v" and not DBG.get("A_nootb"):
                            bi = cnt["tb"] % 2
                            cnt["tb"] += 1
                            o_copy(S, DBG.get("A_otbe", "dve"), otb[:, bi, :], pmm[pi][:, :], reads=[("pmm", pi)] + ([("otf", 0), ("otf", 1)] if DBG.get("A_ser") else []), writes=[("otb", bi)])
                            o_dma(S, DBG.get("A_stq", "sp"), T["V_scr"][row_kv:row_kv + 128, colo:colo + 512], otb[:, bi, :],
                                  reads=[("otb", bi)], grp="otb%d" % bi)
        S.emit()


def stage_B(nc, S, T):
    with St(nc, "B") as st:
        kT = st.sb("kT", [128, 2, KPOS], BF16)
        Vh = st.sb("Vh", [128, 2, 25, 128], BF16)
        qT = st.sb("qT", [128, 2, TOK], BF16)
        pmask = st.sb("pmask", [128, 17, 128], BF16)
        smask = st.sb("smask", [128, 128], BF16)
        smaskn = st.sb("smaskn", [128, 128], BF16)
        vb = st.sb("vb", [128, 24], F32)
        gatt = st.sb("gatt", [128, 8], F32)
        onesb = st.sb("onesb", [128, 128], BF16)
        identb = st.sb("identb", [128, 128], BF16)
        p = st.sb("p", [128, 2, 512], BF16)
        pm = st.sb("pm", [128, 2, 512], BF16)
        onesv = st.sb("onesv", [128, 24, 128], BF16)
        rd = st.sb("rd", [128, 2, 128], F32)
        a32 = st.sb("a32", [128, 2, 128], F32)
        asq = st.sb("asq", [128, 8, TOK], BF16)
        ag = st.sb("ag", [128, 8, TOK], BF16)
        kc = st.sb("kc", [128, 2, 16, 128], BF16)
        vc = st.sb("vc", [128, 2, 16, 128], BF16)
        kcT = st.sb("kcT", [128, 2048], BF16)
        psm = st.sb("psm", [128, 128], BF16)
        psn = st.sb("psn", [128, 128], BF16)
        pmm_ = st.sb("pmm_", [128, 128], BF16)
        pmn = st.sb("pmn", [128, 128], BF16)
        rd8 = st.sb("rd8", [128, 8], F32)
        a8 = st.sb("a8", [128, 8], F32)
        ssa = st.sb("ssa", [128, 16], F32)
        ps_s2 = [st.ps("ps_s%d" % i, [128, 512], F32) for i in range(2)]
        ps_n2 = [st.ps("ps_n%d" % i, [128, 512], F32) for i in range(2)]
        ps_d2 = [st.ps("ps_d%d" % i, [128, 512], F32) for i in range(2)]
        ptk1 = st.ps("ptk", [128, 1024], BF16)
        ptk = [ptk1, ptk1]
        ps_big = st.ps("ps_big", [128, 512], F32)
        ps_sm = ps_big[:, 0:128]
        ps_new = ps_big[:, 128:256]
        ps_snd = ps_big[:, 256:272]
        ps_ss = ps_big[:, 272:288]

        o_memset(S, "dve", onesb[:], 1.0, ["onesb"])
        make_ident(S, identb, "identb")
        o_dma(S, "pool", pmask[:].rearrange("p a b -> p (a b)"), T["pmask"][:, :], writes=["pmask"], grp="pmask")
        o_dma(S, "pool", smask[:], T["smask"][:, :], writes=["smask"], grp="smask")
        o_dma(S, "pool", smaskn[:], T["smaskn"][:, :], writes=["smaskn"], grp="smaskn")
        o_dma(S, "sp", vb[:], T["vbias"][:, :], writes=["vb"], grp="vb")
        o_dma(S, "sp", gatt[:], T["gatt"][:, :], writes=["gatt"], grp="gatt")
        for j in range(24):
            o_ts(S, "pool", onesv[:, j, :], onesb[:], vb[:, j:j + 1], ALU.mult, reads=["onesb", "vb"], writes=["onesv"])
        o_memset(S, "pool", asq[:, :, 1024:TOK], 0.0, [("asq", h, 8) for h in range(8)])
        o_memset(S, "pool", ag[:, :, 1024:TOK], 0.0, [("ag", h, 8) for h in range(8)])
        it = 0
        blk_i = 0
        sh_i = 0
        for h in range(8):
            b = h % 2
            o_dma(S, "sp", kT[:, b, :], T["kT"][h, :, :], writes=[("kT", b)], grp="kT%d" % b)
            o_dma(S, "sp", Vh[:, b, :, :], T["V_scr"][:, h * 128:(h + 1) * 128].rearrange("(c p) n -> p c n", p=128),
                  writes=[("Vh", b)], grp="Vh%d" % b)
            o_dma(S, "sp", qT[:, b, :], T["qT"][h, :, :], writes=[("qT", b)], grp="qT%d" % b)
            groups_ = [(m, c0, n) for m in range(8) for (c0, n) in ((0, 4), (4, 4), (8, 4), (12, 4), (16, 1))]

            def score(gi):
                m, c0, n = groups_[gi]
                sg = (g0 + gi) % 2
                for q in range(n):
                    kcx = m + c0 + q
                    o_mm(S, ps_s2[sg][:, q * 128:(q + 1) * 128], kT[:, b, kcx * 128:(kcx + 1) * 128], qT[:, b, m * 128:(m + 1) * 128],
                         True, True, reads=[("kT", b), ("qT", b)], writes=[("ps_s", sg)])
                o_act(S, p[:, sg, 0:n * 128], ps_s2[sg][:, 0:n * 128], AF.Exp, reads=[("ps_s", sg)], writes=[("p", sg)], scale=SC_ATT)
                o_tt(S, "dve", pm[:, sg, 0:n * 128], p[:, sg, 0:n * 128],
                     pmask[:, c0:c0 + n, :].rearrange("p a b -> p (a b)"), ALU.mult,
                     reads=[("p", sg), "pmask"], writes=[("pm", sg)])

            g0 = it
            score(0)
            for gi, (m, c0, n) in enumerate(groups_):
                if gi + 1 < len(groups_):
                    score(gi + 1)
                sg = (g0 + gi) % 2
                sl = (blk_i + m) % 2
                for q in range(n):
                    kcx = m + c0 + q
                    cc = c0 + q
                    o_mm(S, ps_n2[sl][:, 0:128], Vh[:, b, kcx, :], pm[:, sg, q * 128:(q + 1) * 128], cc == 0, cc == 16,
                         reads=[("Vh", b), ("pm", sg)], writes=[("ps_n", sl)])
                for q in range(n):
                    kcx = m + c0 + q
                    cc = c0 + q
                    o_mm(S, ps_d2[sl][:, 0:128], onesv[:, kcx, :], pm[:, sg, q * 128:(q + 1) * 128], cc == 0, cc == 16,
                         reads=["onesv", ("pm", sg)], writes=[("ps_d", sl)])
                if c0 == 16:
                    o_recip(S, rd[:, sl, :], ps_d2[sl][:, 0:128], reads=[("ps_d", sl)], writes=[("rd", sl)])
                    o_tt(S, "dve", a32[:, sl, :], ps_n2[sl][:, 0:128], rd[:, sl, :], ALU.mult,
                         reads=[("ps_n", sl), ("rd", sl)], writes=[("a32", sl)])
                    o_tt(S, "pool", asq[:, h, m * 128:(m + 1) * 128], a32[:, sl, :], a32[:, sl, :], ALU.mult,
                         reads=[("a32", sl)], writes=[("asq", h, m)])
                    o_copy(S, "act", ag[:, h, m * 128:(m + 1) * 128], a32[:, sl, :],
                           reads=[("a32", sl), "gatt"], writes=[("ag", h, m)], scale=gatt[:, h:h + 1])
            it += len(groups_)
            blk_i += 8
            o_mm(S, ps_new[:, :], kT[:, b, 3072:3200], qT[:, b, 1024:1152], True, True,
                 reads=[("kT", b), ("qT", b)], writes=["ps_big"])
            o_act(S, psn[:], ps_new[:, :], AF.Exp, reads=["ps_big"], writes=["psn"], scale=SC_ATT)
            o_tt(S, "dve", pmn[:], psn[:], smaskn[:], ALU.mult, reads=["psn", "smaskn"], writes=["pmn"])
            for s in range(4):
                sb_ = sh_i % 2
                sh_i += 1
                o_dma(S, "pool", kc[:, sb_, :, :], T["cwk"][s, :, h * 128:(h + 1) * 128].rearrange("(c p) n -> p c n", p=128),
                      writes=[("kc", sb_)], grp="kc%d" % sb_)
                o_dma(S, "pool", vc[:, sb_, :, :], T["cwv"][s, :, h * 128:(h + 1) * 128].rearrange("(c p) n -> p c n", p=128),
                      writes=[("vc", sb_)], grp="vc%d" % sb_)
                for half in range(2):
                    for q8 in range(8):
                        cc = half * 8 + q8
                        o_tr(S, ptk[half][:, q8 * 128:(q8 + 1) * 128], kc[:, sb_, cc, :], identb[:],
                             reads=[("kc", sb_), "identb"], writes=["ptk"])
                    o_copy(S, "act" if half == 0 else "dve", kcT[:, half * 1024:(half + 1) * 1024], ptk[half][:, :],
                           reads=["ptk"], writes=[("kcT", half)])
                q0 = 1024 + 32 * s
                for cc in range(16):
                    o_mm(S, ps_sm[:, cc * 8:(cc + 1) * 8], kcT[:, cc * 128:(cc + 1) * 128], qT[:, b, q0:q0 + 8], True, True,
                         reads=[("kcT", cc // 8), ("qT", b)], writes=["ps_big"])
                o_act(S, psm[:], ps_sm[:, 0:128], AF.Exp, reads=["ps_big"], writes=["psm"], scale=SC_ATT)
                o_tt(S, "dve", pmm_[:], psm[:], smask[:], ALU.mult, reads=["psm", "smask"], writes=["pmm_"])
                for cc in range(16):
                    o_mm(S, ps_snd[:, 0:8], vc[:, sb_, cc, :], pmm_[:, cc * 8:(cc + 1) * 8], cc == 0, False,
                         reads=[("vc", sb_), "pmm_"], writes=["ps_big"])
                o_mm(S, ps_snd[:, 0:8], Vh[:, b, 24, :], pmn[:, 32 * s:32 * s + 8], False, True,
                     reads=[("Vh", b), "pmn"], writes=["ps_big"])
                for cc in range(16):
                    o_mm(S, ps_snd[:, 8:16], onesb[:], pmm_[:, cc * 8:(cc + 1) * 8], cc == 0, False,
                         reads=["onesb", "pmm_"], writes=["ps_big"])
                o_mm(S, ps_snd[:, 8:16], onesb[:], pmn[:, 32 * s:32 * s + 8], False, True,
                     reads=["onesb", "pmn"], writes=["ps_big"])
                o_recip(S, rd8[:], ps_snd[:, 8:16], reads=["ps_big"], writes=["rd8"])
                o_tt(S, "dve", a8[:], ps_snd[:, 0:8], rd8[:], ALU.mult, reads=["ps_big", "rd8"], writes=["a8"])
                o_tt(S, "pool", asq[:, h, q0:q0 + 8], a8[:], a8[:], ALU.mult, reads=["a8"], writes=[("asq", h, 8)])
                o_ts(S, "pool", ag[:, h, q0:q0 + 8], a8[:], gatt[:, h:h + 1], ALU.mult, reads=["a8", "gatt"], writes=[("ag", h, 8)])
        for t in range(NT):
            for h in range(8):
                o_mm(S, ps_ss[:, t:t + 1], asq[:, h, t * 128:(t + 1) * 128], onesb[:, 0:1], h == 0, h == 7,
                     reads=[("asq", h, t), "onesb"], writes=["ps_big"])
        o_act(S, ssa[:, 0:NT], ps_ss[:, 0:NT], AF.Sqrt, reads=["ps_big"], writes=["ssa"], bias=1e-6, scale=1.0 / 1024.0)
        o_recip(S, ssa[:, 0:NT], ssa[:, 0:NT], reads=["ssa"], writes=["ssa"])
        o_dma(S, "sp", T["rstd_a"][:, :], ssa[:, 0:NT], reads=["ssa"], grp="rstd")
        o_dma(S, "sp", T["mixT"][0:8, :, :].rearrange("h p t -> p h t"), ag[:, :, :],
              reads=[("ag", h, m) for h in range(8) for m in range(9)], grp="agst")
        S.emit()


def stage_C(nc, S, T):
    with St(nc, "C") as st:
        lre = st.sb("lre", [128, 32], F32)
        lim = st.sb("lim", [128, 32], F32)
        ldt = st.sb("ldt", [128, 32], F32)
        tA = st.sb("tA", [128, 32], F32)
        tB = st.sb("tB", [128, 32], F32)
        tC = st.sb("tC", [128, 32], F32)
        tI = st.sb("tI", [128, 32], mybir.dt.int32)
        mag = st.sb("mag", [128, 32], F32)
        fre = st.sb("fre", [128, 32], F32)
        fim = st.sb("fim", [128, 32], F32)
        nfim = st.sb("nfim", [128, 32], F32)
        pwr = st.sb("pwr", [128, 11, 32], F32)
        pwi = st.sb("pwi", [128, 11, 32], F32)
        npwi = st.sb("npwi", [128, 11, 32], F32)
        BTre = st.sb("BTre", [128, 32, 128], BF16)
        BTim = st.sb("BTim", [128, 32, 128], BF16)
        CTre = st.sb("CTre", [128, 32, 128], BF16)
        CTim = st.sb("CTim", [128, 32, 128], BF16)
        dcol = st.sb("dcol", [128, 8], F32)
        h0r = st.sb("h0r", [128, 32, 4], F32)
        h0i = st.sb("h0i", [128, 32, 4], F32)
        uT = st.sb("uT", [128, 2, UPOS], BF16)
        xr2 = st.sb("xr", [128, 2, UPOS], F32)
        xi2 = st.sb("xi", [128, 2, UPOS], F32)
        T0r = st.sb("T0r", [128, 1536], F32)
        T0i = st.sb("T0i", [128, 1536], F32)
        T1r = st.sb("T1r", [128, 768], F32)
        T1i = st.sb("T1i", [128, 768], F32)
        identF = st.sb("identF", [128, 128], F32)
        onesF = st.sb("onesF", [128, 128], F32)
        Dre = st.sb("Dre", [128, 2, 512], F32)
        Dim = st.sb("Dim", [128, 2, 512], F32)
        Bsr = st.sb("Bsr", [128, 4, 8], F32)
        Bsi = st.sb("Bsi", [128, 4, 8], F32)
        Hr = st.sb("Hr", [128, 4], F32)
        Hi = st.sb("Hi", [128, 4], F32)
        tmp = st.sb("tmp", [128, 4, 512], F32)
        hbr = st.sb("hbr", [128, TOK], BF16)
        hbi = st.sb("hbi", [128, TOK], BF16)
        hfr = st.sb("hfr", [128, 32], F32)
        hfi = st.sb("hfi", [128, 32], F32)
        hsr = st.sb("hsr", [128, 32, 4], F32)
        hsi = st.sb("hsi", [128, 32, 4], F32)
        ysb = st.sb("ysb", [128, TOK], F32)
        gt1 = st.sb("gt1", [128, TOK], F32)
        yg = st.sb("yg", [128, 2, TOK], F32)
        ygb = st.sb("ygb", [128, 2, TOK], BF16)
        pw = [st.ps("pw%d" % i, [128, 512], F32) for i in range(4)]
        py = [st.ps("py%d" % i, [128, 512], F32) for i in range(3)]

        for (t_, nm) in ((lre, "lamre"), (lim, "lamim"), (ldt, "logdt")):
            o_dma(S, "sp", t_[:], T[nm][:, :], writes=[nm], grp=nm)
        o_dma(S, "sp", dcol[:], T["dcol"][:, :], writes=["dcol"], grp="dcol")
        o_dma(S, "sp", h0r[:].rearrange("p a b -> p (a b)"), T["h0re"][:, :], writes=["h0r"], grp="h0r")
        o_dma(S, "sp", h0i[:].rearrange("p a b -> p (a b)"), T["h0im"][:, :], writes=["h0i"], grp="h0i")
        for (t_, nm) in ((BTre, "BTre"), (BTim, "BTim"), (CTre, "CTre"), (CTim, "CTim")):
            o_dma(S, "pool", t_[:].rearrange("p a b -> p (a b)"), T[nm][:, :], writes=[nm], grp=nm)
        P = ["par"]
        o_act(S, ldt[:], ldt[:], AF.Exp, reads=["logdt"], writes=["logdt"] + P)
        o_tt(S, "dve", tA[:], lre[:], ldt[:], ALU.mult, reads=["lamre", "logdt"], writes=P)
        o_act(S, mag[:], tA[:], AF.Exp, reads=P, writes=P)
        o_tt(S, "dve", tB[:], lim[:], ldt[:], ALU.mult, reads=["lamim", "logdt"], writes=P)

        def sin_of(dst, shift):
            o_ts(S, "dve", tC[:], tB[:], shift, ALU.add, reads=P, writes=P, s2=1.0 / (2 * np.pi), op1=ALU.mult)
            S.op("dve", lambda h: h.tensor_copy(out=tI[:], in_=tC[:]), reads=P, writes=P)
            S.op("dve", lambda h: h.tensor_copy(out=tA[:], in_=tI[:]), reads=P, writes=P)
            o_tt(S, "dve", tC[:], tC[:], tA[:], ALU.subtract, reads=P, writes=P)
            o_ts(S, "dve", tA[:], tC[:], 0.5, ALU.is_gt, reads=P, writes=P)
            o_tt(S, "dve", tC[:], tC[:], tA[:], ALU.subtract, reads=P, writes=P)
            o_ts(S, "dve", tA[:], tC[:], -0.5, ALU.is_lt, reads=P, writes=P)
            o_tt(S, "dve", tC[:], tC[:], tA[:], ALU.add, reads=P, writes=P)
            o_ts(S, "dve", tC[:], tC[:], 2 * np.pi, ALU.mult, reads=P, writes=P, s2=3.14159, op1=ALU.min)
            o_ts(S, "dve", tC[:], tC[:], -3.14159, ALU.max, reads=P, writes=P)
            o_act(S, dst, tC[:], AF.Sin, reads=P, writes=P)

        sin_of(pwi[:, 0, :], 0.0)
        sin_of(pwr[:, 0, :], np.pi / 2)
        o_tt(S, "dve", pwr[:, 0, :], pwr[:, 0, :], mag[:], ALU.mult, reads=P, writes=P)
        o_tt(S, "dve", pwi[:, 0, :], pwi[:, 0, :], mag[:], ALU.mult, reads=P, writes=P)
        o_tt(S, "dve", tA[:], lre[:], lre[:], ALU.mult, reads=P + ["lamre"], writes=P)
        o_tt(S, "dve", tC[:], lim[:], lim[:], ALU.mult, reads=P + ["lamim"], writes=P)
        o_tt(S, "dve", tA[:], tA[:], tC[:], ALU.add, reads=P, writes=P)
        o_recip(S, tA[:], tA[:], reads=P, writes=P)
        o_ts(S, "dve", tB[:], pwr[:, 0, :], -1.0, ALU.add, reads=P, writes=P)
        o_tt(S, "dve", fre[:], tB[:], lre[:], ALU.mult, reads=P, writes=P)
        o_tt(S, "dve", tC[:], pwi[:, 0, :], lim[:], ALU.mult, reads=P, writes=P)
        o_tt(S, "dve", fre[:], fre[:], tC[:], ALU.add, reads=P, writes=P)
        o_tt(S, "dve", fre[:], fre[:], tA[:], ALU.mult, reads=P, writes=P)
        o_tt(S, "dve", fim[:], pwi[:, 0, :], lre[:], ALU.mult, reads=P, writes=P)
        o_tt(S, "dve", tC[:], tB[:], lim[:], ALU.mult, reads=P, writes=P)
        o_tt(S, "dve", fim[:], fim[:], tC[:], ALU.subtract, reads=P, writes=P)
        o_tt(S, "dve", fim[:], fim[:], tA[:], ALU.mult, reads=P, writes=P)
        o_ts(S, "dve", nfim[:], fim[:], -1.0, ALU.mult, reads=P, writes=P)
        for k in range(10):
            o_tt(S, "dve", tA[:], pwr[:, k, :], pwr[:, k, :], ALU.mult, reads=P, writes=P)
            o_tt(S, "dve", tC[:], pwi[:, k, :], pwi[:, k, :], ALU.mult, reads=P, writes=P)
            o_tt(S, "dve", pwr[:, k + 1, :], tA[:], tC[:], ALU.subtract, reads=P, writes=P)
            o_tt(S, "dve", tA[:], pwr[:, k, :], pwi[:, k, :], ALU.mult, reads=P, writes=P)
            o_ts(S, "dve", pwi[:, k + 1, :], tA[:], 2.0, ALU.mult, reads=P, writes=P)
        o_ts(S, "dve", npwi[:].rearrange("p a b -> p (a b)"), pwi[:].rearrange("p a b -> p (a b)"), -1.0, ALU.mult, reads=P, writes=P)
        make_ident(S, identF, "identF")
        o_memset(S, "pool", onesF[:], 1.0, ["onesF"])
        for g in range(8):
            db = g % 2
            for q in range(4):
                tq = 4 * g + q
                o_copy(S, "act", Dre[:, db, 128 * q:128 * (q + 1)], identF[:], reads=["identF"] + P, writes=[("Dre", db)],
                       scale=fre[:, tq:tq + 1])
                o_copy(S, "act", Dim[:, db, 128 * q:128 * (q + 1)], identF[:], reads=["identF"] + P, writes=[("Dim", db)],
                       scale=fim[:, tq:tq + 1])
            o_mm(S, pw[0][:, :], onesF[:], Dre[:, db, :], True, True, reads=["onesF", ("Dre", db)], writes=[("pw", 0)])
            o_mm(S, pw[1][:, :], onesF[:], Dim[:, db, :], True, True, reads=["onesF", ("Dim", db)], writes=[("pw", 1)])
            bre = BTre[:, 4 * g:4 * g + 4, :].rearrange("p a b -> p (a b)")
            bim = BTim[:, 4 * g:4 * g + 4, :].rearrange("p a b -> p (a b)")
            o_tt(S, "dve", tmp[:, 0, :], pw[0][:, :], bre, ALU.mult, reads=[("pw", 0), "BTre"], writes=[("tmp", 0)])
            o_tt(S, "dve", tmp[:, 1, :], pw[1][:, :], bim, ALU.mult, reads=[("pw", 1), "BTim"], writes=[("tmp", 1)])
            o_tt(S, "dve", tmp[:, 2, :], pw[0][:, :], bim, ALU.mult, reads=[("pw", 0), "BTim"], writes=[("tmp", 2)])
            o_tt(S, "dve", tmp[:, 3, :], pw[1][:, :], bre, ALU.mult, reads=[("pw", 1), "BTre"], writes=[("tmp", 3)])
            o_tt(S, "dve", bre, tmp[:, 0, :], tmp[:, 1, :], ALU.subtract, reads=[("tmp", 0), ("tmp", 1)], writes=["BTre"])
            o_tt(S, "dve", bim, tmp[:, 2, :], tmp[:, 3, :], ALU.add, reads=[("tmp", 2), ("tmp", 3)], writes=["BTim"])
        o_memset(S, "pool", hbr[:, 1024:TOK], 0.0, ["hbr_s"])
        o_memset(S, "pool", hbi[:, 1024:TOK], 0.0, ["hbi_s"])

        def cma(dr, di, er, ei, orr, oi, k, tau, rk, wk):
            pr = pwr[:, k, tau:tau + 1]
            pi_ = pwi[:, k, tau:tau + 1]
            npi = npwi[:, k, tau:tau + 1]
            wr_ = [(w_, "r") for w_ in wk]
            wi_ = [(w_, "i") for w_ in wk]
            o_stt(S, dr, er, pr, orr, ALU.mult, ALU.add, reads=rk + P, writes=wr_)
            o_stt(S, di, ei, pr, oi, ALU.mult, ALU.add, reads=rk + P, writes=wi_)
            o_stt(S, dr, ei, npi, dr, ALU.mult, ALU.add, reads=rk + P, writes=wr_)
            o_stt(S, di, er, pi_, di, ALU.mult, ALU.add, reads=rk + P, writes=wi_ + list(wk))
            S.touch(wk, wr_ + wi_)

        blocks = [(i * 512, 512) for i in range(8)] + [(4096, 128)]
        wic = [0]

        def emit_xe(tau, ub):
            xr = xr2[:, tau % 2, :]
            xi = xi2[:, tau % 2, :]
            XK = ("x", tau % 2)
            for (c0, w) in blocks:
                wi = wic[0]
                p0 = pw[(wi * 2) % 4]
                p1 = pw[(wi * 2 + 1) % 4]
                k0 = ("pw", (wi * 2) % 4)
                k1 = ("pw", (wi * 2 + 1) % 4)
                wic[0] += 1
                o_mm(S, p0[:, 0:w], BTre[:, tau, :], uT[:, ub, c0:c0 + w], True, True, reads=["BTre", ("uT", ub)], writes=[k0])
                o_mm(S, p1[:, 0:w], BTim[:, tau, :], uT[:, ub, c0:c0 + w], True, True, reads=["BTim", ("uT", ub)], writes=[k1])
                o_copy(S, "act", xr[:, c0:c0 + w], p0[:, 0:w], reads=[k0], writes=[XK])
                o_copy(S, "act", xi[:, c0:c0 + w], p1[:, 0:w], reads=[k1], writes=[XK])

        o_dma(S, "sp", uT[:, 0, :], T["uT"][0, :, :], writes=[("uT", 0)], grp="uT0")
        emit_xe(0, 0)
        for c in range(8):
            ub = c % 2
            for j in range(4):
                tau = 4 * c + j
                xr = xr2[:, tau % 2, :]
                xi = xi2[:, tau % 2, :]
                XK = ("x", tau % 2)
                if tau + 1 < 32:
                    cn = (tau + 1) // 4
                    if (tau + 1) % 4 == 0:
                        o_dma(S, "sp", uT[:, cn % 2, :], T["uT"][cn, :, :], writes=[("uT", cn % 2)], grp="uT%d" % (cn % 2))
                    emit_xe(tau + 1, cn % 2)
                X = [XK]
                xs_r = xr[:, 4096:UPOS:32]
                xs_i = xi[:, 4096:UPOS:32]
                cma(xs_r, xs_i, h0r[:, tau, :], h0i[:, tau, :], xs_r, xs_i, 0, tau, X + ["h0r", "h0i"], X)
                src_r, src_i, n = xr, xi, 3072
                dsts = [(T0r, T0i), (T1r, T1i)]
                for k in range(10):
                    dr_, di_ = dsts[k % 2]
                    no = n // 2
                    cma(dr_[:, 0:no], di_[:, 0:no], src_r[:, 0:n:2], src_i[:, 0:n:2], src_r[:, 1:n:2], src_i[:, 1:n:2],
                        k, tau, X + ["tree"], ["tree"])
                    src_r, src_i, n = dr_, di_, no
                cma(Hr[:, 0:1], Hi[:, 0:1], src_r[:, 0:1], src_i[:, 0:1], src_r[:, 1:2], src_i[:, 1:2], 10, tau, ["tree"], ["H"])
                cma(Hr[:, 1:2], Hi[:, 1:2], Hr[:, 0:1], Hi[:, 0:1], src_r[:, 2:3], src_i[:, 2:3], 10, tau, ["tree", "H"], ["H"])
                cma(xr[:, 3072:3073], xi[:, 3072:3073], Hr[:, 1:2], Hi[:, 1:2], xr[:, 3072:3073], xi[:, 3072:3073], 0, tau, X + ["H"], X)
                o_ = 3072
                for k in range(10):
                    st_ = 1 << (k + 1)
                    h_ = 1 << k
                    cnt = 1024 // st_
                    t0 = o_ + st_ - 1
                    s0 = o_ + h_ - 1
                    t1 = t0 + (cnt - 1) * st_ + 1
                    s1 = s0 + (cnt - 1) * st_ + 1
                    cma(xr[:, t0:t1:st_], xi[:, t0:t1:st_], xr[:, s0:s1:st_], xi[:, s0:s1:st_],
                        xr[:, t0:t1:st_], xi[:, t0:t1:st_], k, tau, X, X)
                for k in range(8, -1, -1):
                    st_ = 1 << (k + 1)
                    h_ = 1 << k
                    cnt = 1024 // st_ - 1
                    t0 = o_ + st_ + h_ - 1
                    s0 = o_ + st_ - 1
                    t1 = t0 + (cnt - 1) * st_ + 1
                    s1 = s0 + (cnt - 1) * st_ + 1
                    cma(xr[:, t0:t1:st_], xi[:, t0:t1:st_], xr[:, s0:s1:st_], xi[:, s0:s1:st_],
                        xr[:, t0:t1:st_], xi[:, t0:t1:st_], k, tau, X, X)
                sr = xr[:, 4096:UPOS].rearrange("p (s j) -> p s j", j=32)[:, :, 0:8]
                si_ = xi[:, 4096:UPOS].rearrange("p (s j) -> p s j", j=32)[:, :, 0:8]
                cur = (sr, si_, XK)
                alt = (Bsr[:, :, :], Bsi[:, :, :], "Bs")
                for k in range(3):
                    s_ = 1 << k
                    cr, ci, ck = cur
                    ar_, ai_, ak = alt
                    cma(ar_[:, :, s_:8], ai_[:, :, s_:8], cr[:, :, 0:8 - s_], ci[:, :, 0:8 - s_], cr[:, :, s_:8], ci[:, :, s_:8],
                        k, tau, [ck], [ak])
                    o_copy(S, "act", ar_[:, :, 0:s_], cr[:, :, 0:s_], reads=[ck], writes=[ak])
                    o_copy(S, "pool", ai_[:, :, 0:s_], ci[:, :, 0:s_], reads=[ck], writes=[ak])
                    cur, alt = alt, cur
                fr, fi_, fk = cur
                o_copy(S, "act", hfr[:, tau:tau + 1], xr[:, 4095:4096], reads=X, writes=["hf"])
                o_copy(S, "act", hfi[:, tau:tau + 1], xi[:, 4095:4096], reads=X, writes=["hf"])
                o_copy(S, "pool", hsr[:, tau, :], fr[:, :, 7], reads=[fk], writes=["hs"])
                o_copy(S, "pool", hsi[:, tau, :], fi_[:, :, 7], reads=[fk], writes=["hs"])
                o_copy(S, "act", hbr[:, 0:1024], xr[:, 3072:4096], reads=X, writes=["hbr"])
                o_copy(S, "act", hbi[:, 0:1024], xi[:, 3072:4096], reads=X, writes=["hbi"], scale=-1.0)
                o_copy(S, "pool", hbr[:, 1024:TOK].rearrange("p (s j) -> p s j", j=32)[:, :, 0:8], fr, reads=[fk, "hbr_s"], writes=["hbr_s"])
                o_ts(S, "pool", hbi[:, 1024:TOK].rearrange("p (s j) -> p s j", j=32)[:, :, 0:8], fi_, -1.0, ALU.mult,
                     reads=[fk, "hbi_s"], writes=["hbi_s"])
                for bi, (c0, w) in enumerate([(0, 512), (512, 512), (1024, 128)]):
                    o_mm(S, py[bi][:, 0:w], CTre[:, tau, :], hbr[:, c0:c0 + w], j == 0, False,
                         reads=["CTre", "hbr", "hbr_s"], writes=[("py", bi)])
                    o_mm(S, py[bi][:, 0:w], CTim[:, tau, :], hbi[:, c0:c0 + w], False, j == 3,
                         reads=["CTim", "hbi", "hbi_s"], writes=[("py", bi)])
            yb_ = c % 2
            for bi, (c0, w) in enumerate([(0, 512), (512, 512), (1024, 128)]):
                o_stt(S, ysb[:, c0:c0 + w], uT[:, ub, 3072 + c0:3072 + c0 + w], dcol[:, c:c + 1], py[bi][:, 0:w], ALU.mult, ALU.add,
                      reads=[("uT", ub), ("py", bi), "dcol"], writes=["ysb"])
            o_tt(S, "pool", gt1[:], ysb[:], ysb[:], ALU.mult, reads=["ysb"], writes=["gt1"])
            o_ts(S, "dve", gt1[:], gt1[:], 0.044715, ALU.mult, reads=["gt1"], writes=["gt1"], s2=1.0, op1=ALU.add)
            o_tt(S, "dve", gt1[:], gt1[:], ysb[:], ALU.mult, reads=["gt1", "ysb"], writes=["gt1"])
            o_act(S, gt1[:], gt1[:], AF.Tanh, reads=["gt1"], writes=["gt1"], scale=0.7978845608028654)
            o_ts(S, "dve", gt1[:], gt1[:], 1.0, ALU.add, reads=["gt1"], writes=["gt1"], s2=0.5, op1=ALU.mult)
            o_tt(S, "dve", yg[:, yb_, :], gt1[:], ysb[:], ALU.mult, reads=["gt1", "ysb"], writes=[("yg", yb_)])
            o_copy(S, "pool", ygb[:, yb_, :], yg[:, yb_, :], reads=[("yg", yb_)], writes=[("ygb", yb_)])
            o_dma(S, "sp", T["ygT"][c, :, :], yg[:, yb_, :], reads=[("yg", yb_)], grp="yg%d" % yb_)
            o_dma(S, "sp", T["ygTb"][c, :, :], ygb[:, yb_, :], reads=[("ygb", yb_)], grp="ygb%d" % yb_)
        o_dma(S, "sp", T["hfr"][:, :], hfr[:], reads=["hf"], grp="hfr")
        o_dma(S, "sp", T["hfi"][:, :], hfi[:], reads=["hf"], grp="hfi")
        o_dma(S, "sp", T["hsr"][:, :], hsr[:].rearrange("p a b -> p (a b)"), reads=["hs"], grp="hsr")
        o_dma(S, "sp", T["hsi"][:, :], hsi[:].rearrange("p a b -> p (a b)"), reads=["hs"], grp="hsi")
        S.emit()


def stage_D1(nc, S, T):
    with St(nc, "D1") as st:
        ygT = st.sb("ygT", [128, 8, TOK], F32)
        ygb = st.sb("ygb", [128, 8, TOK], BF16)
        Wg = st.sb("Wg", [128, 8, 1024], BF16)
        gssm = st.sb("gssm", [128, 8], F32)
        onesb = st.sb("onesb", [128, 128], BF16)
        ssq = st.sb("ssq", [128, 8, TOK], BF16)
        sgm = st.sb("sgm", [128, 8, TOK], BF16)
        sg = st.sb("sg", [128, 2, 512], F32)
        so = st.sb("so", [128, 2, 512], F32)
        sss = st.sb("sss", [128, 16], F32)
        pz = [st.ps("pz%d" % i, [128, 512], F32) for i in range(4)]
        ps_ss = st.ps("ps_ss", [128, 16], F32)
        o_memset(S, "dve", onesb[:], 1.0, ["onesb"])
        o_dma(S, "sp", ygT[:], T["ygT"].rearrange("c p t -> p c t"), writes=["ygT"], grp="ygT")
        o_dma(S, "sp", ygb[:], T["ygTb"].rearrange("c p t -> p c t"), writes=["ygb"], grp="ygb")
        o_dma(S, "pool", Wg[:], T["w_glu"].rearrange("(c p) n -> p c n", p=128), writes=["Wg"], grp="Wg")
        o_dma(S, "sp", gssm[:], T["gssm"][:, :], writes=["gssm"], grp="gssm")
        i = 0
        for f in range(8):
            for (c0, w) in [(0, 512), (512, 512), (1024, 128)]:
                pi = i % 4
                si = i % 2
                i += 1
                for k in range(8):
                    o_mm(S, pz[pi][:, 0:w], Wg[:, k, f * 128:(f + 1) * 128], ygb[:, k, c0:c0 + w], k == 0, k == 7,
                         reads=["Wg", "ygb"], writes=[("pz", pi)])
                o_act(S, sg[:, si, 0:w], pz[pi][:, 0:w], AF.Sigmoid, reads=[("pz", pi)], writes=[("sg", si)])
                o_tt(S, "dve", so[:, si, 0:w], ygT[:, f, c0:c0 + w], sg[:, si, 0:w], ALU.mult, reads=["ygT", ("sg", si)], writes=[("so", si)])
                o_tt(S, "pool", ssq[:, f, c0:c0 + w], so[:, si, 0:w], so[:, si, 0:w], ALU.mult, reads=[("so", si)], writes=["ssq"])
                o_copy(S, "act", sgm[:, f, c0:c0 + w], so[:, si, 0:w], reads=[("so", si), "gssm"], writes=["sgm"], scale=gssm[:, f:f + 1])
        for t in range(NT):
            for f in range(8):
                o_mm(S, ps_ss[:, t:t + 1], ssq[:, f, t * 128:(t + 1) * 128], onesb[:, 0:1], f == 0, f == 7,
                     reads=["ssq", "onesb"], writes=["ps_ss"])
        o_act(S, sss[:, 0:NT], ps_ss[:, 0:NT], AF.Sqrt, reads=["ps_ss"], writes=["sss"], bias=1e-6, scale=1.0 / 1024.0)
        o_recip(S, sss[:, 0:NT], sss[:, 0:NT], reads=["sss"], writes=["sss"])
        o_dma(S, "sp", T["rstd_s"][:, :], sss[:, 0:NT], reads=["sss"], grp="rstd")
        o_dma(S, "sp", T["mixT"][8:16, :, :].rearrange("h p t -> p h t"), sgm[:, :, :], reads=["sgm"], grp="sgmst")
        S.emit()


def layernorm_tile(S, st_, X, t, lng, lnb, stats, mv, rs, key):
    xt = X[:, t, :]
    for c in range(4):
        S.op("dve", lambda h, c=c: h.bn_stats(out=stats[:, c, :], in_=X[:, t, c * 512:(c + 1) * 512]), reads=[key], writes=["ln_st"])
    S.op("dve", lambda h: h.bn_aggr(out=mv[:], in_=stats[:].rearrange("p a b -> p (a b)")), reads=["ln_st"], writes=["ln_mv"])
    o_act(S, rs[:], mv[:, 1:2], AF.Sqrt, reads=["ln_mv"], writes=["ln_rs"], bias=1e-5, scale=1.0)
    o_recip(S, rs[:], rs[:], reads=["ln_rs"], writes=["ln_rs"])
    o_ts(S, "dve", xt, xt, mv[:, 0:1], ALU.subtract, reads=[key, "ln_mv", "ln_rs"], writes=[key], s2=rs[:, 0:1], op1=ALU.mult)
    o_tt(S, "dve", xt, xt, lng[:], ALU.mult, reads=[key, "lng"], writes=[key])
    o_tt(S, "pool", xt, xt, lnb[:], ALU.add, reads=[key, "lnb"], writes=[key])


def linear_residual_ln(nc, S, T, name, inT_name, w_name, lng_name, lnb_name, xin_fn, xout_name, two_part):
    with St(nc, name) as st:
        X = st.sb("X", [128, NT, 2048], F32)
        inT = st.sb("inT", [128, 16, TOK], BF16)
        Wo = st.sb("Wo", [128, 2, 16, 512], BF16)
        lng = st.sb("lng", [128, 2048], F32)
        lnb = st.sb("lnb", [128, 2048], F32)
        tmp = st.sb("tmp", [128, 2, 512], F32)
        stats = st.sb("stats", [128, 4, 6], F32)
        mv = st.sb("mv", [128, 2], F32)
        rs = st.sb("rs", [128, 1], F32)
        ra = st.sb("ra", [128, NT], F32)
        rsm = st.sb("rsm", [128, NT], F32)
        pa = [st.ps("pa%d" % i, [128, 512], F32) for i in range(2)]
        pb = [st.ps("pb%d" % i, [128, 512], F32) for i in range(2)]
        xin_fn(S, X)
        o_dma(S, "sp", inT[:], T[inT_name].rearrange("c p t -> p c t"), writes=["inT"], grp="inT")
        o_dma(S, "sp", lng[:], T[lng_name].partition_broadcast(128), writes=["lng"], grp="lng")
        o_dma(S, "sp", lnb[:], T[lnb_name].partition_broadcast(128), writes=["lnb"], grp="lnb")
        if two_part:
            o_dma(S, "sp", ra[:], T["rstd_a"][:, :], writes=["ra"], grp="ra")
            o_dma(S, "sp", rsm[:], T["rstd_s"][:, :], writes=["rsm"], grp="rsm")
        i = 0
        for n in range(4):
            wb = n % 2
            o_dma(S, "pool", Wo[:, wb, :, :], T[w_name][:, n * 512:(n + 1) * 512].rearrange("(c p) n -> p c n", p=128),
                  writes=[("Wo", wb)], grp="Wo%d" % wb)
            for t in range(NT):
                pi = i % 2
                i += 1
                xk = ("X", t)
                if two_part:
                    for k in range(8):
                        o_mm(S, pa[pi][:, :], inT[:, k, t * 128:(t + 1) * 128], Wo[:, wb, k, :], k == 0, k == 7,
                             reads=["inT", ("Wo", wb)], writes=[("pa", pi)])
                    for k in range(8):
                        o_mm(S, pb[pi][:, :], inT[:, 8 + k, t * 128:(t + 1) * 128], Wo[:, wb, 8 + k, :], k == 0, k == 7,
                             reads=["inT", ("Wo", wb)], writes=[("pb", pi)])
                    o_copy(S, "act", tmp[:, pi, :], pa[pi][:, :], reads=[("pa", pi), "ra"], writes=[("tmp", pi)], scale=ra[:, t:t + 1])
                    o_stt(S, tmp[:, pi, :], pb[pi][:, :], rsm[:, t:t + 1], tmp[:, pi, :], ALU.mult, ALU.add,
                          reads=[("pb", pi), ("tmp", pi), "rsm"], writes=[("tmp", pi)])
                    o_stt(S, X[:, t, n * 512:(n + 1) * 512], X[:, t, n * 512:(n + 1) * 512], ALPHA, tmp[:, pi, :], ALU.mult, ALU.add,
                          reads=[xk, ("tmp", pi)], writes=[xk])
                else:
                    for k in range(16):
                        o_mm(S, pa[pi][:, :], inT[:, k, t * 128:(t + 1) * 128], Wo[:, wb, k, :], k == 0, k == 15,
                             reads=["inT", ("Wo", wb)], writes=[("pa", pi)])
                    o_stt(S, X[:, t, n * 512:(n + 1) * 512], X[:, t, n * 512:(n + 1) * 512], ALPHA, pa[pi][:, :], ALU.mult, ALU.add,
                          reads=[xk, ("pa", pi)], writes=[xk])
                if n == 3:
                    layernorm_tile(S, st, X, t, lng, lnb, stats, mv, rs, ("X", t))
                    o_dma(S, "sp", T[xout_name][t * 128:(t + 1) * 128, :], X[:, t, :], reads=[("X", t)], grp="xo%d" % (t % 2))
        S.emit()


def xin_from_inputs(T):
    def f(S, X):
        o_dma(S, "sp", X[:, 0:8, :], T["xw"][3072:4096, :].rearrange("(t p) d -> p t d", p=128),
              writes=[("X", t) for t in range(8)], grp="X")
        o_dma(S, "sp", X[:, 8, :], T["xs"][:, :], writes=[("X", 8)], grp="X8")
    return f


def xin_from_scr(T, name):
    def f(S, X):
        o_dma(S, "sp", X[:, :, :], T[name][:, :].rearrange("(t p) d -> p t d", p=128),
              writes=[("X", t) for t in range(NT)], grp="X")
    return f


def transpose_block(S, X, t, ident, ptr, dstT, dst_key, i0, f32copy=None):
    for b4 in range(4):
        pi = (i0 + b4) % 2
        for q in range(4):
            c = b4 * 4 + q
            o_tr(S, ptr[pi][:, q * 128:(q + 1) * 128], X[:, t, c * 128:(c + 1) * 128], ident[:],
                 reads=[("X", t), "ident"], writes=[("ptr", pi)])
        o_copy(S, "act" if pi == 0 else "dve", dstT[:, b4 * 4:(b4 + 1) * 4, t * 128:(t + 1) * 128],
               ptr[pi][:, :].rearrange("p (a b) -> p a b", a=4), reads=[("ptr", pi)], writes=[dst_key])
        if f32copy is not None:
            o_copy(S, "dve" if pi == 0 else "act", f32copy[:, b4 * 4:(b4 + 1) * 4, :],
                   ptr[pi][:, :].rearrange("p (a b) -> p a b", a=4), reads=[("ptr", pi)], writes=["f32copy"])


def stage_E0(nc, S, T):
    with St(nc, "E0") as st:
        ident = st.sb("ident", [128, 128], F32)
        M = st.sb("M", [128, 2, 2048], F32)
        memT = st.sb("memT", [128, 16, 256], BF16)
        Wb = st.sb("Wb", [128, 2, 16, 512], BF16)
        of = st.sb("of", [128, 2, 512], F32)
        ob = st.sb("ob", [128, 2, 512], BF16)
        okT = st.sb("okT", [128, 2, 256], BF16)
        ptr = [st.ps("ptr%d" % i, [128, 512], F32) for i in range(2)]
        pm = [st.ps("pm%d" % i, [128, 512], F32) for i in range(2)]
        pk = [st.ps("pk%d" % i, [128, 256], F32) for i in range(2)]
        make_ident(S, ident, "ident")
        o_dma(S, "sp", M[:], T["memp"][:, :].rearrange("(t p) d -> p t d", p=128), writes=[("X", 0), ("X", 1)], grp="M")
        for t in range(2):
            transpose_block(S, M, t, ident, ptr, memT, "memT", 0)
        wi = 0
        oi = 0
        for (wname, kind) in (("w_mk", "k"), ("w_mv", "v")):
            for n in range(4):
                wb = wi % 2
                wi += 1
                o_dma(S, "pool", Wb[:, wb, :, :], T[wname][:, n * 512:(n + 1) * 512].rearrange("(c p) n -> p c n", p=128),
                      writes=[("Wb", wb)], grp="Wb%d" % wb)
                for t in range(2):
                    pi = oi % 2
                    oi += 1
                    for k in range(16):
                        o_mm(S, pm[pi][:, :], memT[:, k, t * 128:(t + 1) * 128], Wb[:, wb, k, :], k == 0, k == 15,
                             reads=["memT", ("Wb", wb)], writes=[("pm", pi)])
                    o_copy(S, "act", of[:, pi, :], pm[pi][:, :], reads=[("pm", pi)], writes=[("of", pi)])
                    o_dma(S, "sp", T["memKo" if kind == "k" else "memVo"][t * 128:(t + 1) * 128, n * 512:(n + 1) * 512],
                          of[:, pi, :], reads=[("of", pi)], grp="of%d" % pi)
                    if kind == "v":
                        o_copy(S, "dve", ob[:, pi, :], pm[pi][:, :], reads=[("pm", pi)], writes=[("ob", pi)])
                        o_dma(S, "sp", T["mv_scr"][t * 128:(t + 1) * 128, n * 512:(n + 1) * 512], ob[:, pi, :],
                              reads=[("ob", pi)], grp="ob%d" % pi)
                if kind == "k":
                    for fb in range(4):
                        f = n * 4 + fb
                        pi = f % 2
                        for k in range(16):
                            o_mm(S, pk[pi][:, :], Wb[:, wb, k, fb * 128:(fb + 1) * 128], memT[:, k, :], k == 0, k == 15,
                                 reads=["memT", ("Wb", wb)], writes=[("pk", pi)])
                        o_copy(S, "dve", okT[:, pi, :], pk[pi][:, :], reads=[("pk", pi)], writes=[("okT", pi)])
                        o_dma(S, "sp", T["mkT_scr"][f, :, :], okT[:, pi, :], reads=[("okT", pi)], grp="okT%d" % pi)
        S.emit()


def stage_E1(nc, S, T):
    with St(nc, "E1") as st:
        ident = st.sb("ident", [128, 128], F32)
        X = st.sb("X", [128, NT, 2048], F32)
        xT = st.sb("xT", [128, 16, TOK], BF16)
        Wb = st.sb("Wb", [128, 2, 16, 512], BF16)
        oq = st.sb("oq", [128, 2, 512], BF16)
        ptr = [st.ps("ptr%d" % i, [128, 512], F32) for i in range(2)]
        pq = [st.ps("pq%d" % i, [128, 512], F32) for i in range(4)]
        make_ident(S, ident, "ident")
        xin_from_scr(T, "X1")(S, X)
        for t in range(NT):
            transpose_block(S, X, t, ident, ptr, xT, "xT", 0)
        i = 0
        for n in range(4):
            wb = n % 2
            o_dma(S, "pool", Wb[:, wb, :, :], T["w_mq"][:, n * 512:(n + 1) * 512].rearrange("(c p) n -> p c n", p=128),
                  writes=[("Wb", wb)], grp="Wb%d" % wb)
            for fb in range(4):
                f = n * 4 + fb
                for (c0, w) in [(0, 512), (512, 512), (1024, 128)]:
                    pi = i % 4
                    oi = i % 2
                    i += 1
                    for k in range(16):
                        o_mm(S, pq[pi][:, 0:w], Wb[:, wb, k, fb * 128:(fb + 1) * 128], xT[:, k, c0:c0 + w], k == 0, k == 15,
                             reads=["xT", ("Wb", wb)], writes=[("pq", pi)])
                    o_copy(S, "act" if oi == 0 else "dve", oq[:, oi, 0:w], pq[pi][:, 0:w], reads=[("pq", pi)], writes=[("oq", oi)])
                    o_dma(S, "sp", T["qmT"][f, :, c0:c0 + w], oq[:, oi, 0:w], reads=[("oq", oi)], grp="oq%d" % oi)
        S.emit()


def stage_E2(nc, S, T):
    with St(nc, "E2") as st:
        identf = st.sb("identf", [128, 128], F32)
        onesb = st.sb("onesb", [128, 128], BF16)
        mkT = st.sb("mkT", [128, 16, 256], BF16)
        mv = st.sb("mv", [128, 2, 2048], BF16)
        qm = st.sb("qm", [128, 16, TOK], BF16)
        p = st.sb("p", [128, 2, 512], BF16)
        rd = st.sb("rd", [128, 512], F32)
        om = st.sb("om", [128, 2, 4, 512], BF16)
        ck = st.sb("ck", [128, 2, 2048], F32)
        skT = st.sb("skT", [128, 16, 256], BF16)
        sv = st.sb("sv", [128, 2, 2048], BF16)
        p8 = st.sb("p8", [128, 2, 8], BF16)
        rd8 = st.sb("rd8", [128, 8], F32)
        om8 = st.sb("om8", [128, 16, 128], BF16)
        ps_s = [st.ps("ps_s%d" % i, [128, 512], F32) for i in range(2)]
        ps_o = [st.ps("ps_o%d" % i, [128, 512], F32) for i in range(4)]
        ps_d = st.ps("ps_d", [128, 512], F32)
        ptr = st.ps("ptr", [128, 512], F32)
        make_ident(S, identf, "ident")
        o_memset(S, "dve", onesb[:], 1.0, ["onesb"])
        o_memset(S, "pool", om8[:].rearrange("p a b -> p (a b)"), 0.0, ["om8"])
        o_dma(S, "sp", mkT[:], T["mkT_scr"].rearrange("c p t -> p c t"), writes=["mkT"], grp="mkT")
        o_dma(S, "sp", mv[:], T["mv_scr"][:, :].rearrange("(t p) d -> p t d", p=128), writes=["mv"], grp="mv")
        o_dma(S, "sp", qm[:], T["qmT"].rearrange("c p t -> p c t"), writes=["qm"], grp="qm")
        si = 0
        oi = 0
        for hh in range(4):
            for blk in range(2):
                c0 = blk * 512
                ob_ = oi % 2
                oi += 1
                for kc_ in range(2):
                    sl = si % 2
                    si += 1
                    for j in range(4):
                        o_mm(S, ps_s[sl][:, :], mkT[:, 4 * hh + j, kc_ * 128:(kc_ + 1) * 128], qm[:, 4 * hh + j, c0:c0 + 512], j == 0, j == 3,
                             reads=["mkT", "qm"], writes=[("ps_s", sl)])
                    o_act(S, p[:, sl, :], ps_s[sl][:, :], AF.Exp, reads=[("ps_s", sl)], writes=[("p", sl)], scale=SC_MEM)
                    for j in range(4):
                        o_mm(S, ps_o[j][:, :], mv[:, kc_, (4 * hh + j) * 128:(4 * hh + j + 1) * 128], p[:, sl, :], kc_ == 0, kc_ == 1,
                             reads=["mv", ("p", sl)], writes=[("ps_o", j)])
                    o_mm(S, ps_d[:, :], onesb[:], p[:, sl, :], kc_ == 0, kc_ == 1, reads=["onesb", ("p", sl)], writes=["ps_d"])
                o_recip(S, rd[:], ps_d[:, :], reads=["ps_d"], writes=["rd"])
                for j in range(4):
                    o_tt(S, "dve", om[:, ob_, j, :], ps_o[j][:, :], rd[:], ALU.mult, reads=[("ps_o", j), "rd"], writes=[("om", ob_)])
                o_dma(S, "sp", T["omT"][4 * hh:4 * hh + 4, :, c0:c0 + 512].rearrange("c p t -> p c t"), om[:, ob_, :, :],
                      reads=[("om", ob_)], grp="om%d" % ob_)
        for s in range(4):
            q0 = 1024 + 32 * s
            o_dma(S, "sp", ck[:], T["cmk"][s, :, :].rearrange("(t p) d -> p t d", p=128), writes=[("X", 0), ("X", 1)], grp="ck")
            o_dma(S, "pool", sv[:], T["cmv"][s, :, :].rearrange("(t p) d -> p t d", p=128), writes=["sv"], grp="sv")
            for t in range(2):
                for b4 in range(4):
                    for q in range(4):
                        c = b4 * 4 + q
                        o_tr(S, ptr[:, q * 128:(q + 1) * 128], ck[:, t, c * 128:(c + 1) * 128], identf[:],
                             reads=[("X", t), "ident"], writes=["ptr"])
                    o_copy(S, "act" if b4 % 2 == 0 else "dve", skT[:, b4 * 4:(b4 + 1) * 4, t * 128:(t + 1) * 128],
                           ptr[:, :].rearrange("p (a b) -> p a b", a=4), reads=["ptr"], writes=["skT"])
            for hh in range(4):
                for kc_ in range(2):
                    for j in range(4):
                        o_mm(S, ps_s[0][:, kc_ * 8:(kc_ + 1) * 8], skT[:, 4 * hh + j, kc_ * 128:(kc_ + 1) * 128], qm[:, 4 * hh + j, q0:q0 + 8],
                             j == 0, j == 3, reads=["skT", "qm"], writes=[("ps_s", 0)])
                o_act(S, p8[:].rearrange("p a b -> p (a b)"), ps_s[0][:, 0:16], AF.Exp, reads=[("ps_s", 0)], writes=["p8"], scale=SC_MEM)
                for j in range(4):
                    for kc_ in range(2):
                        o_mm(S, ps_o[j][:, 0:8], sv[:, kc_, (4 * hh + j) * 128:(4 * hh + j + 1) * 128], p8[:, kc_, :], kc_ == 0, kc_ == 1,
                             reads=["sv", "p8"], writes=[("ps_o", j)])
                for kc_ in range(2):
                    o_mm(S, ps_d[:, 0:8], onesb[:], p8[:, kc_, :], kc_ == 0, kc_ == 1, reads=["onesb", "p8"], writes=["ps_d"])
                o_recip(S, rd8[:], ps_d[:, 0:8], reads=["ps_d"], writes=["rd8"])
                for j in range(4):
                    o_tt(S, "dve", om8[:, 4 * hh + j, 32 * s:32 * s + 8], ps_o[j][:, 0:8], rd8[:], ALU.mult,
                         reads=[("ps_o", j), "rd8"], writes=["om8"])
        o_dma(S, "sp", T["omT"][:, :, 1024:TOK].rearrange("c p t -> p c t"), om8[:, :, :], reads=["om8"], grp="om8")
        S.emit()


def stage_F1(nc, S, T):
    with St(nc, "F1") as st:
        ident = st.sb("ident", [128, 128], F32)
        X = st.sb("X", [128, NT, 2048], F32)
        xT = st.sb("xT", [128, 16, TOK], BF16)
        xTf = st.sb("xTf", [128, 16, 128], F32)
        Wr = st.sb("Wr", [128, 16, 36], F32)
        br = st.sb("br", [128, 36], F32)
        lg = st.sb("lg", [128, 36], F32)
        G = st.sb("G", [128, NT, 32], F32)
        gm = st.sb("gm", [128, 1], F32)
        goh = st.sb("goh", [128, 4], F32)
        ge = st.sb("ge", [128, 4], F32)
        gs = st.sb("gs", [128, 1], F32)
        gw = st.sb("gw", [128, 1], F32)
        es = st.sb("es", [128, 8], F32)
        m1 = st.sb("m1", [128, 1], F32)
        oh1 = st.sb("oh1", [128, 8], F32)
        em = st.sb("em", [128, 8], F32)
        m2 = st.sb("m2", [128, 1], F32)
        oh2 = st.sb("oh2", [128, 8], F32)
        dd = st.sb("dd", [128, 1], F32)
        w1 = st.sb("w1", [128, 1], F32)
        w2 = st.sb("w2", [128, 1], F32)
        g8 = st.sb("g8", [128, 8], F32)
        ptr = [st.ps("ptr%d" % i, [128, 512], F32) for i in range(2)]
        pl = st.ps("pl", [128, 64], F32)
        make_ident(S, ident, "ident")
        xin_from_scr(T, "X2")(S, X)
        o_dma(S, "sp", Wr[:], T["wr"][:, :].rearrange("(c p) n -> p c n", p=128), writes=["Wr"], grp="Wr")
        o_dma(S, "sp", br[:], T["br"].partition_broadcast(128), writes=["br"], grp="br")
        R = ["rt"]
        for t in range(NT):
            transpose_block(S, X, t, ident, ptr, xT, "xT", 0, f32copy=xTf)
            for k in range(16):
                o_mm(S, pl[:, 0:36], xTf[:, k, :], Wr[:, k, :], k == 0, k == 15, reads=["f32copy", "Wr"], writes=["pl"])
            o_tt(S, "dve", lg[:], pl[:, 0:36], br[:], ALU.add, reads=["pl", "br"], writes=R)
            S.op("dve", lambda h: h.reduce_max(out=gm[:], in_=lg[:, 0:4], axis=mybir.AxisListType.X), reads=R, writes=R)
            o_ts(S, "dve", goh[:], lg[:, 0:4], gm[:, 0:1], ALU.is_equal, reads=R, writes=R)
            o_ts(S, "dve", ge[:], lg[:, 0:4], gm[:, 0:1], ALU.subtract, reads=R, writes=R)
            o_act(S, ge[:], ge[:], AF.Exp, reads=R, writes=R)
            S.op("dve", lambda h: h.reduce_sum(out=gs[:], in_=ge[:], axis=mybir.AxisListType.X), reads=R, writes=R)
            o_recip(S, gw[:], gs[:], reads=R, writes=R)
            o_ts(S, "dve", es[:], lg[:, 4:12], goh[:, 0:1], ALU.mult, reads=R, writes=R)
            for g in range(1, 4):
                o_stt(S, es[:], lg[:, 4 + 8 * g:12 + 8 * g], goh[:, g:g + 1], es[:], ALU.mult, ALU.add, reads=R, writes=R)
            S.op("dve", lambda h: h.reduce_max(out=m1[:], in_=es[:], axis=mybir.AxisListType.X), reads=R, writes=R)
            o_ts(S, "dve", oh1[:], es[:], m1[:, 0:1], ALU.is_equal, reads=R, writes=R)
            o_stt(S, em[:], oh1[:], -1e30, es[:], ALU.mult, ALU.add, reads=R, writes=R)
            S.op("dve", lambda h: h.reduce_max(out=m2[:], in_=em[:], axis=mybir.AxisListType.X), reads=R, writes=R)
            o_ts(S, "dve", oh2[:], em[:], m2[:, 0:1], ALU.is_equal, reads=R, writes=R)
            o_tt(S, "dve", dd[:], m2[:], m1[:], ALU.subtract, reads=R, writes=R)
            o_act(S, dd[:], dd[:], AF.Exp, reads=R, writes=R)
            o_ts(S, "dve", w1[:], dd[:], 1.0, ALU.add, reads=R, writes=R)
            o_recip(S, w1[:], w1[:], reads=R, writes=R)
            o_tt(S, "dve", w1[:], w1[:], gw[:], ALU.mult, reads=R, writes=R)
            o_tt(S, "dve", w2[:], w1[:], dd[:], ALU.mult, reads=R, writes=R)
            o_ts(S, "dve", g8[:], oh1[:], w1[:, 0:1], ALU.mult, reads=R, writes=R)
            o_stt(S, g8[:], oh2[:], w2[:, 0:1], g8[:], ALU.mult, ALU.add, reads=R, writes=R)
            for g in range(4):
                o_ts(S, "dve", G[:, t, 8 * g:8 * g + 8], g8[:], goh[:, g:g + 1], ALU.mult, reads=R, writes=["G"])
            o_copy(S, "act", X[:, t, :], X[:, t, :], reads=[("X", t)], writes=[("X", t)], scale=ALPHA)
            o_dma(S, "sp", T["X3"][t * 128:(t + 1) * 128, :], X[:, t, :], reads=[("X", t)], grp="xo%d" % (t % 2))
        o_dma(S, "sp", T["x2T"].rearrange("c p t -> p c t"), xT[:, :, :], reads=["xT"], grp="xTst")
        o_dma(S, "sp", T["G"][:, :], G[:].rearrange("p a b -> p (a b)"), reads=["G"], grp="Gst")
        S.emit()


def stage_F2(nc, S, T):
    with St(nc, "F2") as st:
        X = st.sb("X", [128, NT, 2048], F32)
        xT = st.sb("xT", [128, 16, TOK], BF16)
        G = st.sb("G", [128, NT, 32], F32)
        Wg = st.sb("Wg", [128, 16, 512], BF16)
        Wu = st.sb("Wu", [128, 16, 512], BF16)
        Wd = st.sb("Wd", [128, 4, 2048], BF16)
        hT = st.sb("hT", [128, 4, TOK], BF16)
        sg = st.sb("sg", [128, 2, 512], F32)
        pg = [st.ps("pg%d" % i, [128, 512], F32) for i in range(2)]
        pu = [st.ps("pu%d" % i, [128, 512], F32) for i in range(2)]
        po = [st.ps("po%d" % i, [128, 512], F32) for i in range(4)]
        xin_from_scr(T, "X3")(S, X)
        o_dma(S, "sp", xT[:], T["x2T"].rearrange("c p t -> p c t"), writes=["xT"], grp="xT")
        o_dma(S, "sp", G[:].rearrange("p a b -> p (a b)"), T["G"][:, :], writes=["G"], grp="G")
        i = 0
        oi = 0
        for e in range(32):
            o_dma(S, "pool", Wg[:], T["w_gate"][e, :, :].rearrange("(c p) n -> p c n", p=128), writes=["Wg"], grp="Wg")
            o_dma(S, "pool", Wu[:], T["w_up"][e, :, :].rearrange("(c p) n -> p c n", p=128), writes=["Wu"], grp="Wu")
            o_dma(S, "pool", Wd[:], T["w_down"][e, :, :].rearrange("(c p) n -> p c n", p=128), writes=["Wd"], grp="Wd")
            for f in range(4):
                for (c0, w) in [(0, 512), (512, 512), (1024, 128)]:
                    pi = i % 2
                    i += 1
                    for k in range(16):
                        o_mm(S, pg[pi][:, 0:w], Wg[:, k, f * 128:(f + 1) * 128], xT[:, k, c0:c0 + w], k == 0, k == 15,
                             reads=["Wg", "xT"], writes=[("pg", pi)])
                    for k in range(16):
                        o_mm(S, pu[pi][:, 0:w], Wu[:, k, f * 128:(f + 1) * 128], xT[:, k, c0:c0 + w], k == 0, k == 15,
                             reads=["Wu", "xT"], writes=[("pu", pi)])
                    o_act(S, sg[:, pi, 0:w], pg[pi][:, 0:w], AF.Silu, reads=[("pg", pi)], writes=[("sg", pi)])
                    o_tt(S, "dve", hT[:, f, c0:c0 + w], sg[:, pi, 0:w], pu[pi][:, 0:w], ALU.mult,
                         reads=[("sg", pi), ("pu", pi)], writes=[("hT", f)])
            for t in range(NT):
                for n in range(4):
                    pi = oi % 4
                    oi += 1
                    for f in range(4):
                        o_mm(S, po[pi][:, :], hT[:, f, t * 128:(t + 1) * 128], Wd[:, f, n * 512:(n + 1) * 512], f == 0, f == 3,
                             reads=[("hT", f), "Wd"], writes=[("po", pi)])
                    o_stt(S, X[:, t, n * 512:(n + 1) * 512], po[pi][:, :], G[:, t, e:e + 1], X[:, t, n * 512:(n + 1) * 512],
                          ALU.mult, ALU.add, reads=[("po", pi), "G", ("X", t)], writes=[("X", t)])
        for t in range(NT):
            o_dma(S, "sp", T["X1"][t * 128:(t + 1) * 128, :], X[:, t, :], reads=[("X", t)], grp="xo%d" % (t % 2))
        S.emit()


def stage_F3(nc, S, T):
    with St(nc, "F3") as st:
        X = st.sb("X", [128, NT, 2048], F32)
        lng = st.sb("lng", [128, 2048], F32)
        lnb = st.sb("lnb", [128, 2048], F32)
        stats = st.sb("stats", [128, 4, 6], F32)
        mv = st.sb("mv", [128, 2], F32)
        rs = st.sb("rs", [128, 1], F32)
        xin_from_scr(T, "X1")(S, X)
        o_dma(S, "sp", lng[:], T["ln3_g"].partition_broadcast(128), writes=["lng"], grp="lng")
        o_dma(S, "sp", lnb[:], T["ln3_b"].partition_broadcast(128), writes=["lnb"], grp="lnb")
        for t in range(NT):
            layernorm_tile(S, st, X, t, lng, lnb, stats, mv, rs, ("X", t))
            o_dma(S, "sp", T["y"][t * 128:(t + 1) * 128, :], X[:, t, :], reads=[("X", t)], grp="xo%d" % (t % 2))
        S.emit()


IN_SPECS = [
    ("xw", [4096, 2048]), ("xs", [128, 2048]), ("vbias", [128, 24]), ("pmask", [128, 17 * 128]),
    ("smask", [128, 128]), ("smaskn", [128, 128]), ("cwk", [4, 2048, 1024]), ("cwv", [4, 2048, 1024]),
    ("h0re", [128, 128]), ("h0im", [128, 128]), ("cmk", [4, 256, 2048]), ("cmv", [4, 256, 2048]),
    ("memp", [256, 2048]), ("w_in", [2048, 4096]), ("lamre", [128, 32]), ("lamim", [128, 32]),
    ("logdt", [128, 32]), ("BTre", [128, 4096]), ("BTim", [128, 4096]), ("CTre", [128, 4096]),
    ("CTim", [128, 4096]), ("dcol", [128, 8]), ("w_glu", [1024, 1024]), ("gatt", [128, 8]), ("gssm", [128, 8]),
    ("w_out", [2048, 2048]), ("ln1_g", [1, 2048]), ("ln1_b", [1, 2048]), ("w_mq", [2048, 2048]),
    ("w_mk", [2048, 2048]), ("w_mv", [2048, 2048]), ("w_mo", [2048, 2048]), ("ln2_g", [1, 2048]),
    ("ln2_b", [1, 2048]), ("wr", [2048, 36]), ("br", [1, 36]), ("w_gate", [32, 2048, 512]),
    ("w_up", [32, 2048, 512]), ("w_down", [32, 512, 2048]), ("ln3_g", [1, 2048]), ("ln3_b", [1, 2048]),
]
OUT_SPECS = [
    ("y", [TOK, 2048]), ("Kout", [TOK, 1024]), ("Vout", [TOK, 1024]), ("hfr", [128, 32]), ("hfi", [128, 32]),
    ("hsr", [128, 128]), ("hsi", [128, 128]), ("memKo", [256, 2048]), ("memVo", [256, 2048]),
]
SCR_SPECS = [
    ("qT", [8, 128, TOK], BF16), ("kT", [8, 128, KPOS], BF16), ("V_scr", [KPOS, 1024], BF16), ("uT", [8, 128, UPOS], BF16),
    ("mixT", [16, 128, TOK], BF16), ("rstd_a", [128, NT], F32), ("rstd_s", [128, NT], F32),
    ("ygT", [8, 128, TOK], F32), ("ygTb", [8, 128, TOK], BF16), ("X1", [TOK, 2048], F32), ("X2", [TOK, 2048], F32),
    ("X3", [TOK, 2048], F32), ("mkT_scr", [16, 128, 256], BF16), ("mv_scr", [256, 2048], BF16),
    ("qmT", [16, 128, TOK], BF16), ("omT", [16, 128, TOK], BF16), ("x2T", [16, 128, TOK], BF16), ("G", [128, NT * 32], F32),
]


def build_program(stages=None, debug=False):
    nc = bass.Bass("TRN2", target_bir_lowering=False)
    T = {}
    for (n, s) in IN_SPECS:
        T[n] = nc.dram_tensor(n, s, F32, kind="ExternalInput").ap()
    for (n, s) in OUT_SPECS:
        T[n] = nc.dram_tensor(n, s, F32, kind="ExternalOutput").ap()
    for (n, s, d) in SCR_SPECS:
        T[n] = nc.dram_tensor("scr_" + n, s, d, kind=("ExternalOutput" if debug else "Internal")).ap()
    S = Sched(nc)
    table = [
        ("A", lambda: stage_A(nc, S, T)),
        ("B", lambda: stage_B(nc, S, T)),
        ("C", lambda: stage_C(nc, S, T)),
        ("D1", lambda: stage_D1(nc, S, T)),
        ("D2", lambda: linear_residual_ln(nc, S, T, "D2", "mixT", "w_out", "ln1_g", "ln1_b", xin_from_inputs(T), "X1", True)),
        ("E0", lambda: stage_E0(nc, S, T)),
        ("E1", lambda: stage_E1(nc, S, T)),
        ("E2", lambda: stage_E2(nc, S, T)),
        ("E3", lambda: linear_residual_ln(nc, S, T, "E3", "omT", "w_mo", "ln2_g", "ln2_b", xin_from_scr(T, "X1"), "X2", False)),
        ("F1", lambda: stage_F1(nc, S, T)),
        ("F2", lambda: stage_F2(nc, S, T)),
        ("F3", lambda: stage_F3(nc, S, T)),
    ]
    for (nm, fn) in table:
        if stages is None or nm in stages:
            fn()
    S.close()
    return nc


def _mult(diff):
    m = ((diff >= 0) & (diff <= 128)).astype(np.float32)
    m += ((diff >= 0) & (diff <= 512) & (diff % 4 == 0)).astype(np.float32)
    m += ((diff >= 0) & (diff <= 2048) & (diff % 16 == 0)).astype(np.float32)
    return m


def make_inputs(inp):
    f = np.float32
    xp = inp["x_prompt"]
    xsm = inp["x_sample"]
    j = np.arange(128)
    pm = np.zeros((128, 17, 128), f)
    for cc in range(17):
        diff = (16 - cc) * 128 + j[None, :] - j[:, None]
        pm[:, cc, :] = _mult(diff)
    sm = np.zeros((128, 16, 8), f)
    for cc in range(16):
        cpos = cc * 128 + j[:, None]
        diff = np.arange(8)[None, :] + 2048 - cpos
        sm[:, cc, :] = _mult(diff)
    smn = np.zeros((128, 128), f)
    d8 = np.arange(8)[None, :] - np.arange(8)[:, None]
    for s in range(4):
        smn[32 * s:32 * s + 8, 32 * s:32 * s + 8] = _mult(d8)
    br_, bi_ = inp["ssm_b_re"][0], inp["ssm_b_im"][0]
    cr_, ci_ = inp["ssm_c_re"][0], inp["ssm_c_im"][0]
    BTre = np.zeros((128, 32, 128), f)
    BTim = np.zeros((128, 32, 128), f)
    CTre = np.zeros((128, 32, 128), f)
    CTim = np.zeros((128, 32, 128), f)
    for tau in range(32):
        for gl in range(2):
            g = 2 * tau + gl
            r0 = 32 * (tau % 4) + 16 * gl
            BTre[r0:r0 + 16, tau, 64 * gl:64 * gl + 64] = br_[g].T
            BTim[r0:r0 + 16, tau, 64 * gl:64 * gl + 64] = bi_[g].T
            CTre[64 * gl:64 * gl + 64, tau, r0:r0 + 16] = cr_[g].T
            CTim[64 * gl:64 * gl + 64, tau, r0:r0 + 16] = ci_[g].T
    common = {
        "pmask": pm.reshape(128, -1), "smask": sm.reshape(128, -1), "smaskn": smn,
        "w_in": inp["w_in"][0],
        "lamre": np.ascontiguousarray(inp["ssm_lam_re"][0].reshape(32, 128).T),
        "lamim": np.ascontiguousarray(inp["ssm_lam_im"][0].reshape(32, 128).T),
        "logdt": np.ascontiguousarray(np.repeat(inp["ssm_log_dt"][0].reshape(32, 2), 64, axis=1).T),
        "BTre": BTre.reshape(128, -1), "BTim": BTim.reshape(128, -1), "CTre": CTre.reshape(128, -1), "CTim": CTim.reshape(128, -1),
        "dcol": np.ascontiguousarray(inp["ssm_d"][0].reshape(8, 128).T),
        "w_glu": inp["w_glu"][0],
        "gatt": np.ascontiguousarray(inp["g_attn"][0].reshape(8, 128).T),
        "gssm": np.ascontiguousarray(inp["g_ssm"][0].reshape(8, 128).T),
        "w_out": inp["w_out"][0], "ln1_g": inp["ln1_g"], "ln1_b": inp["ln1_b"],
        "w_mq": inp["w_mq"][0], "w_mk": inp["w_mk"][0], "w_mv": inp["w_mv"][0], "w_mo": inp["w_mo"][0],
        "ln2_g": inp["ln2_g"], "ln2_b": inp["ln2_b"],
        "wr": np.ascontiguousarray(np.concatenate([inp["w_r1"][0], inp["w_r2"][0].reshape(2048, 32)], axis=1)),
        "br": np.ascontiguousarray(np.concatenate([inp["b_r1"][0], inp["b_r2"][0].reshape(32)])[None, :]),
        "w_gate": inp["w_gate"][0], "w_up": inp["w_up"][0], "w_down": inp["w_down"][0],
        "ln3_g": inp["ln3_g"], "ln3_b": inp["ln3_b"],
    }
    maps = []
    for c in range(8):
        b, r = c // 4, c % 4
        xw = np.zeros((4096, 2048), f)
        lo = 1024 * r - 3072
        src0 = max(lo, 0)
        xw[src0 - lo:, :] = xp[b, src0:1024 * r + 1024, :]
        xs = np.zeros((128, 2048), f)
        for s in range(4):
            xs[32 * s:32 * s + 8, :] = xsm[4 * c + s]
        vb = np.ones((128, 24), f)
        for jj in range(16):
            if 8 * r - 16 + jj < 0:
                vb[:, jj] = 0.0
        m = dict(common)
        m.update({
            "xw": xw, "xs": xs, "vbias": vb,
            "cwk": np.ascontiguousarray(inp["cache_win_k"][0, 4 * c:4 * c + 4].reshape(4, 2048, 1024)),
            "cwv": np.ascontiguousarray(inp["cache_win_v"][0, 4 * c:4 * c + 4].reshape(4, 2048, 1024)),
            "h0re": np.ascontiguousarray(inp["state_ssm_re"][0, 4 * c:4 * c + 4].reshape(4, 32, 128).transpose(2, 1, 0).reshape(128, 128)),
            "h0im": np.ascontiguousarray(inp["state_ssm_im"][0, 4 * c:4 * c + 4].reshape(4, 32, 128).transpose(2, 1, 0).reshape(128, 128)),
            "cmk": np.ascontiguousarray(inp["cache_mem_k"][0, 4 * c:4 * c + 4].reshape(4, 256, 2048)),
            "cmv": np.ascontiguousarray(inp["cache_mem_v"][0, 4 * c:4 * c + 4].reshape(4, 256, 2048)),
            "memp": np.ascontiguousarray(inp["mem_prompt"][b]),
        })
        maps.append({k: np.ascontiguousarray(v, dtype=f) for k, v in m.items()})
    return maps


def assemble(res):
    f = np.float32
    yp = np.zeros((2, 4096, 2048), f)
    ys = np.zeros((32, 8, 2048), f)
    wkp = np.zeros((1, 2, 2048, 8, 128), f)
    wvp = np.zeros((1, 2, 2048, 8, 128), f)
    wks = np.zeros((1, 32, 8, 8, 128), f)
    wvs = np.zeros((1, 32, 8, 8, 128), f)
    srp = np.zeros((1, 2, 64, 64), f)
    sip = np.zeros((1, 2, 64, 64), f)
    srs = np.zeros((1, 32, 64, 64), f)
    sis = np.zeros((1, 32, 64, 64), f)
    mkp = np.zeros((1, 2, 256, 4, 512), f)
    mvp = np.zeros((1, 2, 256, 4, 512), f)
    for c in range(8):
        r_ = res[c]
        b, r = c // 4, c % 4
        yp[b, 1024 * r:1024 * r + 1024] = r_["y"][0:1024]
        for s in range(4):
            ys[4 * c + s] = r_["y"][1024 + 32 * s:1024 + 32 * s + 8]
            wks[0, 4 * c + s] = r_["Kout"][1024 + 32 * s:1024 + 32 * s + 8].reshape(8, 8, 128)
            wvs[0, 4 * c + s] = r_["Vout"][1024 + 32 * s:1024 + 32 * s + 8].reshape(8, 8, 128)
        if r >= 2:
            wkp[0, b, 1024 * (r - 2):1024 * (r - 1)] = r_["Kout"][0:1024].reshape(1024, 8, 128)
            wvp[0, b, 1024 * (r - 2):1024 * (r - 1)] = r_["Vout"][0:1024].reshape(1024, 8, 128)
        if r == 3:
            srp[0, b] = r_["hfr"].T.reshape(64, 64)
            sip[0, b] = r_["hfi"].T.reshape(64, 64)
        hs_r = r_["hsr"].reshape(128, 32, 4).transpose(2, 1, 0).reshape(4, 64, 64)
        hs_i = r_["hsi"].reshape(128, 32, 4).transpose(2, 1, 0).reshape(4, 64, 64)
        srs[0, 4 * c:4 * c + 4] = hs_r
        sis[0, 4 * c:4 * c + 4] = hs_i
        if r == 0:
            mkp[0, b] = r_["memKo"].reshape(256, 4, 512)
            mvp[0, b] = r_["memVo"].reshape(256, 4, 512)
    return (yp, ys, wkp, wvp, wks, wvs, srp, sip, srs, sis, mkp, mvp)


def kernel(**inputs):
    inp = {k: np.asarray(v) for k, v in inputs.items()}
    maps = make_inputs(inp)
    nc = build_program()
    res = run_bass_kernel_spmd(nc, maps, core_ids=list(range(8)))
    return assemble(res.results)
```
